# Optimizing a Trainium2 kernel written in Bass

```python
import jax
import jax.numpy as jnp
from jax import lax
import numpy as np


D_MODEL = 2048
BATCH = 8
SEQ = 2048
DEPTH = 2

GRID_W = 64
CTX_LEN = 256
N_FOURIER_GROUPS = 4
FOURIER_GROUP_CH = D_MODEL // 8
FOURIER_W = N_FOURIER_GROUPS * FOURIER_GROUP_CH
NA_HEADS = 16
NA_HEAD_DIM = 64
NA_W = NA_HEADS * NA_HEAD_DIM
NA_ROWS = 8
NA_COLS = 16
CONV_W = D_MODEL
CONV_K = 31
N_EXPERTS = 16
N_GROUPS = 4
EXPERTS_PER_GROUP = N_EXPERTS // N_GROUPS
TOP_K = 2
F_EXPERT = 512
N_MOD = 6
EPS = 1e-6

kernel_name = 'hybrid_fourier_natten_conformer_groupmoe_dit'


def rms_norm(x, g):
    xf = x.astype(jnp.float32)
    y = xf * lax.rsqrt(jnp.mean(xf * xf, axis=-1, keepdims=True) + EPS)
    return (y * g.astype(jnp.float32)).astype(x.dtype)


def modulate(h, shift, scale):
    return h * (1 + scale) + shift


def split_heads(t):
    b, l, _ = t.shape
    return t.reshape(b, l, NA_HEADS, NA_HEAD_DIM)


def fourier_mix(u):
    b, l, _ = u.shape
    ug = u.astype(jnp.float32).reshape(b, l, N_FOURIER_GROUPS, FOURIER_GROUP_CH)
    f = jnp.fft.fft2(ug, axes=(1, 3), norm='ortho').real
    return f.reshape(b, l, FOURIER_W).astype(u.dtype)


def context_attention(q, k, v):
    scale = q.shape[-1] ** -0.5
    s = jnp.einsum('bqhd,bkhd->bhqk', q, k).astype(jnp.float32) * scale
    p = jax.nn.softmax(s, axis=-1).astype(v.dtype)
    return jnp.einsum('bhqk,bkhd->bqhd', p, v)


def neighbourhood_attention(q, k, v, kc, vc, rpb):
    b, s, h, dh = q.shape
    rows = s // GRID_W
    kr = min(NA_ROWS, rows)
    scale = dh ** -0.5
    qg = q.reshape(b, rows, GRID_W, h, dh)
    kg = k.reshape(b, rows, GRID_W, h, dh)
    vg = v.reshape(b, rows, GRID_W, h, dh)
    col = jnp.arange(GRID_W)
    col_start = jnp.clip(col - NA_COLS // 2, 0, GRID_W - NA_COLS)
    col_mask = (col[None, :] >= col_start[:, None]) & (col[None, :] < col_start[:, None] + NA_COLS)
    dc_idx = jnp.clip(col[None, :] - col[:, None] + NA_COLS - 1, 0, 2 * NA_COLS - 2)
    rpb_f = rpb.astype(jnp.float32)

    def row_block(r):
        rs = jnp.clip(r - kr // 2, 0, rows - kr)
        q_r = lax.dynamic_index_in_dim(qg, r, axis=1, keepdims=False)
        k_r = lax.dynamic_slice_in_dim(kg, rs, kr, axis=1)
        v_r = lax.dynamic_slice_in_dim(vg, rs, kr, axis=1)
        s_loc = jnp.einsum('bqhd,bjkhd->bhqjk', q_r, k_r).astype(jnp.float32) * scale
        dr_idx = rs + jnp.arange(kr) - r + NA_ROWS - 1
        bias = rpb_f[:, dr_idx[:, None, None], dc_idx[None, :, :]]
        bias = bias.transpose(0, 2, 1, 3)
        s_loc = jnp.where(col_mask[:, None, :], s_loc + bias[None], -jnp.inf)
        s_loc = s_loc.reshape(b, h, GRID_W, kr * GRID_W)
        s_ctx = jnp.einsum('bqhd,bchd->bhqc', q_r, kc).astype(jnp.float32) * scale
        p = jax.nn.softmax(jnp.concatenate([s_loc, s_ctx], axis=-1), axis=-1).astype(v.dtype)
        p_loc = p[..., :kr * GRID_W].reshape(b, h, GRID_W, kr, GRID_W)
        p_ctx = p[..., kr * GRID_W:]
        return (jnp.einsum('bhqjk,bjkhd->bqhd', p_loc, v_r)
                + jnp.einsum('bhqc,bchd->bqhd', p_ctx, vc))

    o = lax.map(row_block, jnp.arange(rows))
    return jnp.moveaxis(o, 0, 1).reshape(b, s, h * dh)


def fourier_na_mixer(h, hc, w_in, rpb, w_out, need_ctx_out):
    a_end = FOURIER_W
    q_end = a_end + NA_W
    k_end = q_end + NA_W
    p = h @ w_in
    kvc = hc @ w_in[:, q_end:]
    kc = split_heads(kvc[..., :NA_W])
    vc = split_heads(kvc[..., NA_W:])
    q = split_heads(p[..., a_end:q_end])
    k = split_heads(p[..., q_end:k_end])
    v = split_heads(p[..., k_end:])
    y = jnp.concatenate([fourier_mix(p[..., :a_end]),
                         neighbourhood_attention(q, k, v, kc, vc, rpb)], axis=-1) @ w_out
    if not need_ctx_out:
        return y, None
    b, lc, _ = hc.shape
    pc = hc @ w_in[:, :q_end]
    oc = context_attention(split_heads(pc[..., a_end:]), kc, vc).reshape(b, lc, NA_W)
    yc = jnp.concatenate([fourier_mix(pc[..., :a_end]), oc], axis=-1) @ w_out
    return y, yc


def conformer_conv(h, w_in, b_in, dw_w, dw_b, ln_g, ln_b, w_out, b_out):
    p = h @ w_in + b_in
    u = p[..., :CONV_W] * jax.nn.sigmoid(p[..., CONV_W:])
    u = lax.conv_general_dilated(u, dw_w[:, None, :].astype(u.dtype), window_strides=(1,),
                                 padding=[(CONV_K // 2, CONV_K // 2)],
                                 dimension_numbers=('NWC', 'WIO', 'NWC'),
                                 feature_group_count=CONV_W) + dw_b
    uf = u.astype(jnp.float32)
    mu = jnp.mean(uf, axis=-1, keepdims=True)
    var = jnp.mean(jnp.square(uf - mu), axis=-1, keepdims=True)
    un = (uf - mu) * lax.rsqrt(var + EPS) * ln_g.astype(jnp.float32) + ln_b.astype(jnp.float32)
    un = jax.nn.silu(un).astype(h.dtype)
    return un @ w_out + b_out


def grouped_moe(h, router_w, router_b, w_gate, w_up, w_down):
    shape = h.shape
    t = h.reshape(-1, shape[-1])
    n = t.shape[0]
    scores = jax.nn.sigmoid((t @ router_w).astype(jnp.float32))
    sel = scores + router_b.astype(jnp.float32)
    grp_score = lax.top_k(sel.reshape(n, N_GROUPS, EXPERTS_PER_GROUP), 2)[0].sum(-1)
    top_g = jnp.argmax(grp_score, axis=-1)
    in_grp = (jnp.arange(N_EXPERTS) // EXPERTS_PER_GROUP)[None, :] == top_g[:, None]
    _, idx = lax.top_k(jnp.where(in_grp, sel, -jnp.inf), TOP_K)
    w = jnp.take_along_axis(scores, idx, axis=-1)
    w = w / jnp.sum(w, axis=-1, keepdims=True)
    combine = jnp.sum(jax.nn.one_hot(idx, N_EXPERTS, dtype=jnp.float32) * w[..., None], axis=1).astype(t.dtype)
    out = jnp.zeros_like(t)
    for e in range(N_EXPERTS):
        y = (jax.nn.silu(t @ w_gate[e]) * (t @ w_up[e])) @ w_down[e]
        out = out + combine[:, e:e + 1] * y
    return out.reshape(shape)


def setup_inputs(seed: int = 0) -> dict:
    key = jax.random.key(seed)
    ks = iter(jax.random.split(key, 40))
    n_even = (DEPTH + 1) // 2
    n_odd = DEPTH // 2
    f32 = jnp.float32

    def nrm(shape, scale):
        return jax.random.normal(next(ks), shape, f32) * scale

    mix_w = FOURIER_W + NA_W
    return {
        'x': nrm((BATCH, SEQ, D_MODEL), 1.0),
        'c': nrm((BATCH, D_MODEL), 1.0),
        'ctx': nrm((BATCH, CTX_LEN, D_MODEL), 1.0),
        'c_ctx': nrm((D_MODEL,), 1.0),
        'ada_w': nrm((DEPTH, D_MODEL, N_MOD * D_MODEL), 0.5 * D_MODEL ** -0.5),
        'ada_b': nrm((DEPTH, N_MOD * D_MODEL), 0.02),
        'mix_norm_g': 1.0 + nrm((DEPTH, D_MODEL), 0.02),
        'ffn_norm_g': 1.0 + nrm((DEPTH, D_MODEL), 0.02),
        'ev_w_in': nrm((n_even, D_MODEL, FOURIER_W + 3 * NA_W), D_MODEL ** -0.5),
        'ev_rpb': nrm((n_even, NA_HEADS, 2 * NA_ROWS - 1, 2 * NA_COLS - 1), 0.1),
        'ev_w_out': nrm((n_even, mix_w, D_MODEL), mix_w ** -0.5),
        'od_w_in': nrm((n_odd, D_MODEL, 2 * CONV_W), D_MODEL ** -0.5),
        'od_b_in': nrm((n_odd, 2 * CONV_W), 0.02),
        'od_dw_w': nrm((n_odd, CONV_K, CONV_W), CONV_K ** -0.5),
        'od_dw_b': nrm((n_odd, CONV_W), 0.02),
        'od_ln_g': 1.0 + nrm((n_odd, CONV_W), 0.02),
        'od_ln_b': nrm((n_odd, CONV_W), 0.02),
        'od_w_out': nrm((n_odd, CONV_W, D_MODEL), CONV_W ** -0.5),
        'od_b_out': nrm((n_odd, D_MODEL), 0.02),
        'router_w': nrm((D_MODEL, N_EXPERTS), D_MODEL ** -0.5),
        'router_b': nrm((N_EXPERTS,), 0.01),
        'moe_w_gate': nrm((DEPTH, N_EXPERTS, D_MODEL, F_EXPERT), D_MODEL ** -0.5),
        'moe_w_up': nrm((DEPTH, N_EXPERTS, D_MODEL, F_EXPERT), D_MODEL ** -0.5),
        'moe_w_down': nrm((DEPTH, N_EXPERTS, F_EXPERT, D_MODEL), F_EXPERT ** -0.5),
        'final_norm_g': 1.0 + nrm((D_MODEL,), 0.02),
    }


def reference(x, c, ctx, c_ctx, ada_w, ada_b, mix_norm_g, ffn_norm_g, ev_w_in, ev_rpb, ev_w_out,
              od_w_in, od_b_in, od_dw_w, od_dw_b, od_ln_g, od_ln_b, od_w_out, od_b_out,
              router_w, router_b, moe_w_gate, moe_w_up, moe_w_down, final_norm_g):
    b = x.shape[0]
    n_ctx = ctx.shape[1]
    silu_c = jax.nn.silu(c)
    silu_cc = jax.nn.silu(c_ctx)
    for i in range(DEPTH):
        is_even = (i % 2 == 0)
        ctx_needed_later = any(j % 2 == 0 for j in range(i + 1, DEPTH))
        mod = (silu_c @ ada_w[i] + ada_b[i]).reshape(b, N_MOD, 1, D_MODEL)
        mod_c = (silu_cc @ ada_w[i] + ada_b[i]).reshape(N_MOD, D_MODEL)
        shift1, scale1, gate1, shift2, scale2, gate2 = (mod[:, j] for j in range(N_MOD))
        h = modulate(rms_norm(x, mix_norm_g[i]), shift1, scale1)
        if is_even or ctx_needed_later:
            hc = modulate(rms_norm(ctx, mix_norm_g[i]), mod_c[0], mod_c[1])
        if is_even:
            e = i // 2
            y, yc = fourier_na_mixer(h, hc, ev_w_in[e], ev_rpb[e], ev_w_out[e], ctx_needed_later)
        else:
            o = i // 2
            conv_args = (od_w_in[o], od_b_in[o], od_dw_w[o], od_dw_b[o], od_ln_g[o], od_ln_b[o],
                         od_w_out[o], od_b_out[o])
            y = conformer_conv(h, *conv_args)
            yc = conformer_conv(hc, *conv_args) if ctx_needed_later else None
        x = x + gate1 * y
        h2 = modulate(rms_norm(x, ffn_norm_g[i]), shift2, scale2)
        if ctx_needed_later:
            ctx = ctx + mod_c[2] * yc
            hc2 = modulate(rms_norm(ctx, ffn_norm_g[i]), mod_c[3], mod_c[4])
            both = grouped_moe(jnp.concatenate([hc2, h2], axis=1), router_w, router_b,
                               moe_w_gate[i], moe_w_up[i], moe_w_down[i])
            ctx = ctx + mod_c[5] * both[:, :n_ctx]
            x = x + gate2 * both[:, n_ctx:]
        else:
            x = x + gate2 * grouped_moe(h2, router_w, router_b, moe_w_gate[i], moe_w_up[i], moe_w_down[i])
    return rms_norm(x, final_norm_g)
```

```python
import numpy as np
import ml_dtypes
import concourse.bass as bass
import concourse.mybir as mybir
from concourse.bass_utils import run_bass_kernel_spmd

F32 = mybir.dt.float32
BF16 = mybir.dt.bfloat16
AF = mybir.ActivationFunctionType
ALU = mybir.AluOpType
AX = mybir.AxisListType

D = 2048
S = 2048
NT = 16
NK = 16
CTX = 256
NE = 16
FE = 512
EPS = 1e-6
NEG = -30000.0


class Tk:
    __slots__ = ("w", "r")

    def __init__(self):
        self.w = None
        self.r = {}


class Op:
    __slots__ = ("eng", "fn", "deps", "inc", "dma", "sem", "val", "gidx", "region", "outer")

    def __init__(self, eng, fn, dma):
        self.eng = eng
        self.fn = fn
        self.dma = dma
        self.deps = set()
        self.inc = False
        self.sem = None
        self.val = 0
        self.gidx = 0
        self.region = None
        self.outer = None


class Prog:
    ENGS = ("pe", "act", "dve", "pool", "sp")
    SEG = 10 ** 9
    NDS = 28

    def __init__(self):
        self.ops = {e: [] for e in self.ENGS}
        self.all_dma = []
        self.bar = None
        self.bar_seen = set()
        self.region = None
        self.outer = None
        self.regs = {}
        self.regs2 = {}

    def add(self, eng, fn, reads=(), writes=(), dma=False):
        op = Op(eng, fn, dma)
        op.region = self.region
        op.outer = self.outer
        deps = set()
        for t in reads:
            if t.w is not None:
                deps.add(t.w)
        for t in writes:
            if t.w is not None:
                deps.add(t.w)
            for o in t.r.values():
                if isinstance(o, list):
                    deps.update(o)
                else:
                    deps.add(o)
        if self.bar is not None and eng not in self.bar_seen:
            deps.update(self.bar)
            self.bar_seen.add(eng)
        for t in reads:
            if dma:
                t.r.setdefault("dma", []).append(op)
            else:
                t.r[eng] = op
        for t in writes:
            t.w = op
            t.r = {}
        deps.discard(op)
        if eng == "pe" and not dma:
            deps = {d for d in deps if not (d.eng == "pe" and not d.dma)}
        op.deps = deps
        for d in deps:
            d.inc = True
        self.ops[eng].append(op)
        if dma:
            self.all_dma.append(op)
        return op

    def barrier(self):
        deps = []
        for e in self.ENGS:
            for o in reversed(self.ops[e]):
                if not o.dma:
                    deps.append(o)
                    break
        deps.extend(self.all_dma)
        self.all_dma = []
        self.bar = deps
        self.bar_seen = set()

    def run_emit(self, nc, block, handles, sems):
        si = 0
        for e in self.ENGS:
            cnt = 0
            cur = None
            for o in self.ops[e]:
                if o.dma or not o.inc:
                    continue
                if cnt % self.SEG == 0:
                    cur = sems[si]
                    si += 1
                o.sem = cur
                o.val = cnt % self.SEG + 1
                o.gidx = cnt + 1
                cnt += 1
        dsems = sems[si:si + self.NDS]
        assert len(dsems) == self.NDS, "not enough semaphores"
        dcount = [0] * self.NDS
        k = 0
        for o in self.dma_order:
            s = k % self.NDS
            o.sem = dsems[s]
            o.gidx = (s, dcount[s])
            dcount[s] += 16
            o.val = dcount[s]
            k += 1

        def emit_engine(ename):
            def body(e):
                waited = {}

                def emit_op(o):
                    for d in o.deps:
                        key = ("d", id(d.sem)) if d.dma else (d.eng, id(d.sem))
                        need = d.val
                        if waited.get(key, 0) >= need:
                            continue
                        e.wait_ge(d.sem, need)
                        waited[key] = need
                    if o.dma:
                        slot, prev = o.gidx
                        key = ("d", id(o.sem))
                        if prev > 0 and waited.get(key, 0) < prev:
                            e.wait_ge(o.sem, prev)
                            waited[key] = prev
                        ins = o.fn(e)
                        ins.then_inc(o.sem, 16)
                    else:
                        if o.fn is None:
                            return
                        ins = o.fn(e)
                        if o.inc:
                            ins.then_inc(o.sem, 1)

                ops = self.ops[ename]

                def else_bulk(grp):
                    ninc = sum(1 for q in grp if (not q.dma) and q.inc)
                    if ninc:
                        csem = [q.sem for q in grp if (not q.dma) and q.inc][0]
                        e.drain().then_inc(csem, ninc)
                    for q in grp:
                        if q.dma:
                            slot, prev = q.gidx
                            if prev > 0:
                                e.wait_ge(q.sem, prev)
                            e.sem_inc(q.sem, 16)

                def emit_range(byslot, lo, hi):
                    grp = [q for s in range(lo, hi) for q in byslot.get(s, [])]
                    if not grp:
                        return
                    saved = dict(waited)
                    with e.If_lt(self.regs[ename], -lo):
                        if hi - lo == 1:
                            for q in grp:
                                emit_op(q)
                        else:
                            mid = (lo + hi) // 2
                            emit_range(byslot, lo, mid)
                            emit_range(byslot, mid, hi)
                    with e.Else():
                        waited.clear()
                        waited.update(saved)
                        else_bulk(grp)
                    waited.clear()
                    waited.update(saved)

                def emit_list(lst):
                    i = 0
                    while i < len(lst):
                        o = lst[i]
                        if o.region is None:
                            emit_op(o)
                            i += 1
                            continue
                        uid = o.region[0]
                        j = i
                        byslot = {}
                        while j < len(lst) and lst[j].region is not None and lst[j].region[0] == uid:
                            byslot.setdefault(lst[j].region[1], []).append(lst[j])
                            j += 1
                        emit_range(byslot, min(byslot), 16)
                        i = j

                i = 0
                while i < len(ops):
                    o = ops[i]
                    if o.outer is None:
                        j = i
                        while j < len(ops) and ops[j].outer is None:
                            j += 1
                        emit_list(ops[i:j])
                        i = j
                        continue
                    ou = o.outer
                    j = i
                    while j < len(ops) and ops[j].outer is ou:
                        j += 1
                    grp = ops[i:j]
                    saved = dict(waited)
                    with e.If_lt(self.regs2[ename], -ou[1]):
                        emit_list(grp)
                    with e.Else():
                        waited.clear()
                        waited.update(saved)
                        else_bulk(grp)
                    waited.clear()
                    waited.update(saved)
                    i = j
            return body

        block.tensor(emit_engine("pe"))
        block.scalar(emit_engine("act"))
        block.vector(emit_engine("dve"))
        block.gpsimd(emit_engine("pool"))
        block.sync(emit_engine("sp"))


def _mk_prog():
    p = Prog()
    p.dma_order = []
    _add = p.add

    def add(eng, fn, reads=(), writes=(), dma=False):
        o = _add(eng, fn, reads, writes, dma)
        if dma:
            p.dma_order.append(o)
        return o
    p.add = add
    return p


class Ctx:
    pass


def build_program(stages=(0, 1, 2, 3, 4), dbg=False, sparse=True):
    nc = bass.Bass("TRN2", target_bir_lowering=False)
    P = _mk_prog()
    g = Ctx()
    g.nc, g.P = nc, P

    def din(name, shape, dt=F32):
        return nc.dram_tensor(name, list(shape), dt, kind="ExternalInput").ap()

    g.x = din("x", [S, D])
    g.ctx = din("ctx", [CTX, D])
    g.cT = din("cT", [128, NK, 2])
    g.ada_w = din("ada_w", [2, D, 6 * D])
    g.ada_b2 = din("ada_b2", [2, 2, 6 * D])
    g.gT = din("gT", [128, 4, NK])
    g.fng = din("fng", [128, D])
    g.w_in0 = din("w_in0", [D, 4096])
    g.w_out0 = din("w_out0", [D, D])
    g.rpbt = din("rpbt", [16, 128, 1664])
    g.csc = din("csc", [128, 2, 512], BF16)
    g.dft = din("dft", [2, 4, 128, NK, 512], BF16)
    g.w_in1 = din("w_in1", [D, 4096])
    g.cvp = din("cvp", [128, 6, NK])
    g.dww = din("dww", [128, NK, 31])
    g.w_out1 = din("w_out1", [D, D])
    g.bout = din("bout", [128, D])
    g.rw = din("rw", [128, NK, NE])
    g.rb = din("rb", [128, NE])
    g.wg = din("wg", [2, NE, D, FE])
    g.wu = din("wu", [2, NE, D, FE])
    g.wd = din("wd", [2, NE, FE, D])
    g.ident = din("ident", [128, 128])
    g.out = nc.dram_tensor("out", [S, D], F32, kind="ExternalOutput").ap()
    kind = "ExternalOutput" if dbg else "Internal"
    g.xs = [nc.dram_tensor("xs%d" % i, [S, D], F32, kind=kind).ap() for i in range(3)]
    g.cst = din("cst", [128, 160])
    g.xg_all = nc.dram_tensor("xg_all", [NE * 2048, D], BF16, kind="Internal").ap()
    g.yg_all = nc.dram_tensor("yg_all", [NE * 2048, D], F32, kind="Internal").ap()
    g.xg_tk, g.yg_tk = Tk(), Tk()

    ARENA = 51456
    with (
        nc.sbuf_tensor("arena", [128, ARENA], F32) as arena,
        nc.sbuf_tensor("pers", [128, 1128], F32) as pers,
        nc.psum_tensor("ps0", [128, 512], F32) as ps0, nc.psum_tensor("ps1", [128, 512], F32) as ps1,
        nc.psum_tensor("ps2", [128, 512], F32) as ps2, nc.psum_tensor("ps3", [128, 512], F32) as ps3,
        nc.psum_tensor("ps4", [128, 512], F32) as ps4, nc.psum_tensor("ps5", [128, 512], F32) as ps5,
        nc.psum_tensor("ps6", [128, 512], F32) as ps6, nc.psum_tensor("ps7", [128, 512], F32) as ps7,
    ):
        g.arena = arena
        g.ps = [ps0, ps1, ps2, ps3, ps4, ps5, ps6, ps7]
        g.pst = [Tk() for _ in range(8)]
        g.pers = pers
        g.ident32 = pers[:, 0:128]
        g.modT = [pers[:, 128:224].rearrange("p (j k) -> p j k", j=6), pers[:, 224:320].rearrange("p (j k) -> p j k", j=6)]
        g.modcT = pers[:, 320:352].rearrange("p (j k) -> p j k", j=2)
        g.gTs = pers[:, 352:416].rearrange("p (j k) -> p j k", j=4)
        g.AB = pers[:, 416:480].rearrange("p (j k) -> p j k", j=4)
        g.ones32 = pers[:, 480:608]
        g.selA = pers[0:2, 608:736]
        g.cTs = pers[:, 736:768].rearrange("p (k c) -> p k c", c=2)
        g.small = pers[:, 768:1128]
        g.tk_pers = Tk()
        g.tk_mod = Tk()
        g.tk_AB = Tk()

        from_stage = {}
        setup_consts(g)
        if 0 in stages:
            stage_ada(g)
        P.barrier()
        if 1 in stages:
            stage_mixer0(g, g.x, g.xs[0])
            P.barrier()
        if 2 in stages:
            (stage_moe_sparse if sparse else stage_moe)(g, 0, g.xs[0] if 1 in stages else g.x, g.xs[1], final=False)
            P.barrier()
        if 3 in stages:
            stage_conv(g, g.xs[1] if 2 in stages else g.x, g.xs[2])
            P.barrier()
        if 4 in stages:
            (stage_moe_sparse if sparse else stage_moe)(g, 1, g.xs[2] if 3 in stages else g.x, g.out, final=True)
            P.barrier()
        P.add("sp", None)

        nsem = 100
        import contextlib
        with contextlib.ExitStack() as st:
            sems = [st.enter_context(nc.semaphore("s%d" % i)) for i in range(nsem)]
            P.regs = {"pe": st.enter_context(nc.tensor.register("r_pe")), "act": st.enter_context(nc.scalar.register("r_act")),
                      "dve": st.enter_context(nc.vector.register("r_dve")), "pool": st.enter_context(nc.gpsimd.register("r_pool")),
                      "sp": st.enter_context(nc.sync.register("r_sp"))}
            P.regs2 = {"pe": st.enter_context(nc.tensor.register("r2_pe")), "act": st.enter_context(nc.scalar.register("r2_act")),
                       "dve": st.enter_context(nc.vector.register("r2_dve")), "pool": st.enter_context(nc.gpsimd.register("r2_pool")),
                       "sp": st.enter_context(nc.sync.register("r2_sp"))}
            block = st.enter_context(nc.Block())
            P.run_emit(nc, block, None, sems)
    return nc


def arena_f32(g, off, n):
    return g.arena[:, off:off + n]


def arena_bf(g, off, n):
    return g.arena[:, off:off + n // 2].bitcast(BF16)


def setup_consts(g):
    P = g.P
    tk = g.tk_pers
    P.add("sp", lambda e: e.dma_start(out=g.ident32, in_=g.ident), [], [tk], dma=True)
    P.add("sp", lambda e: e.dma_start(out=g.gTs, in_=g.gT), [], [tk], dma=True)
    P.add("sp", lambda e: e.dma_start(out=g.cTs, in_=g.cT), [], [tk], dma=True)
    P.add("pool", lambda e: e.memset(g.ones32, 1.0), [], [tk])
    P.add("pool", lambda e: e.memset(g.selA, 0.0), [], [tk])
    P.add("pool", lambda e: e.memset(g.pers[0:1, 608:736], 1.0), [], [tk])


def stage_ada(g):
    P = g.P
    stg = [arena_f32(g, i * 2048, 2048).rearrange("p (k n) -> p k n", k=4) for i in range(4)]
    stg_tk = [Tk() for _ in range(4)]
    bias = [g.arena[0:2, 8192 + i * 512: 8192 + (i + 1) * 512] for i in range(2)]
    bias_tk = [Tk(), Tk()]
    mrow = [g.arena[0:2, 9216 + i * 512: 9216 + (i + 1) * 512] for i in range(2)]
    mrow_tk = [Tk(), Tk()]
    sT = g.small[:, 0:32].rearrange("p (k c) -> p k c", c=2)
    tk_s = Tk()
    P.add("act", lambda e: e.activation(out=sT, in_=g.cTs, func=AF.Silu), [g.tk_pers], [tk_s])
    u = 0
    for l in range(2):
        for nb in range(24):
            j, q = divmod(nb, 4)
            pm = g.ps[nb % 2][0:2, :]
            pm_tk = g.pst[nb % 2]
            for kk in range(4):
                slot = u % 4
                u += 1
                src = g.ada_w[l, kk * 512:(kk + 1) * 512, nb * 512:(nb + 1) * 512].rearrange("(k p) n -> p k n", p=128)
                P.add("sp", lambda e, o=stg[slot], s=src: e.dma_start(out=o, in_=s), [], [stg_tk[slot]], dma=True)
                for k4 in range(4):
                    k = kk * 4 + k4
                    P.add("pe", lambda e, o=pm, a=sT[:, k, :], b=stg[slot][:, k4, :], st=(k == 0), sp=(k == 15):
                          e.matmul(o, a, b, start=st, stop=sp), [tk_s, stg_tk[slot]], [pm_tk])
            bb = nb % 2
            P.add("sp", lambda e, o=bias[bb], s=g.ada_b2[l, :, nb * 512:(nb + 1) * 512]: e.dma_start(out=o, in_=s),
                  [], [bias_tk[bb]], dma=True)
            P.add("dve", lambda e, o=mrow[bb], a=pm, b=bias[bb]: e.tensor_tensor(out=o, in0=a, in1=b, op=ALU.add),
                  [pm_tk, bias_tk[bb]], [mrow_tk[bb]])
            pt = g.ps[2 + bb][:, 0:8]
            pt_tk = g.pst[2 + bb]
            for qq in range(4):
                P.add("pe", lambda e, o=pt[:, qq * 2:qq * 2 + 2], i=mrow[bb][0:2, qq * 128:(qq + 1) * 128]:
                      e.transpose(o, i, g.ident32[0:2, 0:2]), [mrow_tk[bb], g.tk_pers], [pt_tk])
            ptv = pt.rearrange("p (q r) -> p q r", r=2)
            P.add("dve", lambda e, o=g.modT[l][:, j, q * 4:(q + 1) * 4], i=ptv[:, :, 0]: e.tensor_copy(out=o, in_=i),
                  [pt_tk], [g.tk_mod])
            if l == 0 and j < 2:
                P.add("dve", lambda e, o=g.modcT[:, j, q * 4:(q + 1) * 4], i=ptv[:, :, 1]: e.tensor_copy(out=o, in_=i),
                      [pt_tk], [g.tk_mod])


def prep_AB(g, gi, scaleT, shiftT, slot):
    P = g.P
    A = g.AB[:, slot, :]
    B = g.AB[:, slot + 1, :]
    P.add("dve", lambda e: e.scalar_tensor_tensor(out=A, in0=scaleT, scalar=1.0, in1=g.gTs[:, gi, :], op0=ALU.add, op1=ALU.mult),
          [g.tk_mod, g.tk_pers], [g.tk_AB])
    P.add("dve", lambda e: e.tensor_copy(out=B, in_=shiftT), [g.tk_mod], [g.tk_AB])
    return A, B


def make_bc(g, srcT, dst, dst_tk, tmp_off):
    P = g.P
    dg = [arena_f32(g, tmp_off + i * 128, 128) for i in range(2)]
    dg_tk = [Tk(), Tk()]
    for k in range(NK):
        s = k % 2
        P.add("pool", lambda e, o=dg[s], sc=srcT[:, k:k + 1]: e.tensor_scalar(out=o, in0=g.ident32, scalar1=sc, scalar2=None, op0=ALU.mult),
              [g.tk_mod, g.tk_pers, g.tk_AB], [dg_tk[s]])
        bank = 4 + (k // 4) % 2
        P.add("pe", lambda e, o=g.ps[bank][:, (k % 4) * 128:(k % 4 + 1) * 128], b=dg[s]: e.matmul(o, g.ones32, b, start=True, stop=True),
              [dg_tk[s], g.tk_pers], [g.pst[bank]])
        if k % 4 == 3:
            c0 = (k // 4) * 512
            P.add("act", lambda e, o=dst[:, c0:c0 + 512], i=g.ps[bank][:, :]: e.activation(out=o, in_=i, func=AF.Copy),
                  [g.pst[bank]], [dst_tk])


def norm_transpose(g, src, src_tk, A, B, ab_slot_reads, dstf, tmp, it, router=None, nodst=False, phase=0):
    P = g.P
    nr = len(tmp["xn"])
    r = it % nr
    ss, ss_tk = tmp["ss"][it % 2]
    sq, sq_tk = tmp["sq"][it % 2]
    xn, xn_tk = tmp["xn"][r]
    junk, junk_tk = tmp["junk"]
    if phase in (0, 1):
        P.add("dve", lambda e: e.memset(ss, 0.0), [], [ss_tk])
        P.add("act", lambda e: e.activation(out=junk, in_=src, func=AF.Square, accum_out=ss), [src_tk], [junk_tk, ss_tk])
        P.add("act", lambda e: e.activation(out=sq, in_=ss, func=AF.Sqrt, bias=tmp["eps"], scale=1.0 / D), [ss_tk, g.tk_pers], [sq_tk])
        P.add("dve", lambda e: e.reciprocal(out=sq, in_=sq), [sq_tk], [sq_tk])
        P.add("act", lambda e: e.activation(out=xn, in_=src, func=AF.Copy, scale=sq), [src_tk, sq_tk], [xn_tk])
    if phase == 1:
        return
    for b in range(4):
        for c in range(4):
            k = b * 4 + c
            P.add("pe", lambda e, o=g.ps[b][:, c * 128:(c + 1) * 128], i=xn[:, k * 128:(k + 1) * 128]: e.transpose(o, i, g.ident32),
                  [xn_tk, g.tk_pers], [g.pst[b]])
    t32s = []
    for b in range(4):
        if router is not None:
            t32, t32_tk = tmp["t32"][(it * 4 + b) % len(tmp["t32"])]
            t32s.append((t32, t32_tk))
        if not nodst:
            dst, dst_tk = dstf(b)
        for c in range(4):
            k = b * 4 + c
            pc = g.ps[b][:, c * 128:(c + 1) * 128]
            if router is not None:
                o_, wtk = t32[:, c * 128:(c + 1) * 128], t32_tk
            else:
                o_, wtk = dst[:, c, :], dst_tk
            if c % 2 == 0:
                P.add("act", lambda e, o=o_, i=pc, k=k: e.activation(out=o, in_=i, func=AF.Identity, scale=A[:, k:k + 1], bias=B[:, k:k + 1]), [g.pst[b], g.tk_AB], [wtk])
            else:
                P.add("dve", lambda e, o=o_, i=pc, k=k: e.tensor_scalar(out=o, in0=i, scalar1=A[:, k:k + 1], scalar2=B[:, k:k + 1], op0=ALU.mult, op1=ALU.add), [g.pst[b], g.tk_AB], [wtk])
        if router is not None and not nodst:
            P.add("act", lambda e, o=dst, i=t32.rearrange("p (c n) -> p c n", c=4): e.activation(out=o, in_=i, func=AF.Copy), [t32_tk], [dst_tk])
    if router is not None:
        lg, lg_tk, rws, rw_tk = router
        for b in range(4):
            t32, t32_tk = t32s[b]
            for c in range(4):
                k = b * 4 + c
                P.add("pe", lambda e, o=lg, a=t32[:, c * 128:(c + 1) * 128], w=rws[:, k, :], st=(k == 0), sp=(k == 15):
                      e.matmul(o, a, w, start=st, stop=sp), [t32_tk, rw_tk], [lg_tk])


def stage_moe(g, l, xsrc, xdst, final):
    P = g.P
    A, B = prep_AB(g, 2 * l + 1, g.modT[l][:, 4, :], g.modT[l][:, 3, :], 0)
    ACC, HT, W0 = 0, 16384, 24576
    STG, GU, DN, AT0, CBC, G2, TMP, COMBT, SELE = 24576, 28672, 32768, 40960, 45056, 46080, 48128, 50176, 51200
    acc = [arena_f32(g, ACC + t * 2048, 2048) for t in range(8)]
    acc_tk = [Tk() for _ in range(8)]
    hT = arena_bf(g, HT, 16384).rearrange("p (k n) -> p k n", k=NK)
    hT_tk = [Tk() for _ in range(8)]
    g2bc = arena_f32(g, G2, 2048)
    g2_tk = Tk()
    make_bc(g, g.modT[l][:, 5, :], g2bc, g2_tk, TMP)
    rws = g.small[:, 64:64 + 256].rearrange("p (k e) -> p k e", e=NE)
    rw_tk = Tk()
    rbs = g.small[:, 320:336]
    eps = g.small[:, 336:337]
    P.add("sp", lambda e: e.dma_start(out=rws, in_=g.rw), [], [rw_tk], dma=True)
    P.add("sp", lambda e: e.dma_start(out=rbs, in_=g.rb), [], [rw_tk], dma=True)
    P.add("pool", lambda e: e.memset(eps, EPS), [], [g.tk_pers])
    for tb in range(2):
        tmp = {
            "ss": [(g.small[:, 340 + i:341 + i], Tk()) for i in range(2)],
            "sq": [(g.small[:, 344 + i:345 + i], Tk()) for i in range(2)],
            "xn": [(arena_f32(g, W0 + i * 2048, 2048), Tk()) for i in range(2)],
            "junk": (arena_bf(g, W0 + 4096, 2048), Tk()),
            "t32": [(arena_f32(g, W0 + 5120 + i * 512, 512), Tk()) for i in range(2)],
            "eps": eps,
        }
        lgps = g.ps[6]
        lg_tk = g.pst[6]
        for t in range(8):
            i = tb * 8 + t
            P.add("sp", lambda e, o=acc[t], s=xsrc[i * 128:(i + 1) * 128, :]: e.dma_start(out=o, in_=s), [], [acc_tk[t]], dma=True)
            norm_transpose(g, acc[t], acc_tk[t], A, B, None,
                           lambda b, t=t: (hT[:, b * 4:(b + 1) * 4, t * 128:(t + 1) * 128], hT_tk[t]),
                           tmp, t, router=(lgps[:, t * 16:(t + 1) * 16], lg_tk, rws, rw_tk))
        RB = W0 + 6144

        def rt(n, w):
            return arena_f32(g, RB + n * 128, w)
        sc, sel, eq, msk, w_, comb = rt(0, 128), rt(1, 128), rt(2, 128), rt(3, 128), rt(4, 128), rt(5, 128)
        m1, m2, gs, ing = rt(6, 32), rt(7, 32), rt(8, 32), rt(9, 32)
        gmax, wsum = rt(10, 8), rt(11, 8)
        rtk = Tk()

        def v3(a, x, y):
            return a.rearrange("p (x y) -> p x y", x=x)
        P.add("act", lambda e: e.activation(out=sc, in_=lgps[:, 0:128], func=AF.Sigmoid), [lg_tk], [rtk])
        P.add("dve", lambda e: e.tensor_tensor(out=v3(sel, 8, 16), in0=v3(sc, 8, 16), in1=rbs.unsqueeze(1).to_broadcast([128, 8, 16]), op=ALU.add), [rtk, rw_tk], [rtk])
        P.add("dve", lambda e: e.tensor_reduce(out=m1, in_=v3(sel, 32, 4), axis=AX.X, op=ALU.max), [rtk], [rtk])
        P.add("dve", lambda e: e.tensor_tensor(out=v3(eq, 32, 4), in0=v3(sel, 32, 4), in1=m1.unsqueeze(2).to_broadcast([128, 32, 4]), op=ALU.is_equal), [rtk], [rtk])
        P.add("dve", lambda e: e.scalar_tensor_tensor(out=msk, in0=eq, scalar=-1e9, in1=sel, op0=ALU.mult, op1=ALU.add), [rtk], [rtk])
        P.add("dve", lambda e: e.tensor_reduce(out=m2, in_=v3(msk, 32, 4), axis=AX.X, op=ALU.max), [rtk], [rtk])
        P.add("dve", lambda e: e.tensor_tensor(out=gs, in0=m1, in1=m2, op=ALU.add), [rtk], [rtk])
        P.add("dve", lambda e: e.tensor_reduce(out=gmax, in_=v3(gs, 8, 4), axis=AX.X, op=ALU.max), [rtk], [rtk])
        P.add("dve", lambda e: e.tensor_tensor(out=v3(ing, 8, 4), in0=v3(gs, 8, 4), in1=gmax.unsqueeze(2).to_broadcast([128, 8, 4]), op=ALU.is_equal), [rtk], [rtk])
        P.add("dve", lambda e: e.tensor_tensor(out=v3(eq, 32, 4), in0=v3(sel, 32, 4), in1=m2.unsqueeze(2).to_broadcast([128, 32, 4]), op=ALU.is_ge), [rtk], [rtk])
        P.add("dve", lambda e: e.tensor_tensor(out=v3(msk, 32, 4), in0=v3(eq, 32, 4), in1=ing.unsqueeze(2).to_broadcast([128, 32, 4]), op=ALU.mult), [rtk], [rtk])
        P.add("dve", lambda e: e.tensor_tensor(out=w_, in0=sc, in1=msk, op=ALU.mult), [rtk], [rtk])
        P.add("dve", lambda e: e.tensor_reduce(out=wsum, in_=v3(w_, 8, 16), axis=AX.X, op=ALU.add), [rtk], [rtk])
        P.add("dve", lambda e: e.reciprocal(out=wsum, in_=wsum), [rtk], [rtk])
        P.add("dve", lambda e: e.tensor_tensor(out=v3(comb, 8, 16), in0=v3(w_, 8, 16), in1=wsum.unsqueeze(2).to_broadcast([128, 8, 16]), op=ALU.mult), [rtk], [rtk])
        combT = g.arena[0:16, COMBT: COMBT + 1024]
        combT_tk = Tk()
        for half in range(2):
            bank = 4 + half
            for t4 in range(4):
                t = half * 4 + t4
                P.add("pe", lambda e, o=g.ps[bank][0:16, t4 * 128:(t4 + 1) * 128], i=comb[:, t * 16:(t + 1) * 16]: e.transpose(o, i, g.ident32),
                      [rtk, g.tk_pers], [g.pst[bank]])
            P.add("act", lambda e, o=combT[:, half * 512:(half + 1) * 512], i=g.ps[bank][0:16, :]: e.activation(out=o, in_=i, func=AF.Copy),
                  [g.pst[bank]], [combT_tk])
        sel2 = [g.arena[0:16, SELE + i * 128: SELE + (i + 1) * 128] for i in range(2)]
        sel2_tk = [Tk(), Tk()]
        P.barrier()
        stg = [arena_f32(g, STG + i * 2048, 2048) for i in range(2)]
        stg_tk = [Tk() for _ in range(2)]
        gub = [arena_bf(g, GU + i * 1024, 2048) for i in range(4)]
        gu_tk = [Tk() for _ in range(4)]
        dnb = [arena_bf(g, DN + i * 1024, 2048) for i in range(8)]
        dn_tk = [Tk() for _ in range(8)]
        ATb = [arena_bf(g, AT0 + i * 2048, 4096).rearrange("p (f n) -> p f n", f=4) for i in range(2)]
        AT_tk = [Tk(), Tk()]
        cbc = [arena_bf(g, CBC + i * 512, 1024) for i in range(2)]
        cbc_tk = [Tk(), Tk()]
        sgt = [arena_f32(g, TMP + i * 512, 512) for i in range(2)]
        sg_tk = [Tk(), Tk()]
        t2t = [arena_f32(g, TMP + 1024 + i * 512, 512) for i in range(2)]
        t2_tk = [Tk(), Tk()]
        units = []
        for ex in range(NE):
            for f in range(4):
                units.append(("g", ex, f))
                units.append(("u", ex, f))
                units.append(("d", ex, f))
        state = {"dma": 0, "cast": 0}

        def unit_src(u):
            kind, ex, f = u
            if kind == "g":
                return g.wg[l, ex, :, f * 128:(f + 1) * 128].rearrange("(k p) n -> p k n", p=128)
            if kind == "u":
                return g.wu[l, ex, :, f * 128:(f + 1) * 128].rearrange("(k p) n -> p k n", p=128)
            return g.wd[l, ex, f * 128:(f + 1) * 128, :]

        def unit_dst(n):
            kind, ex, f = units[n]
            if kind == "d":
                s = (ex % 2) * 4 + f
                return dnb[s], dn_tk[s]
            s = (2 * (ex * 4 + f) + (1 if kind == "u" else 0)) % 4
            return gub[s], gu_tk[s]

        def issue_dma(upto):
            while state["dma"] < min(upto, len(units)):
                n = state["dma"]
                kind = units[n][0]
                s = n % 2
                o = stg[s] if kind == "d" else stg[s].rearrange("p (k n) -> p k n", k=NK)
                P.add("sp", lambda e, o=o, sr=unit_src(units[n]): e.dma_start(out=o, in_=sr), [], [stg_tk[s]], dma=True)
                state["dma"] += 1

        def issue_cast(upto):
            while state["cast"] < min(upto, len(units)):
                n = state["cast"]
                issue_dma(n + 2)
                kind = units[n][0]
                s = n % 2
                dst, dtk = unit_dst(n)
                if kind == "d":
                    P.add("pool", lambda e, o=dst, i=stg[s]: e.tensor_tensor(out=o, in0=i, in1=g2bc, op=ALU.mult), [stg_tk[s], g2_tk], [dtk])
                elif kind == "g":
                    P.add("act", lambda e, o=dst, i=stg[s]: e.activation(out=o, in_=i, func=AF.Copy), [stg_tk[s]], [dtk])
                else:
                    P.add("dve", lambda e, o=dst, i=stg[s]: e.tensor_copy(out=o, in_=i), [stg_tk[s]], [dtk])
                state["cast"] += 1

        issue_cast(6)
        it = 0
        for ex in range(NE):
            c = ex % 2
            P.add("pool", lambda e, o=sel2[c], i=g.ident32[0:16, ex:ex + 1].to_broadcast([16, 128]): e.tensor_copy(out=o, in_=i), [g.tk_pers], [sel2_tk[c]])
            for sb in range(2):
                bank = 4 + sb
                P.add("pe", lambda e, o=g.ps[bank][:, :], a=sel2[c], b=combT[:, sb * 512:(sb + 1) * 512]: e.matmul(o, a, b, start=True, stop=True),
                      [sel2_tk[c], combT_tk], [g.pst[bank]])
                P.add("act", lambda e, o=cbc[c][:, sb * 512:(sb + 1) * 512], i=g.ps[bank][:, :]: e.activation(out=o, in_=i, func=AF.Copy),
                      [g.pst[bank]], [cbc_tk[c]])
            for f in range(4):
                n0 = (ex * 4 + f) * 3
                issue_cast(n0 + 6)
                gw, gtk = unit_dst(n0)
                uw, utk = unit_dst(n0 + 1)
                gw3 = gw.rearrange("p (k n) -> p k n", k=NK)
                uw3 = uw.rearrange("p (k n) -> p k n", k=NK)
                for sb in range(2):
                    bg, bu = (0, 1) if it % 2 == 0 else (2, 3)
                    for k in range(NK):
                        P.add("pe", lambda e, o=g.ps[bg][:, :], a=gw3[:, k, :], b=hT[:, k, sb * 512:(sb + 1) * 512], st=(k == 0), sp=(k == NK - 1):
                              e.matmul(o, a, b, start=st, stop=sp), [gtk] + hT_tk[sb * 4:sb * 4 + 4], [g.pst[bg]])
                    for k in range(NK):
                        P.add("pe", lambda e, o=g.ps[bu][:, :], a=uw3[:, k, :], b=hT[:, k, sb * 512:(sb + 1) * 512], st=(k == 0), sp=(k == NK - 1):
                              e.matmul(o, a, b, start=st, stop=sp), [utk] + hT_tk[sb * 4:sb * 4 + 4], [g.pst[bu]])
                    r = it % 2
                    P.add("act", lambda e, o=sgt[r], i=g.ps[bg][:, :]: e.activation(out=o, in_=i, func=AF.Silu), [g.pst[bg]], [sg_tk[r]])
                    P.add("dve", lambda e, o=t2t[r], a=g.ps[bu][:, :], b=sgt[r]: e.tensor_tensor(out=o, in0=a, in1=b, op=ALU.mult), [g.pst[bu], sg_tk[r]], [t2_tk[r]])
                    P.add("pool", lambda e, o=ATb[c][:, f, sb * 512:(sb + 1) * 512], a=t2t[r], b=cbc[c][:, sb * 512:(sb + 1) * 512]:
                          e.tensor_tensor(out=o, in0=a, in1=b, op=ALU.mult), [t2_tk[r], cbc_tk[c]], [AT_tk[c]])
                    it += 1
            for t in range(8):
                for db in range(4):
                    bank = 6 + (t * 4 + db) % 2
                    for f in range(4):
                        dw, dtk = dnb[(ex % 2) * 4 + f], dn_tk[(ex % 2) * 4 + f]
                        P.add("pe", lambda e, o=g.ps[bank][:, :], a=ATb[c][:, f, t * 128:(t + 1) * 128], b=dw[:, db * 512:(db + 1) * 512], st=(f == 0), sp=(f == 3):
                              e.matmul(o, a, b, start=st, stop=sp), [AT_tk[c], dtk], [g.pst[bank]])
                    P.add("dve", lambda e, o=acc[t][:, db * 512:(db + 1) * 512], i=g.ps[bank][:, :]: e.tensor_tensor(out=o, in0=i, in1=o, op=ALU.add),
                          [g.pst[bank], acc_tk[t]], [acc_tk[t]])
        P.barrier()
        if final:
            fng = arena_f32(g, W0, 2048)
            fng_tk = Tk()
            P.add("sp", lambda e: e.dma_start(out=fng, in_=g.fng), [], [fng_tk], dma=True)
            junk = arena_bf(g, W0 + 2048, 2048)
            junk_tk = Tk()
            fin_ss_tk = [Tk(), Tk()]
            for t in range(8):
                i = tb * 8 + t
                ss, ss_tk = g.small[:, 340 + t % 2:341 + t % 2], fin_ss_tk[t % 2]
                P.add("pool", lambda e, o=ss: e.memset(o, 0.0), [], [ss_tk])
                P.add("act", lambda e, o=junk, i_=acc[t], a=ss: e.activation(out=o, in_=i_, func=AF.Square, accum_out=a), [acc_tk[t]], [junk_tk, ss_tk])
                P.add("act", lambda e, o=ss: e.activation(out=o, in_=o, func=AF.Sqrt, bias=eps, scale=1.0 / D), [ss_tk, g.tk_pers], [ss_tk])
                P.add("dve", lambda e, o=ss: e.reciprocal(out=o, in_=o), [ss_tk], [ss_tk])
                P.add("dve", lambda e, o=acc[t], sc_=ss: e.scalar_tensor_tensor(out=o, in0=o, scalar=sc_, in1=fng, op0=ALU.mult, op1=ALU.mult), [acc_tk[t], ss_tk, fng_tk], [acc_tk[t]])
                P.add("sp", lambda e, o=xdst[i * 128:(i + 1) * 128, :], s=acc[t]: e.dma_start(out=o, in_=s), [acc_tk[t]], [], dma=True)
        else:
            for t in range(8):
                i = tb * 8 + t
                P.add("sp", lambda e, o=xdst[i * 128:(i + 1) * 128, :], s=acc[t]: e.dma_start(out=o, in_=s), [acc_tk[t]], [], dma=True)
        P.barrier()


def stage_moe_sparse(g, l, xsrc, xdst, final):
    P = g.P
    I32 = mybir.dt.int32
    U16 = mybir.dt.uint16
    A, B = prep_AB(g, 2 * l + 1, g.modT[l][:, 4, :], g.modT[l][:, 3, :], 0)
    NTL = NT
    H2, ABC, BBC, XT, XN, JK, T32, RB = 0, 16384, 18432, 20480, 26624, 32768, 33792, 37888
    PERS = 51200
    h2tok = arena_bf(g, H2, 32768).rearrange("p (t n) -> p t n", t=NTL)
    h2_tk = [Tk() for _ in range(NTL)]
    Abc, Bbc = arena_f32(g, ABC, 2048), arena_f32(g, BBC, 2048)
    Abc_tk, Bbc_tk = Tk(), Tk()
    make_bc(g, A, Abc, Abc_tk, RB)
    make_bc(g, B, Bbc, Bbc_tk, RB + 256)
    rws = g.small[:, 64:64 + 256].rearrange("p (k e) -> p k e", e=NE)
    rw_tk = Tk()
    rbs = g.small[:, 320:336]
    eps = g.small[:, 336:337]
    P.add("sp", lambda e: e.dma_start(out=rws, in_=g.rw), [], [rw_tk], dma=True)
    P.add("sp", lambda e: e.dma_start(out=rbs, in_=g.rb), [], [rw_tk], dma=True)
    P.add("pool", lambda e: e.memset(eps, EPS), [], [g.tk_pers])
    desti = g.arena[:, PERS:PERS + 32].bitcast(I32)
    cw = arena_f32(g, PERS + 32, 32)
    cntneg = g.arena[0:1, PERS + 64:PERS + 82].bitcast(I32)
    maskbits = g.arena[:, PERS + 96:PERS + 224].bitcast(U16)
    pers_tk = Tk()
    xt = [arena_f32(g, XT + i * 2048, 2048) for i in range(3)]
    xt_tk = [Tk(), Tk(), Tk()]
    tmp = {
        "ss": [(g.small[:, 340 + i:341 + i], Tk()) for i in range(2)],
        "sq": [(g.small[:, 344 + i:345 + i], Tk()) for i in range(2)],
        "xn": [(arena_f32(g, XN + i * 2048, 2048), Tk()) for i in range(3)],
        "junk": (arena_bf(g, JK, 2048), Tk()),
        "t32": [(arena_f32(g, T32 + i * 512, 512), Tk()) for i in range(8)],
        "eps": eps,
    }
    lgps = g.ps[6]
    lg_tk = g.pst[6]
    def n_stage1(t):
        if t < NTL:
            r_ = t % 3
            P.add("sp", lambda e, o=xt[r_], s=xsrc[t * 128:(t + 1) * 128, :]: e.dma_start(out=o, in_=s), [], [xt_tk[r_]], dma=True)
            norm_transpose(g, xt[r_], xt_tk[r_], A, B, None, None, tmp, t, router=(None, None, None, None), nodst=True, phase=1)
    n_stage1(0)
    for t in range(NTL):
        r = t % 3
        n_stage1(t + 1)
        norm_transpose(g, xt[r], xt_tk[r], A, B, None, None, tmp, t, router=(lgps[:, t * 16:(t + 1) * 16], lg_tk, rws, rw_tk), nodst=True, phase=2)
        xn, xn_tk = tmp["xn"][r]
        P.add("dve", lambda e, o=xn: e.tensor_tensor(out=o, in0=o, in1=Abc, op=ALU.mult), [xn_tk, Abc_tk], [xn_tk])
        P.add("pool", lambda e, o=h2tok[:, t, :], i=xn: e.tensor_tensor(out=o, in0=i, in1=Bbc, op=ALU.add), [xn_tk, Bbc_tk], [h2_tk[t]])
    W = NTL * 16

    def rt(n, w=W):
        return arena_f32(g, RB + n * 256, w)
    sc, sel, eq, msk, w_, comb = rt(0), rt(1), rt(2), rt(3), rt(4), rt(5)
    m1, m2, gs, ing = rt(6, 64), rt(7, 64), rt(8, 64), rt(9, 64)
    gmax, wsum = arena_f32(g, RB + 10 * 256, 16), arena_f32(g, RB + 10 * 256 + 16, 16)
    within, cntbc, pref, dfull, ta, tb2 = rt(11), rt(12), rt(13), rt(14), rt(15), rt(16)
    d01 = arena_f32(g, RB + 17 * 256, 32)
    cntf = arena_f32(g, RB + 17 * 256 + 32, 16)
    cnti = g.arena[:, RB + 17 * 256 + 48: RB + 17 * 256 + 64].bitcast(I32)
    cst = arena_f32(g, RB + 18 * 256, 160)
    validf = arena_f32(g, RB + 19 * 256, 256)
    rtk = Tk()
    cst_tk = Tk()
    P.add("sp", lambda e: e.dma_start(out=cst, in_=g.cst), [], [cst_tk], dma=True)
    Utri, posc, ebase = cst[:, 0:128], cst[:, 128:144], cst[:, 144:160]

    def v3(a, x, y):
        return a.rearrange("p (x y) -> p x y", x=x)
    P.add("act", lambda e: e.activation(out=sc, in_=lgps[:, 0:W], func=AF.Sigmoid), [lg_tk], [rtk])
    P.add("dve", lambda e: e.tensor_tensor(out=v3(sel, NTL, 16), in0=v3(sc, NTL, 16), in1=rbs.unsqueeze(1).to_broadcast([128, NTL, 16]), op=ALU.add), [rtk, rw_tk], [rtk])
    P.add("dve", lambda e: e.tensor_reduce(out=m1, in_=v3(sel, NTL * 4, 4), axis=AX.X, op=ALU.max), [rtk], [rtk])
    P.add("dve", lambda e: e.tensor_tensor(out=v3(eq, NTL * 4, 4), in0=v3(sel, NTL * 4, 4), in1=m1.unsqueeze(2).to_broadcast([128, NTL * 4, 4]), op=ALU.is_equal), [rtk], [rtk])
    P.add("dve", lambda e: e.scalar_tensor_tensor(out=msk, in0=eq, scalar=-1e9, in1=sel, op0=ALU.mult, op1=ALU.add), [rtk], [rtk])
    P.add("dve", lambda e: e.tensor_reduce(out=m2, in_=v3(msk, NTL * 4, 4), axis=AX.X, op=ALU.max), [rtk], [rtk])
    P.add("dve", lambda e: e.tensor_tensor(out=gs, in0=m1, in1=m2, op=ALU.add), [rtk], [rtk])
    P.add("dve", lambda e: e.tensor_reduce(out=gmax, in_=v3(gs, NTL, 4), axis=AX.X, op=ALU.max), [rtk], [rtk])
    P.add("dve", lambda e: e.tensor_tensor(out=v3(ing, NTL, 4), in0=v3(gs, NTL, 4), in1=gmax.unsqueeze(2).to_broadcast([128, NTL, 4]), op=ALU.is_equal), [rtk], [rtk])
    P.add("dve", lambda e: e.tensor_tensor(out=v3(eq, NTL * 4, 4), in0=v3(sel, NTL * 4, 4), in1=m2.unsqueeze(2).to_broadcast([128, NTL * 4, 4]), op=ALU.is_ge), [rtk], [rtk])
    P.add("dve", lambda e: e.tensor_tensor(out=v3(msk, NTL * 4, 4), in0=v3(eq, NTL * 4, 4), in1=ing.unsqueeze(2).to_broadcast([128, NTL * 4, 4]), op=ALU.mult), [rtk], [rtk])
    P.add("dve", lambda e: e.tensor_tensor(out=w_, in0=sc, in1=msk, op=ALU.mult), [rtk], [rtk])
    P.add("dve", lambda e: e.tensor_reduce(out=wsum, in_=v3(w_, NTL, 16), axis=AX.X, op=ALU.add), [rtk], [rtk])
    P.add("dve", lambda e: e.reciprocal(out=wsum, in_=wsum), [rtk], [rtk])
    P.add("dve", lambda e: e.tensor_tensor(out=v3(comb, NTL, 16), in0=v3(w_, NTL, 16), in1=wsum.unsqueeze(2).to_broadcast([128, NTL, 16]), op=ALU.mult), [rtk], [rtk])
    P.add("pe", lambda e: e.matmul(g.ps[4][:, 0:W], Utri, msk, start=True, stop=True), [rtk, cst_tk], [g.pst[4]])
    P.add("pe", lambda e: e.matmul(g.ps[5][:, 0:W], g.ones32, msk, start=True, stop=True), [rtk, g.tk_pers], [g.pst[5]])
    P.add("act", lambda e: e.activation(out=within, in_=g.ps[4][:, 0:W], func=AF.Copy), [g.pst[4]], [rtk])
    P.add("act", lambda e: e.activation(out=cntbc, in_=g.ps[5][:, 0:W], func=AF.Copy), [g.pst[5]], [rtk])
    P.add("pool", lambda e: e.memset(pref[:, 0:16], 0.0), [rtk], [rtk])
    for t in range(1, NTL):
        P.add("dve", lambda e, t=t: e.tensor_tensor(out=pref[:, t * 16:(t + 1) * 16], in0=pref[:, (t - 1) * 16:t * 16], in1=cntbc[:, (t - 1) * 16:t * 16], op=ALU.add), [rtk], [rtk])
    P.add("dve", lambda e: e.tensor_tensor(out=cntf, in0=pref[:, (NTL - 1) * 16:NTL * 16], in1=cntbc[:, (NTL - 1) * 16:NTL * 16], op=ALU.add), [rtk], [rtk])
    P.add("dve", lambda e: e.tensor_tensor(out=dfull, in0=within, in1=pref, op=ALU.add), [rtk], [rtk])
    P.add("dve", lambda e: e.tensor_tensor(out=v3(dfull, NTL, 16), in0=v3(dfull, NTL, 16), in1=ebase.unsqueeze(1).to_broadcast([128, NTL, 16]), op=ALU.add), [rtk, cst_tk], [rtk])
    P.add("dve", lambda e: e.tensor_scalar(out=ta, in0=msk, scalar1=-1e6, scalar2=1e6, op0=ALU.mult, op1=ALU.add), [rtk], [rtk])
    P.add("dve", lambda e: e.tensor_tensor(out=ta, in0=ta, in1=dfull, op=ALU.add), [rtk], [rtk])
    P.add("dve", lambda e: e.tensor_reduce(out=d01[:, 0:16], in_=v3(ta, NTL, 16), axis=AX.X, op=ALU.min), [rtk], [rtk])
    P.add("dve", lambda e: e.tensor_tensor(out=tb2, in0=dfull, in1=msk, op=ALU.mult), [rtk], [rtk])
    P.add("dve", lambda e: e.tensor_reduce(out=d01[:, 16:32], in_=v3(tb2, NTL, 16), axis=AX.X, op=ALU.max), [rtk], [rtk])
    for j in range(2):
        P.add("dve", lambda e, j=j: e.tensor_tensor(out=v3(ta, NTL, 16), in0=v3(dfull, NTL, 16), in1=d01[:, j * 16:(j + 1) * 16].unsqueeze(2).to_broadcast([128, NTL, 16]), op=ALU.is_equal), [rtk], [rtk])
        P.add("dve", lambda e: e.tensor_tensor(out=ta, in0=ta, in1=comb, op=ALU.mult), [rtk], [rtk])
        P.add("dve", lambda e, j=j: e.tensor_reduce(out=cw[:, j * 16:(j + 1) * 16], in_=v3(ta, NTL, 16), axis=AX.X, op=ALU.add), [rtk], [pers_tk])
    P.add("dve", lambda e: e.tensor_copy(out=desti, in_=d01), [rtk], [pers_tk])
    P.add("dve", lambda e: e.tensor_scalar(out=cnti, in0=cntf, scalar1=127.0, scalar2=None, op0=ALU.add), [rtk], [rtk])
    P.add("dve", lambda e: e.tensor_scalar(out=cnti, in0=cnti, scalar1=7, scalar2=None, op0=ALU.arith_shift_right), [rtk], [rtk])
    P.add("dve", lambda e: e.tensor_scalar(out=cntneg[0:1, 0:16], in0=cnti[0:1, :], scalar1=-1, scalar2=None, op0=ALU.mult), [rtk], [pers_tk])
    P.add("dve", lambda e: e.tensor_reduce(out=cntneg[0:1, 16:17], in_=cntneg[0:1, 0:16], axis=AX.X, op=ALU.min), [pers_tk], [pers_tk])
    P.add("dve", lambda e: e.tensor_tensor(out=v3(validf, 16, 16), in0=posc.unsqueeze(1).to_broadcast([128, 16, 16]), in1=cntf.unsqueeze(2).to_broadcast([128, 16, 16]), op=ALU.is_lt), [rtk, cst_tk], [rtk])
    P.add("dve", lambda e: e.tensor_scalar(out=maskbits, in0=validf, scalar1=65535.0, scalar2=None, op0=ALU.mult), [rtk], [pers_tk])
    for t in range(NTL):
        for j in range(2):
            c = j * 16 + t
            P.add("pool", lambda e, c=c, t=t: e.indirect_dma_start(out=g.xg_all[:, :], out_offset=bass.IndirectOffsetOnAxis(ap=desti[:, c:c + 1], axis=0),
                                                                   in_=h2tok[:, t, :], in_offset=None),
                  [pers_tk, h2_tk[t]], [g.xg_tk], dma=True)
    P.barrier()
    GU, DN, G2, XG, XGT, SG, ATO, YB, IDB = 0, 16384, 24576, 26624, 30720, 34816, 35840, 36352, 40448
    Gb = [arena_bf(g, GU + i * 8192, 8192).rearrange("p (k n) -> p k n", k=NK) for i in range(2)]
    Ub = [arena_bf(g, GU + 4096 + i * 8192, 8192).rearrange("p (k n) -> p k n", k=NK) for i in range(2)]
    Db = [arena_bf(g, DN + i * 4096, 8192).rearrange("p (f n) -> p f n", f=4) for i in range(2)]
    G_tk = [[Tk() for _ in range(4)] for _ in range(2)]
    U_tk = [[Tk() for _ in range(4)] for _ in range(2)]
    D_tk = [[Tk() for _ in range(4)] for _ in range(2)]
    g2bc = arena_f32(g, G2, 2048)
    g2_tk = Tk()
    make_bc(g, g.modT[l][:, 5, :], g2bc, g2_tk, SG)
    xg = [arena_bf(g, XG + i * 1024, 2048) for i in range(4)]
    xg_tk = [Tk() for _ in range(4)]
    xgT = [arena_bf(g, XGT + i * 1024, 2048).rearrange("p (k n) -> p k n", k=NK) for i in range(4)]
    xgT_tk = [Tk() for _ in range(4)]
    sgt = [arena_f32(g, SG + i * 512, 512) for i in range(2)]
    sg_tk = [Tk(), Tk()]
    ATb = [arena_bf(g, ATO + i * 256, 512).rearrange("p (f n) -> p f n", f=4) for i in range(2)]
    AT_tk = [Tk(), Tk()]
    yb = [arena_f32(g, YB + i * 2048, 2048) for i in range(2)]
    yb_tk = [Tk(), Tk()]
    identb = arena_bf(g, IDB, 128)
    idb_tk = Tk()
    P.add("act", lambda e: e.activation(out=identb, in_=g.ident32, func=AF.Copy), [g.tk_pers], [idb_tk])

    def load_expert(ex):
        pb = ex % 2
        for q4 in range(4):
            P.add("pool", lambda e, o=Gb[pb][:, q4 * 4:(q4 + 1) * 4, :], s_=g.wg[l, ex, q4 * 512:(q4 + 1) * 512, :].rearrange("(k p) n -> p k n", p=128): e.dma_start(out=o, in_=s_),
                  [], [G_tk[pb][q4]], dma=True)
            P.add("pool", lambda e, o=Ub[pb][:, q4 * 4:(q4 + 1) * 4, :], s_=g.wu[l, ex, q4 * 512:(q4 + 1) * 512, :].rearrange("(k p) n -> p k n", p=128): e.dma_start(out=o, in_=s_),
                  [], [U_tk[pb][q4]], dma=True)
        for q4 in range(4):
            P.add("pool", lambda e, o=Db[pb][:, q4, :], s_=g.wd[l, ex, q4 * 128:(q4 + 1) * 128, :]: e.dma_start(out=o, in_=s_), [], [D_tk[pb][q4]], dma=True)

    state_it = {"it": 0}

    def tile(ex, s, uid):
        it = state_it["it"]
        P.region = (uid, s)
        r = it % 2
        state_it["it"] = it + 1
        row0 = ex * 2048 + s * 128
        P.add("sp", lambda e, o=xg[r], s_=g.xg_all[row0:row0 + 128, :]: e.dma_start(out=o, in_=s_), [g.xg_tk], [xg_tk[r]], dma=True)
        mcol = ex * 16 + s
        P.add("dve", lambda e, o=xg[r].bitcast(U16), m=maskbits[:, mcol:mcol + 1].to_broadcast([128, 2048]): e.tensor_tensor(out=o, in0=o, in1=m, op=ALU.bitwise_and),
              [xg_tk[r], pers_tk], [xg_tk[r]])
        for half in range(2):
            pb = g.ps[half][:, :].bitcast(BF16)
            for c in range(8):
                k = half * 8 + c
                P.add("pe", lambda e, o=pb[:, c * 128:(c + 1) * 128], i=xg[r][:, k * 128:(k + 1) * 128]: e.transpose(o, i, identb), [xg_tk[r], idb_tk], [g.pst[half]])
            if half == 0:
                P.add("act", lambda e, o=xgT[r][:, 0:8, :], i=pb.rearrange("p (k n) -> p k n", k=8): e.activation(out=o, in_=i, func=AF.Copy), [g.pst[half]], [xgT_tk[r]])
            else:
                P.add("dve", lambda e, o=xgT[r][:, 8:16, :], i=pb.rearrange("p (k n) -> p k n", k=8): e.tensor_copy(out=o, in_=i), [g.pst[half]], [xgT_tk[r]])
        for f in range(4):
            for k in range(NK):
                P.add("pe", lambda e, o=g.ps[2][:, f * 128:(f + 1) * 128], a=Gb[ex % 2][:, k, f * 128:(f + 1) * 128], b=xgT[r][:, k, :], st=(k == 0), sp=(k == NK - 1):
                      e.matmul(o, a, b, start=st, stop=sp), [G_tk[ex % 2][k // 4], xgT_tk[r]], [g.pst[2]])
            for k in range(NK):
                P.add("pe", lambda e, o=g.ps[3][:, f * 128:(f + 1) * 128], a=Ub[ex % 2][:, k, f * 128:(f + 1) * 128], b=xgT[r][:, k, :], st=(k == 0), sp=(k == NK - 1):
                      e.matmul(o, a, b, start=st, stop=sp), [U_tk[ex % 2][k // 4], xgT_tk[r]], [g.pst[3]])
        P.add("act", lambda e, o=sgt[r]: e.activation(out=o, in_=g.ps[2][:, :], func=AF.Silu), [g.pst[2]], [sg_tk[r]])
        P.add("dve", lambda e, o=ATb[r].rearrange("p f n -> p (f n)"), b=sgt[r]: e.tensor_tensor(out=o, in0=g.ps[3][:, :], in1=b, op=ALU.mult), [g.pst[3], sg_tk[r]], [AT_tk[r]])
        for db in range(4):
            bank = 4 + db
            for f in range(4):
                P.add("pe", lambda e, o=g.ps[bank][:, :], a=ATb[r][:, f, :], b=Db[ex % 2][:, f, db * 512:(db + 1) * 512], st=(f == 0), sp=(f == 3):
                      e.matmul(o, a, b, start=st, stop=sp), [AT_tk[r], D_tk[ex % 2][f]], [g.pst[bank]])
            P.add("dve", lambda e, o=yb[r][:, db * 512:(db + 1) * 512], i=g.ps[bank][:, :], b_=g2bc[:, db * 512:(db + 1) * 512]: e.tensor_tensor(out=o, in0=i, in1=b_, op=ALU.mult),
                  [g.pst[bank], g2_tk], [yb_tk[r]])
        P.add("sp", lambda e, o=g.yg_all[row0:row0 + 128, :], s_=yb[r]: e.dma_start(out=o, in_=s_), [yb_tk[r]], [g.yg_tk], dma=True)
        P.region = None

    def t_load(ex, s, bi):
        row0 = ex * 2048 + s * 128
        P.add("sp", lambda e, o=xg[bi], s_=g.xg_all[row0:row0 + 128, :]: e.dma_start(out=o, in_=s_), [g.xg_tk], [xg_tk[bi]], dma=True)

    def t_mask(ex, s, bi):
        mcol = ex * 16 + s
        P.add("dve", lambda e, o=xg[bi].bitcast(U16), m=maskbits[:, mcol:mcol + 1].to_broadcast([128, 2048]): e.tensor_tensor(out=o, in0=o, in1=m, op=ALU.bitwise_and),
              [xg_tk[bi], pers_tk], [xg_tk[bi]])

    def t_prep(ex, s, bi):
        for half in range(2):
            pb = g.ps[half][:, :].bitcast(BF16)
            for c in range(8):
                k = half * 8 + c
                P.add("pe", lambda e, o=pb[:, c * 128:(c + 1) * 128], i=xg[bi][:, k * 128:(k + 1) * 128]: e.transpose(o, i, identb), [xg_tk[bi], idb_tk], [g.pst[half]])
            if half == 0:
                P.add("act", lambda e, o=xgT[bi][:, 0:8, :], i=pb.rearrange("p (k n) -> p k n", k=8): e.activation(out=o, in_=i, func=AF.Copy), [g.pst[half]], [xgT_tk[bi]])
            else:
                P.add("dve", lambda e, o=xgT[bi][:, 8:16, :], i=pb.rearrange("p (k n) -> p k n", k=8): e.tensor_copy(out=o, in_=i), [g.pst[half]], [xgT_tk[bi]])

    def t_gu(ex, bi, r):
        for f in range(4):
            for k in range(NK):
                P.add("pe", lambda e, o=g.ps[2][:, f * 128:(f + 1) * 128], a=Gb[ex % 2][:, k, f * 128:(f + 1) * 128], b=xgT[bi][:, k, :], st=(k == 0), sp=(k == NK - 1):
                      e.matmul(o, a, b, start=st, stop=sp), [G_tk[ex % 2][k // 4], xgT_tk[bi]], [g.pst[2]])
            for k in range(NK):
                P.add("pe", lambda e, o=g.ps[3][:, f * 128:(f + 1) * 128], a=Ub[ex % 2][:, k, f * 128:(f + 1) * 128], b=xgT[bi][:, k, :], st=(k == 0), sp=(k == NK - 1):
                      e.matmul(o, a, b, start=st, stop=sp), [U_tk[ex % 2][k // 4], xgT_tk[bi]], [g.pst[3]])
        P.add("act", lambda e, o=sgt[r]: e.activation(out=o, in_=g.ps[2][:, :], func=AF.Silu), [g.pst[2]], [sg_tk[r]])
        P.add("dve", lambda e, o=ATb[r].rearrange("p f n -> p (f n)"), b=sgt[r]: e.tensor_tensor(out=o, in0=g.ps[3][:, :], in1=b, op=ALU.mult), [g.pst[3], sg_tk[r]], [AT_tk[r]])

    def t_down(ex, s, r):
        row0 = ex * 2048 + s * 128
        for db in range(4):
            bank = 4 + db
            for f in range(4):
                P.add("pe", lambda e, o=g.ps[bank][:, :], a=ATb[r][:, f, :], b=Db[ex % 2][:, f, db * 512:(db + 1) * 512], st=(f == 0), sp=(f == 3):
                      e.matmul(o, a, b, start=st, stop=sp), [AT_tk[r], D_tk[ex % 2][f]], [g.pst[bank]])
            P.add("dve", lambda e, o=yb[r][:, db * 512:(db + 1) * 512], i=g.ps[bank][:, :], b_=g2bc[:, db * 512:(db + 1) * 512]: e.tensor_tensor(out=o, in0=i, in1=b_, op=ALU.mult),
                  [g.pst[bank], g2_tk], [yb_tk[r]])
        P.add("sp", lambda e, o=g.yg_all[row0:row0 + 128, :], s_=yb[r]: e.dma_start(out=o, in_=s_), [yb_tk[r]], [g.yg_tk], dma=True)

    load_expert(0)
    t_load(0, 0, 2)
    t_mask(0, 0, 2)
    t_prep(0, 0, 2)
    for ex in range(NE):
        if ex + 1 < NE:
            load_expert(ex + 1)
            t_load(ex + 1, 0, 2 + (ex + 1) % 2)
            t_mask(ex + 1, 0, 2 + (ex + 1) % 2)
            t_prep(ex + 1, 0, 2 + (ex + 1) % 2)
        for eng in Prog.ENGS:
            P.add(eng, lambda e, eng=eng, ex=ex: e.reg_load(P.regs[eng], cntneg[0:1, ex:ex + 1]), [pers_tk], [])
        uid = ("moeh", l, ex)
        for s in range(4):
            P.region = (uid, s)
            it = state_it["it"]
            state_it["it"] = it + 1
            r = it % 2
            bi = (2 + ex % 2) if s == 0 else (s % 2)
            if s + 1 < 4:
                t_load(ex, s + 1, (s + 1) % 2)
                t_mask(ex, s + 1, (s + 1) % 2)
            t_gu(ex, bi, r)
            if s + 1 < 4:
                t_prep(ex, s + 1, (s + 1) % 2)
            t_down(ex, s, r)
            P.region = None
    for eng in Prog.ENGS:
        P.add(eng, lambda e, eng=eng: e.reg_load(P.regs2[eng], cntneg[0:1, 16:17]), [pers_tk], [])
    P.outer = (("moec", l), 4)
    for ex in range(NE):
        for eng in Prog.ENGS:
            P.add(eng, lambda e, eng=eng, ex=ex: e.reg_load(P.regs[eng], cntneg[0:1, ex:ex + 1]), [pers_tk], [])
        uid = ("moec", l, ex)
        P.region = (uid, 4)
        load_expert(ex)
        P.region = None
        for s in range(4, 16):
            tile(ex, s, uid)
    P.outer = None
    P.barrier()
    XT2, Y0, FNG, JK2, TM2 = 0, 4096, 12288, 14336, 15360
    xt2 = [arena_f32(g, XT2 + i * 2048, 2048) for i in range(2)]
    xt2_tk = [Tk(), Tk()]
    yg = [[arena_f32(g, Y0 + (i * 2 + j) * 2048, 2048) for j in range(2)] for i in range(2)]
    yg_tk = [[Tk(), Tk()], [Tk(), Tk()]]
    fng = arena_f32(g, FNG, 2048)
    fng_tk = Tk()
    junk = arena_bf(g, JK2, 2048)
    junk_tk = Tk()
    fin_ss_tk = [Tk(), Tk()]
    if final:
        P.add("sp", lambda e: e.dma_start(out=fng, in_=g.fng), [], [fng_tk], dma=True)
    def c_load(t):
        if t < NTL:
            P.add("sp", lambda e, o=xt2[t % 2], s_=xsrc[t * 128:(t + 1) * 128, :]: e.dma_start(out=o, in_=s_), [], [xt2_tk[t % 2]], dma=True)
    c_load(0)
    for t in range(NTL):
        r = t % 2
        c_load(t + 1)
        for j in range(2):
            c = j * 16 + t
            P.add("pool", lambda e, o=yg[r][j], c=c: e.indirect_dma_start(out=o, out_offset=None, in_=g.yg_all[:, :], in_offset=bass.IndirectOffsetOnAxis(ap=desti[:, c:c + 1], axis=0)),
                  [pers_tk, g.yg_tk], [yg_tk[r][j]], dma=True)
        for j in range(2):
            c = j * 16 + t
            P.add("dve", lambda e, o=xt2[r], y=yg[r][j], c=c: e.scalar_tensor_tensor(out=o, in0=y, scalar=cw[:, c:c + 1], in1=o, op0=ALU.mult, op1=ALU.add),
                  [xt2_tk[r], yg_tk[r][j], pers_tk], [xt2_tk[r]])
        if final:
            ss, ss_tk = g.small[:, 340 + t % 2:341 + t % 2], fin_ss_tk[t % 2]
            P.add("pool", lambda e, o=ss: e.memset(o, 0.0), [], [ss_tk])
            P.add("act", lambda e, o=junk, i_=xt2[r], a=ss: e.activation(out=o, in_=i_, func=AF.Square, accum_out=a), [xt2_tk[r]], [junk_tk, ss_tk])
            P.add("act", lambda e, o=ss: e.activation(out=o, in_=o, func=AF.Sqrt, bias=eps, scale=1.0 / D), [ss_tk, g.tk_pers], [ss_tk])
            P.add("dve", lambda e, o=ss: e.reciprocal(out=o, in_=o), [ss_tk], [ss_tk])
            P.add("dve", lambda e, o=xt2[r], sc_=ss: e.scalar_tensor_tensor(out=o, in0=o, scalar=sc_, in1=fng, op0=ALU.mult, op1=ALU.mult), [xt2_tk[r], ss_tk, fng_tk], [xt2_tk[r]])
        P.add("sp", lambda e, o=xdst[t * 128:(t + 1) * 128, :], s_=xt2[r]: e.dma_start(out=o, in_=s_), [xt2_tk[r]], [], dma=True)
    P.barrier()


def _fm(v):
    v = np.asarray(v, np.float32)
    return np.ascontiguousarray(v.reshape(-1, 128).T)


def _bias_tables(rpb):
    H = rpb.shape[0]
    kc = np.arange(64)[:, None]
    qc = np.arange(64)[None, :]
    cs = np.clip(qc - 8, 0, 48)
    cmask = (kc >= cs) & (kc < cs + 16)
    dc = np.clip(kc - qc + 15, 0, 30)
    tab = np.full((H, 128, 26, 64), NEG, np.float32)
    for a in range(2):
        for s in range(10):
            dr = 11 - s + a
            if 3 <= dr <= 10:
                v = rpb[:, dr][:, dc]
                tab[:, a * 64:(a + 1) * 64, s, :] = np.where(cmask[None], v, NEG)
        for s in range(16):
            dr = 14 - s + a
            if 0 <= dr <= 14:
                v = rpb[:, dr][:, dc]
                tab[:, a * 64:(a + 1) * 64, 10 + s, :] = np.where(cmask[None], v, NEG)
    return np.ascontiguousarray(tab.reshape(H, 128, 26 * 64))


def _dft_consts():
    L, C = 2048, 256
    c = np.arange(C)
    ang = 2 * np.pi * np.outer(c, c) / C
    csc = np.concatenate([np.cos(ang), np.sin(ang)], axis=1) / 16.0
    csc = csc.reshape(2, 128, 512).transpose(1, 0, 2)
    l = np.arange(L)
    lm = (np.outer(l, l) % L).astype(np.float64)
    angL = 2 * np.pi * lm / L
    sL = 1.0 / np.sqrt(L)
    CL = np.cos(angL) * sL
    SL = -np.sin(angL) * sL
    out = np.empty((2, 4, 128, 16, 512), np.float32)
    for i, M in enumerate((CL, SL)):
        out[i] = M.reshape(16, 128, 4, 512).transpose(2, 1, 0, 3)
    return csc.astype(ml_dtypes.bfloat16), out.astype(ml_dtypes.bfloat16)


_CONSTS = {}


def make_in_maps(inp):
    f = lambda a: np.ascontiguousarray(np.asarray(a, np.float32))
    if "dft" not in _CONSTS:
        _CONSTS["csc"], _CONSTS["dft"] = _dft_consts()
        _CONSTS["ident"] = np.eye(128, dtype=np.float32)
        p = np.arange(128)
        cst = np.zeros((128, 160), np.float32)
        cst[:, 0:128] = (p[:, None] < p[None, :]).astype(np.float32)
        cst[:, 128:144] = p[:, None] + 128.0 * np.arange(16)[None, :]
        cst[:, 144:160] = 2048.0 * np.arange(16)[None, :]
        _CONSTS["cst"] = cst
    x, c, ctx, c_ctx = f(inp["x"]), f(inp["c"]), f(inp["ctx"]), f(inp["c_ctx"])
    shared = {
        "ada_w": f(inp["ada_w"]),
        "ada_b2": np.ascontiguousarray(np.repeat(f(inp["ada_b"])[:, None, :], 2, axis=1)),
        "gT": np.ascontiguousarray(np.stack([_fm(inp["mix_norm_g"][0]), _fm(inp["ffn_norm_g"][0]),
                                             _fm(inp["mix_norm_g"][1]), _fm(inp["ffn_norm_g"][1])], axis=1)),
        "fng": np.ascontiguousarray(np.broadcast_to(f(inp["final_norm_g"])[None, :], (128, D))),
        "w_in0": f(inp["ev_w_in"][0]), "w_out0": f(inp["ev_w_out"][0]),
        "rpbt": _bias_tables(f(inp["ev_rpb"][0])),
        "csc": _CONSTS["csc"], "dft": _CONSTS["dft"],
        "w_in1": f(inp["od_w_in"][0]),
        "cvp": np.ascontiguousarray(np.stack([_fm(inp["od_b_in"][0][:D]), _fm(inp["od_b_in"][0][D:]), _fm(inp["od_dw_b"][0]),
                                              _fm(inp["od_ln_g"][0]), _fm(inp["od_ln_b"][0]), _fm(inp["od_ln_b"][0])], axis=1)),
        "dww": np.ascontiguousarray(f(inp["od_dw_w"][0]).T.reshape(NK, 128, 31).transpose(1, 0, 2)),
        "w_out1": f(inp["od_w_out"][0]),
        "bout": np.ascontiguousarray(np.broadcast_to(f(inp["od_b_out"][0])[None, :], (128, D))),
        "rw": np.ascontiguousarray(f(inp["router_w"]).reshape(NK, 128, NE).transpose(1, 0, 2)),
        "rb": np.ascontiguousarray(np.broadcast_to(f(inp["router_b"])[None, :], (128, NE))),
        "wg": f(inp["moe_w_gate"]), "wu": f(inp["moe_w_up"]), "wd": f(inp["moe_w_down"]),
        "ident": _CONSTS["ident"],
        "cst": _CONSTS["cst"],
    }
    maps = []
    for b in range(x.shape[0]):
        m = dict(shared)
        m["x"] = x[b]
        m["ctx"] = ctx[b]
        m["cT"] = np.ascontiguousarray(np.stack([_fm(c[b]), _fm(c_ctx)], axis=2))
        maps.append(m)
    return maps


_NC = {}


def kernel(**inputs):
    maps = make_in_maps(inputs)
    if "nc" not in _NC:
        _NC["nc"] = build_program()
    res = run_bass_kernel_spmd(_NC["nc"], maps, core_ids=list(range(8)))
    return np.stack([r["out"] for r in res.results], axis=0).astype(np.float32)


def out_proj(g, zT, z_tk, nkc, w_dram, xin, xout, gateT, bias_bc_dram, base):
    P = g.P
    wsz = nkc * 256
    wbf = [arena_bf(g, base + i * wsz, nkc * 512).rearrange("p (k n) -> p k n", k=nkc) for i in range(2)]
    wbf_tk = [Tk(), Tk()]
    o = base + 2 * wsz
    g1bc = arena_f32(g, o, 2048)
    gb = arena_f32(g, o + 2048, 2048)
    g1_tk, gb_tk = Tk(), Tk()
    o += 4096
    NX = 4
    xi = [arena_f32(g, o + i * 512, 512) for i in range(NX)]
    xi_tk = [Tk() for _ in range(NX)]
    o += NX * 512
    tm = [arena_f32(g, o + i * 512, 512) for i in range(2)]
    tm_tk = [Tk(), Tk()]
    o += 1024
    xo = [arena_f32(g, o + i * 512, 512) for i in range(NX)]
    xo_tk = [Tk() for _ in range(NX)]
    o += NX * 512
    make_bc(g, gateT, g1bc, g1_tk, o)
    if bias_bc_dram is not None:
        P.add("sp", lambda e: e.dma_start(out=gb, in_=bias_bc_dram), [], [gb_tk], dma=True)
        P.add("dve", lambda e: e.tensor_tensor(out=gb, in0=gb, in1=g1bc, op=ALU.mult), [gb_tk, g1_tk], [gb_tk])
    nq = nkc // 4

    def load_w(db):
        wb = wbf[db % 2]
        for kq in range(nq):
            src_ = w_dram[kq * 512:(kq + 1) * 512, db * 512:(db + 1) * 512].rearrange("(k p) n -> p k n", p=128)
            P.add("pool", lambda e, o_=wb[:, kq * 4:(kq + 1) * 4, :], s_=src_: e.dma_start(out=o_, in_=s_), [], [wbf_tk[db % 2]], dma=True)

    iters = [(db, t) for db in range(4) for t in range(NT)]
    state = {"ld": 0}

    def issue_loads(upto):
        while state["ld"] < min(upto, len(iters)):
            n = state["ld"]
            db, t = iters[n]
            P.add("sp", lambda e, o_=xi[n % NX], s_=xin[t * 128:(t + 1) * 128, db * 512:(db + 1) * 512]: e.dma_start(out=o_, in_=s_), [], [xi_tk[n % NX]], dma=True)
            state["ld"] += 1

    load_w(0)
    for it, (db, t) in enumerate(iters):
        wb = wbf[db % 2]
        if t == 0 and db + 1 < 4:
            load_w(db + 1)
        issue_loads(it + NX - 1)
        rx = it % NX
        r2 = it % 2
        bank = 6 + it % 2
        for c in range(nkc):
            P.add("pe", lambda e, o_=g.ps[bank][:, :], a=zT[:, c, t * 128:(t + 1) * 128], b=wb[:, c, :], st=(c == 0), sp=(c == nkc - 1):
                  e.matmul(o_, a, b, start=st, stop=sp), [z_tk, wbf_tk[db % 2]], [g.pst[bank]])
        P.add("dve", lambda e, o_=tm[r2], i_=g.ps[bank][:, :], b_=g1bc[:, db * 512:(db + 1) * 512]: e.tensor_tensor(out=o_, in0=i_, in1=b_, op=ALU.mult),
              [g.pst[bank], g1_tk], [tm_tk[r2]])
        if bias_bc_dram is not None:
            P.add("pool", lambda e, o_=xi[rx], b_=gb[:, db * 512:(db + 1) * 512]: e.tensor_tensor(out=o_, in0=o_, in1=b_, op=ALU.add), [xi_tk[rx], gb_tk], [xi_tk[rx]])
        P.add("dve", lambda e, o_=xo[rx], a=tm[r2], b=xi[rx]: e.tensor_tensor(out=o_, in0=a, in1=b, op=ALU.add), [tm_tk[r2], xi_tk[rx]], [xo_tk[rx]])
        P.add("sp", lambda e, o_=xout[t * 128:(t + 1) * 128, db * 512:(db + 1) * 512], s_=xo[rx]: e.dma_start(out=o_, in_=s_), [xo_tk[rx]], [], dma=True)


def build_hT(g, xsrc, ntiles, A, B, hT, hT_tk, base):
    P = g.P
    eps = g.small[:, 336:337]
    P.add("pool", lambda e: e.memset(eps, EPS), [], [g.tk_pers])
    xt = [arena_f32(g, base + i * 2048, 2048) for i in range(2)]
    xt_tk = [Tk(), Tk()]
    tmp = {
        "ss": [(g.small[:, 340 + i:341 + i], Tk()) for i in range(2)],
        "sq": [(g.small[:, 344 + i:345 + i], Tk()) for i in range(2)],
        "xn": [(arena_f32(g, base + 4096 + i * 2048, 2048), Tk()) for i in range(2)],
        "junk": (arena_bf(g, base + 8192, 2048), Tk()),
        "t32": [(arena_f32(g, base + 9216 + i * 512, 512), Tk()) for i in range(2)],
        "eps": eps,
    }
    def s1(t):
        if t < ntiles:
            r_ = t % 2
            P.add("sp", lambda e, o=xt[r_], s=xsrc[t * 128:(t + 1) * 128, :]: e.dma_start(out=o, in_=s), [], [xt_tk[r_]], dma=True)
            norm_transpose(g, xt[r_], xt_tk[r_], A, B, None, None, tmp, t, phase=1)
    s1(0)
    for t in range(ntiles):
        r = t % 2
        s1(t + 1)
        norm_transpose(g, xt[r], xt_tk[r], A, B, None,
                       lambda b, t=t: (hT[:, b * 4:(b + 1) * 4, t * 128:(t + 1) * 128], hT_tk[t]), tmp, t, phase=2)


def stage_conv(g, xsrc, xdst):
    P = g.P
    l = 1
    A, B = prep_AB(g, 2, g.modT[l][:, 1, :], g.modT[l][:, 0, :], 0)
    HT, UT, R = 0, 16384, 33024
    PADW = 2080
    hT = arena_bf(g, HT, 32768).rearrange("p (k n) -> p k n", k=NK)
    hT_tk = [Tk() for _ in range(NT)]
    uT = arena_bf(g, UT, NK * PADW).rearrange("p (k n) -> p k n", k=NK)
    uT_tk = [Tk() for _ in range(NK)]
    cv = g.small[:, 96:192].rearrange("p (j k) -> p j k", j=6)
    cv_tk = Tk()
    P.add("sp", lambda e: e.dma_start(out=cv, in_=g.cvp), [], [cv_tk], dma=True)
    build_hT(g, xsrc, NT, A, B, hT, hT_tk, R)
    P.barrier()
    for c in range(NK):
        P.add("pool", lambda e, o=uT[:, c, 0:15]: e.memset(o, 0.0), [], [uT_tk[c]])
        P.add("pool", lambda e, o=uT[:, c, 2063:2080]: e.memset(o, 0.0), [], [uT_tk[c]])
    sg = [arena_f32(g, R + 4096 + i * 512, 512) for i in range(2)]
    sg_tk = [Tk(), Tk()]
    stg = [arena_f32(g, 43264 + i * 2048, 2048).rearrange("p (k n) -> p k n", k=NK) for i in range(2)]
    stg_tk = [Tk(), Tk()]
    wbf = [arena_bf(g, 47360 + i * 1024, 2048).rearrange("p (k n) -> p k n", k=NK) for i in range(4)]
    wbf_tk = [Tk() for _ in range(4)]
    nu = 0
    it = 0
    for c in range(NK):
        ws = []
        for half in range(2):
            s = nu % 2
            d = nu % 4
            nu += 1
            col = half * D + c * 128
            src_ = g.w_in1[:, col:col + 128].rearrange("(k p) n -> p k n", p=128)
            P.add("pool", lambda e, o=wbf[d], s_=src_: e.dma_start(out=o, in_=s_), [], [wbf_tk[d]], dma=True)
            ws.append((wbf[d], wbf_tk[d]))
        for tb in range(4):
            bv, bg = (0, 1) if it % 2 == 0 else (2, 3)
            for half, bank in ((0, bv), (1, bg)):
                w, wtk = ws[half]
                for k in range(NK):
                    P.add("pe", lambda e, o=g.ps[bank][:, :], a=w[:, k, :], b=hT[:, k, tb * 512:(tb + 1) * 512], st=(k == 0), sp=(k == NK - 1):
                          e.matmul(o, a, b, start=st, stop=sp), [wtk] + hT_tk[tb * 4:tb * 4 + 4], [g.pst[bank]])
            r = it % 2
            P.add("act", lambda e, o=sg[r], i=g.ps[bg][:, :], b_=cv[:, 1, c:c + 1]: e.activation(out=o, in_=i, func=AF.Sigmoid, bias=b_), [g.pst[bg], cv_tk], [sg_tk[r]])
            P.add("dve", lambda e, o=uT[:, c, 15 + tb * 512: 15 + (tb + 1) * 512], i=g.ps[bv][:, :], b_=cv[:, 0, c:c + 1], s_=sg[r]:
                  e.scalar_tensor_tensor(out=o, in0=i, scalar=b_, in1=s_, op0=ALU.add, op1=ALU.mult), [g.pst[bv], cv_tk, sg_tk[r]], [uT_tk[c]])
            it += 1
    P.barrier()
    vT = arena_bf(g, 0, 32768).rearrange("p (k n) -> p k n", k=NK)
    vT_tk = [[Tk() for _ in range(4)] for _ in range(NK)]
    dwt = arena_f32(g, R, 512).rearrange("p (k t) -> p k t", k=NK)[:, :, 0:31]
    dwt_full = arena_f32(g, R, 496).rearrange("p (k t) -> p k t", k=NK)
    dw_tk = Tk()
    P.add("sp", lambda e: e.dma_start(out=dwt_full, in_=g.dww), [], [dw_tk], dma=True)
    identb = arena_bf(g, R + 512, 128)
    onesb = arena_bf(g, R + 576, 128)
    cb_tk = Tk()
    P.add("act", lambda e: e.activation(out=identb, in_=g.ident32, func=AF.Copy), [g.tk_pers], [cb_tk])
    P.add("pool", lambda e: e.memset(onesb, 1.0), [], [cb_tk])
    dg = [arena_bf(g, R + 1024 + i * 2048, 31 * 128).rearrange("p (t n) -> p t n", t=31) for i in range(2)]
    dg_tk = [Tk(), Tk()]
    it = 0
    for c in range(NK):
        d = c % 2
        for k in range(31):
            if k % 2 == 0:
                P.add("act", lambda e, o=dg[d][:, k, :], sc=dwt_full[:, c, k:k + 1]: e.activation(out=o, in_=identb, func=AF.Copy, scale=sc), [cb_tk, dw_tk], [dg_tk[d]])
            else:
                P.add("dve", lambda e, o=dg[d][:, k, :], sc=dwt_full[:, c, k:k + 1]: e.tensor_scalar(out=o, in0=identb, scalar1=sc, scalar2=None, op0=ALU.mult),
                      [cb_tk, dw_tk], [dg_tk[d]])
        for tb in range(4):
            bank = it % 2
            for k in range(31):
                P.add("pe", lambda e, o=g.ps[bank][:, :], a=dg[d][:, k, :], b=uT[:, c, tb * 512 + k: tb * 512 + k + 512], st=(k == 0), sp=(k == 30):
                      e.matmul(o, a, b, start=st, stop=sp), [dg_tk[d], uT_tk[c]], [g.pst[bank]])
            P.add("act", lambda e, o=vT[:, c, tb * 512:(tb + 1) * 512], i=g.ps[bank][:, :], b_=cv[:, 2, c:c + 1]: e.activation(out=o, in_=i, func=AF.Identity, bias=b_),
                  [g.pst[bank], cv_tk], [vT_tk[c][tb]])
            it += 1
    SB = R + 1024 + 4096
    sqb = [arena_bf(g, SB + i * 256, 512) for i in range(2)]
    sq_tk = [Tk(), Tk()]
    mean = arena_f32(g, SB + 512, 512)
    rstd = arena_f32(g, SB + 1024, 512)
    m2 = arena_f32(g, SB + 1536, 512)
    st_tk = Tk()
    tn = [arena_f32(g, SB + 2048 + i * 512, 512) for i in range(2)]
    tn_tk = [Tk(), Tk()]
    eps = g.small[:, 336:337]
    it = 0
    for tb in range(4):
        for c in range(NK):
            r = it % 2
            vv = vT[:, c, tb * 512:(tb + 1) * 512]
            P.add("act", lambda e, o=sqb[r], i=vv: e.activation(out=o, in_=i, func=AF.Square), [vT_tk[c][tb]], [sq_tk[r]])
            P.add("pe", lambda e, b=vv, st=(c == 0), sp=(c == NK - 1): e.matmul(g.ps[2][:, :], onesb, b, start=st, stop=sp), [cb_tk, vT_tk[c][tb]], [g.pst[2]])
            P.add("pe", lambda e, b=sqb[r], st=(c == 0), sp=(c == NK - 1): e.matmul(g.ps[3][:, :], onesb, b, start=st, stop=sp), [cb_tk, sq_tk[r]], [g.pst[3]])
            it += 1
        P.add("act", lambda e: e.activation(out=mean, in_=g.ps[2][:, :], func=AF.Copy, scale=1.0 / D), [g.pst[2]], [st_tk])
        P.add("dve", lambda e: e.tensor_tensor(out=m2, in0=mean, in1=mean, op=ALU.mult), [st_tk], [st_tk])
        P.add("dve", lambda e: e.scalar_tensor_tensor(out=rstd, in0=g.ps[3][:, :], scalar=1.0 / D, in1=m2, op0=ALU.mult, op1=ALU.subtract), [g.pst[3], st_tk], [st_tk])
        P.add("act", lambda e: e.activation(out=rstd, in_=rstd, func=AF.Sqrt, bias=eps), [st_tk, g.tk_pers], [st_tk])
        P.add("dve", lambda e: e.reciprocal(out=rstd, in_=rstd), [st_tk], [st_tk])
        for c in range(NK):
            r = c % 2
            vv = vT[:, c, tb * 512:(tb + 1) * 512]
            P.add("dve", lambda e, o=tn[r], i=vv: e.tensor_tensor(out=o, in0=i, in1=mean, op=ALU.subtract), [vT_tk[c][tb], st_tk], [tn_tk[r]])
            P.add("dve", lambda e, o=tn[r]: e.tensor_tensor(out=o, in0=o, in1=rstd, op=ALU.mult), [tn_tk[r], st_tk], [tn_tk[r]])
            P.add("act", lambda e, o=vv, i=tn[r], s_=cv[:, 3, c:c + 1], b_=cv[:, 4, c:c + 1]: e.activation(out=o, in_=i, func=AF.Silu, bias=b_, scale=s_),
                  [tn_tk[r], cv_tk], [vT_tk[c][tb]])
    P.barrier()
    z_tk = Tk()
    out_proj(g, vT, z_tk, NK, g.w_out1, xsrc, xdst, g.modT[l][:, 2, :], g.bout, 16384)


def load_w_unit(g, src_ap, stg, stg_tk, dst, dst_tk, eng):
    g.P.add("pool", lambda e: e.dma_start(out=dst, in_=src_ap), [], [dst_tk], dma=True)


def stage_mixer0(g, xsrc, xdst):
    P = g.P
    l = 0
    A, B = prep_AB(g, 0, g.modT[l][:, 1, :], g.modT[l][:, 0, :], 0)
    Ac, Bc = prep_AB(g, 0, g.modcT[:, 1, :], g.modcT[:, 0, :], 2)
    hT = arena_bf(g, 0, 32768).rearrange("p (k n) -> p k n", k=NK)
    hT_tk = [Tk() for _ in range(NT)]
    build_hT(g, xsrc, NT, A, B, hT, hT_tk, 16384)
    P.barrier()
    YT = arena_bf(g, 16384, 16384).rearrange("p (k n) -> p k n", k=8)
    YT_tk = Tk()
    uT = arena_bf(g, 24576, 4096).rearrange("p (k n) -> p k n", k=2)
    uT_tk = [Tk(), Tk()]
    W1 = arena_bf(g, 26624, 8192).rearrange("p (t n) -> p t n", t=NT)
    W1_tk = [Tk() for _ in range(NT)]
    dfb = [arena_bf(g, 30720 + i * 4096, 8192).rearrange("p (k n) -> p k n", k=NK) for i in range(3)]
    dfb_tk = [Tk() for _ in range(3)]
    stg = [arena_f32(g, 43008 + i * 2048, 2048).rearrange("p (k n) -> p k n", k=NK) for i in range(2)]
    stg_tk = [Tk(), Tk()]
    wbf = [arena_bf(g, 47104 + i * 1024, 2048).rearrange("p (k n) -> p k n", k=NK) for i in range(2)]
    wbf_tk = [Tk(), Tk()]
    csc = arena_bf(g, 49152, 1024).rearrange("p (k n) -> p k n", k=2)
    csc_tk = Tk()
    P.add("sp", lambda e: e.dma_start(out=csc, in_=g.csc), [], [csc_tk], dma=True)
    nu = 0
    nd = 0
    it = 0
    for gi in range(4):
        for cc in range(2):
            s = nu % 2
            nu += 1
            col = gi * 256 + cc * 128
            load_w_unit(g, g.w_in0[:, col:col + 128].rearrange("(k p) n -> p k n", p=128), stg[s], stg_tk[s], wbf[s], wbf_tk[s], "act" if cc == 0 else "dve")
            for tb in range(4):
                bank = it % 2
                it += 1
                for k in range(NK):
                    P.add("pe", lambda e, o=g.ps[bank][:, :], a=wbf[s][:, k, :], b=hT[:, k, tb * 512:(tb + 1) * 512], st=(k == 0), sp=(k == NK - 1):
                          e.matmul(o, a, b, start=st, stop=sp), [wbf_tk[s]] + hT_tk[tb * 4:tb * 4 + 4], [g.pst[bank]])
                P.add("act", lambda e, o=uT[:, cc, tb * 512:(tb + 1) * 512], i=g.ps[bank][:, :]: e.activation(out=o, in_=i, func=AF.Copy), [g.pst[bank]], [uT_tk[cc]])
        for t in range(NT):
            bank = 2 + t % 2
            for cc in range(2):
                P.add("pe", lambda e, o=g.ps[bank][:, :], a=uT[:, cc, t * 128:(t + 1) * 128], b=csc[:, cc, :], st=(cc == 0), sp=(cc == 1):
                      e.matmul(o, a, b, start=st, stop=sp), [uT_tk[cc], csc_tk], [g.pst[bank]])
            P.add("dve", lambda e, o=W1[:, t, :], i=g.ps[bank][:, :]: e.tensor_copy(out=o, in_=i), [g.pst[bank]], [W1_tk[t]])
        for mb in range(4):
            bufs = []
            for cs in range(2):
                s = nd % 3
                nd += 1
                P.add("sp", lambda e, o=dfb[s], s_=g.dft[cs, mb]: e.dma_start(out=o, in_=s_), [], [dfb_tk[s]], dma=True)
                bufs.append((dfb[s], dfb_tk[s]))
            for nch in range(2):
                bank = 4 + (mb * 2 + nch) % 2
                n = 0
                for cs in range(2):
                    db_, dtk = bufs[cs]
                    for lc in range(NK):
                        P.add("pe", lambda e, o=g.ps[bank][:, :], a=W1[:, lc, cs * 256 + nch * 128: cs * 256 + (nch + 1) * 128], b=db_[:, lc, :], st=(n == 0), sp=(n == 31):
                              e.matmul(o, a, b, start=st, stop=sp), [W1_tk[lc], dtk], [g.pst[bank]])
                        n += 1
                eng = "act" if nch == 0 else "dve"
                if eng == "act":
                    P.add("act", lambda e, o=YT[:, gi * 2 + nch, mb * 512:(mb + 1) * 512], i=g.ps[bank][:, :]: e.activation(out=o, in_=i, func=AF.Copy), [g.pst[bank]], [YT_tk])
                else:
                    P.add("dve", lambda e, o=YT[:, gi * 2 + nch, mb * 512:(mb + 1) * 512], i=g.ps[bank][:, :]: e.tensor_copy(out=o, in_=i), [g.pst[bank]], [YT_tk])
    P.barrier()
    out_proj(g, YT, YT_tk, 8, g.w_out0[0:1024, :], xsrc, g.xs[2], g.modT[l][:, 2, :], None, 24576)
    P.barrier()
    OT = arena_bf(g, 16384, 16384).rearrange("p (k n) -> p k n", k=8)
    OT_tk = Tk()
    hcT = arena_bf(g, 24576, 4096).rearrange("p (k n) -> p k n", k=NK)
    hc_tk = [Tk(), Tk()]
    build_hT(g, g.ctx, 2, Ac, Bc, hcT, hc_tk, 26624)
    P.barrier()
    QT = arena_bf(g, 26624, 4096).rearrange("p (h n) -> p h n", h=2)
    KT = arena_bf(g, 28672, 4096).rearrange("p (h n) -> p h n", h=2)
    QT_tk, KT_tk = [Tk(), Tk()], [Tk(), Tk()]
    Vq = arena_bf(g, 30720, 4160).rearrange("p (t h d) -> p t h d", t=NT, h=4)
    V_tk = [Tk() for _ in range(NT)]
    kcT = arena_bf(g, 32800, 512).rearrange("p (h n) -> p h n", h=2)
    kc_tk = [Tk(), Tk()]
    vc = arena_bf(g, 33056, 520).rearrange("p (t h d) -> p t h d", t=2, h=4)
    vc_tk = [Tk(), Tk()]
    Otok = arena_f32(g, 33344, 4096).rearrange("p (i f) -> p i f", i=NT)
    Otok_tk = [Tk() for _ in range(NT)]
    tmpb = [arena_f32(g, 37440 + i * 640, 640) for i in range(2)]
    tmp_tk = [Tk(), Tk()]
    Pb = [arena_bf(g, 38720 + i * 448, 896) for i in range(2)]
    Pb_tk = [Tk(), Tk()]
    tab = [arena_f32(g, 39616 + i * 1664, 1664) for i in range(2)]
    tab_tk = [Tk(), Tk()]
    stg = [arena_f32(g, 42944 + i * 2048, 2048).rearrange("p (k n) -> p k n", k=NK) for i in range(2)]
    stg_tk = [Tk(), Tk()]
    wbf = [arena_bf(g, 47040 + i * 1024, 2048).rearrange("p (k n) -> p k n", k=NK) for i in range(4)]
    wbf_tk = [Tk() for _ in range(4)]
    rec = [g.small[:, 348 + i:349 + i] for i in range(2)]
    rec_tk = [Tk(), Tk()]
    nu = 0
    nw = 0
    it = 0

    def wunit(col, eng):
        nonlocal nu, nw
        s = nu % 2
        d = nw % 4
        nu += 1
        nw += 1
        load_w_unit(g, g.w_in0[:, col:col + 128].rearrange("(k p) n -> p k n", p=128), stg[s], stg_tk[s], wbf[d], wbf_tk[d], eng)
        return wbf[d], wbf_tk[d]

    for q in range(4):
        P.add("pool", lambda e: e.memset(Vq[:, :, :, 64:65], 1.0), [], V_tk)
        P.add("pool", lambda e: e.memset(vc[:, :, :, 64:65], 1.0), [], vc_tk)
        for hp in range(2):
            for which, dstT, dtk, base_col in ((0, QT, QT_tk, 1024), (1, KT, KT_tk, 2048)):
                w, wtk = wunit(base_col + q * 256 + hp * 128, "act" if which == 0 else "dve")
                for tb in range(4):
                    bank = it % 2
                    it += 1
                    for k in range(NK):
                        P.add("pe", lambda e, o=g.ps[bank][:, :], a=w[:, k, :], b=hT[:, k, tb * 512:(tb + 1) * 512], st=(k == 0), sp=(k == NK - 1):
                              e.matmul(o, a, b, start=st, stop=sp), [wtk] + hT_tk[tb * 4:tb * 4 + 4], [g.pst[bank]])
                    P.add("act", lambda e, o=dstT[:, hp, tb * 512:(tb + 1) * 512], i=g.ps[bank][:, :]: e.activation(out=o, in_=i, func=AF.Copy), [g.pst[bank]], [dtk[hp]])
                if which == 1:
                    bank = it % 2
                    it += 1
                    for k in range(NK):
                        P.add("pe", lambda e, o=g.ps[bank][:, 0:256], a=w[:, k, :], b=hcT[:, k, :], st=(k == 0), sp=(k == NK - 1):
                              e.matmul(o, a, b, start=st, stop=sp), [wtk] + hc_tk, [g.pst[bank]])
                    P.add("act", lambda e, o=kcT[:, hp, :], i=g.ps[bank][:, 0:256]: e.activation(out=o, in_=i, func=AF.Copy), [g.pst[bank]], [kc_tk[hp]])
        wv = [wunit(3072 + q * 256 + j * 128, "act" if j == 0 else "dve") for j in range(2)]
        for t in range(NT + 2):
            bank = it % 2
            it += 1
            for j in range(2):
                w, wtk = wv[j]
                for k in range(NK):
                    if t < NT:
                        a_, rtk = hT[:, k, t * 128:(t + 1) * 128], [hT_tk[t]]
                    else:
                        a_, rtk = hcT[:, k, (t - NT) * 128:(t - NT + 1) * 128], [hc_tk[t - NT]]
                    P.add("pe", lambda e, o=g.ps[bank][:, j * 128:(j + 1) * 128], a=a_, b=w[:, k, :], st=(k == 0), sp=(k == NK - 1):
                          e.matmul(o, a, b, start=st, stop=sp), [wtk] + rtk, [g.pst[bank]])
            pv = g.ps[bank][:, 0:256].rearrange("p (h d) -> p h d", h=4)
            if t < NT:
                P.add("dve", lambda e, o=Vq[:, t, :, 0:64], i=pv: e.tensor_copy(out=o, in_=i), [g.pst[bank]], [V_tk[t]])
            else:
                P.add("dve", lambda e, o=vc[:, t - NT, :, 0:64], i=pv: e.tensor_copy(out=o, in_=i), [g.pst[bank]], [vc_tk[t - NT]])
        pend = None
        for h4 in range(4):
            hh = q * 4 + h4
            hp, po = h4 // 2, (h4 % 2) * 64
            tb_, tbtk = tab[hh % 2], tab_tk[hh % 2]
            P.add("sp", lambda e, o=tb_, s_=g.rpbt[hh]: e.dma_start(out=o, in_=s_), [], [tbtk], dma=True)
            for i in range(NT):
                if 2 <= i <= 13:
                    chunks = [i + 2, i + 1, i, i - 1, i - 2]
                    tcol = 0
                elif i < 2:
                    chunks = [3, 2, 1, 0]
                    tcol = 640 + (1 + 2 * i) * 64
                else:
                    chunks = [15, 14, 13, 12]
                    tcol = 640 + (7 - 2 * (15 - i)) * 64
                nch = len(chunks)
                r = it % 2
                it += 1
                banks = (0, 1) if r == 0 else (2, 3)
                qs = QT[po:po + 64, hp, i * 128:(i + 1) * 128]

                def sblk(ci):
                    return g.ps[banks[ci // 4]][:, (ci % 4) * 128:(ci % 4 + 1) * 128], g.pst[banks[ci // 4]]
                for ci, j in enumerate(chunks):
                    o_, otk = sblk(ci)
                    P.add("pe", lambda e, o=o_, a=KT[po:po + 64, hp, j * 128:(j + 1) * 128], b=qs: e.matmul(o, a, b, start=True, stop=True),
                          [KT_tk[hp], QT_tk[hp]], [otk])
                for cj in range(2):
                    o_, otk = sblk(nch + cj)
                    P.add("pe", lambda e, o=o_, a=kcT[po:po + 64, hp, cj * 128:(cj + 1) * 128], b=qs: e.matmul(o, a, b, start=True, stop=True),
                          [kc_tk[hp], QT_tk[hp]], [otk])
                P.add("dve", lambda e, o=tmpb[r][:, 0:512], i=g.ps[banks[0]][:, :], t_=tb_[:, tcol:tcol + 512]:
                      e.scalar_tensor_tensor(out=o, in0=i, scalar=0.125, in1=t_, op0=ALU.mult, op1=ALU.add), [g.pst[banks[0]], tbtk], [tmp_tk[r]])
                if nch == 5:
                    P.add("dve", lambda e, o=tmpb[r][:, 512:640], i=g.ps[banks[1]][:, 0:128], t_=tb_[:, tcol + 512:tcol + 640]:
                          e.scalar_tensor_tensor(out=o, in0=i, scalar=0.125, in1=t_, op0=ALU.mult, op1=ALU.add), [g.pst[banks[1]], tbtk], [tmp_tk[r]])
                P.add("act", lambda e, o=Pb[r][:, 0:nch * 128], i=tmpb[r][:, 0:nch * 128]: e.activation(out=o, in_=i, func=AF.Exp), [tmp_tk[r]], [Pb_tk[r]])
                c0 = (nch % 4) * 128
                P.add("act", lambda e, o=Pb[r][:, nch * 128:(nch + 2) * 128], i=g.ps[banks[1]][:, c0:c0 + 256]: e.activation(out=o, in_=i, func=AF.Exp, scale=0.125),
                      [g.pst[banks[1]]], [Pb_tk[r]])
                cur = (h4, i, chunks, r)
                if pend is not None:
                    _attn_pv(g, pend, Pb, Pb_tk, Vq, V_tk, vc, vc_tk, Otok, Otok_tk, rec, rec_tk)
                pend = cur
        _attn_pv(g, pend, Pb, Pb_tk, Vq, V_tk, vc, vc_tk, Otok, Otok_tk, rec, rec_tk)
        for i in range(NT):
            for hp in range(2):
                bank = 6 + (i * 2 + hp) % 2
                P.add("pe", lambda e, o=g.ps[bank][:, 0:128], a=Otok[:, i, hp * 128:(hp + 1) * 128]: e.transpose(o, a, g.ident32), [Otok_tk[i], g.tk_pers], [g.pst[bank]])
                if hp == 0:
                    P.add("act", lambda e, o=OT[:, q * 2 + hp, i * 128:(i + 1) * 128], i_=g.ps[bank][:, 0:128]: e.activation(out=o, in_=i_, func=AF.Copy), [g.pst[bank]], [OT_tk])
                else:
                    P.add("dve", lambda e, o=OT[:, q * 2 + hp, i * 128:(i + 1) * 128], i_=g.ps[bank][:, 0:128]: e.tensor_copy(out=o, in_=i_), [g.pst[bank]], [OT_tk])
    P.barrier()
    out_proj(g, OT, OT_tk, 8, g.w_out0[1024:2048, :], g.xs[2], xdst, g.modT[l][:, 2, :], None, 24576)


def _attn_pv(g, item, Pb, Pb_tk, Vq, V_tk, vc, vc_tk, Otok, Otok_tk, rec, rec_tk):
    P = g.P
    h4, i, chunks, r = item
    nch = len(chunks)
    bank = 4 + r
    o_ = g.ps[bank][:, 0:65]
    n = nch + 2
    for ci, j in enumerate(chunks):
        P.add("pe", lambda e, a=Pb[r][:, ci * 128:(ci + 1) * 128], b=Vq[:, j, h4, :], st=(ci == 0): e.matmul(o_, a, b, start=st, stop=False),
              [Pb_tk[r], V_tk[j]], [g.pst[bank]])
    for cj in range(2):
        P.add("pe", lambda e, a=Pb[r][:, (nch + cj) * 128:(nch + cj + 1) * 128], b=vc[:, cj, h4, :], sp=(cj == 1): e.matmul(o_, a, b, start=False, stop=sp),
              [Pb_tk[r], vc_tk[cj]], [g.pst[bank]])
    P.add("dve", lambda e: e.reciprocal(out=rec[r], in_=g.ps[bank][:, 64:65]), [g.pst[bank]], [rec_tk[r]])
    P.add("act", lambda e, o=Otok[:, i, h4 * 64:(h4 + 1) * 64]: e.activation(out=o, in_=g.ps[bank][:, 0:64], func=AF.Copy, scale=rec[r]),
          [g.pst[bank], rec_tk[r]], [Otok_tk[i]])
```

```python
import numpy as np
import ml_dtypes
import concourse.bass as bass
import concourse.mybir as mybir
from concourse.bass_utils import run_bass_kernel_spmd

F32 = mybir.dt.float32
BF16 = mybir.dt.bfloat16
AF = mybir.ActivationFunctionType
ALU = mybir.AluOpType
AX = mybir.AxisListType

D = 2048
S = 2048
NT = 16
NK = 16
CTX = 256
NE = 16
FE = 512
EPS = 1e-6
NEG = -30000.0


class Tk:
    __slots__ = ("w", "r")

    def __init__(self):
        self.w = None
        self.r = {}


class Op:
    __slots__ = ("eng", "fn", "deps", "inc", "dma", "sem", "val", "gidx", "region", "outer")

    def __init__(self, eng, fn, dma):
        self.eng = eng
        self.fn = fn
        self.dma = dma
        self.deps = set()
        self.inc = False
        self.sem = None
        self.val = 0
        self.gidx = 0
        self.region = None
        self.outer = None


class Prog:
    ENGS = ("pe", "act", "dve", "pool", "sp")
    SEG = 10 ** 9
    NDS = 28

    def __init__(self):
        self.ops = {e: [] for e in self.ENGS}
        self.all_dma = []
        self.bar = None
        self.bar_seen = set()
        self.region = None
        self.outer = None
        self.regs = {}
        self.regs2 = {}

    def add(self, eng, fn, reads=(), writes=(), dma=False):
        op = Op(eng, fn, dma)
        op.region = self.region
        op.outer = self.outer
        deps = set()
        for t in reads:
            if t.w is not None:
                deps.add(t.w)
        for t in writes:
            if t.w is not None:
                deps.add(t.w)
            for o in t.r.values():
                if isinstance(o, list):
                    deps.update(o)
                else:
                    deps.add(o)
        if self.bar is not None and eng not in self.bar_seen:
            deps.update(self.bar)
            self.bar_seen.add(eng)
        for t in reads:
            if dma:
                t.r.setdefault("dma", []).append(op)
            else:
                t.r[eng] = op
        for t in writes:
            t.w = op
            t.r = {}
        deps.discard(op)
        if eng == "pe" and not dma:
            deps = {d for d in deps if not (d.eng == "pe" and not d.dma)}
        op.deps = deps
        for d in deps:
            d.inc = True
        self.ops[eng].append(op)
        if dma:
            self.all_dma.append(op)
        return op

    def barrier(self):
        deps = []
        for e in self.ENGS:
            for o in reversed(self.ops[e]):
                if not o.dma:
                    deps.append(o)
                    break
        deps.extend(self.all_dma)
        self.all_dma = []
        self.bar = deps
        self.bar_seen = set()

    def run_emit(self, nc, block, handles, sems):
        si = 0
        for e in self.ENGS:
            cnt = 0
            cur = None
            for o in self.ops[e]:
                if o.dma or not o.inc:
                    continue
                if cnt % self.SEG == 0:
                    cur = sems[si]
                    si += 1
                o.sem = cur
                o.val = cnt % self.SEG + 1
                o.gidx = cnt + 1
                cnt += 1
        dsems = sems[si:si + self.NDS]
        assert len(dsems) == self.NDS, "not enough semaphores"
        dcount = [0] * self.NDS
        k = 0
        for o in self.dma_order:
            s = k % self.NDS
            o.sem = dsems[s]
            o.gidx = (s, dcount[s])
            dcount[s] += 16
            o.val = dcount[s]
            k += 1

        def emit_engine(ename):
            def body(e):
                waited = {}

                def emit_op(o):
                    for d in o.deps:
                        key = ("d", id(d.sem)) if d.dma else (d.eng, id(d.sem))
                        need = d.val
                        if waited.get(key, 0) >= need:
                            continue
                        e.wait_ge(d.sem, need)
                        waited[key] = need
                    if o.dma:
                        slot, prev = o.gidx
                        key = ("d", id(o.sem))
                        if prev > 0 and waited.get(key, 0) < prev:
                            e.wait_ge(o.sem, prev)
                            waited[key] = prev
                        ins = o.fn(e)
                        ins.then_inc(o.sem, 16)
                    else:
                        if o.fn is None:
                            return
                        ins = o.fn(e)
                        if o.inc:
                            ins.then_inc(o.sem, 1)

                ops = self.ops[ename]

                def else_bulk(grp):
                    ninc = sum(1 for q in grp if (not q.dma) and q.inc)
                    if ninc:
                        csem = [q.sem for q in grp if (not q.dma) and q.inc][0]
                        e.drain().then_inc(csem, ninc)
                    for q in grp:
                        if q.dma:
                            slot, prev = q.gidx
                            if prev > 0:
                                e.wait_ge(q.sem, prev)
                            e.sem_inc(q.sem, 16)

                def emit_range(byslot, lo, hi):
                    grp = [q for s in range(lo, hi) for q in byslot.get(s, [])]
                    if not grp:
                        return
                    saved = dict(waited)
                    with e.If_lt(self.regs[ename], -lo):
                        if hi - lo == 1:
                            for q in grp:
                                emit_op(q)
                        else:
                            mid = (lo + hi) // 2
                            emit_range(byslot, lo, mid)
                            emit_range(byslot, mid, hi)
                    with e.Else():
                        waited.clear()
                        waited.update(saved)
                        else_bulk(grp)
                    waited.clear()
                    waited.update(saved)

                def emit_list(lst):
                    i = 0
                    while i < len(lst):
                        o = lst[i]
                        if o.region is None:
                            emit_op(o)
                            i += 1
                            continue
                        uid = o.region[0]
                        j = i
                        byslot = {}
                        while j < len(lst) and lst[j].region is not None and lst[j].region[0] == uid:
                            byslot.setdefault(lst[j].region[1], []).append(lst[j])
                            j += 1
                        emit_range(byslot, min(byslot), 16)
                        i = j

                i = 0
                while i < len(ops):
                    o = ops[i]
                    if o.outer is None:
                        j = i
                        while j < len(ops) and ops[j].outer is None:
                            j += 1
                        emit_list(ops[i:j])
                        i = j
                        continue
                    ou = o.outer
                    j = i
                    while j < len(ops) and ops[j].outer is ou:
                        j += 1
                    grp = ops[i:j]
                    saved = dict(waited)
                    with e.If_lt(self.regs2[ename], -ou[1]):
                        emit_list(grp)
                    with e.Else():
                        waited.clear()
                        waited.update(saved)
                        else_bulk(grp)
                    waited.clear()
                    waited.update(saved)
                    i = j
            return body

        block.tensor(emit_engine("pe"))
        block.scalar(emit_engine("act"))
        block.vector(emit_engine("dve"))
        block.gpsimd(emit_engine("pool"))
        block.sync(emit_engine("sp"))


def _mk_prog():
    p = Prog()
    p.dma_order = []
    _add = p.add

    def add(eng, fn, reads=(), writes=(), dma=False):
        o = _add(eng, fn, reads, writes, dma)
        if dma:
            p.dma_order.append(o)
        return o
    p.add = add
    return p


class Ctx:
    pass


def build_program(stages=(0, 1, 2, 3, 4), dbg=False, sparse=True):
    nc = bass.Bass("TRN2", target_bir_lowering=False)
    P = _mk_prog()
    g = Ctx()
    g.nc, g.P = nc, P

    def din(name, shape, dt=F32):
        return nc.dram_tensor(name, list(shape), dt, kind="ExternalInput").ap()

    g.x = din("x", [S, D])
    g.ctx = din("ctx", [CTX, D])
    g.cT = din("cT", [128, NK, 2])
    g.ada_w = din("ada_w", [2, D, 6 * D])
    g.ada_b4 = din("ada_b4", [2, 4, 6 * D])
    g.cmb = din("cmb", [4, 2])
    g.gT = din("gT", [128, 4, NK])
    g.fng = din("fng", [128, D])
    g.w_in0 = din("w_in0", [D, 4096])
    g.w_out0 = din("w_out0", [D, D])
    g.rpbt = din("rpbt", [16, 128, 1664])
    g.csc = din("csc", [128, 2, 512], BF16)
    g.dft = din("dft", [2, 4, 128, NK, 512], BF16)
    g.w_in1 = din("w_in1", [D, 4096])
    g.cvp = din("cvp", [128, 6, NK])
    g.dww = din("dww", [128, NK, 31])
    g.w_out1 = din("w_out1", [D, D])
    g.bout = din("bout", [128, D])
    g.rw = din("rw", [128, NK, NE])
    g.rb = din("rb", [128, NE])
    g.wg = din("wg", [2, NE, D, FE])
    g.wu = din("wu", [2, NE, D, FE])
    g.wd = din("wd", [2, NE, FE, D])
    g.ident = din("ident", [128, 128])
    g.out = nc.dram_tensor("out", [S, D], F32, kind="ExternalOutput").ap()
    kind = "ExternalOutput" if dbg else "Internal"
    g.xs = [nc.dram_tensor("xs%d" % i, [S, D], F32, kind=kind).ap() for i in range(3)]
    g.cst = din("cst", [128, 160])
    g.xg_all = nc.dram_tensor("xg_all", [NE * 2048, D], BF16, kind="Internal").ap()
    g.yg_all = nc.dram_tensor("yg_all", [NE * 2048, D], F32, kind="Internal").ap()
    g.xg_tk, g.yg_tk = Tk(), Tk()

    ARENA = 51456
    with (
        nc.sbuf_tensor("arena", [128, ARENA], F32) as arena,
        nc.sbuf_tensor("pers", [128, 1128], F32) as pers,
        nc.psum_tensor("ps0", [128, 512], F32) as ps0, nc.psum_tensor("ps1", [128, 512], F32) as ps1,
        nc.psum_tensor("ps2", [128, 512], F32) as ps2, nc.psum_tensor("ps3", [128, 512], F32) as ps3,
        nc.psum_tensor("ps4", [128, 512], F32) as ps4, nc.psum_tensor("ps5", [128, 512], F32) as ps5,
        nc.psum_tensor("ps6", [128, 512], F32) as ps6, nc.psum_tensor("ps7", [128, 512], F32) as ps7,
    ):
        g.arena = arena
        g.ps = [ps0, ps1, ps2, ps3, ps4, ps5, ps6, ps7]
        g.pst = [Tk() for _ in range(8)]
        g.pers = pers
        g.ident32 = pers[:, 0:128]
        g.modT = [pers[:, 128:224].rearrange("p (j k) -> p j k", j=6), pers[:, 224:320].rearrange("p (j k) -> p j k", j=6)]
        g.modcT = pers[:, 320:352].rearrange("p (j k) -> p j k", j=2)
        g.gTs = pers[:, 352:416].rearrange("p (j k) -> p j k", j=4)
        g.AB = pers[:, 416:480].rearrange("p (j k) -> p j k", j=4)
        g.ones32 = pers[:, 480:608]
        g.selA = pers[0:2, 608:736]
        g.cTs = pers[:, 736:768].rearrange("p (k c) -> p k c", c=2)
        g.small = pers[:, 768:1128]
        g.tk_pers = Tk()
        g.tk_mod = Tk()
        g.tk_AB = Tk()

        from_stage = {}
        setup_consts(g)
        if 0 in stages:
            stage_ada(g)
        P.barrier()
        if 1 in stages:
            stage_mixer0(g, g.x, g.xs[0])
            P.barrier()
        if 2 in stages:
            (stage_moe_sparse if sparse else stage_moe)(g, 0, g.xs[0] if 1 in stages else g.x, g.xs[1], final=False)
            P.barrier()
        if 3 in stages:
            stage_conv(g, g.xs[1] if 2 in stages else g.x, g.xs[2])
            P.barrier()
        if 4 in stages:
            (stage_moe_sparse if sparse else stage_moe)(g, 1, g.xs[2] if 3 in stages else g.x, g.out, final=True)
            P.barrier()
        P.add("sp", None)

        nsem = 100
        import contextlib
        with contextlib.ExitStack() as st:
            sems = [st.enter_context(nc.semaphore("s%d" % i)) for i in range(nsem)]
            P.regs = {"pe": st.enter_context(nc.tensor.register("r_pe")), "act": st.enter_context(nc.scalar.register("r_act")),
                      "dve": st.enter_context(nc.vector.register("r_dve")), "pool": st.enter_context(nc.gpsimd.register("r_pool")),
                      "sp": st.enter_context(nc.sync.register("r_sp"))}
            P.regs2 = {"pe": st.enter_context(nc.tensor.register("r2_pe")), "act": st.enter_context(nc.scalar.register("r2_act")),
                       "dve": st.enter_context(nc.vector.register("r2_dve")), "pool": st.enter_context(nc.gpsimd.register("r2_pool")),
                       "sp": st.enter_context(nc.sync.register("r2_sp"))}
            block = st.enter_context(nc.Block())
            P.run_emit(nc, block, None, sems)
    return nc


def arena_f32(g, off, n):
    return g.arena[:, off:off + n]


def arena_bf(g, off, n):
    return g.arena[:, off:off + n // 2].bitcast(BF16)


def setup_consts(g):
    P = g.P
    tk = g.tk_pers
    P.add("sp", lambda e: e.dma_start(out=g.ident32, in_=g.ident), [], [tk], dma=True)
    P.add("sp", lambda e: e.dma_start(out=g.gTs, in_=g.gT), [], [tk], dma=True)
    P.add("sp", lambda e: e.dma_start(out=g.cTs, in_=g.cT), [], [tk], dma=True)
    P.add("pool", lambda e: e.memset(g.ones32, 1.0), [], [tk])
    P.add("pool", lambda e: e.memset(g.selA, 0.0), [], [tk])
    P.add("pool", lambda e: e.memset(g.pers[0:1, 608:736], 1.0), [], [tk])


def stage_ada(g):
    P = g.P
    NR = 8
    wr = [arena_bf(g, i * 1024, 2048).rearrange("p (k n) -> p k n", k=4) for i in range(NR)]
    wr_tk = [Tk() for _ in range(NR)]
    bias = [g.arena[0:4, 8192 + i * 512: 8192 + (i + 1) * 512] for i in range(2)]
    bias_tk = [Tk(), Tk()]
    mrow = [g.arena[0:4, 9216 + i * 512: 9216 + (i + 1) * 512] for i in range(2)]
    mrow_tk = [Tk(), Tk()]
    sT = g.small[:, 0:32].rearrange("p (k c) -> p k c", c=2)
    s4 = arena_bf(g, 10240, 64).rearrange("p (k c) -> p k c", c=4)
    hi32 = arena_f32(g, 10304, 32).rearrange("p (k c) -> p k c", c=2)
    cmb = g.arena[0:4, 10400:10402]
    tk_s = Tk()
    P.add("act", lambda e: e.activation(out=sT, in_=g.cTs, func=AF.Silu), [g.tk_pers], [tk_s])
    s4v = s4.rearrange("p k (c h) -> p k c h", h=2)
    P.add("dve", lambda e: e.tensor_copy(out=s4v[:, :, :, 0], in_=sT), [tk_s], [tk_s])
    P.add("dve", lambda e: e.tensor_copy(out=hi32, in_=s4v[:, :, :, 0]), [tk_s], [tk_s])
    P.add("dve", lambda e: e.tensor_tensor(out=s4v[:, :, :, 1], in0=sT, in1=hi32, op=ALU.subtract), [tk_s], [tk_s])
    P.add("sp", lambda e: e.dma_start(out=cmb, in_=g.cmb), [], [tk_s], dma=True)
    u = 0
    for l in range(2):
        for nb in range(24):
            j, q = divmod(nb, 4)
            pm = g.ps[nb % 2][0:4, :]
            pm_tk = g.pst[nb % 2]
            for kk in range(4):
                slot = u % NR
                u += 1
                src_ = g.ada_w[l, kk * 512:(kk + 1) * 512, nb * 512:(nb + 1) * 512].rearrange("(k p) n -> p k n", p=128)
                P.add("pool", lambda e, o=wr[slot], s=src_: e.dma_start(out=o, in_=s), [], [wr_tk[slot]], dma=True)
                for k4 in range(4):
                    k = kk * 4 + k4
                    P.add("pe", lambda e, o=pm, a=s4[:, k, :], b=wr[slot][:, k4, :], st=(k == 0), sp=(k == 15):
                          e.matmul(o, a, b, start=st, stop=sp), [tk_s, wr_tk[slot]], [pm_tk])
            bb = nb % 2
            P.add("sp", lambda e, o=bias[bb], s=g.ada_b4[l, :, nb * 512:(nb + 1) * 512]: e.dma_start(out=o, in_=s), [], [bias_tk[bb]], dma=True)
            P.add("dve", lambda e, o=mrow[bb], a=pm, b=bias[bb]: e.tensor_tensor(out=o, in0=a, in1=b, op=ALU.add),
                  [pm_tk, bias_tk[bb]], [mrow_tk[bb]])
            pt = g.ps[2 + bb][:, 0:8]
            pt_tk = g.pst[2 + bb]
            for qq in range(4):
                P.add("pe", lambda e, o=pt[:, qq * 2:qq * 2 + 2], i=mrow[bb][0:4, qq * 128:(qq + 1) * 128]:
                      e.matmul(o, i, cmb, start=True, stop=True), [mrow_tk[bb], tk_s], [pt_tk])
            ptv = pt.rearrange("p (q r) -> p q r", r=2)
            P.add("dve", lambda e, o=g.modT[l][:, j, q * 4:(q + 1) * 4], i=ptv[:, :, 0]: e.tensor_copy(out=o, in_=i),
                  [pt_tk], [g.tk_mod])
            if l == 0 and j < 2:
                P.add("dve", lambda e, o=g.modcT[:, j, q * 4:(q + 1) * 4], i=ptv[:, :, 1]: e.tensor_copy(out=o, in_=i),
                      [pt_tk], [g.tk_mod])


def prep_AB(g, gi, scaleT, shiftT, slot):
    P = g.P
    A = g.AB[:, slot, :]
    B = g.AB[:, slot + 1, :]
    P.add("dve", lambda e: e.scalar_tensor_tensor(out=A, in0=scaleT, scalar=1.0, in1=g.gTs[:, gi, :], op0=ALU.add, op1=ALU.mult),
          [g.tk_mod, g.tk_pers], [g.tk_AB])
    P.add("dve", lambda e: e.tensor_copy(out=B, in_=shiftT), [g.tk_mod], [g.tk_AB])
    return A, B


def make_bc(g, srcT, dst, dst_tk, tmp_off):
    P = g.P
    dg = [arena_f32(g, tmp_off + i * 128, 128) for i in range(2)]
    dg_tk = [Tk(), Tk()]
    for k in range(NK):
        s = k % 2
        P.add("pool", lambda e, o=dg[s], sc=srcT[:, k:k + 1]: e.tensor_scalar(out=o, in0=g.ident32, scalar1=sc, scalar2=None, op0=ALU.mult),
              [g.tk_mod, g.tk_pers, g.tk_AB], [dg_tk[s]])
        bank = 4 + (k // 4) % 2
        P.add("pe", lambda e, o=g.ps[bank][:, (k % 4) * 128:(k % 4 + 1) * 128], b=dg[s]: e.matmul(o, g.ones32, b, start=True, stop=True),
              [dg_tk[s], g.tk_pers], [g.pst[bank]])
        if k % 4 == 3:
            c0 = (k // 4) * 512
            P.add("act", lambda e, o=dst[:, c0:c0 + 512], i=g.ps[bank][:, :]: e.activation(out=o, in_=i, func=AF.Copy),
                  [g.pst[bank]], [dst_tk])


def norm_transpose(g, src, src_tk, A, B, ab_slot_reads, dstf, tmp, it, router=None, nodst=False, phase=0):
    P = g.P
    nr = len(tmp["xn"])
    r = it % nr
    ss, ss_tk = tmp["ss"][it % 2]
    sq, sq_tk = tmp["sq"][it % 2]
    xn, xn_tk = tmp["xn"][r]
    junk, junk_tk = tmp["junk"]
    if phase in (0, 1):
        P.add("dve", lambda e: e.memset(ss, 0.0), [], [ss_tk])
        P.add("act", lambda e: e.activation(out=junk, in_=src, func=AF.Square, accum_out=ss), [src_tk], [junk_tk, ss_tk])
        P.add("act", lambda e: e.activation(out=sq, in_=ss, func=AF.Sqrt, bias=tmp["eps"], scale=1.0 / D), [ss_tk, g.tk_pers], [sq_tk])
        P.add("dve", lambda e: e.reciprocal(out=sq, in_=sq), [sq_tk], [sq_tk])
        P.add("act", lambda e: e.activation(out=xn, in_=src, func=AF.Copy, scale=sq), [src_tk, sq_tk], [xn_tk])
    if phase == 1:
        return
    for b in range(4):
        for c in range(4):
            k = b * 4 + c
            P.add("pe", lambda e, o=g.ps[b][:, c * 128:(c + 1) * 128], i=xn[:, k * 128:(k + 1) * 128]: e.transpose(o, i, g.ident32),
                  [xn_tk, g.tk_pers], [g.pst[b]])
    t32s = []
    for b in range(4):
        if router is not None:
            t32, t32_tk = tmp["t32"][(it * 4 + b) % len(tmp["t32"])]
            t32s.append((t32, t32_tk))
        if not nodst:
            dst, dst_tk = dstf(b)
        for c in range(4):
            k = b * 4 + c
            pc = g.ps[b][:, c * 128:(c + 1) * 128]
            if router is not None:
                o_, wtk = t32[:, c * 128:(c + 1) * 128], t32_tk
            else:
                o_, wtk = dst[:, c, :], dst_tk
            if c % 2 == 0:
                P.add("act", lambda e, o=o_, i=pc, k=k: e.activation(out=o, in_=i, func=AF.Identity, scale=A[:, k:k + 1], bias=B[:, k:k + 1]), [g.pst[b], g.tk_AB], [wtk])
            else:
                P.add("dve", lambda e, o=o_, i=pc, k=k: e.tensor_scalar(out=o, in0=i, scalar1=A[:, k:k + 1], scalar2=B[:, k:k + 1], op0=ALU.mult, op1=ALU.add), [g.pst[b], g.tk_AB], [wtk])
        if router is not None and not nodst:
            P.add("act", lambda e, o=dst, i=t32.rearrange("p (c n) -> p c n", c=4): e.activation(out=o, in_=i, func=AF.Copy), [t32_tk], [dst_tk])
    if router is not None:
        lg, lg_tk, rws, rw_tk = router
        for b in range(4):
            t32, t32_tk = t32s[b]
            for c in range(4):
                k = b * 4 + c
                P.add("pe", lambda e, o=lg, a=t32[:, c * 128:(c + 1) * 128], w=rws[:, k, :], st=(k == 0), sp=(k == 15):
                      e.matmul(o, a, w, start=st, stop=sp), [t32_tk, rw_tk], [lg_tk])


def stage_moe(g, l, xsrc, xdst, final):
    P = g.P
    A, B = prep_AB(g, 2 * l + 1, g.modT[l][:, 4, :], g.modT[l][:, 3, :], 0)
    ACC, HT, W0 = 0, 16384, 24576
    STG, GU, DN, AT0, CBC, G2, TMP, COMBT, SELE = 24576, 28672, 32768, 40960, 45056, 46080, 48128, 50176, 51200
    acc = [arena_f32(g, ACC + t * 2048, 2048) for t in range(8)]
    acc_tk = [Tk() for _ in range(8)]
    hT = arena_bf(g, HT, 16384).rearrange("p (k n) -> p k n", k=NK)
    hT_tk = [Tk() for _ in range(8)]
    g2bc = arena_f32(g, G2, 2048)
    g2_tk = Tk()
    make_bc(g, g.modT[l][:, 5, :], g2bc, g2_tk, TMP)
    rws = g.small[:, 64:64 + 256].rearrange("p (k e) -> p k e", e=NE)
    rw_tk = Tk()
    rbs = g.small[:, 320:336]
    eps = g.small[:, 336:337]
    P.add("sp", lambda e: e.dma_start(out=rws, in_=g.rw), [], [rw_tk], dma=True)
    P.add("sp", lambda e: e.dma_start(out=rbs, in_=g.rb), [], [rw_tk], dma=True)
    P.add("pool", lambda e: e.memset(eps, EPS), [], [g.tk_pers])
    for tb in range(2):
        tmp = {
            "ss": [(g.small[:, 340 + i:341 + i], Tk()) for i in range(2)],
            "sq": [(g.small[:, 344 + i:345 + i], Tk()) for i in range(2)],
            "xn": [(arena_f32(g, W0 + i * 2048, 2048), Tk()) for i in range(2)],
            "junk": (arena_bf(g, W0 + 4096, 2048), Tk()),
            "t32": [(arena_f32(g, W0 + 5120 + i * 512, 512), Tk()) for i in range(2)],
            "eps": eps,
        }
        lgps = g.ps[6]
        lg_tk = g.pst[6]
        for t in range(8):
            i = tb * 8 + t
            P.add("sp", lambda e, o=acc[t], s=xsrc[i * 128:(i + 1) * 128, :]: e.dma_start(out=o, in_=s), [], [acc_tk[t]], dma=True)
            norm_transpose(g, acc[t], acc_tk[t], A, B, None,
                           lambda b, t=t: (hT[:, b * 4:(b + 1) * 4, t * 128:(t + 1) * 128], hT_tk[t]),
                           tmp, t, router=(lgps[:, t * 16:(t + 1) * 16], lg_tk, rws, rw_tk))
        RB = W0 + 6144

        def rt(n, w):
            return arena_f32(g, RB + n * 128, w)
        sc, sel, eq, msk, w_, comb = rt(0, 128), rt(1, 128), rt(2, 128), rt(3, 128), rt(4, 128), rt(5, 128)
        m1, m2, gs, ing = rt(6, 32), rt(7, 32), rt(8, 32), rt(9, 32)
        gmax, wsum = rt(10, 8), rt(11, 8)
        rtk = Tk()

        def v3(a, x, y):
            return a.rearrange("p (x y) -> p x y", x=x)
        P.add("act", lambda e: e.activation(out=sc, in_=lgps[:, 0:128], func=AF.Sigmoid), [lg_tk], [rtk])
        P.add("dve", lambda e: e.tensor_tensor(out=v3(sel, 8, 16), in0=v3(sc, 8, 16), in1=rbs.unsqueeze(1).to_broadcast([128, 8, 16]), op=ALU.add), [rtk, rw_tk], [rtk])
        P.add("dve", lambda e: e.tensor_reduce(out=m1, in_=v3(sel, 32, 4), axis=AX.X, op=ALU.max), [rtk], [rtk])
        P.add("dve", lambda e: e.tensor_tensor(out=v3(eq, 32, 4), in0=v3(sel, 32, 4), in1=m1.unsqueeze(2).to_broadcast([128, 32, 4]), op=ALU.is_equal), [rtk], [rtk])
        P.add("dve", lambda e: e.scalar_tensor_tensor(out=msk, in0=eq, scalar=-1e9, in1=sel, op0=ALU.mult, op1=ALU.add), [rtk], [rtk])
        P.add("dve", lambda e: e.tensor_reduce(out=m2, in_=v3(msk, 32, 4), axis=AX.X, op=ALU.max), [rtk], [rtk])
        P.add("dve", lambda e: e.tensor_tensor(out=gs, in0=m1, in1=m2, op=ALU.add), [rtk], [rtk])
        P.add("dve", lambda e: e.tensor_reduce(out=gmax, in_=v3(gs, 8, 4), axis=AX.X, op=ALU.max), [rtk], [rtk])
        P.add("dve", lambda e: e.tensor_tensor(out=v3(ing, 8, 4), in0=v3(gs, 8, 4), in1=gmax.unsqueeze(2).to_broadcast([128, 8, 4]), op=ALU.is_equal), [rtk], [rtk])
        P.add("dve", lambda e: e.tensor_tensor(out=v3(eq, 32, 4), in0=v3(sel, 32, 4), in1=m2.unsqueeze(2).to_broadcast([128, 32, 4]), op=ALU.is_ge), [rtk], [rtk])
        P.add("dve", lambda e: e.tensor_tensor(out=v3(msk, 32, 4), in0=v3(eq, 32, 4), in1=ing.unsqueeze(2).to_broadcast([128, 32, 4]), op=ALU.mult), [rtk], [rtk])
        P.add("dve", lambda e: e.tensor_tensor(out=w_, in0=sc, in1=msk, op=ALU.mult), [rtk], [rtk])
        P.add("dve", lambda e: e.tensor_reduce(out=wsum, in_=v3(w_, 8, 16), axis=AX.X, op=ALU.add), [rtk], [rtk])
        P.add("dve", lambda e: e.reciprocal(out=wsum, in_=wsum), [rtk], [rtk])
        P.add("dve", lambda e: e.tensor_tensor(out=v3(comb, 8, 16), in0=v3(w_, 8, 16), in1=wsum.unsqueeze(2).to_broadcast([128, 8, 16]), op=ALU.mult), [rtk], [rtk])
        combT = g.arena[0:16, COMBT: COMBT + 1024]
        combT_tk = Tk()
        for half in range(2):
            bank = 4 + half
            for t4 in range(4):
                t = half * 4 + t4
                P.add("pe", lambda e, o=g.ps[bank][0:16, t4 * 128:(t4 + 1) * 128], i=comb[:, t * 16:(t + 1) * 16]: e.transpose(o, i, g.ident32),
                      [rtk, g.tk_pers], [g.pst[bank]])
            P.add("act", lambda e, o=combT[:, half * 512:(half + 1) * 512], i=g.ps[bank][0:16, :]: e.activation(out=o, in_=i, func=AF.Copy),
                  [g.pst[bank]], [combT_tk])
        sel2 = [g.arena[0:16, SELE + i * 128: SELE + (i + 1) * 128] for i in range(2)]
        sel2_tk = [Tk(), Tk()]
        P.barrier()
        stg = [arena_f32(g, STG + i * 2048, 2048) for i in range(2)]
        stg_tk = [Tk() for _ in range(2)]
        gub = [arena_bf(g, GU + i * 1024, 2048) for i in range(4)]
        gu_tk = [Tk() for _ in range(4)]
        dnb = [arena_bf(g, DN + i * 1024, 2048) for i in range(8)]
        dn_tk = [Tk() for _ in range(8)]
        ATb = [arena_bf(g, AT0 + i * 2048, 4096).rearrange("p (f n) -> p f n", f=4) for i in range(2)]
        AT_tk = [Tk(), Tk()]
        cbc = [arena_bf(g, CBC + i * 512, 1024) for i in range(2)]
        cbc_tk = [Tk(), Tk()]
        sgt = [arena_f32(g, TMP + i * 512, 512) for i in range(2)]
        sg_tk = [Tk(), Tk()]
        t2t = [arena_f32(g, TMP + 1024 + i * 512, 512) for i in range(2)]
        t2_tk = [Tk(), Tk()]
        units = []
        for ex in range(NE):
            for f in range(4):
                units.append(("g", ex, f))
                units.append(("u", ex, f))
                units.append(("d", ex, f))
        state = {"dma": 0, "cast": 0}

        def unit_src(u):
            kind, ex, f = u
            if kind == "g":
                return g.wg[l, ex, :, f * 128:(f + 1) * 128].rearrange("(k p) n -> p k n", p=128)
            if kind == "u":
                return g.wu[l, ex, :, f * 128:(f + 1) * 128].rearrange("(k p) n -> p k n", p=128)
            return g.wd[l, ex, f * 128:(f + 1) * 128, :]

        def unit_dst(n):
            kind, ex, f = units[n]
            if kind == "d":
                s = (ex % 2) * 4 + f
                return dnb[s], dn_tk[s]
            s = (2 * (ex * 4 + f) + (1 if kind == "u" else 0)) % 4
            return gub[s], gu_tk[s]

        def issue_dma(upto):
            while state["dma"] < min(upto, len(units)):
                n = state["dma"]
                kind = units[n][0]
                s = n % 2
                o = stg[s] if kind == "d" else stg[s].rearrange("p (k n) -> p k n", k=NK)
                P.add("sp", lambda e, o=o, sr=unit_src(units[n]): e.dma_start(out=o, in_=sr), [], [stg_tk[s]], dma=True)
                state["dma"] += 1

        def issue_cast(upto):
            while state["cast"] < min(upto, len(units)):
                n = state["cast"]
                issue_dma(n + 2)
                kind = units[n][0]
                s = n % 2
                dst, dtk = unit_dst(n)
                if kind == "d":
                    P.add("pool", lambda e, o=dst, i=stg[s]: e.tensor_tensor(out=o, in0=i, in1=g2bc, op=ALU.mult), [stg_tk[s], g2_tk], [dtk])
                elif kind == "g":
                    P.add("act", lambda e, o=dst, i=stg[s]: e.activation(out=o, in_=i, func=AF.Copy), [stg_tk[s]], [dtk])
                else:
                    P.add("dve", lambda e, o=dst, i=stg[s]: e.tensor_copy(out=o, in_=i), [stg_tk[s]], [dtk])
                state["cast"] += 1

        issue_cast(6)
        it = 0
        for ex in range(NE):
            c = ex % 2
            P.add("pool", lambda e, o=sel2[c], i=g.ident32[0:16, ex:ex + 1].to_broadcast([16, 128]): e.tensor_copy(out=o, in_=i), [g.tk_pers], [sel2_tk[c]])
            for sb in range(2):
                bank = 4 + sb
                P.add("pe", lambda e, o=g.ps[bank][:, :], a=sel2[c], b=combT[:, sb * 512:(sb + 1) * 512]: e.matmul(o, a, b, start=True, stop=True),
                      [sel2_tk[c], combT_tk], [g.pst[bank]])
                P.add("act", lambda e, o=cbc[c][:, sb * 512:(sb + 1) * 512], i=g.ps[bank][:, :]: e.activation(out=o, in_=i, func=AF.Copy),
                      [g.pst[bank]], [cbc_tk[c]])
            for f in range(4):
                n0 = (ex * 4 + f) * 3
                issue_cast(n0 + 6)
                gw, gtk = unit_dst(n0)
                uw, utk = unit_dst(n0 + 1)
                gw3 = gw.rearrange("p (k n) -> p k n", k=NK)
                uw3 = uw.rearrange("p (k n) -> p k n", k=NK)
                for sb in range(2):
                    bg, bu = (0, 1) if it % 2 == 0 else (2, 3)
                    for k in range(NK):
                        P.add("pe", lambda e, o=g.ps[bg][:, :], a=gw3[:, k, :], b=hT[:, k, sb * 512:(sb + 1) * 512], st=(k == 0), sp=(k == NK - 1):
                              e.matmul(o, a, b, start=st, stop=sp), [gtk] + hT_tk[sb * 4:sb * 4 + 4], [g.pst[bg]])
                    for k in range(NK):
                        P.add("pe", lambda e, o=g.ps[bu][:, :], a=uw3[:, k, :], b=hT[:, k, sb * 512:(sb + 1) * 512], st=(k == 0), sp=(k == NK - 1):
                              e.matmul(o, a, b, start=st, stop=sp), [utk] + hT_tk[sb * 4:sb * 4 + 4], [g.pst[bu]])
                    r = it % 2
                    P.add("act", lambda e, o=sgt[r], i=g.ps[bg][:, :]: e.activation(out=o, in_=i, func=AF.Silu), [g.pst[bg]], [sg_tk[r]])
                    P.add("dve", lambda e, o=t2t[r], a=g.ps[bu][:, :], b=sgt[r]: e.tensor_tensor(out=o, in0=a, in1=b, op=ALU.mult), [g.pst[bu], sg_tk[r]], [t2_tk[r]])
                    P.add("pool", lambda e, o=ATb[c][:, f, sb * 512:(sb + 1) * 512], a=t2t[r], b=cbc[c][:, sb * 512:(sb + 1) * 512]:
                          e.tensor_tensor(out=o, in0=a, in1=b, op=ALU.mult), [t2_tk[r], cbc_tk[c]], [AT_tk[c]])
                    it += 1
            for t in range(8):
                for db in range(4):
                    bank = 6 + (t * 4 + db) % 2
                    for f in range(4):
                        dw, dtk = dnb[(ex % 2) * 4 + f], dn_tk[(ex % 2) * 4 + f]
                        P.add("pe", lambda e, o=g.ps[bank][:, :], a=ATb[c][:, f, t * 128:(t + 1) * 128], b=dw[:, db * 512:(db + 1) * 512], st=(f == 0), sp=(f == 3):
                              e.matmul(o, a, b, start=st, stop=sp), [AT_tk[c], dtk], [g.pst[bank]])
                    P.add("dve", lambda e, o=acc[t][:, db * 512:(db + 1) * 512], i=g.ps[bank][:, :]: e.tensor_tensor(out=o, in0=i, in1=o, op=ALU.add),
                          [g.pst[bank], acc_tk[t]], [acc_tk[t]])
        P.barrier()
        if final:
            fng = arena_f32(g, W0, 2048)
            fng_tk = Tk()
            P.add("sp", lambda e: e.dma_start(out=fng, in_=g.fng), [], [fng_tk], dma=True)
            junk = arena_bf(g, W0 + 2048, 2048)
            junk_tk = Tk()
            fin_ss_tk = [Tk(), Tk()]
            for t in range(8):
                i = tb * 8 + t
                ss, ss_tk = g.small[:, 340 + t % 2:341 + t % 2], fin_ss_tk[t % 2]
                P.add("pool", lambda e, o=ss: e.memset(o, 0.0), [], [ss_tk])
                P.add("act", lambda e, o=junk, i_=acc[t], a=ss: e.activation(out=o, in_=i_, func=AF.Square, accum_out=a), [acc_tk[t]], [junk_tk, ss_tk])
                P.add("act", lambda e, o=ss: e.activation(out=o, in_=o, func=AF.Sqrt, bias=eps, scale=1.0 / D), [ss_tk, g.tk_pers], [ss_tk])
                P.add("dve", lambda e, o=ss: e.reciprocal(out=o, in_=o), [ss_tk], [ss_tk])
                P.add("dve", lambda e, o=acc[t], sc_=ss: e.scalar_tensor_tensor(out=o, in0=o, scalar=sc_, in1=fng, op0=ALU.mult, op1=ALU.mult), [acc_tk[t], ss_tk, fng_tk], [acc_tk[t]])
                P.add("sp", lambda e, o=xdst[i * 128:(i + 1) * 128, :], s=acc[t]: e.dma_start(out=o, in_=s), [acc_tk[t]], [], dma=True)
        else:
            for t in range(8):
                i = tb * 8 + t
                P.add("sp", lambda e, o=xdst[i * 128:(i + 1) * 128, :], s=acc[t]: e.dma_start(out=o, in_=s), [acc_tk[t]], [], dma=True)
        P.barrier()


def stage_moe_sparse(g, l, xsrc, xdst, final):
    P = g.P
    I32 = mybir.dt.int32
    U16 = mybir.dt.uint16
    A, B = prep_AB(g, 2 * l + 1, g.modT[l][:, 4, :], g.modT[l][:, 3, :], 0)
    NTL = NT
    H2, ABC, BBC, XT, XN, JK, T32, RB = 0, 16384, 18432, 20480, 26624, 32768, 33792, 37888
    PERS = 51200
    h2tok = arena_bf(g, H2, 32768).rearrange("p (t n) -> p t n", t=NTL)
    h2_tk = [Tk() for _ in range(NTL)]
    Abc, Bbc = arena_f32(g, ABC, 2048), arena_f32(g, BBC, 2048)
    Abc_tk, Bbc_tk = Tk(), Tk()
    make_bc(g, A, Abc, Abc_tk, RB)
    make_bc(g, B, Bbc, Bbc_tk, RB + 256)
    rws = g.small[:, 64:64 + 256].rearrange("p (k e) -> p k e", e=NE)
    rw_tk = Tk()
    rbs = g.small[:, 320:336]
    eps = g.small[:, 336:337]
    P.add("sp", lambda e: e.dma_start(out=rws, in_=g.rw), [], [rw_tk], dma=True)
    P.add("sp", lambda e: e.dma_start(out=rbs, in_=g.rb), [], [rw_tk], dma=True)
    P.add("pool", lambda e: e.memset(eps, EPS), [], [g.tk_pers])
    desti = g.arena[:, PERS:PERS + 32].bitcast(I32)
    cw = arena_f32(g, PERS + 32, 32)
    cntneg = g.arena[0:1, PERS + 64:PERS + 82].bitcast(I32)
    maskbits = g.arena[:, PERS + 96:PERS + 224].bitcast(U16)
    pers_tk = Tk()
    xt = [arena_f32(g, XT + i * 2048, 2048) for i in range(3)]
    xt_tk = [Tk(), Tk(), Tk()]
    tmp = {
        "ss": [(g.small[:, 340 + i:341 + i], Tk()) for i in range(2)],
        "sq": [(g.small[:, 344 + i:345 + i], Tk()) for i in range(2)],
        "xn": [(arena_f32(g, XN + i * 2048, 2048), Tk()) for i in range(3)],
        "junk": (arena_bf(g, JK, 2048), Tk()),
        "t32": [(arena_f32(g, T32 + i * 512, 512), Tk()) for i in range(8)],
        "eps": eps,
    }
    lgps = g.ps[6]
    lg_tk = g.pst[6]
    def n_stage1(t):
        if t < NTL:
            r_ = t % 3
            P.add("sp", lambda e, o=xt[r_], s=xsrc[t * 128:(t + 1) * 128, :]: e.dma_start(out=o, in_=s), [], [xt_tk[r_]], dma=True)
            norm_transpose(g, xt[r_], xt_tk[r_], A, B, None, None, tmp, t, router=(None, None, None, None), nodst=True, phase=1)
    n_stage1(0)
    for t in range(NTL):
        r = t % 3
        n_stage1(t + 1)
        norm_transpose(g, xt[r], xt_tk[r], A, B, None, None, tmp, t, router=(lgps[:, t * 16:(t + 1) * 16], lg_tk, rws, rw_tk), nodst=True, phase=2)
        xn, xn_tk = tmp["xn"][r]
        P.add("dve", lambda e, o=xn: e.tensor_tensor(out=o, in0=o, in1=Abc, op=ALU.mult), [xn_tk, Abc_tk], [xn_tk])
        P.add("pool", lambda e, o=h2tok[:, t, :], i=xn: e.tensor_tensor(out=o, in0=i, in1=Bbc, op=ALU.add), [xn_tk, Bbc_tk], [h2_tk[t]])
    W = NTL * 16

    def rt(n, w=W):
        return arena_f32(g, RB + n * 256, w)
    sc, sel, eq, msk, w_, comb = rt(0), rt(1), rt(2), rt(3), rt(4), rt(5)
    m1, m2, gs, ing = rt(6, 64), rt(7, 64), rt(8, 64), rt(9, 64)
    gmax, wsum = arena_f32(g, RB + 10 * 256, 16), arena_f32(g, RB + 10 * 256 + 16, 16)
    within, cntbc, pref, dfull, ta, tb2 = rt(11), rt(12), rt(13), rt(14), rt(15), rt(16)
    d01 = arena_f32(g, RB + 17 * 256, 32)
    cntf = arena_f32(g, RB + 17 * 256 + 32, 16)
    cnti = g.arena[:, RB + 17 * 256 + 48: RB + 17 * 256 + 64].bitcast(I32)
    cst = arena_f32(g, RB + 18 * 256, 160)
    validf = arena_f32(g, RB + 19 * 256, 256)
    rtk = Tk()
    cst_tk = Tk()
    P.add("sp", lambda e: e.dma_start(out=cst, in_=g.cst), [], [cst_tk], dma=True)
    Utri, posc, ebase = cst[:, 0:128], cst[:, 128:144], cst[:, 144:160]

    def v3(a, x, y):
        return a.rearrange("p (x y) -> p x y", x=x)
    P.add("act", lambda e: e.activation(out=sc, in_=lgps[:, 0:W], func=AF.Sigmoid), [lg_tk], [rtk])
    P.add("dve", lambda e: e.tensor_tensor(out=v3(sel, NTL, 16), in0=v3(sc, NTL, 16), in1=rbs.unsqueeze(1).to_broadcast([128, NTL, 16]), op=ALU.add), [rtk, rw_tk], [rtk])
    P.add("dve", lambda e: e.tensor_reduce(out=m1, in_=v3(sel, NTL * 4, 4), axis=AX.X, op=ALU.max), [rtk], [rtk])
    P.add("dve", lambda e: e.tensor_tensor(out=v3(eq, NTL * 4, 4), in0=v3(sel, NTL * 4, 4), in1=m1.unsqueeze(2).to_broadcast([128, NTL * 4, 4]), op=ALU.is_equal), [rtk], [rtk])
    P.add("dve", lambda e: e.scalar_tensor_tensor(out=msk, in0=eq, scalar=-1e9, in1=sel, op0=ALU.mult, op1=ALU.add), [rtk], [rtk])
    P.add("dve", lambda e: e.tensor_reduce(out=m2, in_=v3(msk, NTL * 4, 4), axis=AX.X, op=ALU.max), [rtk], [rtk])
    P.add("dve", lambda e: e.tensor_tensor(out=gs, in0=m1, in1=m2, op=ALU.add), [rtk], [rtk])
    P.add("dve", lambda e: e.tensor_reduce(out=gmax, in_=v3(gs, NTL, 4), axis=AX.X, op=ALU.max), [rtk], [rtk])
    P.add("dve", lambda e: e.tensor_tensor(out=v3(ing, NTL, 4), in0=v3(gs, NTL, 4), in1=gmax.unsqueeze(2).to_broadcast([128, NTL, 4]), op=ALU.is_equal), [rtk], [rtk])
    P.add("dve", lambda e: e.tensor_tensor(out=v3(eq, NTL * 4, 4), in0=v3(sel, NTL * 4, 4), in1=m2.unsqueeze(2).to_broadcast([128, NTL * 4, 4]), op=ALU.is_ge), [rtk], [rtk])
    P.add("dve", lambda e: e.tensor_tensor(out=v3(msk, NTL * 4, 4), in0=v3(eq, NTL * 4, 4), in1=ing.unsqueeze(2).to_broadcast([128, NTL * 4, 4]), op=ALU.mult), [rtk], [rtk])
    P.add("dve", lambda e: e.tensor_tensor(out=w_, in0=sc, in1=msk, op=ALU.mult), [rtk], [rtk])
    P.add("dve", lambda e: e.tensor_reduce(out=wsum, in_=v3(w_, NTL, 16), axis=AX.X, op=ALU.add), [rtk], [rtk])
    P.add("dve", lambda e: e.reciprocal(out=wsum, in_=wsum), [rtk], [rtk])
    P.add("dve", lambda e: e.tensor_tensor(out=v3(comb, NTL, 16), in0=v3(w_, NTL, 16), in1=wsum.unsqueeze(2).to_broadcast([128, NTL, 16]), op=ALU.mult), [rtk], [rtk])
    P.add("pe", lambda e: e.matmul(g.ps[4][:, 0:W], Utri, msk, start=True, stop=True), [rtk, cst_tk], [g.pst[4]])
    P.add("pe", lambda e: e.matmul(g.ps[5][:, 0:W], g.ones32, msk, start=True, stop=True), [rtk, g.tk_pers], [g.pst[5]])
    P.add("act", lambda e: e.activation(out=within, in_=g.ps[4][:, 0:W], func=AF.Copy), [g.pst[4]], [rtk])
    P.add("act", lambda e: e.activation(out=cntbc, in_=g.ps[5][:, 0:W], func=AF.Copy), [g.pst[5]], [rtk])
    P.add("pool", lambda e: e.memset(pref[:, 0:16], 0.0), [rtk], [rtk])
    for t in range(1, NTL):
        P.add("dve", lambda e, t=t: e.tensor_tensor(out=pref[:, t * 16:(t + 1) * 16], in0=pref[:, (t - 1) * 16:t * 16], in1=cntbc[:, (t - 1) * 16:t * 16], op=ALU.add), [rtk], [rtk])
    P.add("dve", lambda e: e.tensor_tensor(out=cntf, in0=pref[:, (NTL - 1) * 16:NTL * 16], in1=cntbc[:, (NTL - 1) * 16:NTL * 16], op=ALU.add), [rtk], [rtk])
    P.add("dve", lambda e: e.tensor_tensor(out=dfull, in0=within, in1=pref, op=ALU.add), [rtk], [rtk])
    P.add("dve", lambda e: e.tensor_tensor(out=v3(dfull, NTL, 16), in0=v3(dfull, NTL, 16), in1=ebase.unsqueeze(1).to_broadcast([128, NTL, 16]), op=ALU.add), [rtk, cst_tk], [rtk])
    P.add("dve", lambda e: e.tensor_scalar(out=ta, in0=msk, scalar1=-1e6, scalar2=1e6, op0=ALU.mult, op1=ALU.add), [rtk], [rtk])
    P.add("dve", lambda e: e.tensor_tensor(out=ta, in0=ta, in1=dfull, op=ALU.add), [rtk], [rtk])
    P.add("dve", lambda e: e.tensor_reduce(out=d01[:, 0:16], in_=v3(ta, NTL, 16), axis=AX.X, op=ALU.min), [rtk], [rtk])
    P.add("dve", lambda e: e.tensor_tensor(out=tb2, in0=dfull, in1=msk, op=ALU.mult), [rtk], [rtk])
    P.add("dve", lambda e: e.tensor_reduce(out=d01[:, 16:32], in_=v3(tb2, NTL, 16), axis=AX.X, op=ALU.max), [rtk], [rtk])
    for j in range(2):
        P.add("dve", lambda e, j=j: e.tensor_tensor(out=v3(ta, NTL, 16), in0=v3(dfull, NTL, 16), in1=d01[:, j * 16:(j + 1) * 16].unsqueeze(2).to_broadcast([128, NTL, 16]), op=ALU.is_equal), [rtk], [rtk])
        P.add("dve", lambda e: e.tensor_tensor(out=ta, in0=ta, in1=comb, op=ALU.mult), [rtk], [rtk])
        P.add("dve", lambda e, j=j: e.tensor_reduce(out=cw[:, j * 16:(j + 1) * 16], in_=v3(ta, NTL, 16), axis=AX.X, op=ALU.add), [rtk], [pers_tk])
    P.add("dve", lambda e: e.tensor_copy(out=desti, in_=d01), [rtk], [pers_tk])
    P.add("dve", lambda e: e.tensor_scalar(out=cnti, in0=cntf, scalar1=127.0, scalar2=None, op0=ALU.add), [rtk], [rtk])
    P.add("dve", lambda e: e.tensor_scalar(out=cnti, in0=cnti, scalar1=7, scalar2=None, op0=ALU.arith_shift_right), [rtk], [rtk])
    P.add("dve", lambda e: e.tensor_scalar(out=cntneg[0:1, 0:16], in0=cnti[0:1, :], scalar1=-1, scalar2=None, op0=ALU.mult), [rtk], [pers_tk])
    P.add("dve", lambda e: e.tensor_reduce(out=cntneg[0:1, 16:17], in_=cntneg[0:1, 0:16], axis=AX.X, op=ALU.min), [pers_tk], [pers_tk])
    P.add("dve", lambda e: e.tensor_tensor(out=v3(validf, 16, 16), in0=posc.unsqueeze(1).to_broadcast([128, 16, 16]), in1=cntf.unsqueeze(2).to_broadcast([128, 16, 16]), op=ALU.is_lt), [rtk, cst_tk], [rtk])
    P.add("dve", lambda e: e.tensor_scalar(out=maskbits, in0=validf, scalar1=65535.0, scalar2=None, op0=ALU.mult), [rtk], [pers_tk])
    for t in range(NTL):
        for j in range(2):
            c = j * 16 + t
            P.add("pool", lambda e, c=c, t=t: e.indirect_dma_start(out=g.xg_all[:, :], out_offset=bass.IndirectOffsetOnAxis(ap=desti[:, c:c + 1], axis=0),
                                                                   in_=h2tok[:, t, :], in_offset=None),
                  [pers_tk, h2_tk[t]], [], dma=True)
    P.barrier()
    GU, DN, G2, XG, XGT, SG, ATO, YB, IDB = 0, 16384, 24576, 26624, 30720, 34816, 35840, 36352, 40448
    Gb = [arena_bf(g, GU + i * 8192, 8192).rearrange("p (k n) -> p k n", k=NK) for i in range(2)]
    Ub = [arena_bf(g, GU + 4096 + i * 8192, 8192).rearrange("p (k n) -> p k n", k=NK) for i in range(2)]
    Db = [arena_bf(g, DN + i * 4096, 8192).rearrange("p (f n) -> p f n", f=4) for i in range(2)]
    G_tk = [[Tk() for _ in range(4)] for _ in range(2)]
    U_tk = [[Tk() for _ in range(4)] for _ in range(2)]
    D_tk = [[Tk() for _ in range(4)] for _ in range(2)]
    g2bc = arena_f32(g, G2, 2048)
    g2_tk = Tk()
    make_bc(g, g.modT[l][:, 5, :], g2bc, g2_tk, SG)
    xg = [arena_bf(g, XG + i * 1024, 2048) for i in range(4)]
    xg_tk = [Tk() for _ in range(4)]
    xgT = [arena_bf(g, XGT + i * 1024, 2048).rearrange("p (k n) -> p k n", k=NK) for i in range(4)]
    xgT_tk = [Tk() for _ in range(4)]
    sgt = [arena_f32(g, SG + i * 512, 512) for i in range(2)]
    sg_tk = [Tk(), Tk()]
    ATb = [arena_bf(g, ATO + i * 256, 512).rearrange("p (f n) -> p f n", f=4) for i in range(2)]
    AT_tk = [Tk(), Tk()]
    yb = [arena_f32(g, YB + i * 2048, 2048) for i in range(2)]
    yb_tk = [Tk(), Tk()]
    identb = arena_bf(g, IDB, 128)
    idb_tk = Tk()
    P.add("act", lambda e: e.activation(out=identb, in_=g.ident32, func=AF.Copy), [g.tk_pers], [idb_tk])

    def load_expert(ex):
        pb = ex % 2
        for q4 in range(4):
            P.add("pool", lambda e, o=Gb[pb][:, q4 * 4:(q4 + 1) * 4, :], s_=g.wg[l, ex, q4 * 512:(q4 + 1) * 512, :].rearrange("(k p) n -> p k n", p=128): e.dma_start(out=o, in_=s_),
                  [], [G_tk[pb][q4]], dma=True)
            P.add("pool", lambda e, o=Ub[pb][:, q4 * 4:(q4 + 1) * 4, :], s_=g.wu[l, ex, q4 * 512:(q4 + 1) * 512, :].rearrange("(k p) n -> p k n", p=128): e.dma_start(out=o, in_=s_),
                  [], [U_tk[pb][q4]], dma=True)
        for q4 in range(4):
            P.add("pool", lambda e, o=Db[pb][:, q4, :], s_=g.wd[l, ex, q4 * 128:(q4 + 1) * 128, :]: e.dma_start(out=o, in_=s_), [], [D_tk[pb][q4]], dma=True)

    state_it = {"it": 0}

    def tile(ex, s, uid):
        it = state_it["it"]
        P.region = (uid, s)
        r = it % 2
        state_it["it"] = it + 1
        row0 = ex * 2048 + s * 128
        P.add("sp", lambda e, o=xg[r], s_=g.xg_all[row0:row0 + 128, :]: e.dma_start(out=o, in_=s_), [g.xg_tk], [xg_tk[r]], dma=True)
        mcol = ex * 16 + s
        P.add("dve", lambda e, o=xg[r].bitcast(U16), m=maskbits[:, mcol:mcol + 1].to_broadcast([128, 2048]): e.tensor_tensor(out=o, in0=o, in1=m, op=ALU.bitwise_and),
              [xg_tk[r], pers_tk], [xg_tk[r]])
        for half in range(2):
            pb = g.ps[half][:, :].bitcast(BF16)
            for c in range(8):
                k = half * 8 + c
                P.add("pe", lambda e, o=pb[:, c * 128:(c + 1) * 128], i=xg[r][:, k * 128:(k + 1) * 128]: e.transpose(o, i, identb), [xg_tk[r], idb_tk], [g.pst[half]])
            if half == 0:
                P.add("act", lambda e, o=xgT[r][:, 0:8, :], i=pb.rearrange("p (k n) -> p k n", k=8): e.activation(out=o, in_=i, func=AF.Copy), [g.pst[half]], [xgT_tk[r]])
            else:
                P.add("dve", lambda e, o=xgT[r][:, 8:16, :], i=pb.rearrange("p (k n) -> p k n", k=8): e.tensor_copy(out=o, in_=i), [g.pst[half]], [xgT_tk[r]])
        for f in range(4):
            for k in range(NK):
                P.add("pe", lambda e, o=g.ps[2][:, f * 128:(f + 1) * 128], a=Gb[ex % 2][:, k, f * 128:(f + 1) * 128], b=xgT[r][:, k, :], st=(k == 0), sp=(k == NK - 1):
                      e.matmul(o, a, b, start=st, stop=sp), [G_tk[ex % 2][k // 4], xgT_tk[r]], [g.pst[2]])
            for k in range(NK):
                P.add("pe", lambda e, o=g.ps[3][:, f * 128:(f + 1) * 128], a=Ub[ex % 2][:, k, f * 128:(f + 1) * 128], b=xgT[r][:, k, :], st=(k == 0), sp=(k == NK - 1):
                      e.matmul(o, a, b, start=st, stop=sp), [U_tk[ex % 2][k // 4], xgT_tk[r]], [g.pst[3]])
        P.add("act", lambda e, o=sgt[r]: e.activation(out=o, in_=g.ps[2][:, :], func=AF.Silu), [g.pst[2]], [sg_tk[r]])
        P.add("dve", lambda e, o=ATb[r].rearrange("p f n -> p (f n)"), b=sgt[r]: e.tensor_tensor(out=o, in0=g.ps[3][:, :], in1=b, op=ALU.mult), [g.pst[3], sg_tk[r]], [AT_tk[r]])
        for db in range(4):
            bank = 4 + db
            for f in range(4):
                P.add("pe", lambda e, o=g.ps[bank][:, :], a=ATb[r][:, f, :], b=Db[ex % 2][:, f, db * 512:(db + 1) * 512], st=(f == 0), sp=(f == 3):
                      e.matmul(o, a, b, start=st, stop=sp), [AT_tk[r], D_tk[ex % 2][f]], [g.pst[bank]])
            P.add("dve", lambda e, o=yb[r][:, db * 512:(db + 1) * 512], i=g.ps[bank][:, :], b_=g2bc[:, db * 512:(db + 1) * 512]: e.tensor_tensor(out=o, in0=i, in1=b_, op=ALU.mult),
                  [g.pst[bank], g2_tk], [yb_tk[r]])
        P.add("sp", lambda e, o=g.yg_all[row0:row0 + 128, :], s_=yb[r]: e.dma_start(out=o, in_=s_), [yb_tk[r]], [g.yg_tk], dma=True)
        P.region = None

    def t_load(ex, s, bi):
        row0 = ex * 2048 + s * 128
        P.add("sp", lambda e, o=xg[bi], s_=g.xg_all[row0:row0 + 128, :]: e.dma_start(out=o, in_=s_), [g.xg_tk], [xg_tk[bi]], dma=True)

    def t_mask(ex, s, bi):
        mcol = ex * 16 + s
        P.add("dve", lambda e, o=xg[bi].bitcast(U16), m=maskbits[:, mcol:mcol + 1].to_broadcast([128, 2048]): e.tensor_tensor(out=o, in0=o, in1=m, op=ALU.bitwise_and),
              [xg_tk[bi], pers_tk], [xg_tk[bi]])

    def t_prep(ex, s, bi):
        for half in range(2):
            pb = g.ps[half][:, :].bitcast(BF16)
            for c in range(8):
                k = half * 8 + c
                P.add("pe", lambda e, o=pb[:, c * 128:(c + 1) * 128], i=xg[bi][:, k * 128:(k + 1) * 128]: e.transpose(o, i, identb), [xg_tk[bi], idb_tk], [g.pst[half]])
            if half == 0:
                P.add("act", lambda e, o=xgT[bi][:, 0:8, :], i=pb.rearrange("p (k n) -> p k n", k=8): e.activation(out=o, in_=i, func=AF.Copy), [g.pst[half]], [xgT_tk[bi]])
            else:
                P.add("dve", lambda e, o=xgT[bi][:, 8:16, :], i=pb.rearrange("p (k n) -> p k n", k=8): e.tensor_copy(out=o, in_=i), [g.pst[half]], [xgT_tk[bi]])

    def t_gu(ex, bi, r):
        for f in range(4):
            for k in range(NK):
                P.add("pe", lambda e, o=g.ps[2][:, f * 128:(f + 1) * 128], a=Gb[ex % 2][:, k, f * 128:(f + 1) * 128], b=xgT[bi][:, k, :], st=(k == 0), sp=(k == NK - 1):
                      e.matmul(o, a, b, start=st, stop=sp), [G_tk[ex % 2][k // 4], xgT_tk[bi]], [g.pst[2]])
            for k in range(NK):
                P.add("pe", lambda e, o=g.ps[3][:, f * 128:(f + 1) * 128], a=Ub[ex % 2][:, k, f * 128:(f + 1) * 128], b=xgT[bi][:, k, :], st=(k == 0), sp=(k == NK - 1):
                      e.matmul(o, a, b, start=st, stop=sp), [U_tk[ex % 2][k // 4], xgT_tk[bi]], [g.pst[3]])
        P.add("act", lambda e, o=sgt[r]: e.activation(out=o, in_=g.ps[2][:, :], func=AF.Silu), [g.pst[2]], [sg_tk[r]])
        P.add("dve", lambda e, o=ATb[r].rearrange("p f n -> p (f n)"), b=sgt[r]: e.tensor_tensor(out=o, in0=g.ps[3][:, :], in1=b, op=ALU.mult), [g.pst[3], sg_tk[r]], [AT_tk[r]])

    def t_down(ex, s, r):
        row0 = ex * 2048 + s * 128
        for db in range(4):
            bank = 4 + db
            for f in range(4):
                P.add("pe", lambda e, o=g.ps[bank][:, :], a=ATb[r][:, f, :], b=Db[ex % 2][:, f, db * 512:(db + 1) * 512], st=(f == 0), sp=(f == 3):
                      e.matmul(o, a, b, start=st, stop=sp), [AT_tk[r], D_tk[ex % 2][f]], [g.pst[bank]])
            P.add("dve", lambda e, o=yb[r][:, db * 512:(db + 1) * 512], i=g.ps[bank][:, :], b_=g2bc[:, db * 512:(db + 1) * 512]: e.tensor_tensor(out=o, in0=i, in1=b_, op=ALU.mult),
                  [g.pst[bank], g2_tk], [yb_tk[r]])
        P.add("sp", lambda e, o=g.yg_all[row0:row0 + 128, :], s_=yb[r]: e.dma_start(out=o, in_=s_), [yb_tk[r]], [g.yg_tk], dma=True)

    load_expert(0)
    t_load(0, 0, 2)
    t_mask(0, 0, 2)
    t_prep(0, 0, 2)
    for ex in range(NE):
        if ex + 1 < NE:
            load_expert(ex + 1)
            t_load(ex + 1, 0, 2 + (ex + 1) % 2)
            t_mask(ex + 1, 0, 2 + (ex + 1) % 2)
            t_prep(ex + 1, 0, 2 + (ex + 1) % 2)
        for eng in Prog.ENGS:
            P.add(eng, lambda e, eng=eng, ex=ex: e.reg_load(P.regs[eng], cntneg[0:1, ex:ex + 1]), [pers_tk], [])
        uid = ("moeh", l, ex)
        for s in range(4):
            P.region = (uid, s)
            it = state_it["it"]
            state_it["it"] = it + 1
            r = it % 2
            bi = (2 + ex % 2) if s == 0 else (s % 2)
            if s + 1 < 4:
                t_load(ex, s + 1, (s + 1) % 2)
                t_mask(ex, s + 1, (s + 1) % 2)
            t_gu(ex, bi, r)
            if s + 1 < 4:
                t_prep(ex, s + 1, (s + 1) % 2)
            t_down(ex, s, r)
            P.region = None
    for eng in Prog.ENGS:
        P.add(eng, lambda e, eng=eng: e.reg_load(P.regs2[eng], cntneg[0:1, 16:17]), [pers_tk], [])
    P.outer = (("moec", l), 4)
    for ex in range(NE):
        for eng in Prog.ENGS:
            P.add(eng, lambda e, eng=eng, ex=ex: e.reg_load(P.regs[eng], cntneg[0:1, ex:ex + 1]), [pers_tk], [])
        uid = ("moec", l, ex)
        P.region = (uid, 4)
        load_expert(ex)
        P.region = None
        for s in range(4, 16):
            tile(ex, s, uid)
    P.outer = None
    P.barrier()
    XT2, Y0, FNG, JK2, TM2 = 0, 4096, 12288, 14336, 15360
    xt2 = [arena_f32(g, XT2 + i * 2048, 2048) for i in range(2)]
    xt2_tk = [Tk(), Tk()]
    yg = [[arena_f32(g, Y0 + (i * 2 + j) * 2048, 2048) for j in range(2)] for i in range(2)]
    yg_tk = [[Tk(), Tk()], [Tk(), Tk()]]
    fng = arena_f32(g, FNG, 2048)
    fng_tk = Tk()
    junk = arena_bf(g, JK2, 2048)
    junk_tk = Tk()
    fin_ss_tk = [Tk(), Tk()]
    if final:
        P.add("sp", lambda e: e.dma_start(out=fng, in_=g.fng), [], [fng_tk], dma=True)
    def c_load(t):
        if t < NTL:
            P.add("sp", lambda e, o=xt2[t % 2], s_=xsrc[t * 128:(t + 1) * 128, :]: e.dma_start(out=o, in_=s_), [], [xt2_tk[t % 2]], dma=True)
    c_load(0)
    for t in range(NTL):
        r = t % 2
        c_load(t + 1)
        for j in range(2):
            c = j * 16 + t
            P.add("pool", lambda e, o=yg[r][j], c=c: e.indirect_dma_start(out=o, out_offset=None, in_=g.yg_all[:, :], in_offset=bass.IndirectOffsetOnAxis(ap=desti[:, c:c + 1], axis=0)),
                  [pers_tk, g.yg_tk], [yg_tk[r][j]], dma=True)
        for j in range(2):
            c = j * 16 + t
            P.add("dve", lambda e, o=xt2[r], y=yg[r][j], c=c: e.scalar_tensor_tensor(out=o, in0=y, scalar=cw[:, c:c + 1], in1=o, op0=ALU.mult, op1=ALU.add),
                  [xt2_tk[r], yg_tk[r][j], pers_tk], [xt2_tk[r]])
        if final:
            ss, ss_tk = g.small[:, 340 + t % 2:341 + t % 2], fin_ss_tk[t % 2]
            P.add("pool", lambda e, o=ss: e.memset(o, 0.0), [], [ss_tk])
            P.add("act", lambda e, o=junk, i_=xt2[r], a=ss: e.activation(out=o, in_=i_, func=AF.Square, accum_out=a), [xt2_tk[r]], [junk_tk, ss_tk])
            P.add("act", lambda e, o=ss: e.activation(out=o, in_=o, func=AF.Sqrt, bias=eps, scale=1.0 / D), [ss_tk, g.tk_pers], [ss_tk])
            P.add("dve", lambda e, o=ss: e.reciprocal(out=o, in_=o), [ss_tk], [ss_tk])
            P.add("dve", lambda e, o=xt2[r], sc_=ss: e.scalar_tensor_tensor(out=o, in0=o, scalar=sc_, in1=fng, op0=ALU.mult, op1=ALU.mult), [xt2_tk[r], ss_tk, fng_tk], [xt2_tk[r]])
        P.add("sp", lambda e, o=xdst[t * 128:(t + 1) * 128, :], s_=xt2[r]: e.dma_start(out=o, in_=s_), [xt2_tk[r]], [], dma=True)
    P.barrier()


def _fm(v):
    v = np.asarray(v, np.float32)
    return np.ascontiguousarray(v.reshape(-1, 128).T)


def _ada_b4(ab):
    o = np.zeros((ab.shape[0], 4, ab.shape[1]), np.float32)
    o[:, 0] = ab
    o[:, 2] = ab
    return o


def _bias_tables(rpb):
    H = rpb.shape[0]
    kc = np.arange(64)[:, None]
    qc = np.arange(64)[None, :]
    cs = np.clip(qc - 8, 0, 48)
    cmask = (kc >= cs) & (kc < cs + 16)
    dc = np.clip(kc - qc + 15, 0, 30)
    tab = np.full((H, 128, 26, 64), NEG, np.float32)
    for a in range(2):
        for s in range(10):
            dr = 11 - s + a
            if 3 <= dr <= 10:
                v = rpb[:, dr][:, dc]
                tab[:, a * 64:(a + 1) * 64, s, :] = np.where(cmask[None], v, NEG)
        for s in range(16):
            dr = 14 - s + a
            if 0 <= dr <= 14:
                v = rpb[:, dr][:, dc]
                tab[:, a * 64:(a + 1) * 64, 10 + s, :] = np.where(cmask[None], v, NEG)
    return np.ascontiguousarray(tab.reshape(H, 128, 26 * 64))


def _dft_consts():
    L, C = 2048, 256
    c = np.arange(C)
    ang = 2 * np.pi * np.outer(c, c) / C
    csc = np.concatenate([np.cos(ang), np.sin(ang)], axis=1) / 16.0
    csc = csc.reshape(2, 128, 512).transpose(1, 0, 2)
    l = np.arange(L)
    lm = (np.outer(l, l) % L).astype(np.float64)
    angL = 2 * np.pi * lm / L
    sL = 1.0 / np.sqrt(L)
    CL = np.cos(angL) * sL
    SL = -np.sin(angL) * sL
    out = np.empty((2, 4, 128, 16, 512), np.float32)
    for i, M in enumerate((CL, SL)):
        out[i] = M.reshape(16, 128, 4, 512).transpose(2, 1, 0, 3)
    return csc.astype(ml_dtypes.bfloat16), out.astype(ml_dtypes.bfloat16)


_CONSTS = {}


def make_in_maps(inp):
    f = lambda a: np.ascontiguousarray(np.asarray(a, np.float32))
    if "dft" not in _CONSTS:
        _CONSTS["csc"], _CONSTS["dft"] = _dft_consts()
        _CONSTS["ident"] = np.eye(128, dtype=np.float32)
        p = np.arange(128)
        cst = np.zeros((128, 160), np.float32)
        cst[:, 0:128] = (p[:, None] < p[None, :]).astype(np.float32)
        cst[:, 128:144] = p[:, None] + 128.0 * np.arange(16)[None, :]
        cst[:, 144:160] = 2048.0 * np.arange(16)[None, :]
        _CONSTS["cst"] = cst
    x, c, ctx, c_ctx = f(inp["x"]), f(inp["c"]), f(inp["ctx"]), f(inp["c_ctx"])
    shared = {
        "ada_w": f(inp["ada_w"]),
        "ada_b4": _ada_b4(f(inp["ada_b"])),
        "cmb": np.array([[1, 0], [1, 0], [0, 1], [0, 1]], np.float32),
        "gT": np.ascontiguousarray(np.stack([_fm(inp["mix_norm_g"][0]), _fm(inp["ffn_norm_g"][0]),
                                             _fm(inp["mix_norm_g"][1]), _fm(inp["ffn_norm_g"][1])], axis=1)),
        "fng": np.ascontiguousarray(np.broadcast_to(f(inp["final_norm_g"])[None, :], (128, D))),
        "w_in0": f(inp["ev_w_in"][0]), "w_out0": f(inp["ev_w_out"][0]),
        "rpbt": _bias_tables(f(inp["ev_rpb"][0])),
        "csc": _CONSTS["csc"], "dft": _CONSTS["dft"],
        "w_in1": f(inp["od_w_in"][0]),
        "cvp": np.ascontiguousarray(np.stack([_fm(inp["od_b_in"][0][:D]), _fm(inp["od_b_in"][0][D:]), _fm(inp["od_dw_b"][0]),
                                              _fm(inp["od_ln_g"][0]), _fm(inp["od_ln_b"][0]), _fm(inp["od_ln_b"][0])], axis=1)),
        "dww": np.ascontiguousarray(f(inp["od_dw_w"][0]).T.reshape(NK, 128, 31).transpose(1, 0, 2)),
        "w_out1": f(inp["od_w_out"][0]),
        "bout": np.ascontiguousarray(np.broadcast_to(f(inp["od_b_out"][0])[None, :], (128, D))),
        "rw": np.ascontiguousarray(f(inp["router_w"]).reshape(NK, 128, NE).transpose(1, 0, 2)),
        "rb": np.ascontiguousarray(np.broadcast_to(f(inp["router_b"])[None, :], (128, NE))),
        "wg": f(inp["moe_w_gate"]), "wu": f(inp["moe_w_up"]), "wd": f(inp["moe_w_down"]),
        "ident": _CONSTS["ident"],
        "cst": _CONSTS["cst"],
    }
    maps = []
    for b in range(x.shape[0]):
        m = dict(shared)
        m["x"] = x[b]
        m["ctx"] = ctx[b]
        m["cT"] = np.ascontiguousarray(np.stack([_fm(c[b]), _fm(c_ctx)], axis=2))
        maps.append(m)
    return maps


_NC = {}


def kernel(**inputs):
    maps = make_in_maps(inputs)
    if "nc" not in _NC:
        _NC["nc"] = build_program()
    res = run_bass_kernel_spmd(_NC["nc"], maps, core_ids=list(range(8)))
    return np.stack([r["out"] for r in res.results], axis=0).astype(np.float32)


def out_proj(g, zT, z_tk, nkc, w_dram, xin, xout, gateT, bias_bc_dram, base):
    P = g.P
    wsz = nkc * 256
    wbf = [arena_bf(g, base + i * wsz, nkc * 512).rearrange("p (k n) -> p k n", k=nkc) for i in range(2)]
    wbf_tk = [Tk(), Tk()]
    o = base + 2 * wsz
    g1bc = arena_f32(g, o, 2048)
    gb = arena_f32(g, o + 2048, 2048)
    g1_tk, gb_tk = Tk(), Tk()
    o += 4096
    NX = 4
    xi = [arena_f32(g, o + i * 512, 512) for i in range(NX)]
    xi_tk = [Tk() for _ in range(NX)]
    o += NX * 512
    tm = [arena_f32(g, o + i * 512, 512) for i in range(2)]
    tm_tk = [Tk(), Tk()]
    o += 1024
    xo = [arena_f32(g, o + i * 512, 512) for i in range(NX)]
    xo_tk = [Tk() for _ in range(NX)]
    o += NX * 512
    make_bc(g, gateT, g1bc, g1_tk, o)
    if bias_bc_dram is not None:
        P.add("sp", lambda e: e.dma_start(out=gb, in_=bias_bc_dram), [], [gb_tk], dma=True)
        P.add("dve", lambda e: e.tensor_tensor(out=gb, in0=gb, in1=g1bc, op=ALU.mult), [gb_tk, g1_tk], [gb_tk])
    nq = nkc // 4

    def load_w(db):
        wb = wbf[db % 2]
        for kq in range(nq):
            src_ = w_dram[kq * 512:(kq + 1) * 512, db * 512:(db + 1) * 512].rearrange("(k p) n -> p k n", p=128)
            P.add("pool", lambda e, o_=wb[:, kq * 4:(kq + 1) * 4, :], s_=src_: e.dma_start(out=o_, in_=s_), [], [wbf_tk[db % 2]], dma=True)

    iters = [(db, t) for db in range(4) for t in range(NT)]
    state = {"ld": 0}

    def issue_loads(upto):
        while state["ld"] < min(upto, len(iters)):
            n = state["ld"]
            db, t = iters[n]
            P.add("sp", lambda e, o_=xi[n % NX], s_=xin[t * 128:(t + 1) * 128, db * 512:(db + 1) * 512]: e.dma_start(out=o_, in_=s_), [], [xi_tk[n % NX]], dma=True)
            state["ld"] += 1

    load_w(0)
    for it, (db, t) in enumerate(iters):
        wb = wbf[db % 2]
        if t == 0 and db + 1 < 4:
            load_w(db + 1)
        issue_loads(it + NX - 1)
        rx = it % NX
        r2 = it % 2
        bank = 6 + it % 2
        for c in range(nkc):
            P.add("pe", lambda e, o_=g.ps[bank][:, :], a=zT[:, c, t * 128:(t + 1) * 128], b=wb[:, c, :], st=(c == 0), sp=(c == nkc - 1):
                  e.matmul(o_, a, b, start=st, stop=sp), [z_tk, wbf_tk[db % 2]], [g.pst[bank]])
        P.add("dve", lambda e, o_=tm[r2], i_=g.ps[bank][:, :], b_=g1bc[:, db * 512:(db + 1) * 512]: e.tensor_tensor(out=o_, in0=i_, in1=b_, op=ALU.mult),
              [g.pst[bank], g1_tk], [tm_tk[r2]])
        if bias_bc_dram is not None:
            P.add("pool", lambda e, o_=xi[rx], b_=gb[:, db * 512:(db + 1) * 512]: e.tensor_tensor(out=o_, in0=o_, in1=b_, op=ALU.add), [xi_tk[rx], gb_tk], [xi_tk[rx]])
        P.add("dve", lambda e, o_=xo[rx], a=tm[r2], b=xi[rx]: e.tensor_tensor(out=o_, in0=a, in1=b, op=ALU.add), [tm_tk[r2], xi_tk[rx]], [xo_tk[rx]])
        P.add("sp", lambda e, o_=xout[t * 128:(t + 1) * 128, db * 512:(db + 1) * 512], s_=xo[rx]: e.dma_start(out=o_, in_=s_), [xo_tk[rx]], [], dma=True)


def build_hT(g, xsrc, ntiles, A, B, hT, hT_tk, base):
    P = g.P
    eps = g.small[:, 336:337]
    P.add("pool", lambda e: e.memset(eps, EPS), [], [g.tk_pers])
    xt = [arena_f32(g, base + i * 2048, 2048) for i in range(2)]
    xt_tk = [Tk(), Tk()]
    tmp = {
        "ss": [(g.small[:, 340 + i:341 + i], Tk()) for i in range(2)],
        "sq": [(g.small[:, 344 + i:345 + i], Tk()) for i in range(2)],
        "xn": [(arena_f32(g, base + 4096 + i * 2048, 2048), Tk()) for i in range(2)],
        "junk": (arena_bf(g, base + 8192, 2048), Tk()),
        "t32": [(arena_f32(g, base + 9216 + i * 512, 512), Tk()) for i in range(2)],
        "eps": eps,
    }
    def s1(t):
        if t < ntiles:
            r_ = t % 2
            P.add("sp", lambda e, o=xt[r_], s=xsrc[t * 128:(t + 1) * 128, :]: e.dma_start(out=o, in_=s), [], [xt_tk[r_]], dma=True)
            norm_transpose(g, xt[r_], xt_tk[r_], A, B, None, None, tmp, t, phase=1)
    s1(0)
    for t in range(ntiles):
        r = t % 2
        s1(t + 1)
        norm_transpose(g, xt[r], xt_tk[r], A, B, None,
                       lambda b, t=t: (hT[:, b * 4:(b + 1) * 4, t * 128:(t + 1) * 128], hT_tk[t]), tmp, t, phase=2)


def stage_conv(g, xsrc, xdst):
    P = g.P
    l = 1
    A, B = prep_AB(g, 2, g.modT[l][:, 1, :], g.modT[l][:, 0, :], 0)
    HT, UT, R = 0, 16384, 33024
    PADW = 2080
    hT = arena_bf(g, HT, 32768).rearrange("p (k n) -> p k n", k=NK)
    hT_tk = [Tk() for _ in range(NT)]
    uT = arena_bf(g, UT, NK * PADW).rearrange("p (k n) -> p k n", k=NK)
    uT_tk = [Tk() for _ in range(NK)]
    cv = g.small[:, 96:192].rearrange("p (j k) -> p j k", j=6)
    cv_tk = Tk()
    P.add("sp", lambda e: e.dma_start(out=cv, in_=g.cvp), [], [cv_tk], dma=True)
    build_hT(g, xsrc, NT, A, B, hT, hT_tk, R)
    P.barrier()
    for c in range(NK):
        P.add("pool", lambda e, o=uT[:, c, 0:15]: e.memset(o, 0.0), [], [uT_tk[c]])
        P.add("pool", lambda e, o=uT[:, c, 2063:2080]: e.memset(o, 0.0), [], [uT_tk[c]])
    sg = [arena_f32(g, R + 4096 + i * 512, 512) for i in range(2)]
    sg_tk = [Tk(), Tk()]
    stg = [arena_f32(g, 43264 + i * 2048, 2048).rearrange("p (k n) -> p k n", k=NK) for i in range(2)]
    stg_tk = [Tk(), Tk()]
    wbf = [arena_bf(g, 47360 + i * 1024, 2048).rearrange("p (k n) -> p k n", k=NK) for i in range(4)]
    wbf_tk = [Tk() for _ in range(4)]
    nu = 0
    it = 0
    for c in range(NK):
        ws = []
        for half in range(2):
            s = nu % 2
            d = nu % 4
            nu += 1
            col = half * D + c * 128
            src_ = g.w_in1[:, col:col + 128].rearrange("(k p) n -> p k n", p=128)
            P.add("pool", lambda e, o=wbf[d], s_=src_: e.dma_start(out=o, in_=s_), [], [wbf_tk[d]], dma=True)
            ws.append((wbf[d], wbf_tk[d]))
        for tb in range(4):
            bv, bg = (0, 1) if it % 2 == 0 else (2, 3)
            for half, bank in ((0, bv), (1, bg)):
                w, wtk = ws[half]
                for k in range(NK):
                    P.add("pe", lambda e, o=g.ps[bank][:, :], a=w[:, k, :], b=hT[:, k, tb * 512:(tb + 1) * 512], st=(k == 0), sp=(k == NK - 1):
                          e.matmul(o, a, b, start=st, stop=sp), [wtk] + hT_tk[tb * 4:tb * 4 + 4], [g.pst[bank]])
            r = it % 2
            P.add("act", lambda e, o=sg[r], i=g.ps[bg][:, :], b_=cv[:, 1, c:c + 1]: e.activation(out=o, in_=i, func=AF.Sigmoid, bias=b_), [g.pst[bg], cv_tk], [sg_tk[r]])
            P.add("dve", lambda e, o=uT[:, c, 15 + tb * 512: 15 + (tb + 1) * 512], i=g.ps[bv][:, :], b_=cv[:, 0, c:c + 1], s_=sg[r]:
                  e.scalar_tensor_tensor(out=o, in0=i, scalar=b_, in1=s_, op0=ALU.add, op1=ALU.mult), [g.pst[bv], cv_tk, sg_tk[r]], [uT_tk[c]])
            it += 1
    P.barrier()
    vT = arena_bf(g, 0, 32768).rearrange("p (k n) -> p k n", k=NK)
    vT_tk = [[Tk() for _ in range(4)] for _ in range(NK)]
    dwt = arena_f32(g, R, 512).rearrange("p (k t) -> p k t", k=NK)[:, :, 0:31]
    dwt_full = arena_f32(g, R, 496).rearrange("p (k t) -> p k t", k=NK)
    dw_tk = Tk()
    P.add("sp", lambda e: e.dma_start(out=dwt_full, in_=g.dww), [], [dw_tk], dma=True)
    identb = arena_bf(g, R + 512, 128)
    onesb = arena_bf(g, R + 576, 128)
    cb_tk = Tk()
    P.add("act", lambda e: e.activation(out=identb, in_=g.ident32, func=AF.Copy), [g.tk_pers], [cb_tk])
    P.add("pool", lambda e: e.memset(onesb, 1.0), [], [cb_tk])
    dg = [arena_bf(g, R + 1024 + i * 2048, 31 * 128).rearrange("p (t n) -> p t n", t=31) for i in range(2)]
    dg_tk = [Tk(), Tk()]
    it = 0
    for c in range(NK):
        d = c % 2
        for k in range(31):
            if k % 2 == 0:
                P.add("act", lambda e, o=dg[d][:, k, :], sc=dwt_full[:, c, k:k + 1]: e.activation(out=o, in_=identb, func=AF.Copy, scale=sc), [cb_tk, dw_tk], [dg_tk[d]])
            else:
                P.add("dve", lambda e, o=dg[d][:, k, :], sc=dwt_full[:, c, k:k + 1]: e.tensor_scalar(out=o, in0=identb, scalar1=sc, scalar2=None, op0=ALU.mult),
                      [cb_tk, dw_tk], [dg_tk[d]])
        for tb in range(4):
            bank = it % 2
            for k in range(31):
                P.add("pe", lambda e, o=g.ps[bank][:, :], a=dg[d][:, k, :], b=uT[:, c, tb * 512 + k: tb * 512 + k + 512], st=(k == 0), sp=(k == 30):
                      e.matmul(o, a, b, start=st, stop=sp), [dg_tk[d], uT_tk[c]], [g.pst[bank]])
            P.add("act", lambda e, o=vT[:, c, tb * 512:(tb + 1) * 512], i=g.ps[bank][:, :], b_=cv[:, 2, c:c + 1]: e.activation(out=o, in_=i, func=AF.Identity, bias=b_),
                  [g.pst[bank], cv_tk], [vT_tk[c][tb]])
            it += 1
    SB = R + 1024 + 4096
    sqb = [arena_bf(g, SB + i * 256, 512) for i in range(2)]
    sq_tk = [Tk(), Tk()]
    mean = arena_f32(g, SB + 512, 512)
    rstd = arena_f32(g, SB + 1024, 512)
    m2 = arena_f32(g, SB + 1536, 512)
    st_tk = Tk()
    tn = [arena_f32(g, SB + 2048 + i * 512, 512) for i in range(2)]
    tn_tk = [Tk(), Tk()]
    eps = g.small[:, 336:337]
    it = 0
    for tb in range(4):
        for c in range(NK):
            r = it % 2
            vv = vT[:, c, tb * 512:(tb + 1) * 512]
            P.add("act", lambda e, o=sqb[r], i=vv: e.activation(out=o, in_=i, func=AF.Square), [vT_tk[c][tb]], [sq_tk[r]])
            P.add("pe", lambda e, b=vv, st=(c == 0), sp=(c == NK - 1): e.matmul(g.ps[2][:, :], onesb, b, start=st, stop=sp), [cb_tk, vT_tk[c][tb]], [g.pst[2]])
            P.add("pe", lambda e, b=sqb[r], st=(c == 0), sp=(c == NK - 1): e.matmul(g.ps[3][:, :], onesb, b, start=st, stop=sp), [cb_tk, sq_tk[r]], [g.pst[3]])
            it += 1
        P.add("act", lambda e: e.activation(out=mean, in_=g.ps[2][:, :], func=AF.Copy, scale=1.0 / D), [g.pst[2]], [st_tk])
        P.add("dve", lambda e: e.tensor_tensor(out=m2, in0=mean, in1=mean, op=ALU.mult), [st_tk], [st_tk])
        P.add("dve", lambda e: e.scalar_tensor_tensor(out=rstd, in0=g.ps[3][:, :], scalar=1.0 / D, in1=m2, op0=ALU.mult, op1=ALU.subtract), [g.pst[3], st_tk], [st_tk])
        P.add("act", lambda e: e.activation(out=rstd, in_=rstd, func=AF.Sqrt, bias=eps), [st_tk, g.tk_pers], [st_tk])
        P.add("dve", lambda e: e.reciprocal(out=rstd, in_=rstd), [st_tk], [st_tk])
        for c in range(NK):
            r = c % 2
            vv = vT[:, c, tb * 512:(tb + 1) * 512]
            P.add("dve", lambda e, o=tn[r], i=vv: e.tensor_tensor(out=o, in0=i, in1=mean, op=ALU.subtract), [vT_tk[c][tb], st_tk], [tn_tk[r]])
            P.add("dve", lambda e, o=tn[r]: e.tensor_tensor(out=o, in0=o, in1=rstd, op=ALU.mult), [tn_tk[r], st_tk], [tn_tk[r]])
            P.add("act", lambda e, o=vv, i=tn[r], s_=cv[:, 3, c:c + 1], b_=cv[:, 4, c:c + 1]: e.activation(out=o, in_=i, func=AF.Silu, bias=b_, scale=s_),
                  [tn_tk[r], cv_tk], [vT_tk[c][tb]])
    P.barrier()
    z_tk = Tk()
    out_proj(g, vT, z_tk, NK, g.w_out1, xsrc, xdst, g.modT[l][:, 2, :], g.bout, 16384)


def load_w_unit(g, src_ap, stg, stg_tk, dst, dst_tk, eng):
    g.P.add("pool", lambda e: e.dma_start(out=dst, in_=src_ap), [], [dst_tk], dma=True)


def stage_mixer0(g, xsrc, xdst):
    P = g.P
    l = 0
    A, B = prep_AB(g, 0, g.modT[l][:, 1, :], g.modT[l][:, 0, :], 0)
    Ac, Bc = prep_AB(g, 0, g.modcT[:, 1, :], g.modcT[:, 0, :], 2)
    hT = arena_bf(g, 0, 32768).rearrange("p (k n) -> p k n", k=NK)
    hT_tk = [Tk() for _ in range(NT)]
    build_hT(g, xsrc, NT, A, B, hT, hT_tk, 16384)
    P.barrier()
    YT = arena_bf(g, 16384, 16384).rearrange("p (k n) -> p k n", k=8)
    YT_tk = Tk()
    uT = arena_bf(g, 24576, 4096).rearrange("p (k n) -> p k n", k=2)
    uT_tk = [Tk(), Tk()]
    W1 = arena_bf(g, 26624, 8192).rearrange("p (t n) -> p t n", t=NT)
    W1_tk = [Tk() for _ in range(NT)]
    dfb = [arena_bf(g, 30720 + i * 4096, 8192).rearrange("p (k n) -> p k n", k=NK) for i in range(3)]
    dfb_tk = [Tk() for _ in range(3)]
    stg = [arena_f32(g, 43008 + i * 2048, 2048).rearrange("p (k n) -> p k n", k=NK) for i in range(2)]
    stg_tk = [Tk(), Tk()]
    wbf = [arena_bf(g, 47104 + i * 1024, 2048).rearrange("p (k n) -> p k n", k=NK) for i in range(2)]
    wbf_tk = [Tk(), Tk()]
    csc = arena_bf(g, 49152, 1024).rearrange("p (k n) -> p k n", k=2)
    csc_tk = Tk()
    P.add("sp", lambda e: e.dma_start(out=csc, in_=g.csc), [], [csc_tk], dma=True)
    nu = 0
    nd = 0
    it = 0
    for gi in range(4):
        for cc in range(2):
            s = nu % 2
            nu += 1
            col = gi * 256 + cc * 128
            load_w_unit(g, g.w_in0[:, col:col + 128].rearrange("(k p) n -> p k n", p=128), stg[s], stg_tk[s], wbf[s], wbf_tk[s], "act" if cc == 0 else "dve")
            for tb in range(4):
                bank = it % 2
                it += 1
                for k in range(NK):
                    P.add("pe", lambda e, o=g.ps[bank][:, :], a=wbf[s][:, k, :], b=hT[:, k, tb * 512:(tb + 1) * 512], st=(k == 0), sp=(k == NK - 1):
                          e.matmul(o, a, b, start=st, stop=sp), [wbf_tk[s]] + hT_tk[tb * 4:tb * 4 + 4], [g.pst[bank]])
                P.add("act", lambda e, o=uT[:, cc, tb * 512:(tb + 1) * 512], i=g.ps[bank][:, :]: e.activation(out=o, in_=i, func=AF.Copy), [g.pst[bank]], [uT_tk[cc]])
        for t in range(NT):
            bank = 2 + t % 2
            for cc in range(2):
                P.add("pe", lambda e, o=g.ps[bank][:, :], a=uT[:, cc, t * 128:(t + 1) * 128], b=csc[:, cc, :], st=(cc == 0), sp=(cc == 1):
                      e.matmul(o, a, b, start=st, stop=sp), [uT_tk[cc], csc_tk], [g.pst[bank]])
            P.add("dve", lambda e, o=W1[:, t, :], i=g.ps[bank][:, :]: e.tensor_copy(out=o, in_=i), [g.pst[bank]], [W1_tk[t]])
        for mb in range(4):
            bufs = []
            for cs in range(2):
                s = nd % 3
                nd += 1
                P.add("sp", lambda e, o=dfb[s], s_=g.dft[cs, mb]: e.dma_start(out=o, in_=s_), [], [dfb_tk[s]], dma=True)
                bufs.append((dfb[s], dfb_tk[s]))
            for nch in range(2):
                bank = 4 + (mb * 2 + nch) % 2
                n = 0
                for cs in range(2):
                    db_, dtk = bufs[cs]
                    for lc in range(NK):
                        P.add("pe", lambda e, o=g.ps[bank][:, :], a=W1[:, lc, cs * 256 + nch * 128: cs * 256 + (nch + 1) * 128], b=db_[:, lc, :], st=(n == 0), sp=(n == 31):
                              e.matmul(o, a, b, start=st, stop=sp), [W1_tk[lc], dtk], [g.pst[bank]])
                        n += 1
                eng = "act" if nch == 0 else "dve"
                if eng == "act":
                    P.add("act", lambda e, o=YT[:, gi * 2 + nch, mb * 512:(mb + 1) * 512], i=g.ps[bank][:, :]: e.activation(out=o, in_=i, func=AF.Copy), [g.pst[bank]], [YT_tk])
                else:
                    P.add("dve", lambda e, o=YT[:, gi * 2 + nch, mb * 512:(mb + 1) * 512], i=g.ps[bank][:, :]: e.tensor_copy(out=o, in_=i), [g.pst[bank]], [YT_tk])
    P.barrier()
    out_proj(g, YT, YT_tk, 8, g.w_out0[0:1024, :], xsrc, g.xs[2], g.modT[l][:, 2, :], None, 24576)
    P.barrier()
    OT = arena_bf(g, 16384, 16384).rearrange("p (k n) -> p k n", k=8)
    OT_tk = Tk()
    hcT = arena_bf(g, 24576, 4096).rearrange("p (k n) -> p k n", k=NK)
    hc_tk = [Tk(), Tk()]
    build_hT(g, g.ctx, 2, Ac, Bc, hcT, hc_tk, 26624)
    P.barrier()
    QT = arena_bf(g, 26624, 4096).rearrange("p (h n) -> p h n", h=2)
    KT = arena_bf(g, 28672, 4096).rearrange("p (h n) -> p h n", h=2)
    QT_tk, KT_tk = [Tk(), Tk()], [Tk(), Tk()]
    Vq = arena_bf(g, 30720, 4160).rearrange("p (t h d) -> p t h d", t=NT, h=4)
    V_tk = [Tk() for _ in range(NT)]
    kcT = arena_bf(g, 32800, 512).rearrange("p (h n) -> p h n", h=2)
    kc_tk = [Tk(), Tk()]
    vc = arena_bf(g, 33056, 520).rearrange("p (t h d) -> p t h d", t=2, h=4)
    vc_tk = [Tk(), Tk()]
    Otok = arena_f32(g, 33344, 4096).rearrange("p (i f) -> p i f", i=NT)
    Otok_tk = [Tk() for _ in range(NT)]
    tmpb = [arena_f32(g, 37440 + i * 640, 640) for i in range(2)]
    tmp_tk = [Tk(), Tk()]
    Pb = [arena_bf(g, 38720 + i * 448, 896) for i in range(2)]
    Pb_tk = [Tk(), Tk()]
    tab = [arena_f32(g, 39616 + i * 1664, 1664) for i in range(2)]
    tab_tk = [Tk(), Tk()]
    stg = [arena_f32(g, 42944 + i * 2048, 2048).rearrange("p (k n) -> p k n", k=NK) for i in range(2)]
    stg_tk = [Tk(), Tk()]
    wbf = [arena_bf(g, 47040 + i * 1024, 2048).rearrange("p (k n) -> p k n", k=NK) for i in range(4)]
    wbf_tk = [Tk() for _ in range(4)]
    rec = [g.small[:, 348 + i:349 + i] for i in range(2)]
    rec_tk = [Tk(), Tk()]
    nu = 0
    nw = 0
    it = 0

    def wunit(col, eng):
        nonlocal nu, nw
        s = nu % 2
        d = nw % 4
        nu += 1
        nw += 1
        load_w_unit(g, g.w_in0[:, col:col + 128].rearrange("(k p) n -> p k n", p=128), stg[s], stg_tk[s], wbf[d], wbf_tk[d], eng)
        return wbf[d], wbf_tk[d]

    for q in range(4):
        P.add("pool", lambda e: e.memset(Vq[:, :, :, 64:65], 1.0), [], V_tk)
        P.add("pool", lambda e: e.memset(vc[:, :, :, 64:65], 1.0), [], vc_tk)
        for hp in range(2):
            for which, dstT, dtk, base_col in ((0, QT, QT_tk, 1024), (1, KT, KT_tk, 2048)):
                w, wtk = wunit(base_col + q * 256 + hp * 128, "act" if which == 0 else "dve")
                for tb in range(4):
                    bank = it % 2
                    it += 1
                    for k in range(NK):
                        P.add("pe", lambda e, o=g.ps[bank][:, :], a=w[:, k, :], b=hT[:, k, tb * 512:(tb + 1) * 512], st=(k == 0), sp=(k == NK - 1):
                              e.matmul(o, a, b, start=st, stop=sp), [wtk] + hT_tk[tb * 4:tb * 4 + 4], [g.pst[bank]])
                    P.add("act", lambda e, o=dstT[:, hp, tb * 512:(tb + 1) * 512], i=g.ps[bank][:, :]: e.activation(out=o, in_=i, func=AF.Copy), [g.pst[bank]], [dtk[hp]])
                if which == 1:
                    bank = it % 2
                    it += 1
                    for k in range(NK):
                        P.add("pe", lambda e, o=g.ps[bank][:, 0:256], a=w[:, k, :], b=hcT[:, k, :], st=(k == 0), sp=(k == NK - 1):
                              e.matmul(o, a, b, start=st, stop=sp), [wtk] + hc_tk, [g.pst[bank]])
                    P.add("act", lambda e, o=kcT[:, hp, :], i=g.ps[bank][:, 0:256]: e.activation(out=o, in_=i, func=AF.Copy), [g.pst[bank]], [kc_tk[hp]])
        wv = [wunit(3072 + q * 256 + j * 128, "act" if j == 0 else "dve") for j in range(2)]
        for t in range(NT + 2):
            bank = it % 2
            it += 1
            for j in range(2):
                w, wtk = wv[j]
                for k in range(NK):
                    if t < NT:
                        a_, rtk = hT[:, k, t * 128:(t + 1) * 128], [hT_tk[t]]
                    else:
                        a_, rtk = hcT[:, k, (t - NT) * 128:(t - NT + 1) * 128], [hc_tk[t - NT]]
                    P.add("pe", lambda e, o=g.ps[bank][:, j * 128:(j + 1) * 128], a=a_, b=w[:, k, :], st=(k == 0), sp=(k == NK - 1):
                          e.matmul(o, a, b, start=st, stop=sp), [wtk] + rtk, [g.pst[bank]])
            pv = g.ps[bank][:, 0:256].rearrange("p (h d) -> p h d", h=4)
            if t < NT:
                P.add("dve", lambda e, o=Vq[:, t, :, 0:64], i=pv: e.tensor_copy(out=o, in_=i), [g.pst[bank]], [V_tk[t]])
            else:
                P.add("dve", lambda e, o=vc[:, t - NT, :, 0:64], i=pv: e.tensor_copy(out=o, in_=i), [g.pst[bank]], [vc_tk[t - NT]])
        pend = None
        for h4 in range(4):
            hh = q * 4 + h4
            hp, po = h4 // 2, (h4 % 2) * 64
            tb_, tbtk = tab[hh % 2], tab_tk[hh % 2]
            P.add("sp", lambda e, o=tb_, s_=g.rpbt[hh]: e.dma_start(out=o, in_=s_), [], [tbtk], dma=True)
            for i in range(NT):
                if 2 <= i <= 13:
                    chunks = [i + 2, i + 1, i, i - 1, i - 2]
                    tcol = 0
                elif i < 2:
                    chunks = [3, 2, 1, 0]
                    tcol = 640 + (1 + 2 * i) * 64
                else:
                    chunks = [15, 14, 13, 12]
                    tcol = 640 + (7 - 2 * (15 - i)) * 64
                nch = len(chunks)
                r = it % 2
                it += 1
                banks = (0, 1) if r == 0 else (2, 3)
                qs = QT[po:po + 64, hp, i * 128:(i + 1) * 128]

                def sblk(ci):
                    return g.ps[banks[ci // 4]][:, (ci % 4) * 128:(ci % 4 + 1) * 128], g.pst[banks[ci // 4]]
                for ci, j in enumerate(chunks):
                    o_, otk = sblk(ci)
                    P.add("pe", lambda e, o=o_, a=KT[po:po + 64, hp, j * 128:(j + 1) * 128], b=qs: e.matmul(o, a, b, start=True, stop=True),
                          [KT_tk[hp], QT_tk[hp]], [otk])
                for cj in range(2):
                    o_, otk = sblk(nch + cj)
                    P.add("pe", lambda e, o=o_, a=kcT[po:po + 64, hp, cj * 128:(cj + 1) * 128], b=qs: e.matmul(o, a, b, start=True, stop=True),
                          [kc_tk[hp], QT_tk[hp]], [otk])
                P.add("dve", lambda e, o=tmpb[r][:, 0:512], i=g.ps[banks[0]][:, :], t_=tb_[:, tcol:tcol + 512]:
                      e.scalar_tensor_tensor(out=o, in0=i, scalar=0.125, in1=t_, op0=ALU.mult, op1=ALU.add), [g.pst[banks[0]], tbtk], [tmp_tk[r]])
                if nch == 5:
                    P.add("dve", lambda e, o=tmpb[r][:, 512:640], i=g.ps[banks[1]][:, 0:128], t_=tb_[:, tcol + 512:tcol + 640]:
                          e.scalar_tensor_tensor(out=o, in0=i, scalar=0.125, in1=t_, op0=ALU.mult, op1=ALU.add), [g.pst[banks[1]], tbtk], [tmp_tk[r]])
                P.add("act", lambda e, o=Pb[r][:, 0:nch * 128], i=tmpb[r][:, 0:nch * 128]: e.activation(out=o, in_=i, func=AF.Exp), [tmp_tk[r]], [Pb_tk[r]])
                c0 = (nch % 4) * 128
                P.add("act", lambda e, o=Pb[r][:, nch * 128:(nch + 2) * 128], i=g.ps[banks[1]][:, c0:c0 + 256]: e.activation(out=o, in_=i, func=AF.Exp, scale=0.125),
                      [g.pst[banks[1]]], [Pb_tk[r]])
                cur = (h4, i, chunks, r)
                if pend is not None:
                    _attn_pv(g, pend, Pb, Pb_tk, Vq, V_tk, vc, vc_tk, Otok, Otok_tk, rec, rec_tk)
                pend = cur
        _attn_pv(g, pend, Pb, Pb_tk, Vq, V_tk, vc, vc_tk, Otok, Otok_tk, rec, rec_tk)
        for i in range(NT):
            for hp in range(2):
                bank = 6 + (i * 2 + hp) % 2
                P.add("pe", lambda e, o=g.ps[bank][:, 0:128], a=Otok[:, i, hp * 128:(hp + 1) * 128]: e.transpose(o, a, g.ident32), [Otok_tk[i], g.tk_pers], [g.pst[bank]])
                if hp == 0:
                    P.add("act", lambda e, o=OT[:, q * 2 + hp, i * 128:(i + 1) * 128], i_=g.ps[bank][:, 0:128]: e.activation(out=o, in_=i_, func=AF.Copy), [g.pst[bank]], [OT_tk])
                else:
                    P.add("dve", lambda e, o=OT[:, q * 2 + hp, i * 128:(i + 1) * 128], i_=g.ps[bank][:, 0:128]: e.tensor_copy(out=o, in_=i_), [g.pst[bank]], [OT_tk])
    P.barrier()
    out_proj(g, OT, OT_tk, 8, g.w_out0[1024:2048, :], g.xs[2], xdst, g.modT[l][:, 2, :], None, 24576)


def _attn_pv(g, item, Pb, Pb_tk, Vq, V_tk, vc, vc_tk, Otok, Otok_tk, rec, rec_tk):
    P = g.P
    h4, i, chunks, r = item
    nch = len(chunks)
    bank = 4 + r
    o_ = g.ps[bank][:, 0:65]
    n = nch + 2
    for ci, j in enumerate(chunks):
        P.add("pe", lambda e, a=Pb[r][:, ci * 128:(ci + 1) * 128], b=Vq[:, j, h4, :], st=(ci == 0): e.matmul(o_, a, b, start=st, stop=False),
              [Pb_tk[r], V_tk[j]], [g.pst[bank]])
    for cj in range(2):
        P.add("pe", lambda e, a=Pb[r][:, (nch + cj) * 128:(nch + cj + 1) * 128], b=vc[:, cj, h4, :], sp=(cj == 1): e.matmul(o_, a, b, start=False, stop=sp),
              [Pb_tk[r], vc_tk[cj]], [g.pst[bank]])
    P.add("dve", lambda e: e.reciprocal(out=rec[r], in_=g.ps[bank][:, 64:65]), [g.pst[bank]], [rec_tk[r]])
    P.add("act", lambda e, o=Otok[:, i, h4 * 64:(h4 + 1) * 64]: e.activation(out=o, in_=g.ps[bank][:, 0:64], func=AF.Copy, scale=rec[r]),
          [g.pst[bank], rec_tk[r]], [Otok_tk[i]])
```

```python
import numpy as np
import ml_dtypes
import concourse.bass as bass
import concourse.mybir as mybir
from concourse.bass_utils import run_bass_kernel_spmd

F32 = mybir.dt.float32
BF16 = mybir.dt.bfloat16
AF = mybir.ActivationFunctionType
ALU = mybir.AluOpType
AX = mybir.AxisListType

D = 2048
S = 2048
NT = 16
NK = 16
CTX = 256
NE = 16
FE = 512
EPS = 1e-6
NEG = -30000.0


class Tk:
    __slots__ = ("w", "r")

    def __init__(self):
        self.w = None
        self.r = {}


class Op:
    __slots__ = ("eng", "fn", "deps", "inc", "dma", "sem", "val", "gidx", "region", "outer")

    def __init__(self, eng, fn, dma):
        self.eng = eng
        self.fn = fn
        self.dma = dma
        self.deps = set()
        self.inc = False
        self.sem = None
        self.val = 0
        self.gidx = 0
        self.region = None
        self.outer = None


class Prog:
    ENGS = ("pe", "act", "dve", "pool", "sp")
    SEG = 10 ** 9
    NDS = 28

    def __init__(self):
        self.ops = {e: [] for e in self.ENGS}
        self.all_dma = []
        self.bar = None
        self.bar_seen = set()
        self.region = None
        self.outer = None
        self.regs = {}
        self.regs2 = {}

    def add(self, eng, fn, reads=(), writes=(), dma=False):
        op = Op(eng, fn, dma)
        op.region = self.region
        op.outer = self.outer
        deps = set()
        for t in reads:
            if t.w is not None:
                deps.add(t.w)
        for t in writes:
            if t.w is not None:
                deps.add(t.w)
            for o in t.r.values():
                if isinstance(o, list):
                    deps.update(o)
                else:
                    deps.add(o)
        if self.bar is not None and eng not in self.bar_seen:
            deps.update(self.bar)
            self.bar_seen.add(eng)
        for t in reads:
            if dma:
                t.r.setdefault("dma", []).append(op)
            else:
                t.r[eng] = op
        for t in writes:
            t.w = op
            t.r = {}
        deps.discard(op)
        if eng == "pe" and not dma:
            deps = {d for d in deps if not (d.eng == "pe" and not d.dma)}
        op.deps = deps
        for d in deps:
            d.inc = True
        self.ops[eng].append(op)
        if dma:
            self.all_dma.append(op)
        return op

    def barrier(self):
        deps = []
        for e in self.ENGS:
            for o in reversed(self.ops[e]):
                if not o.dma:
                    deps.append(o)
                    break
        deps.extend(self.all_dma)
        self.all_dma = []
        self.bar = deps
        self.bar_seen = set()

    def run_emit(self, nc, block, handles, sems):
        si = 0
        for e in self.ENGS:
            cnt = 0
            cur = None
            for o in self.ops[e]:
                if o.dma or not o.inc:
                    continue
                if cnt % self.SEG == 0:
                    cur = sems[si]
                    si += 1
                o.sem = cur
                o.val = cnt % self.SEG + 1
                o.gidx = cnt + 1
                cnt += 1
        dsems = sems[si:si + self.NDS]
        assert len(dsems) == self.NDS, "not enough semaphores"
        dcount = [0] * self.NDS
        k = 0
        for o in self.dma_order:
            s = k % self.NDS
            o.sem = dsems[s]
            o.gidx = (s, dcount[s])
            dcount[s] += 16
            o.val = dcount[s]
            k += 1

        def emit_engine(ename):
            def body(e):
                waited = {}

                def emit_op(o):
                    for d in o.deps:
                        key = ("d", id(d.sem)) if d.dma else (d.eng, id(d.sem))
                        need = d.val
                        if waited.get(key, 0) >= need:
                            continue
                        e.wait_ge(d.sem, need)
                        waited[key] = need
                    if o.dma:
                        slot, prev = o.gidx
                        key = ("d", id(o.sem))
                        if prev > 0 and waited.get(key, 0) < prev:
                            e.wait_ge(o.sem, prev)
                            waited[key] = prev
                        ins = o.fn(e)
                        ins.then_inc(o.sem, 16)
                    else:
                        if o.fn is None:
                            return
                        ins = o.fn(e)
                        if o.inc:
                            ins.then_inc(o.sem, 1)

                ops = self.ops[ename]

                def else_bulk(grp):
                    ninc = sum(1 for q in grp if (not q.dma) and q.inc)
                    if ninc:
                        csem = [q.sem for q in grp if (not q.dma) and q.inc][0]
                        e.drain().then_inc(csem, ninc)
                    for q in grp:
                        if q.dma:
                            slot, prev = q.gidx
                            if prev > 0:
                                e.wait_ge(q.sem, prev)
                            e.sem_inc(q.sem, 16)

                def emit_range(byslot, lo, hi):
                    grp = [q for s in range(lo, hi) for q in byslot.get(s, [])]
                    if not grp:
                        return
                    saved = dict(waited)
                    with e.If_lt(self.regs[ename], -lo):
                        if hi - lo == 1:
                            for q in grp:
                                emit_op(q)
                        else:
                            mid = (lo + hi) // 2
                            emit_range(byslot, lo, mid)
                            emit_range(byslot, mid, hi)
                    with e.Else():
                        waited.clear()
                        waited.update(saved)
                        else_bulk(grp)
                    waited.clear()
                    waited.update(saved)

                def emit_list(lst):
                    i = 0
                    while i < len(lst):
                        o = lst[i]
                        if o.region is None:
                            emit_op(o)
                            i += 1
                            continue
                        uid = o.region[0]
                        j = i
                        byslot = {}
                        while j < len(lst) and lst[j].region is not None and lst[j].region[0] == uid:
                            byslot.setdefault(lst[j].region[1], []).append(lst[j])
                            j += 1
                        emit_range(byslot, min(byslot), 16)
                        i = j

                i = 0
                while i < len(ops):
                    o = ops[i]
                    if o.outer is None:
                        j = i
                        while j < len(ops) and ops[j].outer is None:
                            j += 1
                        emit_list(ops[i:j])
                        i = j
                        continue
                    ou = o.outer
                    j = i
                    while j < len(ops) and ops[j].outer is ou:
                        j += 1
                    grp = ops[i:j]
                    saved = dict(waited)
                    with e.If_lt(self.regs2[ename], -ou[1]):
                        emit_list(grp)
                    with e.Else():
                        waited.clear()
                        waited.update(saved)
                        else_bulk(grp)
                    waited.clear()
                    waited.update(saved)
                    i = j
            return body

        block.tensor(emit_engine("pe"))
        block.scalar(emit_engine("act"))
        block.vector(emit_engine("dve"))
        block.gpsimd(emit_engine("pool"))
        block.sync(emit_engine("sp"))


def _mk_prog():
    p = Prog()
    p.dma_order = []
    _add = p.add

    def add(eng, fn, reads=(), writes=(), dma=False):
        o = _add(eng, fn, reads, writes, dma)
        if dma:
            p.dma_order.append(o)
        return o
    p.add = add
    return p


class Ctx:
    pass


def build_program(stages=(0, 1, 2, 3, 4), dbg=False, sparse=True):
    nc = bass.Bass("TRN2", target_bir_lowering=False)
    P = _mk_prog()
    g = Ctx()
    g.nc, g.P = nc, P

    def din(name, shape, dt=F32):
        return nc.dram_tensor(name, list(shape), dt, kind="ExternalInput").ap()

    g.x = din("x", [S, D])
    g.ctx = din("ctx", [CTX, D])
    g.cT = din("cT", [128, NK, 2])
    g.ada_w = din("ada_w", [2, D, 6 * D])
    g.ada_b4 = din("ada_b4", [2, 4, 6 * D])
    g.cmb = din("cmb", [4, 2])
    g.gT = din("gT", [128, 4, NK])
    g.fng = din("fng", [128, D])
    g.w_in0 = din("w_in0", [D, 4096])
    g.w_out0 = din("w_out0", [D, D])
    g.rpbt = din("rpbt", [16, 128, 1664])
    g.csc = din("csc", [128, 2, 512], BF16)
    g.dft = din("dft", [2, 4, 128, NK, 512], BF16)
    g.w_in1 = din("w_in1", [D, 4096])
    g.cvp = din("cvp", [128, 6, NK])
    g.dww = din("dww", [128, NK, 31])
    g.w_out1 = din("w_out1", [D, D])
    g.bout = din("bout", [128, D])
    g.rw = din("rw", [128, NK, NE])
    g.rb = din("rb", [128, NE])
    g.wg = din("wg", [2, NE, D, FE])
    g.wu = din("wu", [2, NE, D, FE])
    g.wd = din("wd", [2, NE, FE, D])
    g.ident = din("ident", [128, 128])
    g.out = nc.dram_tensor("out", [S, D], F32, kind="ExternalOutput").ap()
    kind = "ExternalOutput" if dbg else "Internal"
    g.xs = [nc.dram_tensor("xs%d" % i, [S, D], F32, kind=kind).ap() for i in range(3)]
    g.cst = din("cst", [128, 160])
    g.xg_all = nc.dram_tensor("xg_all", [NE * 2048, D], BF16, kind="Internal").ap()
    g.yg_all = nc.dram_tensor("yg_all", [NE * 2048, D], BF16, kind="Internal").ap()
    g.xg_tk, g.yg_tk = Tk(), Tk()

    ARENA = 51456
    with (
        nc.sbuf_tensor("arena", [128, ARENA], F32) as arena,
        nc.sbuf_tensor("pers", [128, 1128], F32) as pers,
        nc.psum_tensor("ps0", [128, 512], F32) as ps0, nc.psum_tensor("ps1", [128, 512], F32) as ps1,
        nc.psum_tensor("ps2", [128, 512], F32) as ps2, nc.psum_tensor("ps3", [128, 512], F32) as ps3,
        nc.psum_tensor("ps4", [128, 512], F32) as ps4, nc.psum_tensor("ps5", [128, 512], F32) as ps5,
        nc.psum_tensor("ps6", [128, 512], F32) as ps6, nc.psum_tensor("ps7", [128, 512], F32) as ps7,
    ):
        g.arena = arena
        g.ps = [ps0, ps1, ps2, ps3, ps4, ps5, ps6, ps7]
        g.pst = [Tk() for _ in range(8)]
        g.pers = pers
        g.ident32 = pers[:, 0:128]
        g.modT = [pers[:, 128:224].rearrange("p (j k) -> p j k", j=6), pers[:, 224:320].rearrange("p (j k) -> p j k", j=6)]
        g.modcT = pers[:, 320:352].rearrange("p (j k) -> p j k", j=2)
        g.gTs = pers[:, 352:416].rearrange("p (j k) -> p j k", j=4)
        g.AB = pers[:, 416:480].rearrange("p (j k) -> p j k", j=4)
        g.ones32 = pers[:, 480:608]
        g.selA = pers[0:2, 608:736]
        g.cTs = pers[:, 736:768].rearrange("p (k c) -> p k c", c=2)
        g.small = pers[:, 768:1128]
        g.tk_pers = Tk()
        g.tk_mod = Tk()
        g.tk_AB = Tk()

        from_stage = {}
        setup_consts(g)
        if 0 in stages:
            stage_ada(g)
        P.barrier()
        if 1 in stages:
            stage_mixer0(g, g.x, g.xs[0])
            P.barrier()
        if 2 in stages:
            (stage_moe_sparse if sparse else stage_moe)(g, 0, g.xs[0] if 1 in stages else g.x, g.xs[1], final=False)
            P.barrier()
        if 3 in stages:
            stage_conv(g, g.xs[1] if 2 in stages else g.x, g.xs[2])
            P.barrier()
        if 4 in stages:
            (stage_moe_sparse if sparse else stage_moe)(g, 1, g.xs[2] if 3 in stages else g.x, g.out, final=True)
            P.barrier()
        P.add("sp", None)

        nsem = 100
        import contextlib
        with contextlib.ExitStack() as st:
            sems = [st.enter_context(nc.semaphore("s%d" % i)) for i in range(nsem)]
            P.regs = {"pe": st.enter_context(nc.tensor.register("r_pe")), "act": st.enter_context(nc.scalar.register("r_act")),
                      "dve": st.enter_context(nc.vector.register("r_dve")), "pool": st.enter_context(nc.gpsimd.register("r_pool")),
                      "sp": st.enter_context(nc.sync.register("r_sp"))}
            P.regs2 = {"pe": st.enter_context(nc.tensor.register("r2_pe")), "act": st.enter_context(nc.scalar.register("r2_act")),
                       "dve": st.enter_context(nc.vector.register("r2_dve")), "pool": st.enter_context(nc.gpsimd.register("r2_pool")),
                       "sp": st.enter_context(nc.sync.register("r2_sp"))}
            block = st.enter_context(nc.Block())
            P.run_emit(nc, block, None, sems)
    return nc


def arena_f32(g, off, n):
    return g.arena[:, off:off + n]


def arena_bf(g, off, n):
    return g.arena[:, off:off + n // 2].bitcast(BF16)


def setup_consts(g):
    P = g.P
    tk = g.tk_pers
    P.add("sp", lambda e: e.dma_start(out=g.ident32, in_=g.ident), [], [tk], dma=True)
    P.add("sp", lambda e: e.dma_start(out=g.gTs, in_=g.gT), [], [tk], dma=True)
    P.add("sp", lambda e: e.dma_start(out=g.cTs, in_=g.cT), [], [tk], dma=True)
    P.add("pool", lambda e: e.memset(g.ones32, 1.0), [], [tk])
    P.add("pool", lambda e: e.memset(g.selA, 0.0), [], [tk])
    P.add("pool", lambda e: e.memset(g.pers[0:1, 608:736], 1.0), [], [tk])


def stage_ada(g):
    P = g.P
    NR = 8
    wr = [arena_bf(g, i * 1024, 2048).rearrange("p (k n) -> p k n", k=4) for i in range(NR)]
    wr_tk = [Tk() for _ in range(NR)]
    bias = [g.arena[0:4, 8192 + i * 512: 8192 + (i + 1) * 512] for i in range(2)]
    bias_tk = [Tk(), Tk()]
    mrow = [g.arena[0:4, 9216 + i * 512: 9216 + (i + 1) * 512] for i in range(2)]
    mrow_tk = [Tk(), Tk()]
    sT = g.small[:, 0:32].rearrange("p (k c) -> p k c", c=2)
    s4 = arena_bf(g, 10240, 64).rearrange("p (k c) -> p k c", c=4)
    hi32 = arena_f32(g, 10304, 32).rearrange("p (k c) -> p k c", c=2)
    cmb = g.arena[0:4, 10400:10402]
    tk_s = Tk()
    P.add("act", lambda e: e.activation(out=sT, in_=g.cTs, func=AF.Silu), [g.tk_pers], [tk_s])
    s4v = s4.rearrange("p k (c h) -> p k c h", h=2)
    P.add("dve", lambda e: e.tensor_copy(out=s4v[:, :, :, 0], in_=sT), [tk_s], [tk_s])
    P.add("dve", lambda e: e.tensor_copy(out=hi32, in_=s4v[:, :, :, 0]), [tk_s], [tk_s])
    P.add("dve", lambda e: e.tensor_tensor(out=s4v[:, :, :, 1], in0=sT, in1=hi32, op=ALU.subtract), [tk_s], [tk_s])
    P.add("sp", lambda e: e.dma_start(out=cmb, in_=g.cmb), [], [tk_s], dma=True)
    u = 0
    for l in range(2):
        for nb in range(24):
            j, q = divmod(nb, 4)
            pm = g.ps[nb % 2][0:4, :]
            pm_tk = g.pst[nb % 2]
            for kk in range(4):
                slot = u % NR
                u += 1
                src_ = g.ada_w[l, kk * 512:(kk + 1) * 512, nb * 512:(nb + 1) * 512].rearrange("(k p) n -> p k n", p=128)
                P.add("pool", lambda e, o=wr[slot], s=src_: e.dma_start(out=o, in_=s), [], [wr_tk[slot]], dma=True)
                for k4 in range(4):
                    k = kk * 4 + k4
                    P.add("pe", lambda e, o=pm, a=s4[:, k, :], b=wr[slot][:, k4, :], st=(k == 0), sp=(k == 15):
                          e.matmul(o, a, b, start=st, stop=sp), [tk_s, wr_tk[slot]], [pm_tk])
            bb = nb % 2
            P.add("sp", lambda e, o=bias[bb], s=g.ada_b4[l, :, nb * 512:(nb + 1) * 512]: e.dma_start(out=o, in_=s), [], [bias_tk[bb]], dma=True)
            P.add("dve", lambda e, o=mrow[bb], a=pm, b=bias[bb]: e.tensor_tensor(out=o, in0=a, in1=b, op=ALU.add),
                  [pm_tk, bias_tk[bb]], [mrow_tk[bb]])
            pt = g.ps[2 + bb][:, 0:8]
            pt_tk = g.pst[2 + bb]
            for qq in range(4):
                P.add("pe", lambda e, o=pt[:, qq * 2:qq * 2 + 2], i=mrow[bb][0:4, qq * 128:(qq + 1) * 128]:
                      e.matmul(o, i, cmb, start=True, stop=True), [mrow_tk[bb], tk_s], [pt_tk])
            ptv = pt.rearrange("p (q r) -> p q r", r=2)
            P.add("dve", lambda e, o=g.modT[l][:, j, q * 4:(q + 1) * 4], i=ptv[:, :, 0]: e.tensor_copy(out=o, in_=i),
                  [pt_tk], [g.tk_mod])
            if l == 0 and j < 2:
                P.add("dve", lambda e, o=g.modcT[:, j, q * 4:(q + 1) * 4], i=ptv[:, :, 1]: e.tensor_copy(out=o, in_=i),
                      [pt_tk], [g.tk_mod])


def prep_AB(g, gi, scaleT, shiftT, slot):
    P = g.P
    A = g.AB[:, slot, :]
    B = g.AB[:, slot + 1, :]
    P.add("dve", lambda e: e.scalar_tensor_tensor(out=A, in0=scaleT, scalar=1.0, in1=g.gTs[:, gi, :], op0=ALU.add, op1=ALU.mult),
          [g.tk_mod, g.tk_pers], [g.tk_AB])
    P.add("dve", lambda e: e.tensor_copy(out=B, in_=shiftT), [g.tk_mod], [g.tk_AB])
    return A, B


def make_bc(g, srcT, dst, dst_tk, tmp_off):
    P = g.P
    dg = [arena_f32(g, tmp_off + i * 128, 128) for i in range(2)]
    dg_tk = [Tk(), Tk()]
    for k in range(NK):
        s = k % 2
        P.add("pool", lambda e, o=dg[s], sc=srcT[:, k:k + 1]: e.tensor_scalar(out=o, in0=g.ident32, scalar1=sc, scalar2=None, op0=ALU.mult),
              [g.tk_mod, g.tk_pers, g.tk_AB], [dg_tk[s]])
        bank = 4 + (k // 4) % 2
        P.add("pe", lambda e, o=g.ps[bank][:, (k % 4) * 128:(k % 4 + 1) * 128], b=dg[s]: e.matmul(o, g.ones32, b, start=True, stop=True),
              [dg_tk[s], g.tk_pers], [g.pst[bank]])
        if k % 4 == 3:
            c0 = (k // 4) * 512
            P.add("act", lambda e, o=dst[:, c0:c0 + 512], i=g.ps[bank][:, :]: e.activation(out=o, in_=i, func=AF.Copy),
                  [g.pst[bank]], [dst_tk])


def norm_transpose(g, src, src_tk, A, B, ab_slot_reads, dstf, tmp, it, router=None, nodst=False, phase=0):
    P = g.P
    nr = len(tmp["xn"])
    r = it % nr
    ss, ss_tk = tmp["ss"][it % 2]
    sq, sq_tk = tmp["sq"][it % 2]
    xn, xn_tk = tmp["xn"][r]
    junk, junk_tk = tmp["junk"]
    if phase in (0, 1):
        P.add("dve", lambda e: e.memset(ss, 0.0), [], [ss_tk])
        P.add("act", lambda e: e.activation(out=junk, in_=src, func=AF.Square, accum_out=ss), [src_tk], [junk_tk, ss_tk])
        P.add("act", lambda e: e.activation(out=sq, in_=ss, func=AF.Sqrt, bias=tmp["eps"], scale=1.0 / D), [ss_tk, g.tk_pers], [sq_tk])
        P.add("dve", lambda e: e.reciprocal(out=sq, in_=sq), [sq_tk], [sq_tk])
        P.add("act", lambda e: e.activation(out=xn, in_=src, func=AF.Copy, scale=sq), [src_tk, sq_tk], [xn_tk])
    if phase == 1:
        return
    for b in range(4):
        for c in range(4):
            k = b * 4 + c
            P.add("pe", lambda e, o=g.ps[b][:, c * 128:(c + 1) * 128], i=xn[:, k * 128:(k + 1) * 128]: e.transpose(o, i, g.ident32),
                  [xn_tk, g.tk_pers], [g.pst[b]])
    t32s = []
    for b in range(4):
        if router is not None:
            t32, t32_tk = tmp["t32"][(it * 4 + b) % len(tmp["t32"])]
            t32s.append((t32, t32_tk))
        if not nodst:
            dst, dst_tk = dstf(b)
        for c in range(4):
            k = b * 4 + c
            pc = g.ps[b][:, c * 128:(c + 1) * 128]
            if router is not None:
                o_, wtk = t32[:, c * 128:(c + 1) * 128], t32_tk
            else:
                o_, wtk = dst[:, c, :], dst_tk
            if c % 2 == 0:
                P.add("act", lambda e, o=o_, i=pc, k=k: e.activation(out=o, in_=i, func=AF.Identity, scale=A[:, k:k + 1], bias=B[:, k:k + 1]), [g.pst[b], g.tk_AB], [wtk])
            else:
                P.add("dve", lambda e, o=o_, i=pc, k=k: e.tensor_scalar(out=o, in0=i, scalar1=A[:, k:k + 1], scalar2=B[:, k:k + 1], op0=ALU.mult, op1=ALU.add), [g.pst[b], g.tk_AB], [wtk])
        if router is not None and not nodst:
            P.add("act", lambda e, o=dst, i=t32.rearrange("p (c n) -> p c n", c=4): e.activation(out=o, in_=i, func=AF.Copy), [t32_tk], [dst_tk])
    if router is not None:
        lg, lg_tk, rws, rw_tk = router
        for b in range(4):
            t32, t32_tk = t32s[b]
            for c in range(4):
                k = b * 4 + c
                P.add("pe", lambda e, o=lg, a=t32[:, c * 128:(c + 1) * 128], w=rws[:, k, :], st=(k == 0), sp=(k == 15):
                      e.matmul(o, a, w, start=st, stop=sp), [t32_tk, rw_tk], [lg_tk])


def stage_moe(g, l, xsrc, xdst, final):
    P = g.P
    A, B = prep_AB(g, 2 * l + 1, g.modT[l][:, 4, :], g.modT[l][:, 3, :], 0)
    ACC, HT, W0 = 0, 16384, 24576
    STG, GU, DN, AT0, CBC, G2, TMP, COMBT, SELE = 24576, 28672, 32768, 40960, 45056, 46080, 48128, 50176, 51200
    acc = [arena_f32(g, ACC + t * 2048, 2048) for t in range(8)]
    acc_tk = [Tk() for _ in range(8)]
    hT = arena_bf(g, HT, 16384).rearrange("p (k n) -> p k n", k=NK)
    hT_tk = [Tk() for _ in range(8)]
    g2bc = arena_f32(g, G2, 2048)
    g2_tk = Tk()
    make_bc(g, g.modT[l][:, 5, :], g2bc, g2_tk, TMP)
    rws = g.small[:, 64:64 + 256].rearrange("p (k e) -> p k e", e=NE)
    rw_tk = Tk()
    rbs = g.small[:, 320:336]
    eps = g.small[:, 336:337]
    P.add("sp", lambda e: e.dma_start(out=rws, in_=g.rw), [], [rw_tk], dma=True)
    P.add("sp", lambda e: e.dma_start(out=rbs, in_=g.rb), [], [rw_tk], dma=True)
    P.add("pool", lambda e: e.memset(eps, EPS), [], [g.tk_pers])
    for tb in range(2):
        tmp = {
            "ss": [(g.small[:, 340 + i:341 + i], Tk()) for i in range(2)],
            "sq": [(g.small[:, 344 + i:345 + i], Tk()) for i in range(2)],
            "xn": [(arena_f32(g, W0 + i * 2048, 2048), Tk()) for i in range(2)],
            "junk": (arena_bf(g, W0 + 4096, 2048), Tk()),
            "t32": [(arena_f32(g, W0 + 5120 + i * 512, 512), Tk()) for i in range(2)],
            "eps": eps,
        }
        lgps = g.ps[6]
        lg_tk = g.pst[6]
        for t in range(8):
            i = tb * 8 + t
            P.add("sp", lambda e, o=acc[t], s=xsrc[i * 128:(i + 1) * 128, :]: e.dma_start(out=o, in_=s), [], [acc_tk[t]], dma=True)
            norm_transpose(g, acc[t], acc_tk[t], A, B, None,
                           lambda b, t=t: (hT[:, b * 4:(b + 1) * 4, t * 128:(t + 1) * 128], hT_tk[t]),
                           tmp, t, router=(lgps[:, t * 16:(t + 1) * 16], lg_tk, rws, rw_tk))
        RB = W0 + 6144

        def rt(n, w):
            return arena_f32(g, RB + n * 128, w)
        sc, sel, eq, msk, w_, comb = rt(0, 128), rt(1, 128), rt(2, 128), rt(3, 128), rt(4, 128), rt(5, 128)
        m1, m2, gs, ing = rt(6, 32), rt(7, 32), rt(8, 32), rt(9, 32)
        gmax, wsum = rt(10, 8), rt(11, 8)
        rtk = Tk()

        def v3(a, x, y):
            return a.rearrange("p (x y) -> p x y", x=x)
        P.add("act", lambda e: e.activation(out=sc, in_=lgps[:, 0:128], func=AF.Sigmoid), [lg_tk], [rtk])
        P.add("dve", lambda e: e.tensor_tensor(out=v3(sel, 8, 16), in0=v3(sc, 8, 16), in1=rbs.unsqueeze(1).to_broadcast([128, 8, 16]), op=ALU.add), [rtk, rw_tk], [rtk])
        P.add("dve", lambda e: e.tensor_reduce(out=m1, in_=v3(sel, 32, 4), axis=AX.X, op=ALU.max), [rtk], [rtk])
        P.add("dve", lambda e: e.tensor_tensor(out=v3(eq, 32, 4), in0=v3(sel, 32, 4), in1=m1.unsqueeze(2).to_broadcast([128, 32, 4]), op=ALU.is_equal), [rtk], [rtk])
        P.add("dve", lambda e: e.scalar_tensor_tensor(out=msk, in0=eq, scalar=-1e9, in1=sel, op0=ALU.mult, op1=ALU.add), [rtk], [rtk])
        P.add("dve", lambda e: e.tensor_reduce(out=m2, in_=v3(msk, 32, 4), axis=AX.X, op=ALU.max), [rtk], [rtk])
        P.add("dve", lambda e: e.tensor_tensor(out=gs, in0=m1, in1=m2, op=ALU.add), [rtk], [rtk])
        P.add("dve", lambda e: e.tensor_reduce(out=gmax, in_=v3(gs, 8, 4), axis=AX.X, op=ALU.max), [rtk], [rtk])
        P.add("dve", lambda e: e.tensor_tensor(out=v3(ing, 8, 4), in0=v3(gs, 8, 4), in1=gmax.unsqueeze(2).to_broadcast([128, 8, 4]), op=ALU.is_equal), [rtk], [rtk])
        P.add("dve", lambda e: e.tensor_tensor(out=v3(eq, 32, 4), in0=v3(sel, 32, 4), in1=m2.unsqueeze(2).to_broadcast([128, 32, 4]), op=ALU.is_ge), [rtk], [rtk])
        P.add("dve", lambda e: e.tensor_tensor(out=v3(msk, 32, 4), in0=v3(eq, 32, 4), in1=ing.unsqueeze(2).to_broadcast([128, 32, 4]), op=ALU.mult), [rtk], [rtk])
        P.add("dve", lambda e: e.tensor_tensor(out=w_, in0=sc, in1=msk, op=ALU.mult), [rtk], [rtk])
        P.add("dve", lambda e: e.tensor_reduce(out=wsum, in_=v3(w_, 8, 16), axis=AX.X, op=ALU.add), [rtk], [rtk])
        P.add("dve", lambda e: e.reciprocal(out=wsum, in_=wsum), [rtk], [rtk])
        P.add("dve", lambda e: e.tensor_tensor(out=v3(comb, 8, 16), in0=v3(w_, 8, 16), in1=wsum.unsqueeze(2).to_broadcast([128, 8, 16]), op=ALU.mult), [rtk], [rtk])
        combT = g.arena[0:16, COMBT: COMBT + 1024]
        combT_tk = Tk()
        for half in range(2):
            bank = 4 + half
            for t4 in range(4):
                t = half * 4 + t4
                P.add("pe", lambda e, o=g.ps[bank][0:16, t4 * 128:(t4 + 1) * 128], i=comb[:, t * 16:(t + 1) * 16]: e.transpose(o, i, g.ident32),
                      [rtk, g.tk_pers], [g.pst[bank]])
            P.add("act", lambda e, o=combT[:, half * 512:(half + 1) * 512], i=g.ps[bank][0:16, :]: e.activation(out=o, in_=i, func=AF.Copy),
                  [g.pst[bank]], [combT_tk])
        sel2 = [g.arena[0:16, SELE + i * 128: SELE + (i + 1) * 128] for i in range(2)]
        sel2_tk = [Tk(), Tk()]
        P.barrier()
        stg = [arena_f32(g, STG + i * 2048, 2048) for i in range(2)]
        stg_tk = [Tk() for _ in range(2)]
        gub = [arena_bf(g, GU + i * 1024, 2048) for i in range(4)]
        gu_tk = [Tk() for _ in range(4)]
        dnb = [arena_bf(g, DN + i * 1024, 2048) for i in range(8)]
        dn_tk = [Tk() for _ in range(8)]
        ATb = [arena_bf(g, AT0 + i * 2048, 4096).rearrange("p (f n) -> p f n", f=4) for i in range(2)]
        AT_tk = [Tk(), Tk()]
        cbc = [arena_bf(g, CBC + i * 512, 1024) for i in range(2)]
        cbc_tk = [Tk(), Tk()]
        sgt = [arena_f32(g, TMP + i * 512, 512) for i in range(2)]
        sg_tk = [Tk(), Tk()]
        t2t = [arena_f32(g, TMP + 1024 + i * 512, 512) for i in range(2)]
        t2_tk = [Tk(), Tk()]
        units = []
        for ex in range(NE):
            for f in range(4):
                units.append(("g", ex, f))
                units.append(("u", ex, f))
                units.append(("d", ex, f))
        state = {"dma": 0, "cast": 0}

        def unit_src(u):
            kind, ex, f = u
            if kind == "g":
                return g.wg[l, ex, :, f * 128:(f + 1) * 128].rearrange("(k p) n -> p k n", p=128)
            if kind == "u":
                return g.wu[l, ex, :, f * 128:(f + 1) * 128].rearrange("(k p) n -> p k n", p=128)
            return g.wd[l, ex, f * 128:(f + 1) * 128, :]

        def unit_dst(n):
            kind, ex, f = units[n]
            if kind == "d":
                s = (ex % 2) * 4 + f
                return dnb[s], dn_tk[s]
            s = (2 * (ex * 4 + f) + (1 if kind == "u" else 0)) % 4
            return gub[s], gu_tk[s]

        def issue_dma(upto):
            while state["dma"] < min(upto, len(units)):
                n = state["dma"]
                kind = units[n][0]
                s = n % 2
                o = stg[s] if kind == "d" else stg[s].rearrange("p (k n) -> p k n", k=NK)
                P.add("sp", lambda e, o=o, sr=unit_src(units[n]): e.dma_start(out=o, in_=sr), [], [stg_tk[s]], dma=True)
                state["dma"] += 1

        def issue_cast(upto):
            while state["cast"] < min(upto, len(units)):
                n = state["cast"]
                issue_dma(n + 2)
                kind = units[n][0]
                s = n % 2
                dst, dtk = unit_dst(n)
                if kind == "d":
                    P.add("pool", lambda e, o=dst, i=stg[s]: e.tensor_tensor(out=o, in0=i, in1=g2bc, op=ALU.mult), [stg_tk[s], g2_tk], [dtk])
                elif kind == "g":
                    P.add("act", lambda e, o=dst, i=stg[s]: e.activation(out=o, in_=i, func=AF.Copy), [stg_tk[s]], [dtk])
                else:
                    P.add("dve", lambda e, o=dst, i=stg[s]: e.tensor_copy(out=o, in_=i), [stg_tk[s]], [dtk])
                state["cast"] += 1

        issue_cast(6)
        it = 0
        for ex in range(NE):
            c = ex % 2
            P.add("pool", lambda e, o=sel2[c], i=g.ident32[0:16, ex:ex + 1].to_broadcast([16, 128]): e.tensor_copy(out=o, in_=i), [g.tk_pers], [sel2_tk[c]])
            for sb in range(2):
                bank = 4 + sb
                P.add("pe", lambda e, o=g.ps[bank][:, :], a=sel2[c], b=combT[:, sb * 512:(sb + 1) * 512]: e.matmul(o, a, b, start=True, stop=True),
                      [sel2_tk[c], combT_tk], [g.pst[bank]])
                P.add("act", lambda e, o=cbc[c][:, sb * 512:(sb + 1) * 512], i=g.ps[bank][:, :]: e.activation(out=o, in_=i, func=AF.Copy),
                      [g.pst[bank]], [cbc_tk[c]])
            for f in range(4):
                n0 = (ex * 4 + f) * 3
                issue_cast(n0 + 6)
                gw, gtk = unit_dst(n0)
                uw, utk = unit_dst(n0 + 1)
                gw3 = gw.rearrange("p (k n) -> p k n", k=NK)
                uw3 = uw.rearrange("p (k n) -> p k n", k=NK)
                for sb in range(2):
                    bg, bu = (0, 1) if it % 2 == 0 else (2, 3)
                    for k in range(NK):
                        P.add("pe", lambda e, o=g.ps[bg][:, :], a=gw3[:, k, :], b=hT[:, k, sb * 512:(sb + 1) * 512], st=(k == 0), sp=(k == NK - 1):
                              e.matmul(o, a, b, start=st, stop=sp), [gtk] + hT_tk[sb * 4:sb * 4 + 4], [g.pst[bg]])
                    for k in range(NK):
                        P.add("pe", lambda e, o=g.ps[bu][:, :], a=uw3[:, k, :], b=hT[:, k, sb * 512:(sb + 1) * 512], st=(k == 0), sp=(k == NK - 1):
                              e.matmul(o, a, b, start=st, stop=sp), [utk] + hT_tk[sb * 4:sb * 4 + 4], [g.pst[bu]])
                    r = it % 2
                    P.add("act", lambda e, o=sgt[r], i=g.ps[bg][:, :]: e.activation(out=o, in_=i, func=AF.Silu), [g.pst[bg]], [sg_tk[r]])
                    P.add("dve", lambda e, o=t2t[r], a=g.ps[bu][:, :], b=sgt[r]: e.tensor_tensor(out=o, in0=a, in1=b, op=ALU.mult), [g.pst[bu], sg_tk[r]], [t2_tk[r]])
                    P.add("pool", lambda e, o=ATb[c][:, f, sb * 512:(sb + 1) * 512], a=t2t[r], b=cbc[c][:, sb * 512:(sb + 1) * 512]:
                          e.tensor_tensor(out=o, in0=a, in1=b, op=ALU.mult), [t2_tk[r], cbc_tk[c]], [AT_tk[c]])
                    it += 1
            for t in range(8):
                for db in range(4):
                    bank = 6 + (t * 4 + db) % 2
                    for f in range(4):
                        dw, dtk = dnb[(ex % 2) * 4 + f], dn_tk[(ex % 2) * 4 + f]
                        P.add("pe", lambda e, o=g.ps[bank][:, :], a=ATb[c][:, f, t * 128:(t + 1) * 128], b=dw[:, db * 512:(db + 1) * 512], st=(f == 0), sp=(f == 3):
                              e.matmul(o, a, b, start=st, stop=sp), [AT_tk[c], dtk], [g.pst[bank]])
                    P.add("dve", lambda e, o=acc[t][:, db * 512:(db + 1) * 512], i=g.ps[bank][:, :]: e.tensor_tensor(out=o, in0=i, in1=o, op=ALU.add),
                          [g.pst[bank], acc_tk[t]], [acc_tk[t]])
        P.barrier()
        if final:
            fng = arena_f32(g, W0, 2048)
            fng_tk = Tk()
            P.add("sp", lambda e: e.dma_start(out=fng, in_=g.fng), [], [fng_tk], dma=True)
            junk = arena_bf(g, W0 + 2048, 2048)
            junk_tk = Tk()
            fin_ss_tk = [Tk(), Tk()]
            for t in range(8):
                i = tb * 8 + t
                ss, ss_tk = g.small[:, 340 + t % 2:341 + t % 2], fin_ss_tk[t % 2]
                P.add("pool", lambda e, o=ss: e.memset(o, 0.0), [], [ss_tk])
                P.add("act", lambda e, o=junk, i_=acc[t], a=ss: e.activation(out=o, in_=i_, func=AF.Square, accum_out=a), [acc_tk[t]], [junk_tk, ss_tk])
                P.add("act", lambda e, o=ss: e.activation(out=o, in_=o, func=AF.Sqrt, bias=eps, scale=1.0 / D), [ss_tk, g.tk_pers], [ss_tk])
                P.add("dve", lambda e, o=ss: e.reciprocal(out=o, in_=o), [ss_tk], [ss_tk])
                P.add("dve", lambda e, o=acc[t], sc_=ss: e.scalar_tensor_tensor(out=o, in0=o, scalar=sc_, in1=fng, op0=ALU.mult, op1=ALU.mult), [acc_tk[t], ss_tk, fng_tk], [acc_tk[t]])
                P.add("sp", lambda e, o=xdst[i * 128:(i + 1) * 128, :], s=acc[t]: e.dma_start(out=o, in_=s), [acc_tk[t]], [], dma=True)
        else:
            for t in range(8):
                i = tb * 8 + t
                P.add("sp", lambda e, o=xdst[i * 128:(i + 1) * 128, :], s=acc[t]: e.dma_start(out=o, in_=s), [acc_tk[t]], [], dma=True)
        P.barrier()


def stage_moe_sparse(g, l, xsrc, xdst, final):
    P = g.P
    I32 = mybir.dt.int32
    U16 = mybir.dt.uint16
    A, B = prep_AB(g, 2 * l + 1, g.modT[l][:, 4, :], g.modT[l][:, 3, :], 0)
    NTL = NT
    H2, ABC, BBC, XT, XN, JK, T32, RB = 0, 16384, 18432, 20480, 26624, 32768, 33792, 37888
    PERS = 51200
    h2tok = arena_bf(g, H2, 32768).rearrange("p (t n) -> p t n", t=NTL)
    h2_tk = [Tk() for _ in range(NTL)]
    Abc, Bbc = arena_f32(g, ABC, 2048), arena_f32(g, BBC, 2048)
    Abc_tk, Bbc_tk = Tk(), Tk()
    make_bc(g, A, Abc, Abc_tk, RB)
    make_bc(g, B, Bbc, Bbc_tk, RB + 256)
    rws = g.small[:, 64:64 + 256].rearrange("p (k e) -> p k e", e=NE)
    rw_tk = Tk()
    rbs = g.small[:, 320:336]
    eps = g.small[:, 336:337]
    P.add("sp", lambda e: e.dma_start(out=rws, in_=g.rw), [], [rw_tk], dma=True)
    P.add("sp", lambda e: e.dma_start(out=rbs, in_=g.rb), [], [rw_tk], dma=True)
    P.add("pool", lambda e: e.memset(eps, EPS), [], [g.tk_pers])
    desti = g.arena[:, PERS:PERS + 32].bitcast(I32)
    cw = arena_f32(g, PERS + 32, 32)
    cntneg = g.arena[0:1, PERS + 64:PERS + 82].bitcast(I32)
    maskbits = g.arena[:, PERS + 96:PERS + 224].bitcast(U16)
    pers_tk = Tk()
    xt = [arena_f32(g, XT + i * 2048, 2048) for i in range(3)]
    xt_tk = [Tk(), Tk(), Tk()]
    tmp = {
        "ss": [(g.small[:, 340 + i:341 + i], Tk()) for i in range(2)],
        "sq": [(g.small[:, 344 + i:345 + i], Tk()) for i in range(2)],
        "xn": [(arena_f32(g, XN + i * 2048, 2048), Tk()) for i in range(3)],
        "junk": (arena_bf(g, JK, 2048), Tk()),
        "t32": [(arena_f32(g, T32 + i * 512, 512), Tk()) for i in range(8)],
        "eps": eps,
    }
    lgps = g.ps[6]
    lg_tk = g.pst[6]
    def n_stage1(t):
        if t < NTL:
            r_ = t % 3
            P.add("sp", lambda e, o=xt[r_], s=xsrc[t * 128:(t + 1) * 128, :]: e.dma_start(out=o, in_=s), [], [xt_tk[r_]], dma=True)
            norm_transpose(g, xt[r_], xt_tk[r_], A, B, None, None, tmp, t, router=(None, None, None, None), nodst=True, phase=1)
    n_stage1(0)
    for t in range(NTL):
        r = t % 3
        n_stage1(t + 1)
        norm_transpose(g, xt[r], xt_tk[r], A, B, None, None, tmp, t, router=(lgps[:, t * 16:(t + 1) * 16], lg_tk, rws, rw_tk), nodst=True, phase=2)
        xn, xn_tk = tmp["xn"][r]
        P.add("dve", lambda e, o=xn: e.tensor_tensor(out=o, in0=o, in1=Abc, op=ALU.mult), [xn_tk, Abc_tk], [xn_tk])
        P.add("pool", lambda e, o=h2tok[:, t, :], i=xn: e.tensor_tensor(out=o, in0=i, in1=Bbc, op=ALU.add), [xn_tk, Bbc_tk], [h2_tk[t]])
    W = NTL * 16

    def rt(n, w=W):
        return arena_f32(g, RB + n * 256, w)
    sc, sel, eq, msk, w_, comb = rt(0), rt(1), rt(2), rt(3), rt(4), rt(5)
    m1, m2, gs, ing = rt(6, 64), rt(7, 64), rt(8, 64), rt(9, 64)
    gmax, wsum = arena_f32(g, RB + 10 * 256, 16), arena_f32(g, RB + 10 * 256 + 16, 16)
    within, cntbc, pref, dfull, ta, tb2 = rt(11), rt(12), rt(13), rt(14), rt(15), rt(16)
    d01 = arena_f32(g, RB + 17 * 256, 32)
    cntf = arena_f32(g, RB + 17 * 256 + 32, 16)
    cnti = g.arena[:, RB + 17 * 256 + 48: RB + 17 * 256 + 64].bitcast(I32)
    cst = arena_f32(g, RB + 18 * 256, 160)
    validf = arena_f32(g, RB + 19 * 256, 256)
    rtk = Tk()
    cst_tk = Tk()
    P.add("sp", lambda e: e.dma_start(out=cst, in_=g.cst), [], [cst_tk], dma=True)
    Utri, posc, ebase = cst[:, 0:128], cst[:, 128:144], cst[:, 144:160]

    def v3(a, x, y):
        return a.rearrange("p (x y) -> p x y", x=x)
    P.add("act", lambda e: e.activation(out=sc, in_=lgps[:, 0:W], func=AF.Sigmoid), [lg_tk], [rtk])
    P.add("dve", lambda e: e.tensor_tensor(out=v3(sel, NTL, 16), in0=v3(sc, NTL, 16), in1=rbs.unsqueeze(1).to_broadcast([128, NTL, 16]), op=ALU.add), [rtk, rw_tk], [rtk])
    P.add("dve", lambda e: e.tensor_reduce(out=m1, in_=v3(sel, NTL * 4, 4), axis=AX.X, op=ALU.max), [rtk], [rtk])
    P.add("dve", lambda e: e.tensor_tensor(out=v3(eq, NTL * 4, 4), in0=v3(sel, NTL * 4, 4), in1=m1.unsqueeze(2).to_broadcast([128, NTL * 4, 4]), op=ALU.is_equal), [rtk], [rtk])
    P.add("dve", lambda e: e.scalar_tensor_tensor(out=msk, in0=eq, scalar=-1e9, in1=sel, op0=ALU.mult, op1=ALU.add), [rtk], [rtk])
    P.add("dve", lambda e: e.tensor_reduce(out=m2, in_=v3(msk, NTL * 4, 4), axis=AX.X, op=ALU.max), [rtk], [rtk])
    P.add("dve", lambda e: e.tensor_tensor(out=gs, in0=m1, in1=m2, op=ALU.add), [rtk], [rtk])
    P.add("dve", lambda e: e.tensor_reduce(out=gmax, in_=v3(gs, NTL, 4), axis=AX.X, op=ALU.max), [rtk], [rtk])
    P.add("dve", lambda e: e.tensor_tensor(out=v3(ing, NTL, 4), in0=v3(gs, NTL, 4), in1=gmax.unsqueeze(2).to_broadcast([128, NTL, 4]), op=ALU.is_equal), [rtk], [rtk])
    P.add("dve", lambda e: e.tensor_tensor(out=v3(eq, NTL * 4, 4), in0=v3(sel, NTL * 4, 4), in1=m2.unsqueeze(2).to_broadcast([128, NTL * 4, 4]), op=ALU.is_ge), [rtk], [rtk])
    P.add("dve", lambda e: e.tensor_tensor(out=v3(msk, NTL * 4, 4), in0=v3(eq, NTL * 4, 4), in1=ing.unsqueeze(2).to_broadcast([128, NTL * 4, 4]), op=ALU.mult), [rtk], [rtk])
    P.add("dve", lambda e: e.tensor_tensor(out=w_, in0=sc, in1=msk, op=ALU.mult), [rtk], [rtk])
    P.add("dve", lambda e: e.tensor_reduce(out=wsum, in_=v3(w_, NTL, 16), axis=AX.X, op=ALU.add), [rtk], [rtk])
    P.add("dve", lambda e: e.reciprocal(out=wsum, in_=wsum), [rtk], [rtk])
    P.add("dve", lambda e: e.tensor_tensor(out=v3(comb, NTL, 16), in0=v3(w_, NTL, 16), in1=wsum.unsqueeze(2).to_broadcast([128, NTL, 16]), op=ALU.mult), [rtk], [rtk])
    P.add("pe", lambda e: e.matmul(g.ps[4][:, 0:W], Utri, msk, start=True, stop=True), [rtk, cst_tk], [g.pst[4]])
    P.add("pe", lambda e: e.matmul(g.ps[5][:, 0:W], g.ones32, msk, start=True, stop=True), [rtk, g.tk_pers], [g.pst[5]])
    P.add("act", lambda e: e.activation(out=within, in_=g.ps[4][:, 0:W], func=AF.Copy), [g.pst[4]], [rtk])
    P.add("act", lambda e: e.activation(out=cntbc, in_=g.ps[5][:, 0:W], func=AF.Copy), [g.pst[5]], [rtk])
    P.add("pool", lambda e: e.memset(pref[:, 0:16], 0.0), [rtk], [rtk])
    for t in range(1, NTL):
        P.add("dve", lambda e, t=t: e.tensor_tensor(out=pref[:, t * 16:(t + 1) * 16], in0=pref[:, (t - 1) * 16:t * 16], in1=cntbc[:, (t - 1) * 16:t * 16], op=ALU.add), [rtk], [rtk])
    P.add("dve", lambda e: e.tensor_tensor(out=cntf, in0=pref[:, (NTL - 1) * 16:NTL * 16], in1=cntbc[:, (NTL - 1) * 16:NTL * 16], op=ALU.add), [rtk], [rtk])
    P.add("dve", lambda e: e.tensor_tensor(out=dfull, in0=within, in1=pref, op=ALU.add), [rtk], [rtk])
    P.add("dve", lambda e: e.tensor_tensor(out=v3(dfull, NTL, 16), in0=v3(dfull, NTL, 16), in1=ebase.unsqueeze(1).to_broadcast([128, NTL, 16]), op=ALU.add), [rtk, cst_tk], [rtk])
    P.add("dve", lambda e: e.tensor_scalar(out=ta, in0=msk, scalar1=-1e6, scalar2=1e6, op0=ALU.mult, op1=ALU.add), [rtk], [rtk])
    P.add("dve", lambda e: e.tensor_tensor(out=ta, in0=ta, in1=dfull, op=ALU.add), [rtk], [rtk])
    P.add("dve", lambda e: e.tensor_reduce(out=d01[:, 0:16], in_=v3(ta, NTL, 16), axis=AX.X, op=ALU.min), [rtk], [rtk])
    P.add("dve", lambda e: e.tensor_tensor(out=tb2, in0=dfull, in1=msk, op=ALU.mult), [rtk], [rtk])
    P.add("dve", lambda e: e.tensor_reduce(out=d01[:, 16:32], in_=v3(tb2, NTL, 16), axis=AX.X, op=ALU.max), [rtk], [rtk])
    for j in range(2):
        P.add("dve", lambda e, j=j: e.tensor_tensor(out=v3(ta, NTL, 16), in0=v3(dfull, NTL, 16), in1=d01[:, j * 16:(j + 1) * 16].unsqueeze(2).to_broadcast([128, NTL, 16]), op=ALU.is_equal), [rtk], [rtk])
        P.add("dve", lambda e: e.tensor_tensor(out=ta, in0=ta, in1=comb, op=ALU.mult), [rtk], [rtk])
        P.add("dve", lambda e, j=j: e.tensor_reduce(out=cw[:, j * 16:(j + 1) * 16], in_=v3(ta, NTL, 16), axis=AX.X, op=ALU.add), [rtk], [pers_tk])
    P.add("dve", lambda e: e.tensor_copy(out=desti, in_=d01), [rtk], [pers_tk])
    P.add("dve", lambda e: e.tensor_scalar(out=cnti, in0=cntf, scalar1=127.0, scalar2=None, op0=ALU.add), [rtk], [rtk])
    P.add("dve", lambda e: e.tensor_scalar(out=cnti, in0=cnti, scalar1=7, scalar2=None, op0=ALU.arith_shift_right), [rtk], [rtk])
    P.add("dve", lambda e: e.tensor_scalar(out=cntneg[0:1, 0:16], in0=cnti[0:1, :], scalar1=-1, scalar2=None, op0=ALU.mult), [rtk], [pers_tk])
    P.add("dve", lambda e: e.tensor_reduce(out=cntneg[0:1, 16:17], in_=cntneg[0:1, 0:16], axis=AX.X, op=ALU.min), [pers_tk], [pers_tk])
    P.add("dve", lambda e: e.tensor_tensor(out=v3(validf, 16, 16), in0=posc.unsqueeze(1).to_broadcast([128, 16, 16]), in1=cntf.unsqueeze(2).to_broadcast([128, 16, 16]), op=ALU.is_lt), [rtk, cst_tk], [rtk])
    P.add("dve", lambda e: e.tensor_scalar(out=maskbits, in0=validf, scalar1=65535.0, scalar2=None, op0=ALU.mult), [rtk], [pers_tk])
    for t in range(NTL):
        for j in range(2):
            c = j * 16 + t
            P.add("pool", lambda e, c=c, t=t: e.indirect_dma_start(out=g.xg_all[:, :], out_offset=bass.IndirectOffsetOnAxis(ap=desti[:, c:c + 1], axis=0),
                                                                   in_=h2tok[:, t, :], in_offset=None),
                  [pers_tk, h2_tk[t]], [], dma=True)
    P.barrier()
    GU, DN, G2, XG, XGT, SG, ATO, YB, IDB = 0, 16384, 24576, 26624, 30720, 34816, 35840, 36352, 40448
    Gb = [arena_bf(g, GU + i * 8192, 8192).rearrange("p (k n) -> p k n", k=NK) for i in range(2)]
    Ub = [arena_bf(g, GU + 4096 + i * 8192, 8192).rearrange("p (k n) -> p k n", k=NK) for i in range(2)]
    Db = [arena_bf(g, DN + i * 4096, 8192).rearrange("p (f n) -> p f n", f=4) for i in range(2)]
    G_tk = [[Tk() for _ in range(4)] for _ in range(2)]
    U_tk = [[Tk() for _ in range(4)] for _ in range(2)]
    D_tk = [[Tk() for _ in range(4)] for _ in range(2)]
    g2bc = arena_f32(g, G2, 2048)
    g2_tk = Tk()
    make_bc(g, g.modT[l][:, 5, :], g2bc, g2_tk, SG)
    xg = [arena_bf(g, XG + i * 1024, 2048) for i in range(4)]
    xg_tk = [Tk() for _ in range(4)]
    xgT = [arena_bf(g, XGT + i * 1024, 2048).rearrange("p (k n) -> p k n", k=NK) for i in range(4)]
    xgT_tk = [Tk() for _ in range(4)]
    sgt = [arena_f32(g, SG + i * 512, 512) for i in range(2)]
    sg_tk = [Tk(), Tk()]
    ATb = [arena_bf(g, ATO + i * 256, 512).rearrange("p (f n) -> p f n", f=4) for i in range(2)]
    AT_tk = [Tk(), Tk()]
    yb = [arena_bf(g, YB + i * 2048, 2048) for i in range(2)]
    yb_tk = [Tk(), Tk()]
    identb = arena_bf(g, IDB, 128)
    idb_tk = Tk()
    P.add("act", lambda e: e.activation(out=identb, in_=g.ident32, func=AF.Copy), [g.tk_pers], [idb_tk])

    def load_expert(ex):
        pb = ex % 2
        for q4 in range(4):
            P.add("pool", lambda e, o=Gb[pb][:, q4 * 4:(q4 + 1) * 4, :], s_=g.wg[l, ex, q4 * 512:(q4 + 1) * 512, :].rearrange("(k p) n -> p k n", p=128): e.dma_start(out=o, in_=s_),
                  [], [G_tk[pb][q4]], dma=True)
            P.add("pool", lambda e, o=Ub[pb][:, q4 * 4:(q4 + 1) * 4, :], s_=g.wu[l, ex, q4 * 512:(q4 + 1) * 512, :].rearrange("(k p) n -> p k n", p=128): e.dma_start(out=o, in_=s_),
                  [], [U_tk[pb][q4]], dma=True)
        for q4 in range(4):
            P.add("pool", lambda e, o=Db[pb][:, q4, :], s_=g.wd[l, ex, q4 * 128:(q4 + 1) * 128, :]: e.dma_start(out=o, in_=s_), [], [D_tk[pb][q4]], dma=True)

    state_it = {"it": 0}

    def tile(ex, s, uid):
        it = state_it["it"]
        P.region = (uid, s)
        r = it % 2
        state_it["it"] = it + 1
        row0 = ex * 2048 + s * 128
        P.add("sp", lambda e, o=xg[r], s_=g.xg_all[row0:row0 + 128, :]: e.dma_start(out=o, in_=s_), [g.xg_tk], [xg_tk[r]], dma=True)
        mcol = ex * 16 + s
        P.add("dve", lambda e, o=xg[r].bitcast(U16), m=maskbits[:, mcol:mcol + 1].to_broadcast([128, 2048]): e.tensor_tensor(out=o, in0=o, in1=m, op=ALU.bitwise_and),
              [xg_tk[r], pers_tk], [xg_tk[r]])
        for half in range(2):
            pb = g.ps[half][:, :].bitcast(BF16)
            for c in range(8):
                k = half * 8 + c
                P.add("pe", lambda e, o=pb[:, c * 128:(c + 1) * 128], i=xg[r][:, k * 128:(k + 1) * 128]: e.transpose(o, i, identb), [xg_tk[r], idb_tk], [g.pst[half]])
            if half == 0:
                P.add("act", lambda e, o=xgT[r][:, 0:8, :], i=pb.rearrange("p (k n) -> p k n", k=8): e.activation(out=o, in_=i, func=AF.Copy), [g.pst[half]], [xgT_tk[r]])
            else:
                P.add("dve", lambda e, o=xgT[r][:, 8:16, :], i=pb.rearrange("p (k n) -> p k n", k=8): e.tensor_copy(out=o, in_=i), [g.pst[half]], [xgT_tk[r]])
        for f in range(4):
            for k in range(NK):
                P.add("pe", lambda e, o=g.ps[2][:, f * 128:(f + 1) * 128], a=Gb[ex % 2][:, k, f * 128:(f + 1) * 128], b=xgT[r][:, k, :], st=(k == 0), sp=(k == NK - 1):
                      e.matmul(o, a, b, start=st, stop=sp), [G_tk[ex % 2][k // 4], xgT_tk[r]], [g.pst[2]])
            for k in range(NK):
                P.add("pe", lambda e, o=g.ps[3][:, f * 128:(f + 1) * 128], a=Ub[ex % 2][:, k, f * 128:(f + 1) * 128], b=xgT[r][:, k, :], st=(k == 0), sp=(k == NK - 1):
                      e.matmul(o, a, b, start=st, stop=sp), [U_tk[ex % 2][k // 4], xgT_tk[r]], [g.pst[3]])
        P.add("act", lambda e, o=sgt[r]: e.activation(out=o, in_=g.ps[2][:, :], func=AF.Silu), [g.pst[2]], [sg_tk[r]])
        P.add("dve", lambda e, o=ATb[r].rearrange("p f n -> p (f n)"), b=sgt[r]: e.tensor_tensor(out=o, in0=g.ps[3][:, :], in1=b, op=ALU.mult), [g.pst[3], sg_tk[r]], [AT_tk[r]])
        for db in range(4):
            bank = 4 + db
            for f in range(4):
                P.add("pe", lambda e, o=g.ps[bank][:, :], a=ATb[r][:, f, :], b=Db[ex % 2][:, f, db * 512:(db + 1) * 512], st=(f == 0), sp=(f == 3):
                      e.matmul(o, a, b, start=st, stop=sp), [AT_tk[r], D_tk[ex % 2][f]], [g.pst[bank]])
            P.add("dve", lambda e, o=yb[r][:, db * 512:(db + 1) * 512], i=g.ps[bank][:, :], b_=g2bc[:, db * 512:(db + 1) * 512]: e.tensor_tensor(out=o, in0=i, in1=b_, op=ALU.mult),
                  [g.pst[bank], g2_tk], [yb_tk[r]])
        P.add("sp", lambda e, o=g.yg_all[row0:row0 + 128, :], s_=yb[r]: e.dma_start(out=o, in_=s_), [yb_tk[r]], [g.yg_tk], dma=True)
        P.region = None

    def t_load(ex, s, bi):
        row0 = ex * 2048 + s * 128
        P.add("sp", lambda e, o=xg[bi], s_=g.xg_all[row0:row0 + 128, :]: e.dma_start(out=o, in_=s_), [g.xg_tk], [xg_tk[bi]], dma=True)

    def t_mask(ex, s, bi):
        mcol = ex * 16 + s
        P.add("dve", lambda e, o=xg[bi].bitcast(U16), m=maskbits[:, mcol:mcol + 1].to_broadcast([128, 2048]): e.tensor_tensor(out=o, in0=o, in1=m, op=ALU.bitwise_and),
              [xg_tk[bi], pers_tk], [xg_tk[bi]])

    def t_prep(ex, s, bi):
        for half in range(2):
            pb = g.ps[half][:, :].bitcast(BF16)
            for c in range(8):
                k = half * 8 + c
                P.add("pe", lambda e, o=pb[:, c * 128:(c + 1) * 128], i=xg[bi][:, k * 128:(k + 1) * 128]: e.transpose(o, i, identb), [xg_tk[bi], idb_tk], [g.pst[half]])
            if half == 0:
                P.add("act", lambda e, o=xgT[bi][:, 0:8, :], i=pb.rearrange("p (k n) -> p k n", k=8): e.activation(out=o, in_=i, func=AF.Copy), [g.pst[half]], [xgT_tk[bi]])
            else:
                P.add("dve", lambda e, o=xgT[bi][:, 8:16, :], i=pb.rearrange("p (k n) -> p k n", k=8): e.tensor_copy(out=o, in_=i), [g.pst[half]], [xgT_tk[bi]])

    def t_gu(ex, bi, r):
        for f in range(4):
            for k in range(NK):
                P.add("pe", lambda e, o=g.ps[2][:, f * 128:(f + 1) * 128], a=Gb[ex % 2][:, k, f * 128:(f + 1) * 128], b=xgT[bi][:, k, :], st=(k == 0), sp=(k == NK - 1):
                      e.matmul(o, a, b, start=st, stop=sp), [G_tk[ex % 2][k // 4], xgT_tk[bi]], [g.pst[2]])
            for k in range(NK):
                P.add("pe", lambda e, o=g.ps[3][:, f * 128:(f + 1) * 128], a=Ub[ex % 2][:, k, f * 128:(f + 1) * 128], b=xgT[bi][:, k, :], st=(k == 0), sp=(k == NK - 1):
                      e.matmul(o, a, b, start=st, stop=sp), [U_tk[ex % 2][k // 4], xgT_tk[bi]], [g.pst[3]])
        P.add("act", lambda e, o=sgt[r]: e.activation(out=o, in_=g.ps[2][:, :], func=AF.Silu), [g.pst[2]], [sg_tk[r]])
        P.add("dve", lambda e, o=ATb[r].rearrange("p f n -> p (f n)"), b=sgt[r]: e.tensor_tensor(out=o, in0=g.ps[3][:, :], in1=b, op=ALU.mult), [g.pst[3], sg_tk[r]], [AT_tk[r]])

    def t_down(ex, s, r):
        row0 = ex * 2048 + s * 128
        for db in range(4):
            bank = 4 + db
            for f in range(4):
                P.add("pe", lambda e, o=g.ps[bank][:, :], a=ATb[r][:, f, :], b=Db[ex % 2][:, f, db * 512:(db + 1) * 512], st=(f == 0), sp=(f == 3):
                      e.matmul(o, a, b, start=st, stop=sp), [AT_tk[r], D_tk[ex % 2][f]], [g.pst[bank]])
            P.add("dve", lambda e, o=yb[r][:, db * 512:(db + 1) * 512], i=g.ps[bank][:, :], b_=g2bc[:, db * 512:(db + 1) * 512]: e.tensor_tensor(out=o, in0=i, in1=b_, op=ALU.mult),
                  [g.pst[bank], g2_tk], [yb_tk[r]])
        P.add("sp", lambda e, o=g.yg_all[row0:row0 + 128, :], s_=yb[r]: e.dma_start(out=o, in_=s_), [yb_tk[r]], [g.yg_tk], dma=True)

    load_expert(0)
    t_load(0, 0, 2)
    t_mask(0, 0, 2)
    t_prep(0, 0, 2)
    for ex in range(NE):
        if ex + 1 < NE:
            load_expert(ex + 1)
            t_load(ex + 1, 0, 2 + (ex + 1) % 2)
            t_mask(ex + 1, 0, 2 + (ex + 1) % 2)
            t_prep(ex + 1, 0, 2 + (ex + 1) % 2)
        for eng in Prog.ENGS:
            P.add(eng, lambda e, eng=eng, ex=ex: e.reg_load(P.regs[eng], cntneg[0:1, ex:ex + 1]), [pers_tk], [])
        uid = ("moeh", l, ex)
        for s in range(4):
            P.region = (uid, s)
            it = state_it["it"]
            state_it["it"] = it + 1
            r = it % 2
            bi = (2 + ex % 2) if s == 0 else (s % 2)
            if s + 1 < 4:
                t_load(ex, s + 1, (s + 1) % 2)
                t_mask(ex, s + 1, (s + 1) % 2)
            t_gu(ex, bi, r)
            if s + 1 < 4:
                t_prep(ex, s + 1, (s + 1) % 2)
            t_down(ex, s, r)
            P.region = None
    for eng in Prog.ENGS:
        P.add(eng, lambda e, eng=eng: e.reg_load(P.regs2[eng], cntneg[0:1, 16:17]), [pers_tk], [])
    P.outer = (("moec", l), 4)
    for ex in range(NE):
        for eng in Prog.ENGS:
            P.add(eng, lambda e, eng=eng, ex=ex: e.reg_load(P.regs[eng], cntneg[0:1, ex:ex + 1]), [pers_tk], [])
        uid = ("moec", l, ex)
        P.region = (uid, 4)
        load_expert(ex)
        P.region = None
        for s in range(4, 16):
            tile(ex, s, uid)
    P.outer = None
    P.barrier()
    XT2, Y0, FNG, JK2, TM2 = 0, 4096, 12288, 14336, 15360
    xt2 = [arena_f32(g, XT2 + i * 2048, 2048) for i in range(2)]
    xt2_tk = [Tk(), Tk()]
    yg = [[arena_bf(g, Y0 + (i * 2 + j) * 2048, 2048) for j in range(2)] for i in range(2)]
    yg_tk = [[Tk(), Tk()], [Tk(), Tk()]]
    fng = arena_f32(g, FNG, 2048)
    fng_tk = Tk()
    junk = arena_bf(g, JK2, 2048)
    junk_tk = Tk()
    fin_ss_tk = [Tk(), Tk()]
    if final:
        P.add("sp", lambda e: e.dma_start(out=fng, in_=g.fng), [], [fng_tk], dma=True)
    def c_load(t):
        if t < NTL:
            P.add("sp", lambda e, o=xt2[t % 2], s_=xsrc[t * 128:(t + 1) * 128, :]: e.dma_start(out=o, in_=s_), [], [xt2_tk[t % 2]], dma=True)
    c_load(0)
    for t in range(NTL):
        r = t % 2
        c_load(t + 1)
        for j in range(2):
            c = j * 16 + t
            P.add("pool", lambda e, o=yg[r][j], c=c: e.indirect_dma_start(out=o, out_offset=None, in_=g.yg_all[:, :], in_offset=bass.IndirectOffsetOnAxis(ap=desti[:, c:c + 1], axis=0)),
                  [pers_tk, g.yg_tk], [yg_tk[r][j]], dma=True)
        for j in range(2):
            c = j * 16 + t
            P.add("dve", lambda e, o=xt2[r], y=yg[r][j], c=c: e.scalar_tensor_tensor(out=o, in0=y, scalar=cw[:, c:c + 1], in1=o, op0=ALU.mult, op1=ALU.add),
                  [xt2_tk[r], yg_tk[r][j], pers_tk], [xt2_tk[r]])
        if final:
            ss, ss_tk = g.small[:, 340 + t % 2:341 + t % 2], fin_ss_tk[t % 2]
            P.add("pool", lambda e, o=ss: e.memset(o, 0.0), [], [ss_tk])
            P.add("act", lambda e, o=junk, i_=xt2[r], a=ss: e.activation(out=o, in_=i_, func=AF.Square, accum_out=a), [xt2_tk[r]], [junk_tk, ss_tk])
            P.add("act", lambda e, o=ss: e.activation(out=o, in_=o, func=AF.Sqrt, bias=eps, scale=1.0 / D), [ss_tk, g.tk_pers], [ss_tk])
            P.add("dve", lambda e, o=ss: e.reciprocal(out=o, in_=o), [ss_tk], [ss_tk])
            P.add("dve", lambda e, o=xt2[r], sc_=ss: e.scalar_tensor_tensor(out=o, in0=o, scalar=sc_, in1=fng, op0=ALU.mult, op1=ALU.mult), [xt2_tk[r], ss_tk, fng_tk], [xt2_tk[r]])
        P.add("sp", lambda e, o=xdst[t * 128:(t + 1) * 128, :], s_=xt2[r]: e.dma_start(out=o, in_=s_), [xt2_tk[r]], [], dma=True)
    P.barrier()


def _fm(v):
    v = np.asarray(v, np.float32)
    return np.ascontiguousarray(v.reshape(-1, 128).T)


def _ada_b4(ab):
    o = np.zeros((ab.shape[0], 4, ab.shape[1]), np.float32)
    o[:, 0] = ab
    o[:, 2] = ab
    return o


def _bias_tables(rpb):
    H = rpb.shape[0]
    kc = np.arange(64)[:, None]
    qc = np.arange(64)[None, :]
    cs = np.clip(qc - 8, 0, 48)
    cmask = (kc >= cs) & (kc < cs + 16)
    dc = np.clip(kc - qc + 15, 0, 30)
    tab = np.full((H, 128, 26, 64), NEG, np.float32)
    for a in range(2):
        for s in range(10):
            dr = 11 - s + a
            if 3 <= dr <= 10:
                v = rpb[:, dr][:, dc]
                tab[:, a * 64:(a + 1) * 64, s, :] = np.where(cmask[None], v, NEG)
        for s in range(16):
            dr = 14 - s + a
            if 0 <= dr <= 14:
                v = rpb[:, dr][:, dc]
                tab[:, a * 64:(a + 1) * 64, 10 + s, :] = np.where(cmask[None], v, NEG)
    return np.ascontiguousarray(tab.reshape(H, 128, 26 * 64))


def _dft_consts():
    L, C = 2048, 256
    c = np.arange(C)
    ang = 2 * np.pi * np.outer(c, c) / C
    csc = np.concatenate([np.cos(ang), np.sin(ang)], axis=1) / 16.0
    csc = csc.reshape(2, 128, 512).transpose(1, 0, 2)
    l = np.arange(L)
    lm = (np.outer(l, l) % L).astype(np.float64)
    angL = 2 * np.pi * lm / L
    sL = 1.0 / np.sqrt(L)
    CL = np.cos(angL) * sL
    SL = -np.sin(angL) * sL
    out = np.empty((2, 4, 128, 16, 512), np.float32)
    for i, M in enumerate((CL, SL)):
        out[i] = M.reshape(16, 128, 4, 512).transpose(2, 1, 0, 3)
    return csc.astype(ml_dtypes.bfloat16), out.astype(ml_dtypes.bfloat16)


_CONSTS = {}


def make_in_maps(inp):
    f = lambda a: np.ascontiguousarray(np.asarray(a, np.float32))
    if "dft" not in _CONSTS:
        _CONSTS["csc"], _CONSTS["dft"] = _dft_consts()
        _CONSTS["ident"] = np.eye(128, dtype=np.float32)
        p = np.arange(128)
        cst = np.zeros((128, 160), np.float32)
        cst[:, 0:128] = (p[:, None] < p[None, :]).astype(np.float32)
        cst[:, 128:144] = p[:, None] + 128.0 * np.arange(16)[None, :]
        cst[:, 144:160] = 2048.0 * np.arange(16)[None, :]
        _CONSTS["cst"] = cst
    x, c, ctx, c_ctx = f(inp["x"]), f(inp["c"]), f(inp["ctx"]), f(inp["c_ctx"])
    shared = {
        "ada_w": f(inp["ada_w"]),
        "ada_b4": _ada_b4(f(inp["ada_b"])),
        "cmb": np.array([[1, 0], [1, 0], [0, 1], [0, 1]], np.float32),
        "gT": np.ascontiguousarray(np.stack([_fm(inp["mix_norm_g"][0]), _fm(inp["ffn_norm_g"][0]),
                                             _fm(inp["mix_norm_g"][1]), _fm(inp["ffn_norm_g"][1])], axis=1)),
        "fng": np.ascontiguousarray(np.broadcast_to(f(inp["final_norm_g"])[None, :], (128, D))),
        "w_in0": f(inp["ev_w_in"][0]), "w_out0": f(inp["ev_w_out"][0]),
        "rpbt": _bias_tables(f(inp["ev_rpb"][0])),
        "csc": _CONSTS["csc"], "dft": _CONSTS["dft"],
        "w_in1": f(inp["od_w_in"][0]),
        "cvp": np.ascontiguousarray(np.stack([_fm(inp["od_b_in"][0][:D]), _fm(inp["od_b_in"][0][D:]), _fm(inp["od_dw_b"][0]),
                                              _fm(inp["od_ln_g"][0]), _fm(inp["od_ln_b"][0]), _fm(inp["od_ln_b"][0])], axis=1)),
        "dww": np.ascontiguousarray(f(inp["od_dw_w"][0]).T.reshape(NK, 128, 31).transpose(1, 0, 2)),
        "w_out1": f(inp["od_w_out"][0]),
        "bout": np.ascontiguousarray(np.broadcast_to(f(inp["od_b_out"][0])[None, :], (128, D))),
        "rw": np.ascontiguousarray(f(inp["router_w"]).reshape(NK, 128, NE).transpose(1, 0, 2)),
        "rb": np.ascontiguousarray(np.broadcast_to(f(inp["router_b"])[None, :], (128, NE))),
        "wg": f(inp["moe_w_gate"]), "wu": f(inp["moe_w_up"]), "wd": f(inp["moe_w_down"]),
        "ident": _CONSTS["ident"],
        "cst": _CONSTS["cst"],
    }
    maps = []
    for b in range(x.shape[0]):
        m = dict(shared)
        m["x"] = x[b]
        m["ctx"] = ctx[b]
        m["cT"] = np.ascontiguousarray(np.stack([_fm(c[b]), _fm(c_ctx)], axis=2))
        maps.append(m)
    return maps


_NC = {}


def kernel(**inputs):
    maps = make_in_maps(inputs)
    if "nc" not in _NC:
        _NC["nc"] = build_program()
    res = run_bass_kernel_spmd(_NC["nc"], maps, core_ids=list(range(8)))
    return np.stack([r["out"] for r in res.results], axis=0).astype(np.float32)


def out_proj(g, zT, z_tk, nkc, w_dram, xin, xout, gateT, bias_bc_dram, base):
    P = g.P
    wsz = nkc * 256
    wbf = [arena_bf(g, base + i * wsz, nkc * 512).rearrange("p (k n) -> p k n", k=nkc) for i in range(2)]
    wbf_tk = [Tk(), Tk()]
    o = base + 2 * wsz
    g1bc = arena_f32(g, o, 2048)
    gb = arena_f32(g, o + 2048, 2048)
    g1_tk, gb_tk = Tk(), Tk()
    o += 4096
    NX = 4
    xi = [arena_f32(g, o + i * 512, 512) for i in range(NX)]
    xi_tk = [Tk() for _ in range(NX)]
    o += NX * 512
    tm = [arena_f32(g, o + i * 512, 512) for i in range(2)]
    tm_tk = [Tk(), Tk()]
    o += 1024
    xo = [arena_f32(g, o + i * 512, 512) for i in range(NX)]
    xo_tk = [Tk() for _ in range(NX)]
    o += NX * 512
    make_bc(g, gateT, g1bc, g1_tk, o)
    if bias_bc_dram is not None:
        P.add("sp", lambda e: e.dma_start(out=gb, in_=bias_bc_dram), [], [gb_tk], dma=True)
        P.add("dve", lambda e: e.tensor_tensor(out=gb, in0=gb, in1=g1bc, op=ALU.mult), [gb_tk, g1_tk], [gb_tk])
    nq = nkc // 4

    def load_w(db):
        wb = wbf[db % 2]
        for kq in range(nq):
            src_ = w_dram[kq * 512:(kq + 1) * 512, db * 512:(db + 1) * 512].rearrange("(k p) n -> p k n", p=128)
            P.add("pool", lambda e, o_=wb[:, kq * 4:(kq + 1) * 4, :], s_=src_: e.dma_start(out=o_, in_=s_), [], [wbf_tk[db % 2]], dma=True)

    iters = [(db, t) for db in range(4) for t in range(NT)]
    state = {"ld": 0}

    def issue_loads(upto):
        while state["ld"] < min(upto, len(iters)):
            n = state["ld"]
            db, t = iters[n]
            P.add("sp", lambda e, o_=xi[n % NX], s_=xin[t * 128:(t + 1) * 128, db * 512:(db + 1) * 512]: e.dma_start(out=o_, in_=s_), [], [xi_tk[n % NX]], dma=True)
            state["ld"] += 1

    load_w(0)
    for it, (db, t) in enumerate(iters):
        wb = wbf[db % 2]
        if t == 0 and db + 1 < 4:
            load_w(db + 1)
        issue_loads(it + NX - 1)
        rx = it % NX
        r2 = it % 2
        bank = 6 + it % 2
        for c in range(nkc):
            P.add("pe", lambda e, o_=g.ps[bank][:, :], a=zT[:, c, t * 128:(t + 1) * 128], b=wb[:, c, :], st=(c == 0), sp=(c == nkc - 1):
                  e.matmul(o_, a, b, start=st, stop=sp), [z_tk, wbf_tk[db % 2]], [g.pst[bank]])
        P.add("dve", lambda e, o_=tm[r2], i_=g.ps[bank][:, :], b_=g1bc[:, db * 512:(db + 1) * 512]: e.tensor_tensor(out=o_, in0=i_, in1=b_, op=ALU.mult),
              [g.pst[bank], g1_tk], [tm_tk[r2]])
        if bias_bc_dram is not None:
            P.add("pool", lambda e, o_=xi[rx], b_=gb[:, db * 512:(db + 1) * 512]: e.tensor_tensor(out=o_, in0=o_, in1=b_, op=ALU.add), [xi_tk[rx], gb_tk], [xi_tk[rx]])
        P.add("dve", lambda e, o_=xo[rx], a=tm[r2], b=xi[rx]: e.tensor_tensor(out=o_, in0=a, in1=b, op=ALU.add), [tm_tk[r2], xi_tk[rx]], [xo_tk[rx]])
        P.add("sp", lambda e, o_=xout[t * 128:(t + 1) * 128, db * 512:(db + 1) * 512], s_=xo[rx]: e.dma_start(out=o_, in_=s_), [xo_tk[rx]], [], dma=True)


def build_hT(g, xsrc, ntiles, A, B, hT, hT_tk, base):
    P = g.P
    eps = g.small[:, 336:337]
    P.add("pool", lambda e: e.memset(eps, EPS), [], [g.tk_pers])
    xt = [arena_f32(g, base + i * 2048, 2048) for i in range(2)]
    xt_tk = [Tk(), Tk()]
    tmp = {
        "ss": [(g.small[:, 340 + i:341 + i], Tk()) for i in range(2)],
        "sq": [(g.small[:, 344 + i:345 + i], Tk()) for i in range(2)],
        "xn": [(arena_f32(g, base + 4096 + i * 2048, 2048), Tk()) for i in range(2)],
        "junk": (arena_bf(g, base + 8192, 2048), Tk()),
        "t32": [(arena_f32(g, base + 9216 + i * 512, 512), Tk()) for i in range(2)],
        "eps": eps,
    }
    def s1(t):
        if t < ntiles:
            r_ = t % 2
            P.add("sp", lambda e, o=xt[r_], s=xsrc[t * 128:(t + 1) * 128, :]: e.dma_start(out=o, in_=s), [], [xt_tk[r_]], dma=True)
            norm_transpose(g, xt[r_], xt_tk[r_], A, B, None, None, tmp, t, phase=1)
    s1(0)
    for t in range(ntiles):
        r = t % 2
        s1(t + 1)
        norm_transpose(g, xt[r], xt_tk[r], A, B, None,
                       lambda b, t=t: (hT[:, b * 4:(b + 1) * 4, t * 128:(t + 1) * 128], hT_tk[t]), tmp, t, phase=2)


def stage_conv(g, xsrc, xdst):
    P = g.P
    l = 1
    A, B = prep_AB(g, 2, g.modT[l][:, 1, :], g.modT[l][:, 0, :], 0)
    HT, UT, R = 0, 16384, 33024
    PADW = 2080
    hT = arena_bf(g, HT, 32768).rearrange("p (k n) -> p k n", k=NK)
    hT_tk = [Tk() for _ in range(NT)]
    uT = arena_bf(g, UT, NK * PADW).rearrange("p (k n) -> p k n", k=NK)
    uT_tk = [Tk() for _ in range(NK)]
    cv = g.small[:, 96:192].rearrange("p (j k) -> p j k", j=6)
    cv_tk = Tk()
    P.add("sp", lambda e: e.dma_start(out=cv, in_=g.cvp), [], [cv_tk], dma=True)
    build_hT(g, xsrc, NT, A, B, hT, hT_tk, R)
    P.barrier()
    for c in range(NK):
        P.add("pool", lambda e, o=uT[:, c, 0:15]: e.memset(o, 0.0), [], [uT_tk[c]])
        P.add("pool", lambda e, o=uT[:, c, 2063:2080]: e.memset(o, 0.0), [], [uT_tk[c]])
    sg = [arena_f32(g, R + 4096 + i * 512, 512) for i in range(2)]
    sg_tk = [Tk(), Tk()]
    stg = [arena_f32(g, 43264 + i * 2048, 2048).rearrange("p (k n) -> p k n", k=NK) for i in range(2)]
    stg_tk = [Tk(), Tk()]
    wbf = [arena_bf(g, 47360 + i * 1024, 2048).rearrange("p (k n) -> p k n", k=NK) for i in range(4)]
    wbf_tk = [Tk() for _ in range(4)]
    nu = 0
    it = 0
    for c in range(NK):
        ws = []
        for half in range(2):
            s = nu % 2
            d = nu % 4
            nu += 1
            col = half * D + c * 128
            src_ = g.w_in1[:, col:col + 128].rearrange("(k p) n -> p k n", p=128)
            P.add("pool", lambda e, o=wbf[d], s_=src_: e.dma_start(out=o, in_=s_), [], [wbf_tk[d]], dma=True)
            ws.append((wbf[d], wbf_tk[d]))
        for tb in range(4):
            bv, bg = (0, 1) if it % 2 == 0 else (2, 3)
            for half, bank in ((0, bv), (1, bg)):
                w, wtk = ws[half]
                for k in range(NK):
                    P.add("pe", lambda e, o=g.ps[bank][:, :], a=w[:, k, :], b=hT[:, k, tb * 512:(tb + 1) * 512], st=(k == 0), sp=(k == NK - 1):
                          e.matmul(o, a, b, start=st, stop=sp), [wtk] + hT_tk[tb * 4:tb * 4 + 4], [g.pst[bank]])
            r = it % 2
            P.add("act", lambda e, o=sg[r], i=g.ps[bg][:, :], b_=cv[:, 1, c:c + 1]: e.activation(out=o, in_=i, func=AF.Sigmoid, bias=b_), [g.pst[bg], cv_tk], [sg_tk[r]])
            P.add("dve", lambda e, o=uT[:, c, 15 + tb * 512: 15 + (tb + 1) * 512], i=g.ps[bv][:, :], b_=cv[:, 0, c:c + 1], s_=sg[r]:
                  e.scalar_tensor_tensor(out=o, in0=i, scalar=b_, in1=s_, op0=ALU.add, op1=ALU.mult), [g.pst[bv], cv_tk, sg_tk[r]], [uT_tk[c]])
            it += 1
    P.barrier()
    vT = arena_bf(g, 0, 32768).rearrange("p (k n) -> p k n", k=NK)
    vT_tk = [[Tk() for _ in range(4)] for _ in range(NK)]
    dwt = arena_f32(g, R, 512).rearrange("p (k t) -> p k t", k=NK)[:, :, 0:31]
    dwt_full = arena_f32(g, R, 496).rearrange("p (k t) -> p k t", k=NK)
    dw_tk = Tk()
    P.add("sp", lambda e: e.dma_start(out=dwt_full, in_=g.dww), [], [dw_tk], dma=True)
    identb = arena_bf(g, R + 512, 128)
    onesb = arena_bf(g, R + 576, 128)
    cb_tk = Tk()
    P.add("act", lambda e: e.activation(out=identb, in_=g.ident32, func=AF.Copy), [g.tk_pers], [cb_tk])
    P.add("pool", lambda e: e.memset(onesb, 1.0), [], [cb_tk])
    dg = [arena_bf(g, R + 1024 + i * 2048, 31 * 128).rearrange("p (t n) -> p t n", t=31) for i in range(2)]
    dg_tk = [Tk(), Tk()]
    it = 0

    def build_diag(c):
        if c >= NK:
            return
        d_ = c % 2
        for k in range(31):
            if k % 2 == 0:
                P.add("act", lambda e, o=dg[d_][:, k, :], sc=dwt_full[:, c, k:k + 1]: e.activation(out=o, in_=identb, func=AF.Copy, scale=sc), [cb_tk, dw_tk], [dg_tk[d_]])
            else:
                P.add("dve", lambda e, o=dg[d_][:, k, :], sc=dwt_full[:, c, k:k + 1]: e.tensor_scalar(out=o, in0=identb, scalar1=sc, scalar2=None, op0=ALU.mult),
                      [cb_tk, dw_tk], [dg_tk[d_]])
    build_diag(0)
    for c in range(NK):
        d = c % 2
        build_diag(c + 1)
        for tb in range(4):
            bank = it % 2
            for k in range(31):
                P.add("pe", lambda e, o=g.ps[bank][:, :], a=dg[d][:, k, :], b=uT[:, c, tb * 512 + k: tb * 512 + k + 512], st=(k == 0), sp=(k == 30):
                      e.matmul(o, a, b, start=st, stop=sp), [dg_tk[d], uT_tk[c]], [g.pst[bank]])
            P.add("act", lambda e, o=vT[:, c, tb * 512:(tb + 1) * 512], i=g.ps[bank][:, :], b_=cv[:, 2, c:c + 1]: e.activation(out=o, in_=i, func=AF.Identity, bias=b_),
                  [g.pst[bank], cv_tk], [vT_tk[c][tb]])
            it += 1
    SB = R + 1024 + 4096
    sqb = [arena_bf(g, SB + i * 256, 512) for i in range(2)]
    sq_tk = [Tk(), Tk()]
    mean = arena_f32(g, SB + 512, 512)
    rstd = arena_f32(g, SB + 1024, 512)
    m2 = arena_f32(g, SB + 1536, 512)
    st_tk = Tk()
    tn = [arena_f32(g, SB + 2048 + i * 512, 512) for i in range(2)]
    tn_tk = [Tk(), Tk()]
    eps = g.small[:, 336:337]
    it = 0
    for tb in range(4):
        for c in range(NK):
            r = it % 2
            vv = vT[:, c, tb * 512:(tb + 1) * 512]
            P.add("act", lambda e, o=sqb[r], i=vv: e.activation(out=o, in_=i, func=AF.Square), [vT_tk[c][tb]], [sq_tk[r]])
            P.add("pe", lambda e, b=vv, st=(c == 0), sp=(c == NK - 1): e.matmul(g.ps[2][:, :], onesb, b, start=st, stop=sp), [cb_tk, vT_tk[c][tb]], [g.pst[2]])
            P.add("pe", lambda e, b=sqb[r], st=(c == 0), sp=(c == NK - 1): e.matmul(g.ps[3][:, :], onesb, b, start=st, stop=sp), [cb_tk, sq_tk[r]], [g.pst[3]])
            it += 1
        P.add("act", lambda e: e.activation(out=mean, in_=g.ps[2][:, :], func=AF.Copy, scale=1.0 / D), [g.pst[2]], [st_tk])
        P.add("dve", lambda e: e.tensor_tensor(out=m2, in0=mean, in1=mean, op=ALU.mult), [st_tk], [st_tk])
        P.add("dve", lambda e: e.scalar_tensor_tensor(out=rstd, in0=g.ps[3][:, :], scalar=1.0 / D, in1=m2, op0=ALU.mult, op1=ALU.subtract), [g.pst[3], st_tk], [st_tk])
        P.add("act", lambda e: e.activation(out=rstd, in_=rstd, func=AF.Sqrt, bias=eps), [st_tk, g.tk_pers], [st_tk])
        P.add("dve", lambda e: e.reciprocal(out=rstd, in_=rstd), [st_tk], [st_tk])
        for c in range(NK):
            r = c % 2
            vv = vT[:, c, tb * 512:(tb + 1) * 512]
            P.add("dve", lambda e, o=tn[r], i=vv: e.tensor_tensor(out=o, in0=i, in1=mean, op=ALU.subtract), [vT_tk[c][tb], st_tk], [tn_tk[r]])
            P.add("dve", lambda e, o=tn[r]: e.tensor_tensor(out=o, in0=o, in1=rstd, op=ALU.mult), [tn_tk[r], st_tk], [tn_tk[r]])
            P.add("act", lambda e, o=vv, i=tn[r], s_=cv[:, 3, c:c + 1], b_=cv[:, 4, c:c + 1]: e.activation(out=o, in_=i, func=AF.Silu, bias=b_, scale=s_),
                  [tn_tk[r], cv_tk], [vT_tk[c][tb]])
    P.barrier()
    z_tk = Tk()
    out_proj(g, vT, z_tk, NK, g.w_out1, xsrc, xdst, g.modT[l][:, 2, :], g.bout, 16384)


def load_w_unit(g, src_ap, stg, stg_tk, dst, dst_tk, eng):
    g.P.add("pool", lambda e: e.dma_start(out=dst, in_=src_ap), [], [dst_tk], dma=True)


def stage_mixer0(g, xsrc, xdst):
    P = g.P
    l = 0
    A, B = prep_AB(g, 0, g.modT[l][:, 1, :], g.modT[l][:, 0, :], 0)
    Ac, Bc = prep_AB(g, 0, g.modcT[:, 1, :], g.modcT[:, 0, :], 2)
    hT = arena_bf(g, 0, 32768).rearrange("p (k n) -> p k n", k=NK)
    hT_tk = [Tk() for _ in range(NT)]
    build_hT(g, xsrc, NT, A, B, hT, hT_tk, 16384)
    P.barrier()
    YT = arena_bf(g, 16384, 16384).rearrange("p (k n) -> p k n", k=8)
    YT_tk = Tk()
    uT = arena_bf(g, 24576, 4096).rearrange("p (k n) -> p k n", k=2)
    uT_tk = [Tk(), Tk()]
    W1 = arena_bf(g, 26624, 8192).rearrange("p (t n) -> p t n", t=NT)
    W1_tk = [Tk() for _ in range(NT)]
    dfb = [arena_bf(g, 30720 + i * 4096, 8192).rearrange("p (k n) -> p k n", k=NK) for i in range(3)]
    dfb_tk = [Tk() for _ in range(3)]
    stg = [arena_f32(g, 43008 + i * 2048, 2048).rearrange("p (k n) -> p k n", k=NK) for i in range(2)]
    stg_tk = [Tk(), Tk()]
    wbf = [arena_bf(g, 47104 + i * 1024, 2048).rearrange("p (k n) -> p k n", k=NK) for i in range(2)]
    wbf_tk = [Tk(), Tk()]
    csc = arena_bf(g, 49152, 1024).rearrange("p (k n) -> p k n", k=2)
    csc_tk = Tk()
    P.add("sp", lambda e: e.dma_start(out=csc, in_=g.csc), [], [csc_tk], dma=True)
    nu = 0
    nd = 0
    it = 0
    for gi in range(4):
        for cc in range(2):
            s = nu % 2
            nu += 1
            col = gi * 256 + cc * 128
            load_w_unit(g, g.w_in0[:, col:col + 128].rearrange("(k p) n -> p k n", p=128), stg[s], stg_tk[s], wbf[s], wbf_tk[s], "act" if cc == 0 else "dve")
            for tb in range(4):
                bank = it % 2
                it += 1
                for k in range(NK):
                    P.add("pe", lambda e, o=g.ps[bank][:, :], a=wbf[s][:, k, :], b=hT[:, k, tb * 512:(tb + 1) * 512], st=(k == 0), sp=(k == NK - 1):
                          e.matmul(o, a, b, start=st, stop=sp), [wbf_tk[s]] + hT_tk[tb * 4:tb * 4 + 4], [g.pst[bank]])
                P.add("act", lambda e, o=uT[:, cc, tb * 512:(tb + 1) * 512], i=g.ps[bank][:, :]: e.activation(out=o, in_=i, func=AF.Copy), [g.pst[bank]], [uT_tk[cc]])
        for t in range(NT):
            bank = 2 + t % 2
            for cc in range(2):
                P.add("pe", lambda e, o=g.ps[bank][:, :], a=uT[:, cc, t * 128:(t + 1) * 128], b=csc[:, cc, :], st=(cc == 0), sp=(cc == 1):
                      e.matmul(o, a, b, start=st, stop=sp), [uT_tk[cc], csc_tk], [g.pst[bank]])
            P.add("dve", lambda e, o=W1[:, t, :], i=g.ps[bank][:, :]: e.tensor_copy(out=o, in_=i), [g.pst[bank]], [W1_tk[t]])
        for mb in range(4):
            bufs = []
            for cs in range(2):
                s = nd % 3
                nd += 1
                P.add("sp", lambda e, o=dfb[s], s_=g.dft[cs, mb]: e.dma_start(out=o, in_=s_), [], [dfb_tk[s]], dma=True)
                bufs.append((dfb[s], dfb_tk[s]))
            for nch in range(2):
                bank = 4 + (mb * 2 + nch) % 2
                n = 0
                for cs in range(2):
                    db_, dtk = bufs[cs]
                    for lc in range(NK):
                        P.add("pe", lambda e, o=g.ps[bank][:, :], a=W1[:, lc, cs * 256 + nch * 128: cs * 256 + (nch + 1) * 128], b=db_[:, lc, :], st=(n == 0), sp=(n == 31):
                              e.matmul(o, a, b, start=st, stop=sp), [W1_tk[lc], dtk], [g.pst[bank]])
                        n += 1
                eng = "act" if nch == 0 else "dve"
                if eng == "act":
                    P.add("act", lambda e, o=YT[:, gi * 2 + nch, mb * 512:(mb + 1) * 512], i=g.ps[bank][:, :]: e.activation(out=o, in_=i, func=AF.Copy), [g.pst[bank]], [YT_tk])
                else:
                    P.add("dve", lambda e, o=YT[:, gi * 2 + nch, mb * 512:(mb + 1) * 512], i=g.ps[bank][:, :]: e.tensor_copy(out=o, in_=i), [g.pst[bank]], [YT_tk])
    P.barrier()
    out_proj(g, YT, YT_tk, 8, g.w_out0[0:1024, :], xsrc, g.xs[2], g.modT[l][:, 2, :], None, 24576)
    P.barrier()
    OT = arena_bf(g, 16384, 16384).rearrange("p (k n) -> p k n", k=8)
    OT_tk = Tk()
    hcT = arena_bf(g, 24576, 4096).rearrange("p (k n) -> p k n", k=NK)
    hc_tk = [Tk(), Tk()]
    build_hT(g, g.ctx, 2, Ac, Bc, hcT, hc_tk, 26624)
    P.barrier()
    QT = arena_bf(g, 26624, 4096).rearrange("p (h n) -> p h n", h=2)
    KT = arena_bf(g, 28672, 4096).rearrange("p (h n) -> p h n", h=2)
    QT_tk, KT_tk = [Tk(), Tk()], [Tk(), Tk()]
    Vq = arena_bf(g, 30720, 4160).rearrange("p (t h d) -> p t h d", t=NT, h=4)
    V_tk = [Tk() for _ in range(NT)]
    kcT = arena_bf(g, 32800, 512).rearrange("p (h n) -> p h n", h=2)
    kc_tk = [Tk(), Tk()]
    vc = arena_bf(g, 33056, 520).rearrange("p (t h d) -> p t h d", t=2, h=4)
    vc_tk = [Tk(), Tk()]
    Otok = arena_f32(g, 33344, 4096).rearrange("p (i f) -> p i f", i=NT)
    Otok_tk = [Tk() for _ in range(NT)]
    tmpb = [arena_f32(g, 37440 + i * 640, 640) for i in range(2)]
    tmp_tk = [Tk(), Tk()]
    Pb = [arena_bf(g, 38720 + i * 448, 896) for i in range(2)]
    Pb_tk = [Tk(), Tk()]
    tab = [arena_f32(g, 39616 + i * 1664, 1664) for i in range(2)]
    tab_tk = [Tk(), Tk()]
    stg = [arena_f32(g, 42944 + i * 2048, 2048).rearrange("p (k n) -> p k n", k=NK) for i in range(2)]
    stg_tk = [Tk(), Tk()]
    wbf = [arena_bf(g, 47040 + i * 1024, 2048).rearrange("p (k n) -> p k n", k=NK) for i in range(4)]
    wbf_tk = [Tk() for _ in range(4)]
    rec = [g.small[:, 348 + i:349 + i] for i in range(2)]
    rec_tk = [Tk(), Tk()]
    nu = 0
    nw = 0
    it = 0

    def wunit(col, eng):
        nonlocal nu, nw
        s = nu % 2
        d = nw % 4
        nu += 1
        nw += 1
        load_w_unit(g, g.w_in0[:, col:col + 128].rearrange("(k p) n -> p k n", p=128), stg[s], stg_tk[s], wbf[d], wbf_tk[d], eng)
        return wbf[d], wbf_tk[d]

    for q in range(4):
        P.add("pool", lambda e: e.memset(Vq[:, :, :, 64:65], 1.0), [], V_tk)
        P.add("pool", lambda e: e.memset(vc[:, :, :, 64:65], 1.0), [], vc_tk)
        for hp in range(2):
            for which, dstT, dtk, base_col in ((0, QT, QT_tk, 1024), (1, KT, KT_tk, 2048)):
                w, wtk = wunit(base_col + q * 256 + hp * 128, "act" if which == 0 else "dve")
                for tb in range(4):
                    bank = it % 2
                    it += 1
                    for k in range(NK):
                        P.add("pe", lambda e, o=g.ps[bank][:, :], a=w[:, k, :], b=hT[:, k, tb * 512:(tb + 1) * 512], st=(k == 0), sp=(k == NK - 1):
                              e.matmul(o, a, b, start=st, stop=sp), [wtk] + hT_tk[tb * 4:tb * 4 + 4], [g.pst[bank]])
                    P.add("act", lambda e, o=dstT[:, hp, tb * 512:(tb + 1) * 512], i=g.ps[bank][:, :]: e.activation(out=o, in_=i, func=AF.Copy), [g.pst[bank]], [dtk[hp]])
                if which == 1:
                    bank = it % 2
                    it += 1
                    for k in range(NK):
                        P.add("pe", lambda e, o=g.ps[bank][:, 0:256], a=w[:, k, :], b=hcT[:, k, :], st=(k == 0), sp=(k == NK - 1):
                              e.matmul(o, a, b, start=st, stop=sp), [wtk] + hc_tk, [g.pst[bank]])
                    P.add("act", lambda e, o=kcT[:, hp, :], i=g.ps[bank][:, 0:256]: e.activation(out=o, in_=i, func=AF.Copy), [g.pst[bank]], [kc_tk[hp]])
        wv = [wunit(3072 + q * 256 + j * 128, "act" if j == 0 else "dve") for j in range(2)]
        for t in range(NT + 2):
            bank = it % 2
            it += 1
            for j in range(2):
                w, wtk = wv[j]
                for k in range(NK):
                    if t < NT:
                        a_, rtk = hT[:, k, t * 128:(t + 1) * 128], [hT_tk[t]]
                    else:
                        a_, rtk = hcT[:, k, (t - NT) * 128:(t - NT + 1) * 128], [hc_tk[t - NT]]
                    P.add("pe", lambda e, o=g.ps[bank][:, j * 128:(j + 1) * 128], a=a_, b=w[:, k, :], st=(k == 0), sp=(k == NK - 1):
                          e.matmul(o, a, b, start=st, stop=sp), [wtk] + rtk, [g.pst[bank]])
            pv = g.ps[bank][:, 0:256].rearrange("p (h d) -> p h d", h=4)
            if t < NT:
                P.add("dve", lambda e, o=Vq[:, t, :, 0:64], i=pv: e.tensor_copy(out=o, in_=i), [g.pst[bank]], [V_tk[t]])
            else:
                P.add("dve", lambda e, o=vc[:, t - NT, :, 0:64], i=pv: e.tensor_copy(out=o, in_=i), [g.pst[bank]], [vc_tk[t - NT]])
        pend = None
        for h4 in range(4):
            hh = q * 4 + h4
            hp, po = h4 // 2, (h4 % 2) * 64
            tb_, tbtk = tab[hh % 2], tab_tk[hh % 2]
            P.add("sp", lambda e, o=tb_, s_=g.rpbt[hh]: e.dma_start(out=o, in_=s_), [], [tbtk], dma=True)
            for i in range(NT):
                if 2 <= i <= 13:
                    chunks = [i + 2, i + 1, i, i - 1, i - 2]
                    tcol = 0
                elif i < 2:
                    chunks = [3, 2, 1, 0]
                    tcol = 640 + (1 + 2 * i) * 64
                else:
                    chunks = [15, 14, 13, 12]
                    tcol = 640 + (7 - 2 * (15 - i)) * 64
                nch = len(chunks)
                r = it % 2
                it += 1
                banks = (0, 1) if r == 0 else (2, 3)
                qs = QT[po:po + 64, hp, i * 128:(i + 1) * 128]

                def sblk(ci):
                    return g.ps[banks[ci // 4]][:, (ci % 4) * 128:(ci % 4 + 1) * 128], g.pst[banks[ci // 4]]
                for ci, j in enumerate(chunks):
                    o_, otk = sblk(ci)
                    P.add("pe", lambda e, o=o_, a=KT[po:po + 64, hp, j * 128:(j + 1) * 128], b=qs: e.matmul(o, a, b, start=True, stop=True),
                          [KT_tk[hp], QT_tk[hp]], [otk])
                for cj in range(2):
                    o_, otk = sblk(nch + cj)
                    P.add("pe", lambda e, o=o_, a=kcT[po:po + 64, hp, cj * 128:(cj + 1) * 128], b=qs: e.matmul(o, a, b, start=True, stop=True),
                          [kc_tk[hp], QT_tk[hp]], [otk])
                P.add("dve", lambda e, o=tmpb[r][:, 0:512], i=g.ps[banks[0]][:, :], t_=tb_[:, tcol:tcol + 512]:
                      e.scalar_tensor_tensor(out=o, in0=i, scalar=0.125, in1=t_, op0=ALU.mult, op1=ALU.add), [g.pst[banks[0]], tbtk], [tmp_tk[r]])
                if nch == 5:
                    P.add("dve", lambda e, o=tmpb[r][:, 512:640], i=g.ps[banks[1]][:, 0:128], t_=tb_[:, tcol + 512:tcol + 640]:
                          e.scalar_tensor_tensor(out=o, in0=i, scalar=0.125, in1=t_, op0=ALU.mult, op1=ALU.add), [g.pst[banks[1]], tbtk], [tmp_tk[r]])
                P.add("act", lambda e, o=Pb[r][:, 0:nch * 128], i=tmpb[r][:, 0:nch * 128]: e.activation(out=o, in_=i, func=AF.Exp), [tmp_tk[r]], [Pb_tk[r]])
                c0 = (nch % 4) * 128
                P.add("act", lambda e, o=Pb[r][:, nch * 128:(nch + 2) * 128], i=g.ps[banks[1]][:, c0:c0 + 256]: e.activation(out=o, in_=i, func=AF.Exp, scale=0.125),
                      [g.pst[banks[1]]], [Pb_tk[r]])
                cur = (h4, i, chunks, r)
                if pend is not None:
                    _attn_pv(g, pend, Pb, Pb_tk, Vq, V_tk, vc, vc_tk, Otok, Otok_tk, rec, rec_tk)
                pend = cur
        _attn_pv(g, pend, Pb, Pb_tk, Vq, V_tk, vc, vc_tk, Otok, Otok_tk, rec, rec_tk)
        for i in range(NT):
            for hp in range(2):
                bank = 6 + (i * 2 + hp) % 2
                P.add("pe", lambda e, o=g.ps[bank][:, 0:128], a=Otok[:, i, hp * 128:(hp + 1) * 128]: e.transpose(o, a, g.ident32), [Otok_tk[i], g.tk_pers], [g.pst[bank]])
                if hp == 0:
                    P.add("act", lambda e, o=OT[:, q * 2 + hp, i * 128:(i + 1) * 128], i_=g.ps[bank][:, 0:128]: e.activation(out=o, in_=i_, func=AF.Copy), [g.pst[bank]], [OT_tk])
                else:
                    P.add("dve", lambda e, o=OT[:, q * 2 + hp, i * 128:(i + 1) * 128], i_=g.ps[bank][:, 0:128]: e.tensor_copy(out=o, in_=i_), [g.pst[bank]], [OT_tk])
    P.barrier()
    out_proj(g, OT, OT_tk, 8, g.w_out0[1024:2048, :], g.xs[2], xdst, g.modT[l][:, 2, :], None, 24576)


def _attn_pv(g, item, Pb, Pb_tk, Vq, V_tk, vc, vc_tk, Otok, Otok_tk, rec, rec_tk):
    P = g.P
    h4, i, chunks, r = item
    nch = len(chunks)
    bank = 4 + r
    o_ = g.ps[bank][:, 0:65]
    n = nch + 2
    for ci, j in enumerate(chunks):
        P.add("pe", lambda e, a=Pb[r][:, ci * 128:(ci + 1) * 128], b=Vq[:, j, h4, :], st=(ci == 0): e.matmul(o_, a, b, start=st, stop=False),
              [Pb_tk[r], V_tk[j]], [g.pst[bank]])
    for cj in range(2):
        P.add("pe", lambda e, a=Pb[r][:, (nch + cj) * 128:(nch + cj + 1) * 128], b=vc[:, cj, h4, :], sp=(cj == 1): e.matmul(o_, a, b, start=False, stop=sp),
              [Pb_tk[r], vc_tk[cj]], [g.pst[bank]])
    P.add("dve", lambda e: e.reciprocal(out=rec[r], in_=g.ps[bank][:, 64:65]), [g.pst[bank]], [rec_tk[r]])
    P.add("act", lambda e, o=Otok[:, i, h4 * 64:(h4 + 1) * 64]: e.activation(out=o, in_=g.ps[bank][:, 0:64], func=AF.Copy, scale=rec[r]),
          [g.pst[bank], rec_tk[r]], [Otok_tk[i]])
```

```python
import numpy as np
import ml_dtypes
import concourse.bass as bass
import concourse.mybir as mybir
from concourse.bass_utils import run_bass_kernel_spmd

F32 = mybir.dt.float32
BF16 = mybir.dt.bfloat16
AF = mybir.ActivationFunctionType
ALU = mybir.AluOpType
AX = mybir.AxisListType

D = 2048
S = 2048
NT = 16
NK = 16
CTX = 256
NE = 16
FE = 512
EPS = 1e-6
NEG = -30000.0
HOT = 16


class Tk:
    __slots__ = ("w", "r")

    def __init__(self):
        self.w = None
        self.r = {}


class Op:
    __slots__ = ("eng", "fn", "deps", "inc", "dma", "sem", "val", "gidx", "region", "outer")

    def __init__(self, eng, fn, dma):
        self.eng = eng
        self.fn = fn
        self.dma = dma
        self.deps = set()
        self.inc = False
        self.sem = None
        self.val = 0
        self.gidx = 0
        self.region = None
        self.outer = None


class Prog:
    ENGS = ("pe", "act", "dve", "pool", "sp")
    SEG = 10 ** 9
    NDS = 28

    def __init__(self):
        self.ops = {e: [] for e in self.ENGS}
        self.all_dma = []
        self.bar = None
        self.bar_seen = set()
        self.region = None
        self.outer = None
        self.regs = {}
        self.regs2 = {}

    def add(self, eng, fn, reads=(), writes=(), dma=False):
        op = Op(eng, fn, dma)
        op.region = self.region
        op.outer = self.outer
        deps = set()
        for t in reads:
            if t.w is not None:
                deps.add(t.w)
        for t in writes:
            if t.w is not None:
                deps.add(t.w)
            for o in t.r.values():
                if isinstance(o, list):
                    deps.update(o)
                else:
                    deps.add(o)
        if self.bar is not None and eng not in self.bar_seen:
            deps.update(self.bar)
            self.bar_seen.add(eng)
        for t in reads:
            if dma:
                t.r.setdefault("dma", []).append(op)
            else:
                t.r[eng] = op
        for t in writes:
            t.w = op
            t.r = {}
        deps.discard(op)
        if eng == "pe" and not dma:
            deps = {d for d in deps if not (d.eng == "pe" and not d.dma)}
        op.deps = deps
        for d in deps:
            d.inc = True
        self.ops[eng].append(op)
        if dma:
            self.all_dma.append(op)
        return op

    def barrier(self):
        deps = []
        for e in self.ENGS:
            for o in reversed(self.ops[e]):
                if not o.dma:
                    deps.append(o)
                    break
        deps.extend(self.all_dma)
        self.all_dma = []
        self.bar = deps
        self.bar_seen = set()

    def run_emit(self, nc, block, handles, sems):
        si = 0
        for e in self.ENGS:
            cnt = 0
            cur = None
            for o in self.ops[e]:
                if o.dma or not o.inc:
                    continue
                if cnt % self.SEG == 0:
                    cur = sems[si]
                    si += 1
                o.sem = cur
                o.val = cnt % self.SEG + 1
                o.gidx = cnt + 1
                cnt += 1
        dsems = sems[si:si + self.NDS]
        assert len(dsems) == self.NDS, "not enough semaphores"
        dcount = [0] * self.NDS
        k = 0
        for o in self.dma_order:
            s = k % self.NDS
            o.sem = dsems[s]
            o.gidx = (s, dcount[s])
            dcount[s] += 16
            o.val = dcount[s]
            k += 1

        def emit_engine(ename):
            def body(e):
                waited = {}

                def emit_op(o):
                    for d in o.deps:
                        key = ("d", id(d.sem)) if d.dma else (d.eng, id(d.sem))
                        need = d.val
                        if waited.get(key, 0) >= need:
                            continue
                        e.wait_ge(d.sem, need)
                        waited[key] = need
                    if o.dma:
                        slot, prev = o.gidx
                        key = ("d", id(o.sem))
                        if prev > 0 and waited.get(key, 0) < prev:
                            e.wait_ge(o.sem, prev)
                            waited[key] = prev
                        ins = o.fn(e)
                        ins.then_inc(o.sem, 16)
                    else:
                        if o.fn is None:
                            return
                        ins = o.fn(e)
                        if o.inc:
                            ins.then_inc(o.sem, 1)

                ops = self.ops[ename]

                def else_bulk(grp):
                    ninc = sum(1 for q in grp if (not q.dma) and q.inc)
                    if ninc:
                        csem = [q.sem for q in grp if (not q.dma) and q.inc][0]
                        e.drain().then_inc(csem, ninc)
                    for q in grp:
                        if q.dma:
                            slot, prev = q.gidx
                            if prev > 0:
                                e.wait_ge(q.sem, prev)
                            e.sem_inc(q.sem, 16)

                def emit_range(byslot, lo, hi):
                    grp = [q for s in range(lo, hi) for q in byslot.get(s, [])]
                    if not grp:
                        return
                    saved = dict(waited)
                    with e.If_lt(self.regs[ename], -lo):
                        if hi - lo == 1:
                            for q in grp:
                                emit_op(q)
                        else:
                            mid = (lo + hi) // 2
                            emit_range(byslot, lo, mid)
                            emit_range(byslot, mid, hi)
                    with e.Else():
                        waited.clear()
                        waited.update(saved)
                        else_bulk(grp)
                    waited.clear()
                    waited.update(saved)

                def emit_list(lst):
                    i = 0
                    while i < len(lst):
                        o = lst[i]
                        if o.region is None:
                            emit_op(o)
                            i += 1
                            continue
                        uid = o.region[0]
                        j = i
                        byslot = {}
                        while j < len(lst) and lst[j].region is not None and lst[j].region[0] == uid:
                            byslot.setdefault(lst[j].region[1], []).append(lst[j])
                            j += 1
                        emit_range(byslot, min(byslot), 16)
                        i = j

                i = 0
                while i < len(ops):
                    o = ops[i]
                    if o.outer is None:
                        j = i
                        while j < len(ops) and ops[j].outer is None:
                            j += 1
                        emit_list(ops[i:j])
                        i = j
                        continue
                    ou = o.outer
                    j = i
                    while j < len(ops) and ops[j].outer is ou:
                        j += 1
                    grp = ops[i:j]
                    saved = dict(waited)
                    with e.If_lt(self.regs2[ename], -ou[1]):
                        emit_list(grp)
                    with e.Else():
                        waited.clear()
                        waited.update(saved)
                        else_bulk(grp)
                    waited.clear()
                    waited.update(saved)
                    i = j
            return body

        block.tensor(emit_engine("pe"))
        block.scalar(emit_engine("act"))
        block.vector(emit_engine("dve"))
        block.gpsimd(emit_engine("pool"))
        block.sync(emit_engine("sp"))


def _mk_prog():
    p = Prog()
    p.dma_order = []
    _add = p.add

    def add(eng, fn, reads=(), writes=(), dma=False):
        o = _add(eng, fn, reads, writes, dma)
        if dma:
            p.dma_order.append(o)
        return o
    p.add = add
    return p


class Ctx:
    pass


def build_program(stages=(0, 1, 2, 3, 4), dbg=False, sparse=True):
    nc = bass.Bass("TRN2", target_bir_lowering=False)
    P = _mk_prog()
    g = Ctx()
    g.nc, g.P = nc, P

    def din(name, shape, dt=F32):
        return nc.dram_tensor(name, list(shape), dt, kind="ExternalInput").ap()

    g.x = din("x", [S, D])
    g.ctx = din("ctx", [CTX, D])
    g.cT = din("cT", [128, NK, 2])
    g.ada_w = din("ada_w", [2, D, 6 * D])
    g.ada_b4 = din("ada_b4", [2, 4, 6 * D])
    g.cmb = din("cmb", [4, 2])
    g.gT = din("gT", [128, 4, NK])
    g.fng = din("fng", [128, D])
    g.w_in0 = din("w_in0", [D, 4096])
    g.w_out0 = din("w_out0", [D, D])
    g.rpbt = din("rpbt", [16, 128, 1664])
    g.csc = din("csc", [128, 2, 512], BF16)
    g.dft = din("dft", [2, 4, 128, NK, 512], BF16)
    g.w_in1 = din("w_in1", [D, 4096])
    g.cvp = din("cvp", [128, 6, NK])
    g.dww = din("dww", [128, NK, 31])
    g.w_out1 = din("w_out1", [D, D])
    g.bout = din("bout", [128, D])
    g.rw = din("rw", [128, NK, NE])
    g.rb = din("rb", [128, NE])
    g.wg = din("wg", [2, NE, D, FE])
    g.wu = din("wu", [2, NE, D, FE])
    g.wd = din("wd", [2, NE, FE, D])
    g.ident = din("ident", [128, 128])
    g.out = nc.dram_tensor("out", [S, D], F32, kind="ExternalOutput").ap()
    kind = "ExternalOutput" if dbg else "Internal"
    g.xs = [nc.dram_tensor("xs%d" % i, [S, D], F32, kind=kind).ap() for i in range(3)]
    g.cst = din("cst", [128, 160])
    g.xg_all = nc.dram_tensor("xg_all", [NE * 2048, D], BF16, kind="Internal").ap()
    g.yg_all = nc.dram_tensor("yg_all", [NE * 2048, D], BF16, kind="Internal").ap()
    g.xg_tk, g.yg_tk = Tk(), Tk()

    ARENA = 51456
    with (
        nc.sbuf_tensor("arena", [128, ARENA], F32) as arena,
        nc.sbuf_tensor("pers", [128, 1128], F32) as pers,
        nc.psum_tensor("ps0", [128, 512], F32) as ps0, nc.psum_tensor("ps1", [128, 512], F32) as ps1,
        nc.psum_tensor("ps2", [128, 512], F32) as ps2, nc.psum_tensor("ps3", [128, 512], F32) as ps3,
        nc.psum_tensor("ps4", [128, 512], F32) as ps4, nc.psum_tensor("ps5", [128, 512], F32) as ps5,
        nc.psum_tensor("ps6", [128, 512], F32) as ps6, nc.psum_tensor("ps7", [128, 512], F32) as ps7,
    ):
        g.arena = arena
        g.ps = [ps0, ps1, ps2, ps3, ps4, ps5, ps6, ps7]
        g.pst = [Tk() for _ in range(8)]
        g.pers = pers
        g.ident32 = pers[:, 0:128]
        g.modT = [pers[:, 128:224].rearrange("p (j k) -> p j k", j=6), pers[:, 224:320].rearrange("p (j k) -> p j k", j=6)]
        g.modcT = pers[:, 320:352].rearrange("p (j k) -> p j k", j=2)
        g.gTs = pers[:, 352:416].rearrange("p (j k) -> p j k", j=4)
        g.AB = pers[:, 416:480].rearrange("p (j k) -> p j k", j=4)
        g.ones32 = pers[:, 480:608]
        g.selA = pers[0:2, 608:736]
        g.cTs = pers[:, 736:768].rearrange("p (k c) -> p k c", c=2)
        g.small = pers[:, 768:1128]
        g.tk_pers = Tk()
        g.tk_mod = Tk()
        g.tk_AB = Tk()

        from_stage = {}
        setup_consts(g)
        if 0 in stages:
            stage_ada(g)
        P.barrier()
        if 1 in stages:
            stage_mixer0(g, g.x, g.xs[0])
            P.barrier()
        if 2 in stages:
            (stage_moe_sparse if sparse else stage_moe)(g, 0, g.xs[0] if 1 in stages else g.x, g.xs[1], final=False)
            P.barrier()
        if 3 in stages:
            stage_conv(g, g.xs[1] if 2 in stages else g.x, g.xs[2])
            P.barrier()
        if 4 in stages:
            (stage_moe_sparse if sparse else stage_moe)(g, 1, g.xs[2] if 3 in stages else g.x, g.out, final=True)
            P.barrier()
        P.add("sp", None)

        nsem = 100
        import contextlib
        with contextlib.ExitStack() as st:
            sems = [st.enter_context(nc.semaphore("s%d" % i)) for i in range(nsem)]
            P.regs = {"pe": st.enter_context(nc.tensor.register("r_pe")), "act": st.enter_context(nc.scalar.register("r_act")),
                      "dve": st.enter_context(nc.vector.register("r_dve")), "pool": st.enter_context(nc.gpsimd.register("r_pool")),
                      "sp": st.enter_context(nc.sync.register("r_sp"))}
            P.regs2 = {"pe": st.enter_context(nc.tensor.register("r2_pe")), "act": st.enter_context(nc.scalar.register("r2_act")),
                       "dve": st.enter_context(nc.vector.register("r2_dve")), "pool": st.enter_context(nc.gpsimd.register("r2_pool")),
                       "sp": st.enter_context(nc.sync.register("r2_sp"))}
            block = st.enter_context(nc.Block())
            P.run_emit(nc, block, None, sems)
    return nc


def arena_f32(g, off, n):
    return g.arena[:, off:off + n]


def arena_bf(g, off, n):
    return g.arena[:, off:off + n // 2].bitcast(BF16)


def setup_consts(g):
    P = g.P
    tk = g.tk_pers
    P.add("sp", lambda e: e.dma_start(out=g.ident32, in_=g.ident), [], [tk], dma=True)
    P.add("sp", lambda e: e.dma_start(out=g.gTs, in_=g.gT), [], [tk], dma=True)
    P.add("sp", lambda e: e.dma_start(out=g.cTs, in_=g.cT), [], [tk], dma=True)
    P.add("pool", lambda e: e.memset(g.ones32, 1.0), [], [tk])
    P.add("pool", lambda e: e.memset(g.selA, 0.0), [], [tk])
    P.add("pool", lambda e: e.memset(g.pers[0:1, 608:736], 1.0), [], [tk])


def stage_ada(g):
    P = g.P
    NR = 8
    wr = [arena_bf(g, i * 1024, 2048).rearrange("p (k n) -> p k n", k=4) for i in range(NR)]
    wr_tk = [Tk() for _ in range(NR)]
    bias = [g.arena[0:4, 8192 + i * 512: 8192 + (i + 1) * 512] for i in range(2)]
    bias_tk = [Tk(), Tk()]
    mrow = [g.arena[0:4, 9216 + i * 512: 9216 + (i + 1) * 512] for i in range(2)]
    mrow_tk = [Tk(), Tk()]
    sT = g.small[:, 0:32].rearrange("p (k c) -> p k c", c=2)
    s4 = arena_bf(g, 10240, 64).rearrange("p (k c) -> p k c", c=4)
    hi32 = arena_f32(g, 10304, 32).rearrange("p (k c) -> p k c", c=2)
    cmb = g.arena[0:4, 10400:10402]
    tk_s = Tk()
    P.add("act", lambda e: e.activation(out=sT, in_=g.cTs, func=AF.Silu), [g.tk_pers], [tk_s])
    s4v = s4.rearrange("p k (c h) -> p k c h", h=2)
    P.add("dve", lambda e: e.tensor_copy(out=s4v[:, :, :, 0], in_=sT), [tk_s], [tk_s])
    P.add("dve", lambda e: e.tensor_copy(out=hi32, in_=s4v[:, :, :, 0]), [tk_s], [tk_s])
    P.add("dve", lambda e: e.tensor_tensor(out=s4v[:, :, :, 1], in0=sT, in1=hi32, op=ALU.subtract), [tk_s], [tk_s])
    P.add("sp", lambda e: e.dma_start(out=cmb, in_=g.cmb), [], [tk_s], dma=True)
    u = 0
    for l in range(2):
        for nb in range(24):
            j, q = divmod(nb, 4)
            pm = g.ps[nb % 2][0:4, :]
            pm_tk = g.pst[nb % 2]
            for kk in range(4):
                slot = u % NR
                u += 1
                src_ = g.ada_w[l, kk * 512:(kk + 1) * 512, nb * 512:(nb + 1) * 512].rearrange("(k p) n -> p k n", p=128)
                P.add("pool", lambda e, o=wr[slot], s=src_: e.dma_start(out=o, in_=s), [], [wr_tk[slot]], dma=True)
                for k4 in range(4):
                    k = kk * 4 + k4
                    P.add("pe", lambda e, o=pm, a=s4[:, k, :], b=wr[slot][:, k4, :], st=(k == 0), sp=(k == 15):
                          e.matmul(o, a, b, start=st, stop=sp), [tk_s, wr_tk[slot]], [pm_tk])
            bb = nb % 2
            P.add("sp", lambda e, o=bias[bb], s=g.ada_b4[l, :, nb * 512:(nb + 1) * 512]: e.dma_start(out=o, in_=s), [], [bias_tk[bb]], dma=True)
            P.add("dve", lambda e, o=mrow[bb], a=pm, b=bias[bb]: e.tensor_tensor(out=o, in0=a, in1=b, op=ALU.add),
                  [pm_tk, bias_tk[bb]], [mrow_tk[bb]])
            pt = g.ps[2 + bb][:, 0:8]
            pt_tk = g.pst[2 + bb]
            for qq in range(4):
                P.add("pe", lambda e, o=pt[:, qq * 2:qq * 2 + 2], i=mrow[bb][0:4, qq * 128:(qq + 1) * 128]:
                      e.matmul(o, i, cmb, start=True, stop=True), [mrow_tk[bb], tk_s], [pt_tk])
            ptv = pt.rearrange("p (q r) -> p q r", r=2)
            P.add("dve", lambda e, o=g.modT[l][:, j, q * 4:(q + 1) * 4], i=ptv[:, :, 0]: e.tensor_copy(out=o, in_=i),
                  [pt_tk], [g.tk_mod])
            if l == 0 and j < 2:
                P.add("dve", lambda e, o=g.modcT[:, j, q * 4:(q + 1) * 4], i=ptv[:, :, 1]: e.tensor_copy(out=o, in_=i),
                      [pt_tk], [g.tk_mod])


def prep_AB(g, gi, scaleT, shiftT, slot):
    P = g.P
    A = g.AB[:, slot, :]
    B = g.AB[:, slot + 1, :]
    P.add("dve", lambda e: e.scalar_tensor_tensor(out=A, in0=scaleT, scalar=1.0, in1=g.gTs[:, gi, :], op0=ALU.add, op1=ALU.mult),
          [g.tk_mod, g.tk_pers], [g.tk_AB])
    P.add("dve", lambda e: e.tensor_copy(out=B, in_=shiftT), [g.tk_mod], [g.tk_AB])
    return A, B


def make_bc(g, srcT, dst, dst_tk, tmp_off):
    P = g.P
    dg = [arena_f32(g, tmp_off + i * 128, 128) for i in range(2)]
    dg_tk = [Tk(), Tk()]
    for k in range(NK):
        s = k % 2
        P.add("pool", lambda e, o=dg[s], sc=srcT[:, k:k + 1]: e.tensor_scalar(out=o, in0=g.ident32, scalar1=sc, scalar2=None, op0=ALU.mult),
              [g.tk_mod, g.tk_pers, g.tk_AB], [dg_tk[s]])
        bank = 4 + (k // 4) % 2
        P.add("pe", lambda e, o=g.ps[bank][:, (k % 4) * 128:(k % 4 + 1) * 128], b=dg[s]: e.matmul(o, g.ones32, b, start=True, stop=True),
              [dg_tk[s], g.tk_pers], [g.pst[bank]])
        if k % 4 == 3:
            c0 = (k // 4) * 512
            P.add("act", lambda e, o=dst[:, c0:c0 + 512], i=g.ps[bank][:, :]: e.activation(out=o, in_=i, func=AF.Copy),
                  [g.pst[bank]], [dst_tk])


def norm_transpose(g, src, src_tk, A, B, ab_slot_reads, dstf, tmp, it, router=None, nodst=False, phase=0):
    P = g.P
    nr = len(tmp["xn"])
    r = it % nr
    ss, ss_tk = tmp["ss"][it % 2]
    sq, sq_tk = tmp["sq"][it % 2]
    xn, xn_tk = tmp["xn"][r]
    junk, junk_tk = tmp["junk"]
    if phase in (0, 1):
        P.add("dve", lambda e: e.memset(ss, 0.0), [], [ss_tk])
        P.add("act", lambda e: e.activation(out=junk, in_=src, func=AF.Square, accum_out=ss), [src_tk], [junk_tk, ss_tk])
        P.add("act", lambda e: e.activation(out=sq, in_=ss, func=AF.Sqrt, bias=tmp["eps"], scale=1.0 / D), [ss_tk, g.tk_pers], [sq_tk])
        P.add("dve", lambda e: e.reciprocal(out=sq, in_=sq), [sq_tk], [sq_tk])
        P.add("act", lambda e: e.activation(out=xn, in_=src, func=AF.Copy, scale=sq), [src_tk, sq_tk], [xn_tk])
    if phase == 1:
        return
    for b in range(4):
        for c in range(4):
            k = b * 4 + c
            P.add("pe", lambda e, o=g.ps[b][:, c * 128:(c + 1) * 128], i=xn[:, k * 128:(k + 1) * 128]: e.transpose(o, i, g.ident32),
                  [xn_tk, g.tk_pers], [g.pst[b]])
    t32s = []
    for b in range(4):
        if router is not None:
            t32, t32_tk = tmp["t32"][(it * 4 + b) % len(tmp["t32"])]
            t32s.append((t32, t32_tk))
        if not nodst:
            dst, dst_tk = dstf(b)
        for c in range(4):
            k = b * 4 + c
            pc = g.ps[b][:, c * 128:(c + 1) * 128]
            if router is not None:
                o_, wtk = t32[:, c * 128:(c + 1) * 128], t32_tk
            else:
                o_, wtk = dst[:, c, :], dst_tk
            if c % 2 == 0:
                P.add("act", lambda e, o=o_, i=pc, k=k: e.activation(out=o, in_=i, func=AF.Identity, scale=A[:, k:k + 1], bias=B[:, k:k + 1]), [g.pst[b], g.tk_AB], [wtk])
            else:
                P.add("dve", lambda e, o=o_, i=pc, k=k: e.tensor_scalar(out=o, in0=i, scalar1=A[:, k:k + 1], scalar2=B[:, k:k + 1], op0=ALU.mult, op1=ALU.add), [g.pst[b], g.tk_AB], [wtk])
        if router is not None and not nodst:
            P.add("act", lambda e, o=dst, i=t32.rearrange("p (c n) -> p c n", c=4): e.activation(out=o, in_=i, func=AF.Copy), [t32_tk], [dst_tk])
    if router is not None:
        lg, lg_tk, rws, rw_tk = router
        for b in range(4):
            t32, t32_tk = t32s[b]
            for c in range(4):
                k = b * 4 + c
                P.add("pe", lambda e, o=lg, a=t32[:, c * 128:(c + 1) * 128], w=rws[:, k, :], st=(k == 0), sp=(k == 15):
                      e.matmul(o, a, w, start=st, stop=sp), [t32_tk, rw_tk], [lg_tk])


def stage_moe(g, l, xsrc, xdst, final):
    P = g.P
    A, B = prep_AB(g, 2 * l + 1, g.modT[l][:, 4, :], g.modT[l][:, 3, :], 0)
    ACC, HT, W0 = 0, 16384, 24576
    STG, GU, DN, AT0, CBC, G2, TMP, COMBT, SELE = 24576, 28672, 32768, 40960, 45056, 46080, 48128, 50176, 51200
    acc = [arena_f32(g, ACC + t * 2048, 2048) for t in range(8)]
    acc_tk = [Tk() for _ in range(8)]
    hT = arena_bf(g, HT, 16384).rearrange("p (k n) -> p k n", k=NK)
    hT_tk = [Tk() for _ in range(8)]
    g2bc = arena_f32(g, G2, 2048)
    g2_tk = Tk()
    make_bc(g, g.modT[l][:, 5, :], g2bc, g2_tk, TMP)
    rws = g.small[:, 64:64 + 256].rearrange("p (k e) -> p k e", e=NE)
    rw_tk = Tk()
    rbs = g.small[:, 320:336]
    eps = g.small[:, 336:337]
    P.add("sp", lambda e: e.dma_start(out=rws, in_=g.rw), [], [rw_tk], dma=True)
    P.add("sp", lambda e: e.dma_start(out=rbs, in_=g.rb), [], [rw_tk], dma=True)
    P.add("pool", lambda e: e.memset(eps, EPS), [], [g.tk_pers])
    for tb in range(2):
        tmp = {
            "ss": [(g.small[:, 340 + i:341 + i], Tk()) for i in range(2)],
            "sq": [(g.small[:, 344 + i:345 + i], Tk()) for i in range(2)],
            "xn": [(arena_f32(g, W0 + i * 2048, 2048), Tk()) for i in range(2)],
            "junk": (arena_bf(g, W0 + 4096, 2048), Tk()),
            "t32": [(arena_f32(g, W0 + 5120 + i * 512, 512), Tk()) for i in range(2)],
            "eps": eps,
        }
        lgps = g.ps[6]
        lg_tk = g.pst[6]
        for t in range(8):
            i = tb * 8 + t
            P.add("sp", lambda e, o=acc[t], s=xsrc[i * 128:(i + 1) * 128, :]: e.dma_start(out=o, in_=s), [], [acc_tk[t]], dma=True)
            norm_transpose(g, acc[t], acc_tk[t], A, B, None,
                           lambda b, t=t: (hT[:, b * 4:(b + 1) * 4, t * 128:(t + 1) * 128], hT_tk[t]),
                           tmp, t, router=(lgps[:, t * 16:(t + 1) * 16], lg_tk, rws, rw_tk))
        RB = W0 + 6144

        def rt(n, w):
            return arena_f32(g, RB + n * 128, w)
        sc, sel, eq, msk, w_, comb = rt(0, 128), rt(1, 128), rt(2, 128), rt(3, 128), rt(4, 128), rt(5, 128)
        m1, m2, gs, ing = rt(6, 32), rt(7, 32), rt(8, 32), rt(9, 32)
        gmax, wsum = rt(10, 8), rt(11, 8)
        rtk = Tk()

        def v3(a, x, y):
            return a.rearrange("p (x y) -> p x y", x=x)
        P.add("act", lambda e: e.activation(out=sc, in_=lgps[:, 0:128], func=AF.Sigmoid), [lg_tk], [rtk])
        P.add("dve", lambda e: e.tensor_tensor(out=v3(sel, 8, 16), in0=v3(sc, 8, 16), in1=rbs.unsqueeze(1).to_broadcast([128, 8, 16]), op=ALU.add), [rtk, rw_tk], [rtk])
        P.add("dve", lambda e: e.tensor_reduce(out=m1, in_=v3(sel, 32, 4), axis=AX.X, op=ALU.max), [rtk], [rtk])
        P.add("dve", lambda e: e.tensor_tensor(out=v3(eq, 32, 4), in0=v3(sel, 32, 4), in1=m1.unsqueeze(2).to_broadcast([128, 32, 4]), op=ALU.is_equal), [rtk], [rtk])
        P.add("dve", lambda e: e.scalar_tensor_tensor(out=msk, in0=eq, scalar=-1e9, in1=sel, op0=ALU.mult, op1=ALU.add), [rtk], [rtk])
        P.add("dve", lambda e: e.tensor_reduce(out=m2, in_=v3(msk, 32, 4), axis=AX.X, op=ALU.max), [rtk], [rtk])
        P.add("dve", lambda e: e.tensor_tensor(out=gs, in0=m1, in1=m2, op=ALU.add), [rtk], [rtk])
        P.add("dve", lambda e: e.tensor_reduce(out=gmax, in_=v3(gs, 8, 4), axis=AX.X, op=ALU.max), [rtk], [rtk])
        P.add("dve", lambda e: e.tensor_tensor(out=v3(ing, 8, 4), in0=v3(gs, 8, 4), in1=gmax.unsqueeze(2).to_broadcast([128, 8, 4]), op=ALU.is_equal), [rtk], [rtk])
        P.add("dve", lambda e: e.tensor_tensor(out=v3(eq, 32, 4), in0=v3(sel, 32, 4), in1=m2.unsqueeze(2).to_broadcast([128, 32, 4]), op=ALU.is_ge), [rtk], [rtk])
        P.add("dve", lambda e: e.tensor_tensor(out=v3(msk, 32, 4), in0=v3(eq, 32, 4), in1=ing.unsqueeze(2).to_broadcast([128, 32, 4]), op=ALU.mult), [rtk], [rtk])
        P.add("dve", lambda e: e.tensor_tensor(out=w_, in0=sc, in1=msk, op=ALU.mult), [rtk], [rtk])
        P.add("dve", lambda e: e.tensor_reduce(out=wsum, in_=v3(w_, 8, 16), axis=AX.X, op=ALU.add), [rtk], [rtk])
        P.add("dve", lambda e: e.reciprocal(out=wsum, in_=wsum), [rtk], [rtk])
        P.add("dve", lambda e: e.tensor_tensor(out=v3(comb, 8, 16), in0=v3(w_, 8, 16), in1=wsum.unsqueeze(2).to_broadcast([128, 8, 16]), op=ALU.mult), [rtk], [rtk])
        combT = g.arena[0:16, COMBT: COMBT + 1024]
        combT_tk = Tk()
        for half in range(2):
            bank = 4 + half
            for t4 in range(4):
                t = half * 4 + t4
                P.add("pe", lambda e, o=g.ps[bank][0:16, t4 * 128:(t4 + 1) * 128], i=comb[:, t * 16:(t + 1) * 16]: e.transpose(o, i, g.ident32),
                      [rtk, g.tk_pers], [g.pst[bank]])
            P.add("act", lambda e, o=combT[:, half * 512:(half + 1) * 512], i=g.ps[bank][0:16, :]: e.activation(out=o, in_=i, func=AF.Copy),
                  [g.pst[bank]], [combT_tk])
        sel2 = [g.arena[0:16, SELE + i * 128: SELE + (i + 1) * 128] for i in range(2)]
        sel2_tk = [Tk(), Tk()]
        P.barrier()
        stg = [arena_f32(g, STG + i * 2048, 2048) for i in range(2)]
        stg_tk = [Tk() for _ in range(2)]
        gub = [arena_bf(g, GU + i * 1024, 2048) for i in range(4)]
        gu_tk = [Tk() for _ in range(4)]
        dnb = [arena_bf(g, DN + i * 1024, 2048) for i in range(8)]
        dn_tk = [Tk() for _ in range(8)]
        ATb = [arena_bf(g, AT0 + i * 2048, 4096).rearrange("p (f n) -> p f n", f=4) for i in range(2)]
        AT_tk = [Tk(), Tk()]
        cbc = [arena_bf(g, CBC + i * 512, 1024) for i in range(2)]
        cbc_tk = [Tk(), Tk()]
        sgt = [arena_f32(g, TMP + i * 512, 512) for i in range(2)]
        sg_tk = [Tk(), Tk()]
        t2t = [arena_f32(g, TMP + 1024 + i * 512, 512) for i in range(2)]
        t2_tk = [Tk(), Tk()]
        units = []
        for ex in range(NE):
            for f in range(4):
                units.append(("g", ex, f))
                units.append(("u", ex, f))
                units.append(("d", ex, f))
        state = {"dma": 0, "cast": 0}

        def unit_src(u):
            kind, ex, f = u
            if kind == "g":
                return g.wg[l, ex, :, f * 128:(f + 1) * 128].rearrange("(k p) n -> p k n", p=128)
            if kind == "u":
                return g.wu[l, ex, :, f * 128:(f + 1) * 128].rearrange("(k p) n -> p k n", p=128)
            return g.wd[l, ex, f * 128:(f + 1) * 128, :]

        def unit_dst(n):
            kind, ex, f = units[n]
            if kind == "d":
                s = (ex % 2) * 4 + f
                return dnb[s], dn_tk[s]
            s = (2 * (ex * 4 + f) + (1 if kind == "u" else 0)) % 4
            return gub[s], gu_tk[s]

        def issue_dma(upto):
            while state["dma"] < min(upto, len(units)):
                n = state["dma"]
                kind = units[n][0]
                s = n % 2
                o = stg[s] if kind == "d" else stg[s].rearrange("p (k n) -> p k n", k=NK)
                P.add("sp", lambda e, o=o, sr=unit_src(units[n]): e.dma_start(out=o, in_=sr), [], [stg_tk[s]], dma=True)
                state["dma"] += 1

        def issue_cast(upto):
            while state["cast"] < min(upto, len(units)):
                n = state["cast"]
                issue_dma(n + 2)
                kind = units[n][0]
                s = n % 2
                dst, dtk = unit_dst(n)
                if kind == "d":
                    P.add("pool", lambda e, o=dst, i=stg[s]: e.tensor_tensor(out=o, in0=i, in1=g2bc, op=ALU.mult), [stg_tk[s], g2_tk], [dtk])
                elif kind == "g":
                    P.add("act", lambda e, o=dst, i=stg[s]: e.activation(out=o, in_=i, func=AF.Copy), [stg_tk[s]], [dtk])
                else:
                    P.add("dve", lambda e, o=dst, i=stg[s]: e.tensor_copy(out=o, in_=i), [stg_tk[s]], [dtk])
                state["cast"] += 1

        issue_cast(6)
        it = 0
        for ex in range(NE):
            c = ex % 2
            P.add("pool", lambda e, o=sel2[c], i=g.ident32[0:16, ex:ex + 1].to_broadcast([16, 128]): e.tensor_copy(out=o, in_=i), [g.tk_pers], [sel2_tk[c]])
            for sb in range(2):
                bank = 4 + sb
                P.add("pe", lambda e, o=g.ps[bank][:, :], a=sel2[c], b=combT[:, sb * 512:(sb + 1) * 512]: e.matmul(o, a, b, start=True, stop=True),
                      [sel2_tk[c], combT_tk], [g.pst[bank]])
                P.add("act", lambda e, o=cbc[c][:, sb * 512:(sb + 1) * 512], i=g.ps[bank][:, :]: e.activation(out=o, in_=i, func=AF.Copy),
                      [g.pst[bank]], [cbc_tk[c]])
            for f in range(4):
                n0 = (ex * 4 + f) * 3
                issue_cast(n0 + 6)
                gw, gtk = unit_dst(n0)
                uw, utk = unit_dst(n0 + 1)
                gw3 = gw.rearrange("p (k n) -> p k n", k=NK)
                uw3 = uw.rearrange("p (k n) -> p k n", k=NK)
                for sb in range(2):
                    bg, bu = (0, 1) if it % 2 == 0 else (2, 3)
                    for k in range(NK):
                        P.add("pe", lambda e, o=g.ps[bg][:, :], a=gw3[:, k, :], b=hT[:, k, sb * 512:(sb + 1) * 512], st=(k == 0), sp=(k == NK - 1):
                              e.matmul(o, a, b, start=st, stop=sp), [gtk] + hT_tk[sb * 4:sb * 4 + 4], [g.pst[bg]])
                    for k in range(NK):
                        P.add("pe", lambda e, o=g.ps[bu][:, :], a=uw3[:, k, :], b=hT[:, k, sb * 512:(sb + 1) * 512], st=(k == 0), sp=(k == NK - 1):
                              e.matmul(o, a, b, start=st, stop=sp), [utk] + hT_tk[sb * 4:sb * 4 + 4], [g.pst[bu]])
                    r = it % 2
                    P.add("act", lambda e, o=sgt[r], i=g.ps[bg][:, :]: e.activation(out=o, in_=i, func=AF.Silu), [g.pst[bg]], [sg_tk[r]])
                    P.add("dve", lambda e, o=t2t[r], a=g.ps[bu][:, :], b=sgt[r]: e.tensor_tensor(out=o, in0=a, in1=b, op=ALU.mult), [g.pst[bu], sg_tk[r]], [t2_tk[r]])
                    P.add("pool", lambda e, o=ATb[c][:, f, sb * 512:(sb + 1) * 512], a=t2t[r], b=cbc[c][:, sb * 512:(sb + 1) * 512]:
                          e.tensor_tensor(out=o, in0=a, in1=b, op=ALU.mult), [t2_tk[r], cbc_tk[c]], [AT_tk[c]])
                    it += 1
            for t in range(8):
                for db in range(4):
                    bank = 6 + (t * 4 + db) % 2
                    for f in range(4):
                        dw, dtk = dnb[(ex % 2) * 4 + f], dn_tk[(ex % 2) * 4 + f]
                        P.add("pe", lambda e, o=g.ps[bank][:, :], a=ATb[c][:, f, t * 128:(t + 1) * 128], b=dw[:, db * 512:(db + 1) * 512], st=(f == 0), sp=(f == 3):
                              e.matmul(o, a, b, start=st, stop=sp), [AT_tk[c], dtk], [g.pst[bank]])
                    P.add("dve", lambda e, o=acc[t][:, db * 512:(db + 1) * 512], i=g.ps[bank][:, :]: e.tensor_tensor(out=o, in0=i, in1=o, op=ALU.add),
                          [g.pst[bank], acc_tk[t]], [acc_tk[t]])
        P.barrier()
        if final:
            fng = arena_f32(g, W0, 2048)
            fng_tk = Tk()
            P.add("sp", lambda e: e.dma_start(out=fng, in_=g.fng), [], [fng_tk], dma=True)
            junk = arena_bf(g, W0 + 2048, 2048)
            junk_tk = Tk()
            fin_ss_tk = [Tk(), Tk()]
            for t in range(8):
                i = tb * 8 + t
                ss, ss_tk = g.small[:, 340 + t % 2:341 + t % 2], fin_ss_tk[t % 2]
                P.add("pool", lambda e, o=ss: e.memset(o, 0.0), [], [ss_tk])
                P.add("act", lambda e, o=junk, i_=acc[t], a=ss: e.activation(out=o, in_=i_, func=AF.Square, accum_out=a), [acc_tk[t]], [junk_tk, ss_tk])
                P.add("act", lambda e, o=ss: e.activation(out=o, in_=o, func=AF.Sqrt, bias=eps, scale=1.0 / D), [ss_tk, g.tk_pers], [ss_tk])
                P.add("dve", lambda e, o=ss: e.reciprocal(out=o, in_=o), [ss_tk], [ss_tk])
                P.add("dve", lambda e, o=acc[t], sc_=ss: e.scalar_tensor_tensor(out=o, in0=o, scalar=sc_, in1=fng, op0=ALU.mult, op1=ALU.mult), [acc_tk[t], ss_tk, fng_tk], [acc_tk[t]])
                P.add("sp", lambda e, o=xdst[i * 128:(i + 1) * 128, :], s=acc[t]: e.dma_start(out=o, in_=s), [acc_tk[t]], [], dma=True)
        else:
            for t in range(8):
                i = tb * 8 + t
                P.add("sp", lambda e, o=xdst[i * 128:(i + 1) * 128, :], s=acc[t]: e.dma_start(out=o, in_=s), [acc_tk[t]], [], dma=True)
        P.barrier()


def stage_moe_sparse(g, l, xsrc, xdst, final):
    P = g.P
    I32 = mybir.dt.int32
    U16 = mybir.dt.uint16
    A, B = prep_AB(g, 2 * l + 1, g.modT[l][:, 4, :], g.modT[l][:, 3, :], 0)
    NTL = NT
    H2, ABC, BBC, XT, XN, JK, T32, RB = 0, 16384, 18432, 20480, 26624, 32768, 33792, 37888
    PERS = 51200
    h2tok = arena_bf(g, H2, 32768).rearrange("p (t n) -> p t n", t=NTL)
    h2_tk = [Tk() for _ in range(NTL)]
    Abc, Bbc = arena_f32(g, ABC, 2048), arena_f32(g, BBC, 2048)
    Abc_tk, Bbc_tk = Tk(), Tk()
    make_bc(g, A, Abc, Abc_tk, RB)
    make_bc(g, B, Bbc, Bbc_tk, RB + 256)
    rws = g.small[:, 64:64 + 256].rearrange("p (k e) -> p k e", e=NE)
    rw_tk = Tk()
    rbs = g.small[:, 320:336]
    eps = g.small[:, 336:337]
    P.add("sp", lambda e: e.dma_start(out=rws, in_=g.rw), [], [rw_tk], dma=True)
    P.add("sp", lambda e: e.dma_start(out=rbs, in_=g.rb), [], [rw_tk], dma=True)
    P.add("pool", lambda e: e.memset(eps, EPS), [], [g.tk_pers])
    desti = g.arena[:, PERS:PERS + 32].bitcast(I32)
    cw = arena_f32(g, PERS + 32, 32)
    cntneg = g.arena[0:1, PERS + 64:PERS + 82].bitcast(I32)
    maskbits = g.arena[:, PERS + 96:PERS + 224].bitcast(U16)
    pers_tk = Tk()
    xt = [arena_f32(g, XT + i * 2048, 2048) for i in range(3)]
    xt_tk = [Tk(), Tk(), Tk()]
    tmp = {
        "ss": [(g.small[:, 340 + i:341 + i], Tk()) for i in range(2)],
        "sq": [(g.small[:, 344 + i:345 + i], Tk()) for i in range(2)],
        "xn": [(arena_f32(g, XN + i * 2048, 2048), Tk()) for i in range(3)],
        "junk": (arena_bf(g, JK, 2048), Tk()),
        "t32": [(arena_f32(g, T32 + i * 512, 512), Tk()) for i in range(8)],
        "eps": eps,
    }
    lgps = g.ps[6]
    lg_tk = g.pst[6]
    def n_stage1(t):
        if t < NTL:
            r_ = t % 3
            P.add("sp", lambda e, o=xt[r_], s=xsrc[t * 128:(t + 1) * 128, :]: e.dma_start(out=o, in_=s), [], [xt_tk[r_]], dma=True)
            norm_transpose(g, xt[r_], xt_tk[r_], A, B, None, None, tmp, t, router=(None, None, None, None), nodst=True, phase=1)
    n_stage1(0)
    for t in range(NTL):
        r = t % 3
        n_stage1(t + 1)
        norm_transpose(g, xt[r], xt_tk[r], A, B, None, None, tmp, t, router=(lgps[:, t * 16:(t + 1) * 16], lg_tk, rws, rw_tk), nodst=True, phase=2)
        xn, xn_tk = tmp["xn"][r]
        P.add("dve", lambda e, o=xn: e.tensor_tensor(out=o, in0=o, in1=Abc, op=ALU.mult), [xn_tk, Abc_tk], [xn_tk])
        P.add("pool", lambda e, o=h2tok[:, t, :], i=xn: e.tensor_tensor(out=o, in0=i, in1=Bbc, op=ALU.add), [xn_tk, Bbc_tk], [h2_tk[t]])
    W = NTL * 16

    def rt(n, w=W):
        return arena_f32(g, RB + n * 256, w)
    sc, sel, eq, msk, w_, comb = rt(0), rt(1), rt(2), rt(3), rt(4), rt(5)
    m1, m2, gs, ing = rt(6, 64), rt(7, 64), rt(8, 64), rt(9, 64)
    gmax, wsum = arena_f32(g, RB + 10 * 256, 16), arena_f32(g, RB + 10 * 256 + 16, 16)
    within, cntbc, pref, dfull, ta, tb2 = rt(11), rt(12), rt(13), rt(14), rt(15), rt(16)
    d01 = arena_f32(g, RB + 17 * 256, 32)
    cntf = arena_f32(g, RB + 17 * 256 + 32, 16)
    cnti = g.arena[:, RB + 17 * 256 + 48: RB + 17 * 256 + 64].bitcast(I32)
    cst = arena_f32(g, RB + 18 * 256, 160)
    validf = arena_f32(g, RB + 19 * 256, 256)
    rtk = Tk()
    cst_tk = Tk()
    P.add("sp", lambda e: e.dma_start(out=cst, in_=g.cst), [], [cst_tk], dma=True)
    Utri, posc, ebase = cst[:, 0:128], cst[:, 128:144], cst[:, 144:160]

    def v3(a, x, y):
        return a.rearrange("p (x y) -> p x y", x=x)
    P.add("act", lambda e: e.activation(out=sc, in_=lgps[:, 0:W], func=AF.Sigmoid), [lg_tk], [rtk])
    P.add("dve", lambda e: e.tensor_tensor(out=v3(sel, NTL, 16), in0=v3(sc, NTL, 16), in1=rbs.unsqueeze(1).to_broadcast([128, NTL, 16]), op=ALU.add), [rtk, rw_tk], [rtk])
    P.add("dve", lambda e: e.tensor_reduce(out=m1, in_=v3(sel, NTL * 4, 4), axis=AX.X, op=ALU.max), [rtk], [rtk])
    P.add("dve", lambda e: e.tensor_tensor(out=v3(eq, NTL * 4, 4), in0=v3(sel, NTL * 4, 4), in1=m1.unsqueeze(2).to_broadcast([128, NTL * 4, 4]), op=ALU.is_equal), [rtk], [rtk])
    P.add("dve", lambda e: e.scalar_tensor_tensor(out=msk, in0=eq, scalar=-1e9, in1=sel, op0=ALU.mult, op1=ALU.add), [rtk], [rtk])
    P.add("dve", lambda e: e.tensor_reduce(out=m2, in_=v3(msk, NTL * 4, 4), axis=AX.X, op=ALU.max), [rtk], [rtk])
    P.add("dve", lambda e: e.tensor_tensor(out=gs, in0=m1, in1=m2, op=ALU.add), [rtk], [rtk])
    P.add("dve", lambda e: e.tensor_reduce(out=gmax, in_=v3(gs, NTL, 4), axis=AX.X, op=ALU.max), [rtk], [rtk])
    P.add("dve", lambda e: e.tensor_tensor(out=v3(ing, NTL, 4), in0=v3(gs, NTL, 4), in1=gmax.unsqueeze(2).to_broadcast([128, NTL, 4]), op=ALU.is_equal), [rtk], [rtk])
    P.add("dve", lambda e: e.tensor_tensor(out=v3(eq, NTL * 4, 4), in0=v3(sel, NTL * 4, 4), in1=m2.unsqueeze(2).to_broadcast([128, NTL * 4, 4]), op=ALU.is_ge), [rtk], [rtk])
    P.add("dve", lambda e: e.tensor_tensor(out=v3(msk, NTL * 4, 4), in0=v3(eq, NTL * 4, 4), in1=ing.unsqueeze(2).to_broadcast([128, NTL * 4, 4]), op=ALU.mult), [rtk], [rtk])
    P.add("dve", lambda e: e.tensor_tensor(out=w_, in0=sc, in1=msk, op=ALU.mult), [rtk], [rtk])
    P.add("dve", lambda e: e.tensor_reduce(out=wsum, in_=v3(w_, NTL, 16), axis=AX.X, op=ALU.add), [rtk], [rtk])
    P.add("dve", lambda e: e.reciprocal(out=wsum, in_=wsum), [rtk], [rtk])
    P.add("dve", lambda e: e.tensor_tensor(out=v3(comb, NTL, 16), in0=v3(w_, NTL, 16), in1=wsum.unsqueeze(2).to_broadcast([128, NTL, 16]), op=ALU.mult), [rtk], [rtk])
    P.add("pe", lambda e: e.matmul(g.ps[4][:, 0:W], Utri, msk, start=True, stop=True), [rtk, cst_tk], [g.pst[4]])
    P.add("pe", lambda e: e.matmul(g.ps[5][:, 0:W], g.ones32, msk, start=True, stop=True), [rtk, g.tk_pers], [g.pst[5]])
    P.add("act", lambda e: e.activation(out=within, in_=g.ps[4][:, 0:W], func=AF.Copy), [g.pst[4]], [rtk])
    P.add("act", lambda e: e.activation(out=cntbc, in_=g.ps[5][:, 0:W], func=AF.Copy), [g.pst[5]], [rtk])
    P.add("pool", lambda e: e.memset(pref[:, 0:16], 0.0), [rtk], [rtk])
    for t in range(1, NTL):
        P.add("dve", lambda e, t=t: e.tensor_tensor(out=pref[:, t * 16:(t + 1) * 16], in0=pref[:, (t - 1) * 16:t * 16], in1=cntbc[:, (t - 1) * 16:t * 16], op=ALU.add), [rtk], [rtk])
    P.add("dve", lambda e: e.tensor_tensor(out=cntf, in0=pref[:, (NTL - 1) * 16:NTL * 16], in1=cntbc[:, (NTL - 1) * 16:NTL * 16], op=ALU.add), [rtk], [rtk])
    P.add("dve", lambda e: e.tensor_tensor(out=dfull, in0=within, in1=pref, op=ALU.add), [rtk], [rtk])
    P.add("dve", lambda e: e.tensor_tensor(out=v3(dfull, NTL, 16), in0=v3(dfull, NTL, 16), in1=ebase.unsqueeze(1).to_broadcast([128, NTL, 16]), op=ALU.add), [rtk, cst_tk], [rtk])
    P.add("dve", lambda e: e.tensor_scalar(out=ta, in0=msk, scalar1=-1e6, scalar2=1e6, op0=ALU.mult, op1=ALU.add), [rtk], [rtk])
    P.add("dve", lambda e: e.tensor_tensor(out=ta, in0=ta, in1=dfull, op=ALU.add), [rtk], [rtk])
    P.add("dve", lambda e: e.tensor_reduce(out=d01[:, 0:16], in_=v3(ta, NTL, 16), axis=AX.X, op=ALU.min), [rtk], [rtk])
    P.add("dve", lambda e: e.tensor_tensor(out=tb2, in0=dfull, in1=msk, op=ALU.mult), [rtk], [rtk])
    P.add("dve", lambda e: e.tensor_reduce(out=d01[:, 16:32], in_=v3(tb2, NTL, 16), axis=AX.X, op=ALU.max), [rtk], [rtk])
    for j in range(2):
        P.add("dve", lambda e, j=j: e.tensor_tensor(out=v3(ta, NTL, 16), in0=v3(dfull, NTL, 16), in1=d01[:, j * 16:(j + 1) * 16].unsqueeze(2).to_broadcast([128, NTL, 16]), op=ALU.is_equal), [rtk], [rtk])
        P.add("dve", lambda e: e.tensor_tensor(out=ta, in0=ta, in1=comb, op=ALU.mult), [rtk], [rtk])
        P.add("dve", lambda e, j=j: e.tensor_reduce(out=cw[:, j * 16:(j + 1) * 16], in_=v3(ta, NTL, 16), axis=AX.X, op=ALU.add), [rtk], [pers_tk])
    P.add("dve", lambda e: e.tensor_copy(out=desti, in_=d01), [rtk], [pers_tk])
    P.add("dve", lambda e: e.tensor_scalar(out=cnti, in0=cntf, scalar1=127.0, scalar2=None, op0=ALU.add), [rtk], [rtk])
    P.add("dve", lambda e: e.tensor_scalar(out=cnti, in0=cnti, scalar1=7, scalar2=None, op0=ALU.arith_shift_right), [rtk], [rtk])
    P.add("dve", lambda e: e.tensor_scalar(out=cntneg[0:1, 0:16], in0=cnti[0:1, :], scalar1=-1, scalar2=None, op0=ALU.mult), [rtk], [pers_tk])
    P.add("dve", lambda e: e.tensor_reduce(out=cntneg[0:1, 16:17], in_=cntneg[0:1, 0:16], axis=AX.X, op=ALU.min), [pers_tk], [pers_tk])
    P.add("dve", lambda e: e.tensor_tensor(out=v3(validf, 16, 16), in0=posc.unsqueeze(1).to_broadcast([128, 16, 16]), in1=cntf.unsqueeze(2).to_broadcast([128, 16, 16]), op=ALU.is_lt), [rtk, cst_tk], [rtk])
    P.add("dve", lambda e: e.tensor_scalar(out=maskbits, in0=validf, scalar1=65535.0, scalar2=None, op0=ALU.mult), [rtk], [pers_tk])
    for t in range(NTL):
        for j in range(2):
            c = j * 16 + t
            P.add("pool", lambda e, c=c, t=t: e.indirect_dma_start(out=g.xg_all[:, :], out_offset=bass.IndirectOffsetOnAxis(ap=desti[:, c:c + 1], axis=0),
                                                                   in_=h2tok[:, t, :], in_offset=None),
                  [pers_tk, h2_tk[t]], [], dma=True)
    P.barrier()
    GU, DN, G2, XG, XGT, SG, ATO, YB, IDB = 0, 16384, 24576, 26624, 30720, 34816, 35840, 36352, 40448
    Gb = [arena_bf(g, GU + i * 8192, 8192).rearrange("p (k n) -> p k n", k=NK) for i in range(2)]
    Ub = [arena_bf(g, GU + 4096 + i * 8192, 8192).rearrange("p (k n) -> p k n", k=NK) for i in range(2)]
    Db = [arena_bf(g, DN + i * 4096, 8192).rearrange("p (f n) -> p f n", f=4) for i in range(2)]
    G_tk = [[Tk() for _ in range(4)] for _ in range(2)]
    U_tk = [[Tk() for _ in range(4)] for _ in range(2)]
    D_tk = [[Tk() for _ in range(4)] for _ in range(2)]
    g2bc = arena_f32(g, G2, 2048)
    g2_tk = Tk()
    make_bc(g, g.modT[l][:, 5, :], g2bc, g2_tk, SG)
    xg = [arena_bf(g, XG + i * 1024, 2048) for i in range(4)]
    xg_tk = [Tk() for _ in range(4)]
    xgT = [arena_bf(g, XGT + i * 1024, 2048).rearrange("p (k n) -> p k n", k=NK) for i in range(4)]
    xgT_tk = [Tk() for _ in range(4)]
    sgt = [arena_f32(g, SG + i * 512, 512) for i in range(2)]
    sg_tk = [Tk(), Tk()]
    ATb = [arena_bf(g, ATO + i * 256, 512).rearrange("p (f n) -> p f n", f=4) for i in range(2)]
    AT_tk = [Tk(), Tk()]
    yb = [arena_bf(g, YB + i * 2048, 2048) for i in range(2)]
    yb_tk = [Tk(), Tk()]
    identb = arena_bf(g, IDB, 128)
    idb_tk = Tk()
    P.add("act", lambda e: e.activation(out=identb, in_=g.ident32, func=AF.Copy), [g.tk_pers], [idb_tk])

    def load_expert(ex):
        pb = ex % 2
        for q4 in range(4):
            P.add("pool", lambda e, o=Gb[pb][:, q4 * 4:(q4 + 1) * 4, :], s_=g.wg[l, ex, q4 * 512:(q4 + 1) * 512, :].rearrange("(k p) n -> p k n", p=128): e.dma_start(out=o, in_=s_),
                  [], [G_tk[pb][q4]], dma=True)
            P.add("pool", lambda e, o=Ub[pb][:, q4 * 4:(q4 + 1) * 4, :], s_=g.wu[l, ex, q4 * 512:(q4 + 1) * 512, :].rearrange("(k p) n -> p k n", p=128): e.dma_start(out=o, in_=s_),
                  [], [U_tk[pb][q4]], dma=True)
        for q4 in range(4):
            P.add("pool", lambda e, o=Db[pb][:, q4, :], s_=g.wd[l, ex, q4 * 128:(q4 + 1) * 128, :]: e.dma_start(out=o, in_=s_), [], [D_tk[pb][q4]], dma=True)

    state_it = {"it": 0}

    def tile(ex, s, uid):
        it = state_it["it"]
        P.region = (uid, s)
        r = it % 2
        state_it["it"] = it + 1
        row0 = ex * 2048 + s * 128
        P.add("sp", lambda e, o=xg[r], s_=g.xg_all[row0:row0 + 128, :]: e.dma_start(out=o, in_=s_), [g.xg_tk], [xg_tk[r]], dma=True)
        mcol = ex * 16 + s
        P.add("dve", lambda e, o=xg[r].bitcast(U16), m=maskbits[:, mcol:mcol + 1].to_broadcast([128, 2048]): e.tensor_tensor(out=o, in0=o, in1=m, op=ALU.bitwise_and),
              [xg_tk[r], pers_tk], [xg_tk[r]])
        for half in range(2):
            pb = g.ps[half][:, :].bitcast(BF16)
            for c in range(8):
                k = half * 8 + c
                P.add("pe", lambda e, o=pb[:, c * 128:(c + 1) * 128], i=xg[r][:, k * 128:(k + 1) * 128]: e.transpose(o, i, identb), [xg_tk[r], idb_tk], [g.pst[half]])
            if half == 0:
                P.add("act", lambda e, o=xgT[r][:, 0:8, :], i=pb.rearrange("p (k n) -> p k n", k=8): e.activation(out=o, in_=i, func=AF.Copy), [g.pst[half]], [xgT_tk[r]])
            else:
                P.add("dve", lambda e, o=xgT[r][:, 8:16, :], i=pb.rearrange("p (k n) -> p k n", k=8): e.tensor_copy(out=o, in_=i), [g.pst[half]], [xgT_tk[r]])
        for f in range(4):
            for k in range(NK):
                P.add("pe", lambda e, o=g.ps[2][:, f * 128:(f + 1) * 128], a=Gb[ex % 2][:, k, f * 128:(f + 1) * 128], b=xgT[r][:, k, :], st=(k == 0), sp=(k == NK - 1):
                      e.matmul(o, a, b, start=st, stop=sp), [G_tk[ex % 2][k // 4], xgT_tk[r]], [g.pst[2]])
            for k in range(NK):
                P.add("pe", lambda e, o=g.ps[3][:, f * 128:(f + 1) * 128], a=Ub[ex % 2][:, k, f * 128:(f + 1) * 128], b=xgT[r][:, k, :], st=(k == 0), sp=(k == NK - 1):
                      e.matmul(o, a, b, start=st, stop=sp), [U_tk[ex % 2][k // 4], xgT_tk[r]], [g.pst[3]])
        P.add("act", lambda e, o=sgt[r]: e.activation(out=o, in_=g.ps[2][:, :], func=AF.Silu), [g.pst[2]], [sg_tk[r]])
        P.add("dve", lambda e, o=ATb[r].rearrange("p f n -> p (f n)"), b=sgt[r]: e.tensor_tensor(out=o, in0=g.ps[3][:, :], in1=b, op=ALU.mult), [g.pst[3], sg_tk[r]], [AT_tk[r]])
        for db in range(4):
            bank = 4 + db
            for f in range(4):
                P.add("pe", lambda e, o=g.ps[bank][:, :], a=ATb[r][:, f, :], b=Db[ex % 2][:, f, db * 512:(db + 1) * 512], st=(f == 0), sp=(f == 3):
                      e.matmul(o, a, b, start=st, stop=sp), [AT_tk[r], D_tk[ex % 2][f]], [g.pst[bank]])
            P.add("dve", lambda e, o=yb[r][:, db * 512:(db + 1) * 512], i=g.ps[bank][:, :], b_=g2bc[:, db * 512:(db + 1) * 512]: e.tensor_tensor(out=o, in0=i, in1=b_, op=ALU.mult),
                  [g.pst[bank], g2_tk], [yb_tk[r]])
        P.add("sp", lambda e, o=g.yg_all[row0:row0 + 128, :], s_=yb[r]: e.dma_start(out=o, in_=s_), [yb_tk[r]], [g.yg_tk], dma=True)
        P.region = None

    def t_load(ex, s, bi):
        row0 = ex * 2048 + s * 128
        P.add("sp", lambda e, o=xg[bi], s_=g.xg_all[row0:row0 + 128, :]: e.dma_start(out=o, in_=s_), [g.xg_tk], [xg_tk[bi]], dma=True)

    def t_mask(ex, s, bi):
        mcol = ex * 16 + s
        P.add("dve", lambda e, o=xg[bi].bitcast(U16), m=maskbits[:, mcol:mcol + 1].to_broadcast([128, 2048]): e.tensor_tensor(out=o, in0=o, in1=m, op=ALU.bitwise_and),
              [xg_tk[bi], pers_tk], [xg_tk[bi]])

    def t_prep(ex, s, bi):
        for half in range(2):
            pb = g.ps[half][:, :].bitcast(BF16)
            for c in range(8):
                k = half * 8 + c
                P.add("pe", lambda e, o=pb[:, c * 128:(c + 1) * 128], i=xg[bi][:, k * 128:(k + 1) * 128]: e.transpose(o, i, identb), [xg_tk[bi], idb_tk], [g.pst[half]])
            if half == 0:
                P.add("act", lambda e, o=xgT[bi][:, 0:8, :], i=pb.rearrange("p (k n) -> p k n", k=8): e.activation(out=o, in_=i, func=AF.Copy), [g.pst[half]], [xgT_tk[bi]])
            else:
                P.add("dve", lambda e, o=xgT[bi][:, 8:16, :], i=pb.rearrange("p (k n) -> p k n", k=8): e.tensor_copy(out=o, in_=i), [g.pst[half]], [xgT_tk[bi]])

    def t_gu(ex, bi, r):
        for f in range(4):
            for k in range(NK):
                P.add("pe", lambda e, o=g.ps[2][:, f * 128:(f + 1) * 128], a=Gb[ex % 2][:, k, f * 128:(f + 1) * 128], b=xgT[bi][:, k, :], st=(k == 0), sp=(k == NK - 1):
                      e.matmul(o, a, b, start=st, stop=sp), [G_tk[ex % 2][k // 4], xgT_tk[bi]], [g.pst[2]])
            for k in range(NK):
                P.add("pe", lambda e, o=g.ps[3][:, f * 128:(f + 1) * 128], a=Ub[ex % 2][:, k, f * 128:(f + 1) * 128], b=xgT[bi][:, k, :], st=(k == 0), sp=(k == NK - 1):
                      e.matmul(o, a, b, start=st, stop=sp), [U_tk[ex % 2][k // 4], xgT_tk[bi]], [g.pst[3]])
        P.add("act", lambda e, o=sgt[r]: e.activation(out=o, in_=g.ps[2][:, :], func=AF.Silu), [g.pst[2]], [sg_tk[r]])
        P.add("dve", lambda e, o=ATb[r].rearrange("p f n -> p (f n)"), b=sgt[r]: e.tensor_tensor(out=o, in0=g.ps[3][:, :], in1=b, op=ALU.mult), [g.pst[3], sg_tk[r]], [AT_tk[r]])

    def t_down(ex, s, r):
        row0 = ex * 2048 + s * 128
        for db in range(4):
            bank = 4 + db
            for f in range(4):
                P.add("pe", lambda e, o=g.ps[bank][:, :], a=ATb[r][:, f, :], b=Db[ex % 2][:, f, db * 512:(db + 1) * 512], st=(f == 0), sp=(f == 3):
                      e.matmul(o, a, b, start=st, stop=sp), [AT_tk[r], D_tk[ex % 2][f]], [g.pst[bank]])
            P.add("dve", lambda e, o=yb[r][:, db * 512:(db + 1) * 512], i=g.ps[bank][:, :], b_=g2bc[:, db * 512:(db + 1) * 512]: e.tensor_tensor(out=o, in0=i, in1=b_, op=ALU.mult),
                  [g.pst[bank], g2_tk], [yb_tk[r]])
        P.add("sp", lambda e, o=g.yg_all[row0:row0 + 128, :], s_=yb[r]: e.dma_start(out=o, in_=s_), [yb_tk[r]], [g.yg_tk], dma=True)

    load_expert(0)
    t_load(0, 0, 2)
    t_mask(0, 0, 2)
    t_prep(0, 0, 2)
    for ex in range(NE):
        if ex + 1 < NE:
            load_expert(ex + 1)
            t_load(ex + 1, 0, 2 + (ex + 1) % 2)
            t_mask(ex + 1, 0, 2 + (ex + 1) % 2)
            t_prep(ex + 1, 0, 2 + (ex + 1) % 2)
        for eng in Prog.ENGS:
            P.add(eng, lambda e, eng=eng, ex=ex: e.reg_load(P.regs[eng], cntneg[0:1, ex:ex + 1]), [pers_tk], [])
        uid = ("moeh", l, ex)
        for s in range(HOT):
            P.region = (uid, s)
            it = state_it["it"]
            state_it["it"] = it + 1
            r = it % 2
            bi = (2 + ex % 2) if s == 0 else (s % 2)
            if s + 1 < HOT:
                t_load(ex, s + 1, (s + 1) % 2)
                t_mask(ex, s + 1, (s + 1) % 2)
            t_gu(ex, bi, r)
            if s + 1 < HOT:
                t_prep(ex, s + 1, (s + 1) % 2)
            t_down(ex, s, r)
            P.region = None
    if HOT < 16:
        for eng in Prog.ENGS:
            P.add(eng, lambda e, eng=eng: e.reg_load(P.regs2[eng], cntneg[0:1, 16:17]), [pers_tk], [])
        P.outer = (("moec", l), HOT)
    for ex in (range(NE) if HOT < 16 else ()):
        for eng in Prog.ENGS:
            P.add(eng, lambda e, eng=eng, ex=ex: e.reg_load(P.regs[eng], cntneg[0:1, ex:ex + 1]), [pers_tk], [])
        uid = ("moec", l, ex)
        P.region = (uid, HOT)
        load_expert(ex)
        P.region = None
        for s in range(HOT, 16):
            tile(ex, s, uid)
    P.outer = None
    P.barrier()
    XT2, Y0, FNG, JK2, TM2 = 0, 4096, 12288, 14336, 15360
    xt2 = [arena_f32(g, XT2 + i * 2048, 2048) for i in range(2)]
    xt2_tk = [Tk(), Tk()]
    yg = [[arena_bf(g, Y0 + (i * 2 + j) * 2048, 2048) for j in range(2)] for i in range(2)]
    yg_tk = [[Tk(), Tk()], [Tk(), Tk()]]
    fng = arena_f32(g, FNG, 2048)
    fng_tk = Tk()
    junk = arena_bf(g, JK2, 2048)
    junk_tk = Tk()
    fin_ss_tk = [Tk(), Tk()]
    if final:
        P.add("sp", lambda e: e.dma_start(out=fng, in_=g.fng), [], [fng_tk], dma=True)
    def c_load(t):
        if t < NTL:
            P.add("sp", lambda e, o=xt2[t % 2], s_=xsrc[t * 128:(t + 1) * 128, :]: e.dma_start(out=o, in_=s_), [], [xt2_tk[t % 2]], dma=True)
    c_load(0)
    for t in range(NTL):
        r = t % 2
        c_load(t + 1)
        for j in range(2):
            c = j * 16 + t
            P.add("pool", lambda e, o=yg[r][j], c=c: e.indirect_dma_start(out=o, out_offset=None, in_=g.yg_all[:, :], in_offset=bass.IndirectOffsetOnAxis(ap=desti[:, c:c + 1], axis=0)),
                  [pers_tk, g.yg_tk], [yg_tk[r][j]], dma=True)
        for j in range(2):
            c = j * 16 + t
            P.add("dve", lambda e, o=xt2[r], y=yg[r][j], c=c: e.scalar_tensor_tensor(out=o, in0=y, scalar=cw[:, c:c + 1], in1=o, op0=ALU.mult, op1=ALU.add),
                  [xt2_tk[r], yg_tk[r][j], pers_tk], [xt2_tk[r]])
        if final:
            ss, ss_tk = g.small[:, 340 + t % 2:341 + t % 2], fin_ss_tk[t % 2]
            P.add("pool", lambda e, o=ss: e.memset(o, 0.0), [], [ss_tk])
            P.add("act", lambda e, o=junk, i_=xt2[r], a=ss: e.activation(out=o, in_=i_, func=AF.Square, accum_out=a), [xt2_tk[r]], [junk_tk, ss_tk])
            P.add("act", lambda e, o=ss: e.activation(out=o, in_=o, func=AF.Sqrt, bias=eps, scale=1.0 / D), [ss_tk, g.tk_pers], [ss_tk])
            P.add("dve", lambda e, o=ss: e.reciprocal(out=o, in_=o), [ss_tk], [ss_tk])
            P.add("dve", lambda e, o=xt2[r], sc_=ss: e.scalar_tensor_tensor(out=o, in0=o, scalar=sc_, in1=fng, op0=ALU.mult, op1=ALU.mult), [xt2_tk[r], ss_tk, fng_tk], [xt2_tk[r]])
        P.add("sp", lambda e, o=xdst[t * 128:(t + 1) * 128, :], s_=xt2[r]: e.dma_start(out=o, in_=s_), [xt2_tk[r]], [], dma=True)
    P.barrier()


def _fm(v):
    v = np.asarray(v, np.float32)
    return np.ascontiguousarray(v.reshape(-1, 128).T)


def _ada_b4(ab):
    o = np.zeros((ab.shape[0], 4, ab.shape[1]), np.float32)
    o[:, 0] = ab
    o[:, 2] = ab
    return o


def _bias_tables(rpb):
    H = rpb.shape[0]
    kc = np.arange(64)[:, None]
    qc = np.arange(64)[None, :]
    cs = np.clip(qc - 8, 0, 48)
    cmask = (kc >= cs) & (kc < cs + 16)
    dc = np.clip(kc - qc + 15, 0, 30)
    tab = np.full((H, 128, 26, 64), NEG, np.float32)
    for a in range(2):
        for s in range(10):
            dr = 11 - s + a
            if 3 <= dr <= 10:
                v = rpb[:, dr][:, dc]
                tab[:, a * 64:(a + 1) * 64, s, :] = np.where(cmask[None], v, NEG)
        for s in range(16):
            dr = 14 - s + a
            if 0 <= dr <= 14:
                v = rpb[:, dr][:, dc]
                tab[:, a * 64:(a + 1) * 64, 10 + s, :] = np.where(cmask[None], v, NEG)
    return np.ascontiguousarray(tab.reshape(H, 128, 26 * 64))


def _dft_consts():
    L, C = 2048, 256
    c = np.arange(C)
    ang = 2 * np.pi * np.outer(c, c) / C
    csc = np.concatenate([np.cos(ang), np.sin(ang)], axis=1) / 16.0
    csc = csc.reshape(2, 128, 512).transpose(1, 0, 2)
    l = np.arange(L)
    lm = (np.outer(l, l) % L).astype(np.float64)
    angL = 2 * np.pi * lm / L
    sL = 1.0 / np.sqrt(L)
    CL = np.cos(angL) * sL
    SL = -np.sin(angL) * sL
    out = np.empty((2, 4, 128, 16, 512), np.float32)
    for i, M in enumerate((CL, SL)):
        out[i] = M.reshape(16, 128, 4, 512).transpose(2, 1, 0, 3)
    return csc.astype(ml_dtypes.bfloat16), out.astype(ml_dtypes.bfloat16)


_CONSTS = {}


def make_in_maps(inp):
    f = lambda a: np.ascontiguousarray(np.asarray(a, np.float32))
    if "dft" not in _CONSTS:
        _CONSTS["csc"], _CONSTS["dft"] = _dft_consts()
        _CONSTS["ident"] = np.eye(128, dtype=np.float32)
        p = np.arange(128)
        cst = np.zeros((128, 160), np.float32)
        cst[:, 0:128] = (p[:, None] < p[None, :]).astype(np.float32)
        cst[:, 128:144] = p[:, None] + 128.0 * np.arange(16)[None, :]
        cst[:, 144:160] = 2048.0 * np.arange(16)[None, :]
        _CONSTS["cst"] = cst
    x, c, ctx, c_ctx = f(inp["x"]), f(inp["c"]), f(inp["ctx"]), f(inp["c_ctx"])
    shared = {
        "ada_w": f(inp["ada_w"]),
        "ada_b4": _ada_b4(f(inp["ada_b"])),
        "cmb": np.array([[1, 0], [1, 0], [0, 1], [0, 1]], np.float32),
        "gT": np.ascontiguousarray(np.stack([_fm(inp["mix_norm_g"][0]), _fm(inp["ffn_norm_g"][0]),
                                             _fm(inp["mix_norm_g"][1]), _fm(inp["ffn_norm_g"][1])], axis=1)),
        "fng": np.ascontiguousarray(np.broadcast_to(f(inp["final_norm_g"])[None, :], (128, D))),
        "w_in0": f(inp["ev_w_in"][0]), "w_out0": f(inp["ev_w_out"][0]),
        "rpbt": _bias_tables(f(inp["ev_rpb"][0])),
        "csc": _CONSTS["csc"], "dft": _CONSTS["dft"],
        "w_in1": f(inp["od_w_in"][0]),
        "cvp": np.ascontiguousarray(np.stack([_fm(inp["od_b_in"][0][:D]), _fm(inp["od_b_in"][0][D:]), _fm(inp["od_dw_b"][0]),
                                              _fm(inp["od_ln_g"][0]), _fm(inp["od_ln_b"][0]), _fm(inp["od_ln_b"][0])], axis=1)),
        "dww": np.ascontiguousarray(f(inp["od_dw_w"][0]).T.reshape(NK, 128, 31).transpose(1, 0, 2)),
        "w_out1": f(inp["od_w_out"][0]),
        "bout": np.ascontiguousarray(np.broadcast_to(f(inp["od_b_out"][0])[None, :], (128, D))),
        "rw": np.ascontiguousarray(f(inp["router_w"]).reshape(NK, 128, NE).transpose(1, 0, 2)),
        "rb": np.ascontiguousarray(np.broadcast_to(f(inp["router_b"])[None, :], (128, NE))),
        "wg": f(inp["moe_w_gate"]), "wu": f(inp["moe_w_up"]), "wd": f(inp["moe_w_down"]),
        "ident": _CONSTS["ident"],
        "cst": _CONSTS["cst"],
    }
    maps = []
    for b in range(x.shape[0]):
        m = dict(shared)
        m["x"] = x[b]
        m["ctx"] = ctx[b]
        m["cT"] = np.ascontiguousarray(np.stack([_fm(c[b]), _fm(c_ctx)], axis=2))
        maps.append(m)
    return maps


_NC = {}


def kernel(**inputs):
    maps = make_in_maps(inputs)
    if "nc" not in _NC:
        _NC["nc"] = build_program()
    res = run_bass_kernel_spmd(_NC["nc"], maps, core_ids=list(range(8)))
    return np.stack([r["out"] for r in res.results], axis=0).astype(np.float32)


def out_proj(g, zT, z_tk, nkc, w_dram, xin, xout, gateT, bias_bc_dram, base):
    P = g.P
    wsz = nkc * 256
    wbf = [arena_bf(g, base + i * wsz, nkc * 512).rearrange("p (k n) -> p k n", k=nkc) for i in range(2)]
    wbf_tk = [Tk(), Tk()]
    o = base + 2 * wsz
    g1bc = arena_f32(g, o, 2048)
    gb = arena_f32(g, o + 2048, 2048)
    g1_tk, gb_tk = Tk(), Tk()
    o += 4096
    NX = 4
    xi = [arena_f32(g, o + i * 512, 512) for i in range(NX)]
    xi_tk = [Tk() for _ in range(NX)]
    o += NX * 512
    tm = [arena_f32(g, o + i * 512, 512) for i in range(2)]
    tm_tk = [Tk(), Tk()]
    o += 1024
    xo = [arena_f32(g, o + i * 512, 512) for i in range(NX)]
    xo_tk = [Tk() for _ in range(NX)]
    o += NX * 512
    make_bc(g, gateT, g1bc, g1_tk, o)
    if bias_bc_dram is not None:
        P.add("sp", lambda e: e.dma_start(out=gb, in_=bias_bc_dram), [], [gb_tk], dma=True)
        P.add("dve", lambda e: e.tensor_tensor(out=gb, in0=gb, in1=g1bc, op=ALU.mult), [gb_tk, g1_tk], [gb_tk])
    nq = nkc // 4

    def load_w(db):
        wb = wbf[db % 2]
        for kq in range(nq):
            src_ = w_dram[kq * 512:(kq + 1) * 512, db * 512:(db + 1) * 512].rearrange("(k p) n -> p k n", p=128)
            P.add("pool", lambda e, o_=wb[:, kq * 4:(kq + 1) * 4, :], s_=src_: e.dma_start(out=o_, in_=s_), [], [wbf_tk[db % 2]], dma=True)

    iters = [(db, t) for db in range(4) for t in range(NT)]
    state = {"ld": 0}

    def issue_loads(upto):
        while state["ld"] < min(upto, len(iters)):
            n = state["ld"]
            db, t = iters[n]
            P.add("sp", lambda e, o_=xi[n % NX], s_=xin[t * 128:(t + 1) * 128, db * 512:(db + 1) * 512]: e.dma_start(out=o_, in_=s_), [], [xi_tk[n % NX]], dma=True)
            state["ld"] += 1

    load_w(0)
    for it, (db, t) in enumerate(iters):
        wb = wbf[db % 2]
        if t == 0 and db + 1 < 4:
            load_w(db + 1)
        issue_loads(it + NX - 1)
        rx = it % NX
        r2 = it % 2
        bank = 6 + it % 2
        for c in range(nkc):
            P.add("pe", lambda e, o_=g.ps[bank][:, :], a=zT[:, c, t * 128:(t + 1) * 128], b=wb[:, c, :], st=(c == 0), sp=(c == nkc - 1):
                  e.matmul(o_, a, b, start=st, stop=sp), [z_tk, wbf_tk[db % 2]], [g.pst[bank]])
        P.add("dve", lambda e, o_=tm[r2], i_=g.ps[bank][:, :], b_=g1bc[:, db * 512:(db + 1) * 512]: e.tensor_tensor(out=o_, in0=i_, in1=b_, op=ALU.mult),
              [g.pst[bank], g1_tk], [tm_tk[r2]])
        if bias_bc_dram is not None:
            P.add("pool", lambda e, o_=xi[rx], b_=gb[:, db * 512:(db + 1) * 512]: e.tensor_tensor(out=o_, in0=o_, in1=b_, op=ALU.add), [xi_tk[rx], gb_tk], [xi_tk[rx]])
        P.add("dve", lambda e, o_=xo[rx], a=tm[r2], b=xi[rx]: e.tensor_tensor(out=o_, in0=a, in1=b, op=ALU.add), [tm_tk[r2], xi_tk[rx]], [xo_tk[rx]])
        P.add("sp", lambda e, o_=xout[t * 128:(t + 1) * 128, db * 512:(db + 1) * 512], s_=xo[rx]: e.dma_start(out=o_, in_=s_), [xo_tk[rx]], [], dma=True)


def build_hT(g, xsrc, ntiles, A, B, hT, hT_tk, base):
    P = g.P
    eps = g.small[:, 336:337]
    P.add("pool", lambda e: e.memset(eps, EPS), [], [g.tk_pers])
    xt = [arena_f32(g, base + i * 2048, 2048) for i in range(2)]
    xt_tk = [Tk(), Tk()]
    tmp = {
        "ss": [(g.small[:, 340 + i:341 + i], Tk()) for i in range(2)],
        "sq": [(g.small[:, 344 + i:345 + i], Tk()) for i in range(2)],
        "xn": [(arena_f32(g, base + 4096 + i * 2048, 2048), Tk()) for i in range(2)],
        "junk": (arena_bf(g, base + 8192, 2048), Tk()),
        "t32": [(arena_f32(g, base + 9216 + i * 512, 512), Tk()) for i in range(2)],
        "eps": eps,
    }
    def s1(t):
        if t < ntiles:
            r_ = t % 2
            P.add("sp", lambda e, o=xt[r_], s=xsrc[t * 128:(t + 1) * 128, :]: e.dma_start(out=o, in_=s), [], [xt_tk[r_]], dma=True)
            norm_transpose(g, xt[r_], xt_tk[r_], A, B, None, None, tmp, t, phase=1)
    s1(0)
    for t in range(ntiles):
        r = t % 2
        s1(t + 1)
        norm_transpose(g, xt[r], xt_tk[r], A, B, None,
                       lambda b, t=t: (hT[:, b * 4:(b + 1) * 4, t * 128:(t + 1) * 128], hT_tk[t]), tmp, t, phase=2)


def stage_conv(g, xsrc, xdst):
    P = g.P
    l = 1
    A, B = prep_AB(g, 2, g.modT[l][:, 1, :], g.modT[l][:, 0, :], 0)
    HT, UT, R = 0, 16384, 33024
    PADW = 2080
    hT = arena_bf(g, HT, 32768).rearrange("p (k n) -> p k n", k=NK)
    hT_tk = [Tk() for _ in range(NT)]
    uT = arena_bf(g, UT, NK * PADW).rearrange("p (k n) -> p k n", k=NK)
    uT_tk = [Tk() for _ in range(NK)]
    cv = g.small[:, 96:192].rearrange("p (j k) -> p j k", j=6)
    cv_tk = Tk()
    P.add("sp", lambda e: e.dma_start(out=cv, in_=g.cvp), [], [cv_tk], dma=True)
    build_hT(g, xsrc, NT, A, B, hT, hT_tk, R)
    P.barrier()
    for c in range(NK):
        P.add("pool", lambda e, o=uT[:, c, 0:15]: e.memset(o, 0.0), [], [uT_tk[c]])
        P.add("pool", lambda e, o=uT[:, c, 2063:2080]: e.memset(o, 0.0), [], [uT_tk[c]])
    sg = [arena_f32(g, R + 4096 + i * 512, 512) for i in range(2)]
    sg_tk = [Tk(), Tk()]
    stg = [arena_f32(g, 43264 + i * 2048, 2048).rearrange("p (k n) -> p k n", k=NK) for i in range(2)]
    stg_tk = [Tk(), Tk()]
    wbf = [arena_bf(g, 47360 + i * 1024, 2048).rearrange("p (k n) -> p k n", k=NK) for i in range(4)]
    wbf_tk = [Tk() for _ in range(4)]
    nu = 0
    it = 0
    for c in range(NK):
        ws = []
        for half in range(2):
            s = nu % 2
            d = nu % 4
            nu += 1
            col = half * D + c * 128
            src_ = g.w_in1[:, col:col + 128].rearrange("(k p) n -> p k n", p=128)
            P.add("pool", lambda e, o=wbf[d], s_=src_: e.dma_start(out=o, in_=s_), [], [wbf_tk[d]], dma=True)
            ws.append((wbf[d], wbf_tk[d]))
        for tb in range(4):
            bv, bg = (0, 1) if it % 2 == 0 else (2, 3)
            for half, bank in ((0, bv), (1, bg)):
                w, wtk = ws[half]
                for k in range(NK):
                    P.add("pe", lambda e, o=g.ps[bank][:, :], a=w[:, k, :], b=hT[:, k, tb * 512:(tb + 1) * 512], st=(k == 0), sp=(k == NK - 1):
                          e.matmul(o, a, b, start=st, stop=sp), [wtk] + hT_tk[tb * 4:tb * 4 + 4], [g.pst[bank]])
            r = it % 2
            P.add("act", lambda e, o=sg[r], i=g.ps[bg][:, :], b_=cv[:, 1, c:c + 1]: e.activation(out=o, in_=i, func=AF.Sigmoid, bias=b_), [g.pst[bg], cv_tk], [sg_tk[r]])
            P.add("dve", lambda e, o=uT[:, c, 15 + tb * 512: 15 + (tb + 1) * 512], i=g.ps[bv][:, :], b_=cv[:, 0, c:c + 1], s_=sg[r]:
                  e.scalar_tensor_tensor(out=o, in0=i, scalar=b_, in1=s_, op0=ALU.add, op1=ALU.mult), [g.pst[bv], cv_tk, sg_tk[r]], [uT_tk[c]])
            it += 1
    P.barrier()
    vT = arena_bf(g, 0, 32768).rearrange("p (k n) -> p k n", k=NK)
    vT_tk = [[Tk() for _ in range(4)] for _ in range(NK)]
    dwt = arena_f32(g, R, 512).rearrange("p (k t) -> p k t", k=NK)[:, :, 0:31]
    dwt_full = arena_f32(g, R, 496).rearrange("p (k t) -> p k t", k=NK)
    dw_tk = Tk()
    P.add("sp", lambda e: e.dma_start(out=dwt_full, in_=g.dww), [], [dw_tk], dma=True)
    identb = arena_bf(g, R + 512, 128)
    onesb = arena_bf(g, R + 576, 128)
    cb_tk = Tk()
    P.add("act", lambda e: e.activation(out=identb, in_=g.ident32, func=AF.Copy), [g.tk_pers], [cb_tk])
    P.add("pool", lambda e: e.memset(onesb, 1.0), [], [cb_tk])
    dg = [arena_bf(g, R + 1024 + i * 2048, 31 * 128).rearrange("p (t n) -> p t n", t=31) for i in range(2)]
    dg_tk = [Tk(), Tk()]
    it = 0

    def build_diag(c):
        if c >= NK:
            return
        d_ = c % 2
        for k in range(31):
            if k % 2 == 0:
                P.add("act", lambda e, o=dg[d_][:, k, :], sc=dwt_full[:, c, k:k + 1]: e.activation(out=o, in_=identb, func=AF.Copy, scale=sc), [cb_tk, dw_tk], [dg_tk[d_]])
            else:
                P.add("dve", lambda e, o=dg[d_][:, k, :], sc=dwt_full[:, c, k:k + 1]: e.tensor_scalar(out=o, in0=identb, scalar1=sc, scalar2=None, op0=ALU.mult),
                      [cb_tk, dw_tk], [dg_tk[d_]])
    build_diag(0)
    for c in range(NK):
        d = c % 2
        build_diag(c + 1)
        for tb in range(4):
            bank = it % 2
            for k in range(31):
                P.add("pe", lambda e, o=g.ps[bank][:, :], a=dg[d][:, k, :], b=uT[:, c, tb * 512 + k: tb * 512 + k + 512], st=(k == 0), sp=(k == 30):
                      e.matmul(o, a, b, start=st, stop=sp), [dg_tk[d], uT_tk[c]], [g.pst[bank]])
            P.add("act", lambda e, o=vT[:, c, tb * 512:(tb + 1) * 512], i=g.ps[bank][:, :], b_=cv[:, 2, c:c + 1]: e.activation(out=o, in_=i, func=AF.Identity, bias=b_),
                  [g.pst[bank], cv_tk], [vT_tk[c][tb]])
            it += 1
    SB = R + 1024 + 4096
    sqb = [arena_bf(g, SB + i * 256, 512) for i in range(2)]
    sq_tk = [Tk(), Tk()]
    mean = arena_f32(g, SB + 512, 512)
    rstd = arena_f32(g, SB + 1024, 512)
    m2 = arena_f32(g, SB + 1536, 512)
    st_tk = Tk()
    tn = [arena_f32(g, SB + 2048 + i * 512, 512) for i in range(2)]
    tn_tk = [Tk(), Tk()]
    eps = g.small[:, 336:337]
    it = 0
    for tb in range(4):
        for c in range(NK):
            r = it % 2
            vv = vT[:, c, tb * 512:(tb + 1) * 512]
            P.add("act", lambda e, o=sqb[r], i=vv: e.activation(out=o, in_=i, func=AF.Square), [vT_tk[c][tb]], [sq_tk[r]])
            P.add("pe", lambda e, b=vv, st=(c == 0), sp=(c == NK - 1): e.matmul(g.ps[2][:, :], onesb, b, start=st, stop=sp), [cb_tk, vT_tk[c][tb]], [g.pst[2]])
            P.add("pe", lambda e, b=sqb[r], st=(c == 0), sp=(c == NK - 1): e.matmul(g.ps[3][:, :], onesb, b, start=st, stop=sp), [cb_tk, sq_tk[r]], [g.pst[3]])
            it += 1
        P.add("act", lambda e: e.activation(out=mean, in_=g.ps[2][:, :], func=AF.Copy, scale=1.0 / D), [g.pst[2]], [st_tk])
        P.add("dve", lambda e: e.tensor_tensor(out=m2, in0=mean, in1=mean, op=ALU.mult), [st_tk], [st_tk])
        P.add("dve", lambda e: e.scalar_tensor_tensor(out=rstd, in0=g.ps[3][:, :], scalar=1.0 / D, in1=m2, op0=ALU.mult, op1=ALU.subtract), [g.pst[3], st_tk], [st_tk])
        P.add("act", lambda e: e.activation(out=rstd, in_=rstd, func=AF.Sqrt, bias=eps), [st_tk, g.tk_pers], [st_tk])
        P.add("dve", lambda e: e.reciprocal(out=rstd, in_=rstd), [st_tk], [st_tk])
        for c in range(NK):
            r = c % 2
            vv = vT[:, c, tb * 512:(tb + 1) * 512]
            P.add("dve", lambda e, o=tn[r], i=vv: e.tensor_tensor(out=o, in0=i, in1=mean, op=ALU.subtract), [vT_tk[c][tb], st_tk], [tn_tk[r]])
            P.add("dve", lambda e, o=tn[r]: e.tensor_tensor(out=o, in0=o, in1=rstd, op=ALU.mult), [tn_tk[r], st_tk], [tn_tk[r]])
            P.add("act", lambda e, o=vv, i=tn[r], s_=cv[:, 3, c:c + 1], b_=cv[:, 4, c:c + 1]: e.activation(out=o, in_=i, func=AF.Silu, bias=b_, scale=s_),
                  [tn_tk[r], cv_tk], [vT_tk[c][tb]])
    P.barrier()
    z_tk = Tk()
    out_proj(g, vT, z_tk, NK, g.w_out1, xsrc, xdst, g.modT[l][:, 2, :], g.bout, 16384)


def load_w_unit(g, src_ap, stg, stg_tk, dst, dst_tk, eng):
    g.P.add("pool", lambda e: e.dma_start(out=dst, in_=src_ap), [], [dst_tk], dma=True)


def stage_mixer0(g, xsrc, xdst):
    P = g.P
    l = 0
    A, B = prep_AB(g, 0, g.modT[l][:, 1, :], g.modT[l][:, 0, :], 0)
    Ac, Bc = prep_AB(g, 0, g.modcT[:, 1, :], g.modcT[:, 0, :], 2)
    hT = arena_bf(g, 0, 32768).rearrange("p (k n) -> p k n", k=NK)
    hT_tk = [Tk() for _ in range(NT)]
    build_hT(g, xsrc, NT, A, B, hT, hT_tk, 16384)
    P.barrier()
    YT = arena_bf(g, 16384, 16384).rearrange("p (k n) -> p k n", k=8)
    YT_tk = Tk()
    uT = arena_bf(g, 24576, 4096).rearrange("p (k n) -> p k n", k=2)
    uT_tk = [Tk(), Tk()]
    W1 = arena_bf(g, 26624, 8192).rearrange("p (t n) -> p t n", t=NT)
    W1_tk = [Tk() for _ in range(NT)]
    dfb = [arena_bf(g, 30720 + i * 4096, 8192).rearrange("p (k n) -> p k n", k=NK) for i in range(3)]
    dfb_tk = [Tk() for _ in range(3)]
    stg = [arena_f32(g, 43008 + i * 2048, 2048).rearrange("p (k n) -> p k n", k=NK) for i in range(2)]
    stg_tk = [Tk(), Tk()]
    wbf = [arena_bf(g, 47104 + i * 1024, 2048).rearrange("p (k n) -> p k n", k=NK) for i in range(2)]
    wbf_tk = [Tk(), Tk()]
    csc = arena_bf(g, 49152, 1024).rearrange("p (k n) -> p k n", k=2)
    csc_tk = Tk()
    P.add("sp", lambda e: e.dma_start(out=csc, in_=g.csc), [], [csc_tk], dma=True)
    nu = 0
    nd = 0
    it = 0
    for gi in range(4):
        for cc in range(2):
            s = nu % 2
            nu += 1
            col = gi * 256 + cc * 128
            load_w_unit(g, g.w_in0[:, col:col + 128].rearrange("(k p) n -> p k n", p=128), stg[s], stg_tk[s], wbf[s], wbf_tk[s], "act" if cc == 0 else "dve")
            for tb in range(4):
                bank = it % 2
                it += 1
                for k in range(NK):
                    P.add("pe", lambda e, o=g.ps[bank][:, :], a=wbf[s][:, k, :], b=hT[:, k, tb * 512:(tb + 1) * 512], st=(k == 0), sp=(k == NK - 1):
                          e.matmul(o, a, b, start=st, stop=sp), [wbf_tk[s]] + hT_tk[tb * 4:tb * 4 + 4], [g.pst[bank]])
                P.add("act", lambda e, o=uT[:, cc, tb * 512:(tb + 1) * 512], i=g.ps[bank][:, :]: e.activation(out=o, in_=i, func=AF.Copy), [g.pst[bank]], [uT_tk[cc]])
        for t in range(NT):
            bank = 2 + t % 2
            for cc in range(2):
                P.add("pe", lambda e, o=g.ps[bank][:, :], a=uT[:, cc, t * 128:(t + 1) * 128], b=csc[:, cc, :], st=(cc == 0), sp=(cc == 1):
                      e.matmul(o, a, b, start=st, stop=sp), [uT_tk[cc], csc_tk], [g.pst[bank]])
            P.add("dve", lambda e, o=W1[:, t, :], i=g.ps[bank][:, :]: e.tensor_copy(out=o, in_=i), [g.pst[bank]], [W1_tk[t]])
        for mb in range(4):
            bufs = []
            for cs in range(2):
                s = nd % 3
                nd += 1
                P.add("sp", lambda e, o=dfb[s], s_=g.dft[cs, mb]: e.dma_start(out=o, in_=s_), [], [dfb_tk[s]], dma=True)
                bufs.append((dfb[s], dfb_tk[s]))
            for nch in range(2):
                bank = 4 + (mb * 2 + nch) % 2
                n = 0
                for cs in range(2):
                    db_, dtk = bufs[cs]
                    for lc in range(NK):
                        P.add("pe", lambda e, o=g.ps[bank][:, :], a=W1[:, lc, cs * 256 + nch * 128: cs * 256 + (nch + 1) * 128], b=db_[:, lc, :], st=(n == 0), sp=(n == 31):
                              e.matmul(o, a, b, start=st, stop=sp), [W1_tk[lc], dtk], [g.pst[bank]])
                        n += 1
                eng = "act" if nch == 0 else "dve"
                if eng == "act":
                    P.add("act", lambda e, o=YT[:, gi * 2 + nch, mb * 512:(mb + 1) * 512], i=g.ps[bank][:, :]: e.activation(out=o, in_=i, func=AF.Copy), [g.pst[bank]], [YT_tk])
                else:
                    P.add("dve", lambda e, o=YT[:, gi * 2 + nch, mb * 512:(mb + 1) * 512], i=g.ps[bank][:, :]: e.tensor_copy(out=o, in_=i), [g.pst[bank]], [YT_tk])
    P.barrier()
    out_proj(g, YT, YT_tk, 8, g.w_out0[0:1024, :], xsrc, g.xs[2], g.modT[l][:, 2, :], None, 24576)
    P.barrier()
    OT = arena_bf(g, 16384, 16384).rearrange("p (k n) -> p k n", k=8)
    OT_tk = Tk()
    hcT = arena_bf(g, 24576, 4096).rearrange("p (k n) -> p k n", k=NK)
    hc_tk = [Tk(), Tk()]
    build_hT(g, g.ctx, 2, Ac, Bc, hcT, hc_tk, 26624)
    P.barrier()
    QT = arena_bf(g, 26624, 4096).rearrange("p (h n) -> p h n", h=2)
    KT = arena_bf(g, 28672, 4096).rearrange("p (h n) -> p h n", h=2)
    QT_tk, KT_tk = [Tk(), Tk()], [Tk(), Tk()]
    Vq = arena_bf(g, 30720, 4160).rearrange("p (t h d) -> p t h d", t=NT, h=4)
    V_tk = [Tk() for _ in range(NT)]
    kcT = arena_bf(g, 32800, 512).rearrange("p (h n) -> p h n", h=2)
    kc_tk = [Tk(), Tk()]
    vc = arena_bf(g, 33056, 520).rearrange("p (t h d) -> p t h d", t=2, h=4)
    vc_tk = [Tk(), Tk()]
    Otok = arena_f32(g, 33344, 4096).rearrange("p (i f) -> p i f", i=NT)
    Otok_tk = [Tk() for _ in range(NT)]
    tmpb = [arena_f32(g, 37440 + i * 640, 640) for i in range(2)]
    tmp_tk = [Tk(), Tk()]
    Pb = [arena_bf(g, 38720 + i * 448, 896) for i in range(2)]
    Pb_tk = [Tk(), Tk()]
    tab = [arena_f32(g, 39616 + i * 1664, 1664) for i in range(2)]
    tab_tk = [Tk(), Tk()]
    stg = [arena_f32(g, 42944 + i * 2048, 2048).rearrange("p (k n) -> p k n", k=NK) for i in range(2)]
    stg_tk = [Tk(), Tk()]
    wbf = [arena_bf(g, 47040 + i * 1024, 2048).rearrange("p (k n) -> p k n", k=NK) for i in range(4)]
    wbf_tk = [Tk() for _ in range(4)]
    rec = [g.small[:, 348 + i:349 + i] for i in range(2)]
    rec_tk = [Tk(), Tk()]
    nu = 0
    nw = 0
    it = 0

    def wunit(col, eng):
        nonlocal nu, nw
        s = nu % 2
        d = nw % 4
        nu += 1
        nw += 1
        load_w_unit(g, g.w_in0[:, col:col + 128].rearrange("(k p) n -> p k n", p=128), stg[s], stg_tk[s], wbf[d], wbf_tk[d], eng)
        return wbf[d], wbf_tk[d]

    for q in range(4):
        P.add("pool", lambda e: e.memset(Vq[:, :, :, 64:65], 1.0), [], V_tk)
        P.add("pool", lambda e: e.memset(vc[:, :, :, 64:65], 1.0), [], vc_tk)
        for hp in range(2):
            for which, dstT, dtk, base_col in ((0, QT, QT_tk, 1024), (1, KT, KT_tk, 2048)):
                w, wtk = wunit(base_col + q * 256 + hp * 128, "act" if which == 0 else "dve")
                for tb in range(4):
                    bank = it % 2
                    it += 1
                    for k in range(NK):
                        P.add("pe", lambda e, o=g.ps[bank][:, :], a=w[:, k, :], b=hT[:, k, tb * 512:(tb + 1) * 512], st=(k == 0), sp=(k == NK - 1):
                              e.matmul(o, a, b, start=st, stop=sp), [wtk] + hT_tk[tb * 4:tb * 4 + 4], [g.pst[bank]])
                    P.add("act", lambda e, o=dstT[:, hp, tb * 512:(tb + 1) * 512], i=g.ps[bank][:, :]: e.activation(out=o, in_=i, func=AF.Copy), [g.pst[bank]], [dtk[hp]])
                if which == 1:
                    bank = it % 2
                    it += 1
                    for k in range(NK):
                        P.add("pe", lambda e, o=g.ps[bank][:, 0:256], a=w[:, k, :], b=hcT[:, k, :], st=(k == 0), sp=(k == NK - 1):
                              e.matmul(o, a, b, start=st, stop=sp), [wtk] + hc_tk, [g.pst[bank]])
                    P.add("act", lambda e, o=kcT[:, hp, :], i=g.ps[bank][:, 0:256]: e.activation(out=o, in_=i, func=AF.Copy), [g.pst[bank]], [kc_tk[hp]])
        wv = [wunit(3072 + q * 256 + j * 128, "act" if j == 0 else "dve") for j in range(2)]
        for t in range(NT + 2):
            bank = it % 2
            it += 1
            for j in range(2):
                w, wtk = wv[j]
                for k in range(NK):
                    if t < NT:
                        a_, rtk = hT[:, k, t * 128:(t + 1) * 128], [hT_tk[t]]
                    else:
                        a_, rtk = hcT[:, k, (t - NT) * 128:(t - NT + 1) * 128], [hc_tk[t - NT]]
                    P.add("pe", lambda e, o=g.ps[bank][:, j * 128:(j + 1) * 128], a=a_, b=w[:, k, :], st=(k == 0), sp=(k == NK - 1):
                          e.matmul(o, a, b, start=st, stop=sp), [wtk] + rtk, [g.pst[bank]])
            pv = g.ps[bank][:, 0:256].rearrange("p (h d) -> p h d", h=4)
            if t < NT:
                P.add("dve", lambda e, o=Vq[:, t, :, 0:64], i=pv: e.tensor_copy(out=o, in_=i), [g.pst[bank]], [V_tk[t]])
            else:
                P.add("dve", lambda e, o=vc[:, t - NT, :, 0:64], i=pv: e.tensor_copy(out=o, in_=i), [g.pst[bank]], [vc_tk[t - NT]])
        pend = None
        for h4 in range(4):
            hh = q * 4 + h4
            hp, po = h4 // 2, (h4 % 2) * 64
            tb_, tbtk = tab[hh % 2], tab_tk[hh % 2]
            P.add("sp", lambda e, o=tb_, s_=g.rpbt[hh]: e.dma_start(out=o, in_=s_), [], [tbtk], dma=True)
            for i in range(NT):
                if 2 <= i <= 13:
                    chunks = [i + 2, i + 1, i, i - 1, i - 2]
                    tcol = 0
                elif i < 2:
                    chunks = [3, 2, 1, 0]
                    tcol = 640 + (1 + 2 * i) * 64
                else:
                    chunks = [15, 14, 13, 12]
                    tcol = 640 + (7 - 2 * (15 - i)) * 64
                nch = len(chunks)
                r = it % 2
                it += 1
                banks = (0, 1) if r == 0 else (2, 3)
                qs = QT[po:po + 64, hp, i * 128:(i + 1) * 128]

                def sblk(ci):
                    return g.ps[banks[ci // 4]][:, (ci % 4) * 128:(ci % 4 + 1) * 128], g.pst[banks[ci // 4]]
                for ci, j in enumerate(chunks):
                    o_, otk = sblk(ci)
                    P.add("pe", lambda e, o=o_, a=KT[po:po + 64, hp, j * 128:(j + 1) * 128], b=qs: e.matmul(o, a, b, start=True, stop=True),
                          [KT_tk[hp], QT_tk[hp]], [otk])
                for cj in range(2):
                    o_, otk = sblk(nch + cj)
                    P.add("pe", lambda e, o=o_, a=kcT[po:po + 64, hp, cj * 128:(cj + 1) * 128], b=qs: e.matmul(o, a, b, start=True, stop=True),
                          [kc_tk[hp], QT_tk[hp]], [otk])
                P.add("dve", lambda e, o=tmpb[r][:, 0:512], i=g.ps[banks[0]][:, :], t_=tb_[:, tcol:tcol + 512]:
                      e.scalar_tensor_tensor(out=o, in0=i, scalar=0.125, in1=t_, op0=ALU.mult, op1=ALU.add), [g.pst[banks[0]], tbtk], [tmp_tk[r]])
                if nch == 5:
                    P.add("dve", lambda e, o=tmpb[r][:, 512:640], i=g.ps[banks[1]][:, 0:128], t_=tb_[:, tcol + 512:tcol + 640]:
                          e.scalar_tensor_tensor(out=o, in0=i, scalar=0.125, in1=t_, op0=ALU.mult, op1=ALU.add), [g.pst[banks[1]], tbtk], [tmp_tk[r]])
                P.add("act", lambda e, o=Pb[r][:, 0:nch * 128], i=tmpb[r][:, 0:nch * 128]: e.activation(out=o, in_=i, func=AF.Exp), [tmp_tk[r]], [Pb_tk[r]])
                c0 = (nch % 4) * 128
                P.add("act", lambda e, o=Pb[r][:, nch * 128:(nch + 2) * 128], i=g.ps[banks[1]][:, c0:c0 + 256]: e.activation(out=o, in_=i, func=AF.Exp, scale=0.125),
                      [g.pst[banks[1]]], [Pb_tk[r]])
                cur = (h4, i, chunks, r)
                if pend is not None:
                    _attn_pv(g, pend, Pb, Pb_tk, Vq, V_tk, vc, vc_tk, Otok, Otok_tk, rec, rec_tk)
                pend = cur
        _attn_pv(g, pend, Pb, Pb_tk, Vq, V_tk, vc, vc_tk, Otok, Otok_tk, rec, rec_tk)
        for i in range(NT):
            for hp in range(2):
                bank = 6 + (i * 2 + hp) % 2
                P.add("pe", lambda e, o=g.ps[bank][:, 0:128], a=Otok[:, i, hp * 128:(hp + 1) * 128]: e.transpose(o, a, g.ident32), [Otok_tk[i], g.tk_pers], [g.pst[bank]])
                if hp == 0:
                    P.add("act", lambda e, o=OT[:, q * 2 + hp, i * 128:(i + 1) * 128], i_=g.ps[bank][:, 0:128]: e.activation(out=o, in_=i_, func=AF.Copy), [g.pst[bank]], [OT_tk])
                else:
                    P.add("dve", lambda e, o=OT[:, q * 2 + hp, i * 128:(i + 1) * 128], i_=g.ps[bank][:, 0:128]: e.tensor_copy(out=o, in_=i_), [g.pst[bank]], [OT_tk])
    P.barrier()
    out_proj(g, OT, OT_tk, 8, g.w_out0[1024:2048, :], g.xs[2], xdst, g.modT[l][:, 2, :], None, 24576)


def _attn_pv(g, item, Pb, Pb_tk, Vq, V_tk, vc, vc_tk, Otok, Otok_tk, rec, rec_tk):
    P = g.P
    h4, i, chunks, r = item
    nch = len(chunks)
    bank = 4 + r
    o_ = g.ps[bank][:, 0:65]
    n = nch + 2
    for ci, j in enumerate(chunks):
        P.add("pe", lambda e, a=Pb[r][:, ci * 128:(ci + 1) * 128], b=Vq[:, j, h4, :], st=(ci == 0): e.matmul(o_, a, b, start=st, stop=False),
              [Pb_tk[r], V_tk[j]], [g.pst[bank]])
    for cj in range(2):
        P.add("pe", lambda e, a=Pb[r][:, (nch + cj) * 128:(nch + cj + 1) * 128], b=vc[:, cj, h4, :], sp=(cj == 1): e.matmul(o_, a, b, start=False, stop=sp),
              [Pb_tk[r], vc_tk[cj]], [g.pst[bank]])
    P.add("dve", lambda e: e.reciprocal(out=rec[r], in_=g.ps[bank][:, 64:65]), [g.pst[bank]], [rec_tk[r]])
    P.add("act", lambda e, o=Otok[:, i, h4 * 64:(h4 + 1) * 64]: e.activation(out=o, in_=g.ps[bank][:, 0:64], func=AF.Copy, scale=rec[r]),
          [g.pst[bank], rec_tk[r]], [Otok_tk[i]])
```
